# Optimizing a Trainium2 kernel written in Bass

```python
import jax, jax.numpy as jnp
from jax import lax
import numpy as np

D_MODEL = 1024
BATCH = 2
SEQ = 8192
DEPTH = 1

D_MIX = D_MODEL
RET_HEADS = 4
RET_DK = 128
RET_DV = 128
RET_CHUNK = 128
NSA_HEADS = 8
NSA_KV_GROUPS = 2
NSA_HPG = NSA_HEADS // NSA_KV_GROUPS
NSA_DH = 64
CMP_LEN = 32
CMP_STRIDE = 16
CMP_HID = 256
SEL_BLOCK = 64
SEL_TOPN = 16
WINDOW = 512
Q_BLOCK = 128
FORCED_SCORE = 1e9
ROPE_THETA = 10000.0
PEER_HEADS = 8
PEER_NKEYS = 128
PEER_EXPERTS = PEER_NKEYS * PEER_NKEYS
PEER_TOPK = 16
PEER_DQ = 256
PEER_DHALF = PEER_DQ // 2
PEER_TOK_BLOCK = 128
NORM_EPS = 1e-6
KV_COLS = NSA_KV_GROUPS * NSA_DH
IN_SPLITS = (RET_HEADS * RET_DK, RET_HEADS * RET_DK, RET_HEADS * RET_DV, RET_HEADS * RET_DV,
             NSA_HEADS * NSA_DH, KV_COLS, KV_COLS, KV_COLS, KV_COLS, KV_COLS, KV_COLS,
             NSA_HEADS * 3)
IN_COLS = sum(IN_SPLITS)

kernel_name = "hymba_retention_nsa_peer_block"


def _rms_norm(x, g):
    xf = x.astype(jnp.float32)
    y = xf * lax.rsqrt(jnp.mean(xf * xf, axis=-1, keepdims=True) + NORM_EPS)
    return (y * g.astype(jnp.float32)).astype(x.dtype)


def _modulate(h, shift, scale):
    return h * (1.0 + scale[:, None, :]) + shift[:, None, :]


def _rope(x, pos):
    d = x.shape[-1]
    inv = ROPE_THETA ** (-jnp.arange(0, d, 2, dtype=jnp.float32) / d)
    ang = pos.astype(jnp.float32)[:, None] * inv[None, :]
    cos = jnp.cos(ang)[None, :, None, :].astype(x.dtype)
    sin = jnp.sin(ang)[None, :, None, :].astype(x.dtype)
    x1, x2 = jnp.split(x, 2, axis=-1)
    return jnp.concatenate([x1 * cos - x2 * sin, x2 * cos + x1 * sin], axis=-1)


def _masked_softmax(s, mask):
    s = jnp.where(mask, s, -jnp.inf)
    m = jnp.max(s, axis=-1, keepdims=True)
    m = jnp.where(jnp.isfinite(m), m, 0.0)
    e = jnp.where(mask, jnp.exp(s - m), 0.0)
    return e / jnp.maximum(jnp.sum(e, axis=-1, keepdims=True), jnp.finfo(jnp.float32).tiny)


def _retention(q, k, v, g):
    B, S, H, DK = q.shape
    DV = v.shape[-1]
    C = RET_CHUNK
    nc = S // C
    f32 = jnp.float32
    log_g = jnp.log1p(-jnp.exp2(-5.0 - jnp.arange(H, dtype=f32)))
    n = jnp.arange(C, dtype=f32)
    diff = n[:, None] - n[None, :]
    causal = diff >= 0
    dmat = jnp.where(causal[None], jnp.exp(jnp.where(causal, diff, 0.0)[None] * log_g[:, None, None]), 0.0)
    qc = q.astype(f32).reshape(B, nc, C, H, DK)
    kc = (k.astype(f32) * DK ** -0.5).reshape(B, nc, C, H, DK)
    vc = v.astype(f32).reshape(B, nc, C, H, DV)
    scores = jnp.einsum('bcnhk,bcmhk->bchnm', qc, kc) * dmat[None, None]
    inner = jnp.einsum('bchnm,bcmhv->bcnhv', scores, vc)
    zeta = jnp.exp((C - 1 - n)[None, :] * log_g[:, None])
    kv = jnp.einsum('bcmhk,bcmhv,hm->cbhkv', kc, vc, zeta)
    cdecay = jnp.exp(C * log_g)[None, :, None, None]

    def step(state, kv_c):
        return cdecay * state + kv_c, state

    _, s_prev = lax.scan(step, jnp.zeros((B, H, DK, DV), f32), kv)
    xi = jnp.exp((n + 1.0)[None, :] * log_g[:, None])
    cross = jnp.einsum('bcnhk,cbhkv,hn->bcnhv', qc, s_prev, xi)
    y = (inner + cross).reshape(B, S, H, DV)
    mu = jnp.mean(y, axis=-1, keepdims=True)
    var = jnp.mean((y - mu) ** 2, axis=-1, keepdims=True)
    y = (y - mu) * lax.rsqrt(var + NORM_EPS)
    y = jax.nn.silu(g.astype(f32)) * y
    return y.reshape(B, S, H * DV).astype(q.dtype)


def _compress(kraw, pe, w1, b1, w2):
    B, S, G, DH = kraw.shape
    ncb = (S - CMP_LEN) // CMP_STRIDE + 1
    idx = jnp.arange(ncb)[:, None] * CMP_STRIDE + jnp.arange(CMP_LEN)[None, :]
    blk = kraw[:, idx] + pe[None, None, :, None, :]
    blk = blk.transpose(0, 1, 3, 2, 4).reshape(B, ncb, G, CMP_LEN * DH)
    return jax.nn.gelu(blk @ w1 + b1) @ w2


def _nsa(q, k_c, v_c, k_s, v_s, k_w, v_w, gate_logits, pe_k, pe_v, wk1, bk1, wk2, wv1, bv1, wv2):
    B, S = q.shape[:2]
    G, HPG, DH = NSA_KV_GROUPS, NSA_HPG, NSA_DH
    f32 = jnp.float32
    kc = _compress(k_c, pe_k, wk1, bk1, wk2)
    vc = _compress(v_c, pe_v, wv1, bv1, wv2)
    ncb = kc.shape[1]
    nsel = S // SEL_BLOCK
    n_top = min(SEL_TOPN, nsel)
    nqb = S // Q_BLOCK
    cmp_start = jnp.arange(ncb) * CMP_STRIDE
    cmp_end = cmp_start + CMP_LEN - 1
    sel_start = jnp.arange(nsel) * SEL_BLOCK
    overlap = jnp.clip(jnp.minimum(cmp_start[:, None] + CMP_LEN, sel_start[None, :] + SEL_BLOCK)
                       - jnp.maximum(cmp_start[:, None], sel_start[None, :]), 0).astype(f32) / CMP_STRIDE
    ks_g = k_s.reshape(B, nsel, SEL_BLOCK, G, DH).transpose(0, 3, 1, 2, 4)
    vs_g = v_s.reshape(B, nsel, SEL_BLOCK, G, DH).transpose(0, 3, 1, 2, 4)
    kw_pad = jnp.pad(k_w, ((0, 0), (WINDOW, 0), (0, 0), (0, 0)))
    vw_pad = jnp.pad(v_w, ((0, 0), (WINDOW, 0), (0, 0), (0, 0)))
    qg = q.reshape(B, nqb, Q_BLOCK, G, HPG, DH).swapaxes(0, 1)
    gg = gate_logits.reshape(B, nqb, Q_BLOCK, G, HPG, 3).swapaxes(0, 1)
    scale = DH ** -0.5
    gather = jax.vmap(jax.vmap(lambda tbl, ix: tbl[ix]))
    blk_ids = jnp.arange(nsel)

    def body(args):
        qb, qblk, gblk = args
        t = qb * Q_BLOCK + jnp.arange(Q_BLOCK)
        s_c = jnp.einsum('bqghd,bngd->bghqn', qblk, kc).astype(f32) * scale
        p_c = _masked_softmax(s_c, cmp_end[None, :] <= t[:, None])
        o_c = jnp.einsum('bghqn,bngd->bqghd', p_c.astype(vc.dtype), vc)
        imp = jnp.einsum('bghqn,nj->bgqj', p_c, overlap)
        cur = t // SEL_BLOCK
        forced = (blk_ids[None, :] == 0) | (blk_ids[None, :] == cur[:, None]) | (blk_ids[None, :] == cur[:, None] - 1)
        imp = jnp.where(forced, FORCED_SCORE, imp)
        imp = jnp.where(blk_ids[None, :] <= cur[:, None], imp, -jnp.inf)
        top_v, top_i = lax.top_k(imp, n_top)
        ksel = gather(ks_g, top_i)
        vsel = gather(vs_g, top_i)
        pos = top_i[..., None] * SEL_BLOCK + jnp.arange(SEL_BLOCK)
        m_s = jnp.isfinite(top_v)[..., None] & (pos <= t[None, None, :, None, None])
        s_s = jnp.einsum('bqghd,bgqnkd->bghqnk', qblk, ksel).astype(f32) * scale
        p_s = _masked_softmax(s_s.reshape(B, G, HPG, Q_BLOCK, n_top * SEL_BLOCK),
                              m_s.reshape(B, G, 1, Q_BLOCK, n_top * SEL_BLOCK))
        o_s = jnp.einsum('bghqm,bgqmd->bqghd', p_s.astype(vsel.dtype),
                         vsel.reshape(B, G, Q_BLOCK, n_top * SEL_BLOCK, DH))
        kwb = lax.dynamic_slice_in_dim(kw_pad, qb * Q_BLOCK, Q_BLOCK + WINDOW, axis=1)
        vwb = lax.dynamic_slice_in_dim(vw_pad, qb * Q_BLOCK, Q_BLOCK + WINDOW, axis=1)
        sp = qb * Q_BLOCK - WINDOW + jnp.arange(Q_BLOCK + WINDOW)
        m_w = (sp[None, :] >= 0) & (sp[None, :] <= t[:, None]) & (sp[None, :] > t[:, None] - WINDOW)
        s_w = jnp.einsum('bqghd,bkgd->bghqk', qblk, kwb).astype(f32) * scale
        p_w = _masked_softmax(s_w, m_w)
        o_w = jnp.einsum('bghqk,bkgd->bqghd', p_w.astype(vwb.dtype), vwb)
        gate = jax.nn.sigmoid(gblk.astype(f32))
        out = gate[..., 0:1] * o_c + gate[..., 1:2] * o_s + gate[..., 2:3] * o_w
        return out.astype(qblk.dtype)

    out = lax.map(body, (jnp.arange(nqb), qg, gg))
    return out.swapaxes(0, 1).reshape(B, S, NSA_HEADS * DH)


def _peer(h, w_q, subkeys, w_down, w_up):
    B, S, D = h.shape
    TB = PEER_TOK_BLOCK
    nb = S // TB
    hb = h.reshape(B, nb, TB, D).swapaxes(0, 1)

    def body(hblk):
        q = (hblk @ w_q).reshape(B, TB, PEER_HEADS, 2, PEER_DHALF)
        s = jnp.einsum('btphd,phnd->btphn', q, subkeys).astype(jnp.float32)
        v, i = lax.top_k(s, PEER_TOPK)
        cand = (v[..., 0, :, None] + v[..., 1, None, :]).reshape(B, TB, PEER_HEADS, PEER_TOPK * PEER_TOPK)
        cidx = (i[..., 0, :, None] * PEER_NKEYS + i[..., 1, None, :]).reshape(B, TB, PEER_HEADS, PEER_TOPK * PEER_TOPK)
        tv, tj = lax.top_k(cand, PEER_TOPK)
        eidx = jnp.take_along_axis(cidx, tj, axis=-1)
        w = jax.nn.softmax(tv, axis=-1)
        u = w_down[eidx]
        a = jax.nn.gelu(jnp.einsum('btd,btpkd->btpk', hblk, u).astype(jnp.float32))
        vv = w_up[eidx]
        return jnp.einsum('btpk,btpkd->btd', (w * a).astype(hblk.dtype), vv)

    out = lax.map(body, hb)
    return out.swapaxes(0, 1).reshape(B, S, D)


def setup_inputs(seed: int = 0) -> dict:
    key = jax.random.key(seed)
    ks = jax.random.split(key, 24)
    L = DEPTH

    def nrm(k, shape, s):
        return jax.random.normal(k, shape, jnp.float32) * s

    return {
        "x": nrm(ks[0], (BATCH, SEQ, D_MODEL), 1.0),
        "c": nrm(ks[1], (BATCH, D_MODEL), 1.0),
        "w_ada": nrm(ks[2], (L, D_MODEL, 6 * D_MODEL), 0.5 * D_MODEL ** -0.5),
        "b_ada": nrm(ks[3], (L, 6 * D_MODEL), 0.02),
        "g_norm_mix": 1.0 + nrm(ks[4], (L, D_MODEL), 0.02),
        "w_in": nrm(ks[5], (L, D_MODEL, IN_COLS), D_MODEL ** -0.5),
        "pe_cmp_k": nrm(ks[6], (L, CMP_LEN, NSA_DH), 0.1),
        "pe_cmp_v": nrm(ks[7], (L, CMP_LEN, NSA_DH), 0.1),
        "w_cmp_k1": nrm(ks[8], (L, CMP_LEN * NSA_DH, CMP_HID), (CMP_LEN * NSA_DH) ** -0.5),
        "b_cmp_k1": nrm(ks[9], (L, CMP_HID), 0.02),
        "w_cmp_k2": nrm(ks[10], (L, CMP_HID, NSA_DH), CMP_HID ** -0.5),
        "w_cmp_v1": nrm(ks[11], (L, CMP_LEN * NSA_DH, CMP_HID), (CMP_LEN * NSA_DH) ** -0.5),
        "b_cmp_v1": nrm(ks[12], (L, CMP_HID), 0.02),
        "w_cmp_v2": nrm(ks[13], (L, CMP_HID, NSA_DH), CMP_HID ** -0.5),
        "w_out": nrm(ks[14], (L, D_MIX, D_MODEL), D_MIX ** -0.5),
        "g_norm_ffn": 1.0 + nrm(ks[15], (L, D_MODEL), 0.02),
        "w_peer_q": nrm(ks[16], (L, D_MODEL, PEER_HEADS * PEER_DQ), D_MODEL ** -0.5),
        "peer_subkeys": nrm(ks[17], (L, PEER_HEADS, 2, PEER_NKEYS, PEER_DHALF), PEER_DHALF ** -0.5),
        "peer_down": nrm(ks[18], (L, PEER_EXPERTS, D_MODEL), D_MODEL ** -0.5),
        "peer_up": nrm(ks[19], (L, PEER_EXPERTS, D_MODEL), PEER_HEADS ** -0.5),
        "g_norm_final": 1.0 + nrm(ks[20], (D_MODEL,), 0.02),
    }


def reference(x, c, w_ada, b_ada, g_norm_mix, w_in, pe_cmp_k, pe_cmp_v, w_cmp_k1, b_cmp_k1, w_cmp_k2,
              w_cmp_v1, b_cmp_v1, w_cmp_v2, w_out, g_norm_ffn, w_peer_q, peer_subkeys, peer_down, peer_up,
              g_norm_final):
    B, S, D = x.shape
    pos = jnp.arange(S)
    split_points = [int(v) for v in np.cumsum(IN_SPLITS)[:-1]]
    for l in range(DEPTH):
        mod = jax.nn.silu(c) @ w_ada[l] + b_ada[l]
        sh1, sc1, ga1, sh2, sc2, ga2 = jnp.split(mod, 6, axis=-1)
        h = _modulate(_rms_norm(x, g_norm_mix[l]), sh1, sc1)
        proj = h @ w_in[l]
        (r_q, r_k, r_v, r_g, n_q, n_kc, n_vc, n_ks, n_vs, n_kw, n_vw, n_gate) = jnp.split(proj, split_points, axis=-1)
        r_q = _rope(r_q.reshape(B, S, RET_HEADS, RET_DK), pos)
        r_k = _rope(r_k.reshape(B, S, RET_HEADS, RET_DK), pos)
        ret_out = _retention(r_q, r_k, r_v.reshape(B, S, RET_HEADS, RET_DV), r_g.reshape(B, S, RET_HEADS, RET_DV))
        kvshape = (B, S, NSA_KV_GROUPS, NSA_DH)
        nsa_out = _nsa(_rope(n_q.reshape(B, S, NSA_HEADS, NSA_DH), pos),
                       _rope(n_kc.reshape(kvshape), pos), n_vc.reshape(kvshape),
                       _rope(n_ks.reshape(kvshape), pos), n_vs.reshape(kvshape),
                       _rope(n_kw.reshape(kvshape), pos), n_vw.reshape(kvshape),
                       n_gate, pe_cmp_k[l], pe_cmp_v[l], w_cmp_k1[l], b_cmp_k1[l], w_cmp_k2[l],
                       w_cmp_v1[l], b_cmp_v1[l], w_cmp_v2[l])
        mix = jnp.concatenate([ret_out, nsa_out], axis=-1) @ w_out[l]
        x = x + ga1[:, None, :] * mix
        h = _modulate(_rms_norm(x, g_norm_ffn[l]), sh2, sc2)
        x = x + ga2[:, None, :] * _peer(h, w_peer_q[l], peer_subkeys[l], peer_down[l], peer_up[l])
    return _rms_norm(x, g_norm_final)
```

```python
import numpy as np
import ml_dtypes
from contextlib import ExitStack
import concourse.bass as bass
import concourse.mybir as mybir
from concourse.bass_utils import run_bass_kernel_spmd

F32 = mybir.dt.float32
BF16 = mybir.dt.bfloat16
U32 = mybir.dt.uint32
F32R = mybir.dt.float32r
ALU = mybir.AluOpType
AF = mybir.ActivationFunctionType
AX = mybir.AxisListType

import os
ROPE_ENG = os.environ.get("ROPE_ENG", "dve")
NEG = -30000.0
BIG = 1.0e30
COMPUTE = ("pe", "act", "dve", "pool")


class _Op:
    __slots__ = ("eng", "fn", "reads", "writes", "deps", "is_dma", "signal", "ev", "id")


class SemState:
    def __init__(self, nc, stack, n_dma_sems=24):
        self.n_dma_sems = n_dma_sems
        self.sems = {}
        for e in COMPUTE:
            self.sems[e] = stack.enter_context(nc.semaphore("s_" + e))
        for k in range(n_dma_sems):
            self.sems["d%d" % k] = stack.enter_context(nc.semaphore("s_d%d" % k))
        self.cnt = {e: 0 for e in COMPUTE}
        self.dma_cnt = [0] * n_dma_sems


class Prog:
    def __init__(self, nc, state):
        self.nc = nc
        self.state = state
        self.ops = []
        self.last_writer = {}
        self.readers = {}
        self.n_dma_sems = state.n_dma_sems
        self.dma_rr = 0
        self.sw_rr = 0
        self.dma_last = [None] * self.n_dma_sems
        self.dma_cnt = state.dma_cnt

    def op(self, eng, fn, reads=(), writes=(), dma=False):
        o = _Op()
        o.eng, o.fn, o.is_dma = eng, fn, dma
        o.reads, o.writes = tuple(reads), tuple(writes)
        o.deps = set()
        o.signal = False
        o.ev = None
        o.id = len(self.ops)
        for r in o.reads:
            w = self.last_writer.get(r)
            if w is not None:
                o.deps.add(w)
        for w_ in o.writes:
            w = self.last_writer.get(w_)
            if w is not None:
                o.deps.add(w)
            lastc = {}
            for rd in self.readers.get(w_, ()):
                p_ = self.ops[rd]
                if p_.is_dma:
                    o.deps.add(rd)
                else:
                    lastc[p_.eng] = rd
            o.deps.update(lastc.values())
        for r in o.reads:
            self.readers.setdefault(r, []).append(o.id)
        for w_ in o.writes:
            self.last_writer[w_] = o.id
            self.readers[w_] = []
        o.deps.discard(o.id)
        if dma:
            if eng == "pool":
                k = self.n_dma_sems - 4 + self.sw_rr
                self.sw_rr = (self.sw_rr + 1) % 4
            else:
                k = self.dma_rr
                self.dma_rr = (self.dma_rr + 1) % (self.n_dma_sems - 4)
            prev = self.dma_last[k]
            if prev is not None:
                o.deps.add(prev)
            self.dma_last[k] = o.id
            self.dma_cnt[k] += 16
            o.ev = ("d%d" % k, self.dma_cnt[k])
        self.ops.append(o)
        return o.id

    def dma(self, eng, out, in_, reads=(), writes=(), **kw):
        return self.op(eng, lambda e: e.dma_start(out=out, in_=in_, **kw), reads, writes, dma=True)

    def emit(self):
        nc, ops = self.nc, self.ops
        for o in ops:
            nd = set()
            for d in o.deps:
                p = ops[d]
                if p.is_dma or o.is_dma or p.eng != o.eng:
                    nd.add(d)
                elif o.eng != "pe":
                    nd.add(d)
            o.deps = nd
            for d in nd:
                if not ops[d].is_dma:
                    ops[d].signal = True
        cnt = self.state.cnt
        for o in ops:
            if not o.is_dma and o.signal:
                cnt[o.eng] += 1
                o.ev = (o.eng, cnt[o.eng])
        finals = [ops[i] for i in self.dma_last if i is not None]
        with ExitStack() as st:
            sems = self.state.sems
            block = st.enter_context(nc.Block())
            streams = {}
            for o in ops:
                streams.setdefault(o.eng, []).append(o)

            def run_stream(ename, e, final=False):
                waited = {}

                def wait(ev):
                    if waited.get(ev[0], 0) < ev[1]:
                        e.wait_ge(sems[ev[0]], ev[1])
                        waited[ev[0]] = ev[1]

                for o in streams.get(ename, []):
                    for d in sorted(o.deps):
                        wait(ops[d].ev)
                    ins = o.fn(e)
                    if o.is_dma:
                        ins.then_inc(sems[o.ev[0]], 16)
                    elif o.signal:
                        ins.then_inc(sems[o.eng], 1)
                if final:
                    for o in finals:
                        wait(o.ev)

            block.sync(lambda e: run_stream("sp", e, final=True))
            block.scalar(lambda e: run_stream("act", e))
            block.vector(lambda e: run_stream("dve", e))
            block.gpsimd(lambda e: run_stream("pool", e))
            block.tensor(lambda e: run_stream("pe", e))


def _blk(flag):
    if flag:
        with ExitStack() as st:
            yield st


class CFG:
    def __init__(self, NW=64, NO=16, NEXP=16384, stop=9):
        self.NW, self.NO, self.NEXP, self.stop = NW, NO, NEXP, stop
        self.astop = 99
        self.NB = NW * 8
        self.NCH = max(1, self.NB // 128)
        self.NWK = min(NW, NO + 4)
        self.NSB = NW * 2


LOGG = [float(np.log1p(-np.exp2(-5.0 - h))) for h in range(4)]


def host_tables(cfg, off):
    NW, NO, NB, NCH = cfg.NW, cfg.NO, cfg.NB, cfg.NCH
    S = NW * 128
    p = (off + np.arange(S)).astype(np.float32)
    t = {}
    inv128 = (10000.0 ** (-np.arange(0, 128, 2, dtype=np.float32) / 128)).astype(np.float32)
    inv64 = (10000.0 ** (-np.arange(0, 64, 2, dtype=np.float32) / 64)).astype(np.float32)
    a128 = p[:, None] * inv128[None]
    a64 = p[:, None] * inv64[None]
    t["rope"] = np.concatenate([np.cos(a128), np.sin(a128), np.cos(a64), np.sin(a64)], 1).astype(np.float32)
    valid_tile = ((off + 128 * np.arange(NW)) >= 0).astype(np.float32)
    n = np.arange(128, dtype=np.float32)
    sc = 128 ** -0.5
    zt = np.stack([np.exp((127 - n) * LOGG[h]) * sc for h in range(4)], 1)
    t["zeta"] = (zt[:, None, :] * valid_tile[None, :, None]).astype(np.float32).reshape(128, NW * 4)
    dm = np.zeros((128, 4, 128), np.float32)
    for h in range(4):
        d = n[None, :] - n[:, None]
        dm[:, h, :] = np.where(d >= 0, np.exp(np.where(d >= 0, d, 0) * LOGG[h]), 0.0) * sc
    t["dmat"] = dm.reshape(128, 512)
    xi = np.stack([np.exp((n + 1.0) * LOGG[h]) for h in range(4)], 0)
    t["xi"] = np.broadcast_to(xi[None], (128, 4, 128)).reshape(128, 512).astype(np.float32).copy()
    t["keybias"] = np.broadcast_to(np.where(valid_tile > 0, 0.0, NEG)[None], (128, NW)).astype(np.float32).copy()
    nb = np.arange(NCH * 128)
    cvalid = ((off + 16 * nb) >= 0) & (nb <= NB - 2)
    t["cmpbias"] = np.where(cvalid, 0.0, NEG).astype(np.float32).reshape(NCH, 128).T.copy()
    tq = (NW - NO) * 128 + np.arange(NO * 128)
    cp = np.where((16 * nb[:, None] + 31) <= tq[None, :], 0.0, NEG)
    cp = cp.reshape(NCH, 128, NO, 128).transpose(2, 1, 0, 3)
    cp = np.broadcast_to(cp[:, :, :, None, :], (NO, 128, NCH, 4, 128))
    t["cmppen"] = cp.astype(ml_dtypes.bfloat16).reshape(NO * 128, NCH * 512)
    k = np.arange(128)
    t["tri"] = np.concatenate([np.tile(np.where(k[:, None] <= k[None, :], 0.0, NEG), (1, 4)),
                               np.tile(np.where(k[:, None] > k[None, :], 0.0, NEG), (1, 4))], 1).astype(ml_dtypes.bfloat16)
    t["iota128"] = np.broadcast_to(np.arange(128, dtype=np.float32)[None], (128, 128)).copy()
    mb = np.arange(128)
    cur = tq // 64
    b0 = (-off) // 64
    validb = (mb[None, :] >= b0) & (mb[None, :] <= cur[:, None]) & (mb[None, :] < NW * 2)
    forced = ((mb[None, :] == b0) | (mb[None, :] == cur[:, None]) | (mb[None, :] == cur[:, None] - 1)) & validb
    m1 = (validb & ~forced).astype(np.float32)
    m2 = np.where(forced, 1e9, np.where(validb, 0.0, -BIG)).astype(np.float32)
    t["selm"] = np.concatenate([m1, m2], 1)
    key = np.arange(S)
    ex = np.zeros((128, S), np.float32)
    ex[key // 64, key] = 1.0
    t["expand"] = ex.astype(ml_dtypes.bfloat16)
    cs = 16 * nb
    ss = 64 * mb
    ov = np.clip(np.minimum(cs[:, None] + 32, ss[None, :] + 64) - np.maximum(cs[:, None], ss[None, :]), 0, None) / 16.0
    t["overlap"] = ov.reshape(NCH, 128, 128).transpose(1, 0, 2).reshape(128, NCH * 128).astype(ml_dtypes.bfloat16)
    t["ident"] = np.eye(128, dtype=np.float32)
    sel = np.zeros((128, 128, 128), np.float32)
    sel[k, k, :] = 1.0
    t["selrow"] = sel.reshape(128, 128 * 128).astype(ml_dtypes.bfloat16)
    t["iota16"] = np.broadcast_to(np.arange(16, dtype=np.float32)[None], (128, 16)).copy()
    return t


def w_in_perm():
    r = lambda a, b: list(range(a, b))
    return np.array(r(512, 1024) + r(1024, 1536) + r(2560, 2688) + r(2816, 2944) + r(3072, 3200)
                    + r(2688, 2816) + r(2944, 3072) + r(3200, 3328)
                    + r(0, 512) + r(1536, 2048)
                    + [2048 + (g * 4 + h) * 64 + d for h in range(4) for g in range(2) for d in range(64)]
                    + r(3328, 3352))


B_RK, B_RV, B_NK, B_NV, B_RQ, B_RG, B_NQ, B_GT = (0, 512), (512, 512), (1024, 384), (1408, 384), \
    (1792, 512), (2304, 512), (2816, 512), (3328, 24)


def build(cfg):
    NW, NO, NB, NCH, NWK = cfg.NW, cfg.NO, cfg.NB, cfg.NCH, cfg.NWK
    T0 = NW - NO
    S = NW * 128
    nc = bass.Bass("TRN2", target_bir_lowering=False)
    dram = lambda name, shape, dt=F32, kind="ExternalInput": nc.dram_tensor(name, shape, dt, kind=kind).ap()
    xw = dram("xw", [S, 1024])
    ccol = dram("ccol", [128, 8])
    w_ada = dram("w_ada", [1024, 6144])
    b_adaT = dram("b_adaT", [128, 48])
    gmixT = dram("gmixT", [128, 8])
    gffnT = dram("gffnT", [128, 8])
    gfin = dram("gfin", [1, 1024])
    w_in = dram("w_in", [1024, 3352])
    w_out = dram("w_out", [1024, 1024])
    w1k = dram("w1k", [64, 32 * 256])
    w1v = dram("w1v", [64, 32 * 256])
    peTk = dram("peTk", [64, 32])
    peTv = dram("peTv", [64, 32])
    b1k = dram("b1k", [128, 2])
    b1v = dram("b1v", [128, 2])
    w2k = dram("w2k", [128, 2 * 64])
    w2v = dram("w2v", [128, 2 * 64])
    w_pq = dram("w_pq", [1024, 2048])
    skT = dram("skT", [128, 2048])
    pdown = dram("pdown", [cfg.NEXP, 1024])
    pup = dram("pup", [cfg.NEXP, 1024])
    tb = {}
    for name, shape, dt in [("rope", [S, 192], F32), ("zeta", [128, NW * 4], F32), ("dmat", [128, 512], F32),
                            ("xi", [128, 512], F32), ("keybias", [128, NW], F32), ("cmpbias", [128, NCH], F32),
                            ("cmppen", [NO * 128, NCH * 512], BF16), ("tri", [128, 1024], BF16), ("iota128", [128, 128], F32),
                            ("selm", [NO * 128, 256], F32), ("expand", [128, S], BF16),
                            ("overlap", [128, NCH * 128], BF16), ("ident", [128, 128], F32),
                            ("selrow", [128, 128 * 128], BF16), ("iota16", [128, 16], F32)]:
        tb[name] = dram("t_" + name, shape, dt)
    out = dram("out", [NO * 128, 1024], kind="ExternalOutput")
    rawd = dram("rawd", [2, 128, S], BF16, kind="Internal")
    RAWLEN = max(S + 16, 16 * NCH * 128 + 32)
    x1d = dram("x1d", [NO * 128, 1024], kind="Internal")

    with ExitStack() as outer:
        sbo = lambda name, shape, dt=F32: outer.enter_context(nc.sbuf_tensor(name, shape, dt))
        SEM = SemState(nc, outer)
        vec = sbo("vec", [128, 48])

        _bc = [0]

        def bcast_rows(P, sb, pbrk, items):
            _bc[0] += 1
            onesf = sb("onesf%d" % _bc[0], [128, 128]); diag = [sb("diag%d_%d" % (i_, _bc[0]), [128, 128]) for i_ in range(2)]
            P.op("dve", lambda e: e.memset(onesf[:], 1.0), writes=["onesf"])
            n = 0
            for vi, dst, dkey in items:
                for half in range(2):
                    pb, pkey = pbrk[n % 2]
                    for q in range(4):
                        fc = half * 4 + q
                        dg = diag[q % 2]
                        P.op("dve", lambda e, dg=dg, vi=vi, fc=fc: e.tensor_scalar(out=dg[:], in0=identf[:], scalar1=vec[:, vi * 8 + fc:vi * 8 + fc + 1],
                                                                                  scalar2=None, op0=ALU.mult),
                             reads=["identf", "vec"], writes=[("diag", q % 2)])
                        P.op("pe", lambda e, dg=dg, pb=pb, q=q: e.matmul(pb[:, q * 128:(q + 1) * 128], lhsT=onesf[:], rhs=dg[:], start=True, stop=True),
                             reads=[("diag", q % 2), "onesf"], writes=[pkey])
                    P.op("act", lambda e, pb=pb, dst=dst, half=half: e.copy(out=dst[:, half * 512:(half + 1) * 512], in_=pb[:]),
                         reads=[pkey], writes=[dkey])
                    n += 1
        identf = sbo("identf", [128, 128]); identb = sbo("identb", [128, 128], BF16)
        epst = sbo("epst", [128, 1])
        mid = ExitStack()
        sbm = lambda name, shape, dt=F32: mid.enter_context(nc.sbuf_tensor(name, shape, dt))
        ksT = sbm("ksT", [128, S], BF16)
        vs = sbm("vs", [128, NW * 2 * 65], BF16)
        kwT = sbm("kwT", [128, NWK * 128], BF16)
        vw = sbm("vw", [128, NWK * 2 * 65], BF16)
        kcT = sbm("kcT", [128, NCH * 128], BF16)
        vca = sbm("vca", [128, NCH * 2 * 193], BF16)
        reto = sbm("reto", [128, NO * 512], BF16)
        qTs = sbm("qTs", [128, NO * 512], BF16)
        gts = sbm("gts", [128, NO * 24])
        vs4 = vs[:].rearrange("p (t g d) -> p t g d", g=2, d=65)
        vw4 = vw[:].rearrange("p (t g d) -> p t g d", g=2, d=65)
        vca4 = vca[:].rearrange("p (c g d) -> p c g d", g=2, d=193)

        for st in _blk(cfg.stop >= 0):
            sb = lambda name, shape, dt=F32: st.enter_context(nc.sbuf_tensor(name, shape, dt))
            ps = lambda name, shape, dt=F32: st.enter_context(nc.psum_tensor(name, shape, dt))
            P = Prog(nc, SEM)
            wad = [sb("wad%d" % i, [128, 8 * 1024]) for i in range(2)]
            cc = sb("cc", [128, 8]); sil = sb("sil", [128, 8])
            badT = sb("badT", [128, 48]); gm = sb("gm", [128, 8]); gf_ = sb("gf_", [128, 8])
            modT = sb("modT", [128, 48])
            pm = ps("pm", [128, 48])
            P.dma("sp", cc[:], ccol, writes=["cc"])
            P.dma("sp", badT[:], b_adaT, writes=["badT"])
            P.dma("sp", gm[:], gmixT, writes=["gm"])
            P.dma("sp", gf_[:], gffnT, writes=["gf_"])
            P.dma("sp", identf[:], tb["ident"], writes=["identf"])
            P.op("dve", lambda e: e.memset(epst[:], 1e-6), writes=["eps"])
            P.op("act", lambda e: e.activation(out=sil[:], in_=cc[:], func=AF.Silu), reads=["cc"], writes=["sil"])
            P.op("dve", lambda e: e.tensor_copy(out=identb[:], in_=identf[:]), reads=["identf"], writes=["identb"])
            for s in range(6):
                wt = wad[s % 2]
                wt3 = wt[:].rearrange("p (k n) -> p k n", k=8)
                for kc in range(8):
                    P.dma("sp" if kc % 2 == 0 else "act", wt3[:, kc, :], w_ada[kc * 128:(kc + 1) * 128, s * 1024:(s + 1) * 1024],
                          writes=[("wad", s % 2, kc)])
                for fc in range(8):
                    for kc in range(8):
                        P.op("pe", lambda e, fc=fc, kc=kc, wt3=wt3, s=s: e.matmul(
                            pm[:, s * 8 + fc:s * 8 + fc + 1], lhsT=wt3[:, kc, fc * 128:(fc + 1) * 128], rhs=sil[:, kc:kc + 1],
                            start=(kc == 0), stop=(kc == 7)), reads=[("wad", s % 2, kc), "sil"], writes=["pm"])
            P.op("dve", lambda e: e.tensor_tensor(out=modT[:], in0=pm[:], in1=badT[:], op=ALU.add), reads=["pm", "badT"], writes=["modT"])
            P.op("dve", lambda e: e.scalar_tensor_tensor(out=vec[:, 0:8], in0=modT[:, 8:16], scalar=1.0, in1=gm[:], op0=ALU.add, op1=ALU.mult),
                 reads=["modT", "gm"], writes=["vec"])
            P.op("dve", lambda e: e.scalar_tensor_tensor(out=vec[:, 16:24], in0=modT[:, 32:40], scalar=1.0, in1=gf_[:], op0=ALU.add, op1=ALU.mult),
                 reads=["modT", "gf_"], writes=["vec"])
            P.op("dve", lambda e: e.tensor_copy(out=vec[:, 8:16], in_=modT[:, 0:8]), reads=["modT"], writes=["vec"])
            P.op("dve", lambda e: e.tensor_copy(out=vec[:, 24:32], in_=modT[:, 24:32]), reads=["modT"], writes=["vec"])
            P.op("dve", lambda e: e.tensor_copy(out=vec[:, 32:40], in_=modT[:, 16:24]), reads=["modT"], writes=["vec"])
            P.op("dve", lambda e: e.tensor_copy(out=vec[:, 40:48], in_=modT[:, 40:48]), reads=["modT"], writes=["vec"])
            P.emit()

        for st in _blk(cfg.stop >= 1):
            sb = lambda name, shape, dt=F32: st.enter_context(nc.sbuf_tensor(name, shape, dt))
            ps = lambda name, shape, dt=F32: st.enter_context(nc.psum_tensor(name, shape, dt))
            P = Prog(nc, SEM)
            wib = sb("wib", [128, 8 * 3352], BF16)
            wib3 = wib[:].rearrange("p (k n) -> p k n", k=8)
            stg = [sb("stg%d" % i, [128, 838]) for i in range(2)]
            A1R = sb("A1R", [128, 1024]); B1R = sb("B1R", [128, 1024])
            n_ = 0
            for kc in range(8):
                for cq in range(4):
                    sl = slice(cq * 838, (cq + 1) * 838)
                    P.dma("sp", stg[n_ % 2][:], w_in[kc * 128:(kc + 1) * 128, sl], writes=[("stg", n_ % 2)])
                    if n_ % 2:
                        P.op("act", lambda e, kc=kc, sl=sl, n_=n_: e.copy(out=wib3[:, kc, sl], in_=stg[n_ % 2][:]), reads=[("stg", n_ % 2)], writes=["wib"])
                    else:
                        P.op("pool", lambda e, kc=kc, sl=sl, n_=n_: e.tensor_copy(out=wib3[:, kc, sl], in_=stg[n_ % 2][:]), reads=[("stg", n_ % 2)], writes=["wib"])
                    n_ += 1
            xt = [sb("xt%d" % i, [128, 1024]) for i in range(2)]
            sq = sb("sq", [128, 1024])
            ss = sb("ss", [128, 2]); rs = sb("rs", [128, 2])
            tmod = sb("tmod", [128, 1024])
            hb = sb("hb", [128, 1024], BF16)
            hT = [sb("hT%d" % i, [128, 1024], BF16) for i in range(2)]
            rp = [sb("rp%d" % i, [128, 192]) for i in range(2)]
            pj = [sb("pj%d" % i, [128, 512]) for i in range(3)]
            ra = sb("ra", [128, 256]); rb_ = sb("rb_", [128, 256]); rc_ = sb("rc_", [128, 256]); rd_ = sb("rd_", [128, 256])
            ktok = sb("ktok", [128, 512], BF16)
            qtok = sb("qtok", [128, 512], BF16)
            vtok = sb("vtok", [128, 512], BF16)
            vz = sb("vz", [128, 512], BF16)
            nk = sb("nk", [128, 384], BF16)
            nv = sb("nv", [128, 384], BF16)
            nq = sb("nq", [128, 512], BF16)
            Sst = sb("Sst", [128, 512]); Sb = sb("Sb", [128, 512], BF16)
            zt = sb("zt", [128, NW * 4]); dmat = sb("dmat", [128, 512]); xit = sb("xit", [128, 512])
            kTr = sb("kTr", [128, 512], BF16); qTr = sb("qTr", [128, 512], BF16); qxT = sb("qxT", [128, 512], BF16)
            pT = sb("pT", [128, 512], BF16)
            yv = sb("yv", [128, 512]); sg = sb("sg", [128, 512])
            st6 = sb("st6", [128, 4 * 6]); mv = sb("mv", [128, 4 * 2]); rstd4 = sb("rstd4", [128, 4])
            rawst = [sb("rawst%d" % i, [128, 256], BF16) for i in range(2)]
            p_tr = ps("p_tr", [128, 1024], BF16)
            p_mm = [ps("p_mm%d" % i, [128, 512]) for i in range(3)]
            p_kv = ps("p_kv", [128, 512])
            p_sc = ps("p_sc", [128, 512])
            p_y = ps("p_y", [128, 512])
            bcast_rows(P, sb, [(p_y, "p_y"), (p_sc, "p_sc")], [(0, A1R, "A1R"), (1, B1R, "B1R")])
            P.dma("sp", zt[:], tb["zeta"], writes=["zt"])
            P.dma("sp", dmat[:], tb["dmat"], writes=["dmat"])
            P.dma("sp", xit[:], tb["xi"], writes=["xit"])
            P.op("dve", lambda e: e.memset(Sst[:], 0.0), writes=["Sst"])
            P.op("dve", lambda e: e.memset(Sb[:], 0.0), writes=["Sb"])
            P.op("pool", lambda e: e.memset(vs[:], 1.0), writes=["vs"])
            P.op("pool", lambda e: e.memset(vw[:], 1.0), writes=["vw"])

            def rope(src, dst, H, D, cos, sin, keys_r, key_w):
                if os.environ.get("SKIP_ROPE"):
                    return
                h2 = D // 2
                s3 = src.rearrange("p (h d) -> p h d", h=H)
                d3 = dst.rearrange("p (h d) -> p h d", h=H)
                x1, x2 = s3[:, :, 0:h2], s3[:, :, h2:D]
                cb = cos.unsqueeze(1).to_broadcast([128, H, h2])
                sbb = sin.unsqueeze(1).to_broadcast([128, H, h2])
                n_ = H * h2
                v = lambda t_: t_[:, 0:n_].rearrange("p (h d) -> p h d", h=H)
                P.op("dve", lambda e: e.tensor_tensor(out=v(ra), in0=x1, in1=cb, op=ALU.mult), reads=keys_r, writes=["ra"])
                P.op(ROPE_ENG, lambda e: e.tensor_tensor(out=v(rb_), in0=x2, in1=sbb, op=ALU.mult), reads=keys_r, writes=["rb"])
                P.op("dve", lambda e: e.tensor_tensor(out=d3[:, :, 0:h2], in0=v(ra), in1=v(rb_), op=ALU.subtract), reads=["ra", "rb"], writes=[key_w + "_lo"])
                P.op(ROPE_ENG, lambda e: e.tensor_tensor(out=v(rc_), in0=x2, in1=cb, op=ALU.mult), reads=keys_r, writes=["rc"])
                P.op("dve", lambda e: e.tensor_tensor(out=v(rd_), in0=x1, in1=sbb, op=ALU.mult), reads=keys_r, writes=["rd"])
                P.op(ROPE_ENG, lambda e: e.tensor_tensor(out=d3[:, :, h2:D], in0=v(rc_), in1=v(rd_), op=ALU.add), reads=["rc", "rd"], writes=[key_w + "_hi"])

            def proj(blk, pdst, pkey, hTt, hkey):
                c0, w = blk
                for kc in range(8):
                    P.op("pe", lambda e, kc=kc: e.matmul(pdst[:, 0:w], lhsT=hTt[:, kc * 128:(kc + 1) * 128], rhs=wib3[:, kc, c0:c0 + w],
                                                        start=(kc == 0), stop=(kc == 7)), reads=[hkey, "wib"], writes=[pkey])

            for T in range(NW if cfg.astop >= 1 else 0):
                b = T % 2
                own = T >= T0
                i = T - T0
                P.dma("sp", xt[b][:], xw[T * 128:(T + 1) * 128, :], writes=[("xt", b)])
                P.dma("sp", rp[b][:], tb["rope"][T * 128:(T + 1) * 128, :], writes=[("rp", b)])
                P.op("act", lambda e, b=b: e.activation(out=sq[:], in_=xt[b][:], func=AF.Square), reads=[("xt", b)], writes=["sq"])
                P.op("dve", lambda e, b=b: e.reduce_sum(out=ss[:, b:b + 1], in_=sq[:], axis=AX.X), reads=["sq"], writes=[("ss", b)])
                P.op("act", lambda e, b=b: e.activation(out=rs[:, b:b + 1], in_=ss[:, b:b + 1], func=AF.Sqrt, scale=1.0 / 1024, bias=epst[:]),
                     reads=[("ss", b), "eps"], writes=[("rs", b)])
                P.op("dve", lambda e, b=b: e.reciprocal(out=rs[:, b:b + 1], in_=rs[:, b:b + 1]), reads=[("rs", b)], writes=[("rs", b)])
                P.op("dve", lambda e, b=b: e.scalar_tensor_tensor(out=tmod[:], in0=xt[b][:], scalar=rs[:, b:b + 1], in1=A1R[:], op0=ALU.mult, op1=ALU.mult),
                     reads=[("xt", b), ("rs", b), "A1R"], writes=["tmod"])
                P.op("pool", lambda e: e.tensor_tensor(out=hb[:], in0=tmod[:], in1=B1R[:], op=ALU.add), reads=["tmod", "B1R"], writes=["hb"])
                for kc in range(8):
                    P.op("pe", lambda e, kc=kc: e.transpose(p_tr[:, kc * 128:(kc + 1) * 128], hb[:, kc * 128:(kc + 1) * 128], identb[:]),
                         reads=["hb"], writes=["p_tr"])
                P.op("act", lambda e, b=b: e.copy(out=hT[b][:], in_=p_tr[:]), reads=["p_tr"], writes=[("hT", b)])
                hkey = ("hT", b)
                cos128, sin128, cos64, sin64 = rp[b][:, 0:64], rp[b][:, 64:128], rp[b][:, 128:160], rp[b][:, 160:192]
                if cfg.astop < 2:
                    continue
                proj(B_RK, p_mm[0], ("p_mm", 0), hT[b], hkey)
                P.op("act", lambda e: e.copy(out=pj[0][:], in_=p_mm[0][:]), reads=[("p_mm", 0)], writes=[("pj", 0)])
                rope(pj[0][:], ktok[:], 4, 128, cos128, sin128, [("pj", 0), ("rp", b)], "ktok")
                proj(B_RV, p_mm[1], ("p_mm", 1), hT[b], hkey)
                P.op("act", lambda e: e.copy(out=vtok[:], in_=p_mm[1][:]), reads=[("p_mm", 1)], writes=["vtok"])
                for h in range(0 if os.environ.get("SKIP_VZ") else 4):
                    P.op("dve", lambda e, h=h, T=T: e.tensor_scalar(out=vz[:, h * 128:(h + 1) * 128], in0=vtok[:, h * 128:(h + 1) * 128],
                                                                    scalar1=zt[:, T * 4 + h:T * 4 + h + 1], scalar2=None, op0=ALU.mult),
                         reads=["vtok", "zt"], writes=[("vz", h)])
                if cfg.astop < 2.2:
                    continue
                proj(B_NK, p_mm[2], ("p_mm", 2), hT[b], hkey)
                P.op("act", lambda e: e.copy(out=pj[2][:, 0:384], in_=p_mm[2][:, 0:384]), reads=[("p_mm", 2)], writes=[("pj", 2)])
                if cfg.astop < 2.5:
                    continue
                rope(pj[2][:, 0:384], nk[:], 6, 64, cos64, sin64, [("pj", 2), ("rp", b)], "nk")
                if cfg.astop < 2.8:
                    continue
                proj(B_NV, p_mm[0], ("p_mm", 0), hT[b], hkey)
                P.op("act", lambda e: e.copy(out=nv[:], in_=p_mm[0][:, 0:384]), reads=[("p_mm", 0)], writes=["nv"])
                if cfg.astop < 4:
                    continue
                P.op("dve", lambda e, T=T: e.tensor_copy(out=vs4[:, T, :, 0:64], in_=nv[:, 128:256].rearrange("p (g d) -> p g d", g=2)),
                     reads=["nv"], writes=["vs"])
                wk = T - (NW - NWK)
                if wk >= 0:
                    P.op("dve", lambda e, wk=wk: e.tensor_copy(out=vw4[:, wk, :, 0:64], in_=nv[:, 256:384].rearrange("p (g d) -> p g d", g=2)),
                         reads=["nv"], writes=["vw"])
                srcs = [nk[:, 0:128], nk[:, 128:256], nk[:, 256:384], nv[:, 0:128]]
                for q, s_ in enumerate(srcs):
                    P.op("pe", lambda e, q=q, s_=s_: e.transpose(p_tr[:, q * 128:(q + 1) * 128], s_, identb[:]),
                         reads=["nk_lo", "nk_hi", "nv"], writes=["p_tr"])
                P.op("act", lambda e, T=T: e.copy(out=ksT[:, T * 128:(T + 1) * 128], in_=p_tr[:, 128:256]), reads=["p_tr"], writes=["ksT"])
                if wk >= 0:
                    P.op("act", lambda e, wk=wk: e.copy(out=kwT[:, wk * 128:(wk + 1) * 128], in_=p_tr[:, 256:384]), reads=["p_tr"], writes=["kwT"])
                rw = rawst[T % 2]
                P.op("act", lambda e, rw=rw: e.copy(out=rw[:, 0:128], in_=p_tr[:, 0:128]), reads=["p_tr"], writes=[("rawst", T % 2)])
                P.op("act", lambda e, rw=rw: e.copy(out=rw[:, 128:256], in_=p_tr[:, 384:512]), reads=["p_tr"], writes=[("rawst", T % 2)])
                for kind in range(2):
                    P.dma("sp", rawd[kind, :, T * 128:(T + 1) * 128], rw[:, kind * 128:(kind + 1) * 128], reads=[("rawst", T % 2)])
                if cfg.astop < 5:
                    continue
                if own and cfg.astop >= 6:
                    proj(B_RQ, p_mm[1], ("p_mm", 1), hT[b], hkey)
                    P.op("act", lambda e: e.copy(out=pj[1][:], in_=p_mm[1][:]), reads=[("p_mm", 1)], writes=[("pj", 1)])
                    rope(pj[1][:], qtok[:], 4, 128, cos128, sin128, [("pj", 1), ("rp", b)], "qtok")
                    for h in range(4):
                        P.op("pe", lambda e, h=h: e.transpose(p_tr[:, h * 128:(h + 1) * 128], ktok[:, h * 128:(h + 1) * 128], identb[:]),
                             reads=["ktok_lo", "ktok_hi"], writes=["p_tr"])
                    P.op("act", lambda e: e.copy(out=kTr[:], in_=p_tr[:, 0:512]), reads=["p_tr"], writes=["kTr"])
                    for h in range(4):
                        P.op("pe", lambda e, h=h: e.transpose(p_tr[:, 512 + h * 128:512 + (h + 1) * 128], qtok[:, h * 128:(h + 1) * 128], identb[:]),
                             reads=["qtok_lo", "qtok_hi"], writes=["p_tr"])
                    P.op("act", lambda e: e.copy(out=qTr[:], in_=p_tr[:, 512:1024]), reads=["p_tr"], writes=["qTr"])
                    P.op("dve", lambda e: e.tensor_tensor(out=qxT[:], in0=qTr[:], in1=xit[:], op=ALU.mult), reads=["qTr", "xit"], writes=["qxT"])
                    for h in range(4):
                        hs = slice(h * 128, (h + 1) * 128)
                        P.op("pe", lambda e, hs=hs: e.matmul(p_sc[:, hs], lhsT=kTr[:, hs], rhs=qTr[:, hs], start=True, stop=True),
                             reads=["kTr", "qTr"], writes=["p_sc"])
                    P.op("dve", lambda e: e.tensor_tensor(out=pT[:], in0=p_sc[:], in1=dmat[:], op=ALU.mult), reads=["p_sc", "dmat"], writes=["pT"])
                    for h in range(4):
                        hs = slice(h * 128, (h + 1) * 128)
                        P.op("pe", lambda e, hs=hs: e.matmul(p_y[:, hs], lhsT=pT[:, hs], rhs=vtok[:, hs], start=True, stop=False),
                             reads=["pT", "vtok"], writes=["p_y"])
                        P.op("pe", lambda e, hs=hs: e.matmul(p_y[:, hs], lhsT=qxT[:, hs], rhs=Sb[:, hs], start=False, stop=True),
                             reads=["qxT", "Sb"], writes=["p_y"])
                    P.op("act", lambda e: e.copy(out=yv[:], in_=p_y[:]), reads=["p_y"], writes=["yv"])
                    for h in range(4):
                        P.op("dve", lambda e, h=h: e.bn_stats(out=st6[:, h * 6:(h + 1) * 6], in_=yv[:, h * 128:(h + 1) * 128]), reads=["yv"], writes=[("st6", h)])
                        P.op("dve", lambda e, h=h: e.bn_aggr(out=mv[:, h * 2:(h + 1) * 2], in_=st6[:, h * 6:(h + 1) * 6]), reads=[("st6", h)], writes=[("mv", h)])
                    mv3 = mv[:].rearrange("p (h t) -> p h t", t=2)
                    P.op("act", lambda e: e.activation(out=rstd4[:], in_=mv3[:, :, 1], func=AF.Sqrt, bias=epst[:]),
                         reads=[("mv", 0), ("mv", 1), ("mv", 2), ("mv", 3), "eps"], writes=["rstd4"])
                    P.op("dve", lambda e: e.reciprocal(out=rstd4[:], in_=rstd4[:]), reads=["rstd4"], writes=["rstd4"])
                    proj(B_RG, p_mm[2], ("p_mm", 2), hT[b], hkey)
                    P.op("act", lambda e: e.activation(out=sg[:], in_=p_mm[2][:], func=AF.Silu), reads=[("p_mm", 2)], writes=["sg"])
                    for h in range(4):
                        hs = slice(h * 128, (h + 1) * 128)
                        P.op("dve", lambda e, h=h, hs=hs: e.tensor_scalar(out=yv[:, hs], in0=yv[:, hs], scalar1=mv[:, 2 * h:2 * h + 1], scalar2=rstd4[:, h:h + 1],
                                                                         op0=ALU.subtract, op1=ALU.mult),
                             reads=["yv", ("mv", h), "rstd4"], writes=[("yn", h)])
                        P.op("pool", lambda e, h=h, hs=hs, i=i: e.tensor_tensor(out=reto[:, i * 512 + h * 128:i * 512 + (h + 1) * 128], in0=yv[:, hs], in1=sg[:, hs], op=ALU.mult),
                             reads=[("yn", h), "sg"], writes=["reto"])
                    proj(B_NQ, p_mm[0], ("p_mm", 0), hT[b], hkey)
                    P.op("act", lambda e: e.copy(out=pj[0][:], in_=p_mm[0][:]), reads=[("p_mm", 0)], writes=[("pj", 0)])
                    rope(pj[0][:], nq[:], 8, 64, cos64, sin64, [("pj", 0), ("rp", b)], "nq")
                    for h in range(4):
                        P.op("pe", lambda e, h=h: e.transpose(p_tr[:, h * 128:(h + 1) * 128], nq[:, h * 128:(h + 1) * 128], identb[:]),
                             reads=["nq_lo", "nq_hi"], writes=["p_tr"])
                    P.op("act", lambda e, i=i: e.copy(out=qTs[:, i * 512:(i + 1) * 512], in_=p_tr[:, 0:512]), reads=["p_tr"], writes=["qTs"])
                    proj(B_GT, p_mm[1], ("p_mm", 1), hT[b], hkey)
                    P.op("act", lambda e, i=i: e.activation(out=gts[:, i * 24:(i + 1) * 24], in_=p_mm[1][:, 0:24], func=AF.Sigmoid),
                         reads=[("p_mm", 1)], writes=["gts"])
                for h in range(4):
                    hs = slice(h * 128, (h + 1) * 128)
                    P.op("pe", lambda e, hs=hs: e.matmul(p_kv[:, hs], lhsT=ktok[:, hs], rhs=vz[:, hs], start=True, stop=True),
                         reads=["ktok_lo", "ktok_hi", ("vz", 0), ("vz", 1), ("vz", 2), ("vz", 3)], writes=["p_kv"])
                for h in range(4):
                    hs = slice(h * 128, (h + 1) * 128)
                    P.op("dve", lambda e, h=h, hs=hs: e.scalar_tensor_tensor(out=Sst[:, hs], in0=Sst[:, hs], scalar=float(np.exp(128 * LOGG[h])), in1=p_kv[:, hs],
                                                                            op0=ALU.mult, op1=ALU.add), reads=["p_kv", "Sst"], writes=["Sst"])
                P.op("act", lambda e: e.copy(out=Sb[:], in_=Sst[:]), reads=["Sst"], writes=["Sb"])
            P.emit()

        for st in _blk(cfg.stop >= 2):
            sb = lambda name, shape, dt=F32: st.enter_context(nc.sbuf_tensor(name, shape, dt))
            ps = lambda name, shape, dt=F32: st.enter_context(nc.psum_tensor(name, shape, dt))
            P = Prog(nc, SEM)
            NBP = NCH * 128
            raw = [sb("raw%d" % k, [128, RAWLEN], BF16) for k in range(2)]
            w1s = sb("w1s", [128, 32 * 256]); w1b = [sb("w1b%d" % k, [128, 32 * 256], BF16) for k in range(2)]
            pes = sb("pes", [128, 32]); peb = [sb("peb%d" % k, [128, 32], BF16) for k in range(2)]
            b1s = [sb("b1s%d" % k, [128, 2]) for k in range(2)]
            w2s = sb("w2s", [128, 128]); w2b = [sb("w2b%d" % k, [128, 128], BF16) for k in range(2)]
            cb = sb("cb", [128, 2])
            u = sb("u", [128, 512]); u2 = sb("u2", [128, 512]); u3 = sb("u3", [128, 512])
            hid = [sb("hid%d" % i, [128, 512], BF16) for i in range(2)]
            ovl = sb("ovl", [128, NCH * 128], BF16)
            p_h = ps("p_h", [128, 512]); p_c = ps("p_c", [128, 2]); p_o = ps("p_o", [128, 512])
            P.dma("sp", ovl[:], tb["overlap"], writes=["ovl"])
            P.op("pool", lambda e: e.memset(vca[:], 1.0), writes=["vca"])
            for cc_ in range(NCH):
                for g in range(2):
                    P.op("pool", lambda e, cc_=cc_, g=g: e.tensor_copy(out=vca4[:, cc_, g, 65:193], in_=ovl[:, cc_ * 128:(cc_ + 1) * 128]), reads=["ovl"], writes=["vca"])
            for kind, (w1d, ped, b1d, w2d) in enumerate([(w1k, peTk, b1k, w2k), (w1v, peTv, b1v, w2v)]):
                P.op("pool", lambda e, kind=kind: e.memset(raw[kind][:], 0.0), writes=[("raw", kind)])
                P.dma("sp", raw[kind][:, 0:S], rawd[kind], writes=[("raw", kind)])
                for half in range(2):
                    P.dma("sp", w1s[half * 64:(half + 1) * 64, :], w1d, writes=["w1s"])
                    P.dma("sp", pes[half * 64:(half + 1) * 64, :], ped, writes=["pes"])
                P.op("act", lambda e, kind=kind: e.copy(out=w1b[kind][:], in_=w1s[:]), reads=["w1s"], writes=[("w1b", kind)])
                P.op("dve", lambda e, kind=kind: e.tensor_copy(out=peb[kind][:], in_=pes[:]), reads=["pes"], writes=[("peb", kind)])
                P.dma("sp", b1s[kind][:], b1d, writes=[("b1s", kind)])
                P.dma("sp", w2s[:], w2d, writes=["w2s"])
                P.op("dve", lambda e, kind=kind: e.tensor_copy(out=w2b[kind][:], in_=w2s[:]), reads=["w2s"], writes=[("w2b", kind)])
                w13 = w1b[kind][:].rearrange("p (l n) -> p l n", l=32)
                for hc in range(2):
                    for l in range(32):
                        P.op("pe", lambda e, hc=hc, l=l, w13=w13, kind=kind: e.matmul(p_c[:, hc:hc + 1], lhsT=w13[0:64, l, hc * 128:(hc + 1) * 128], rhs=peb[kind][0:64, l:l + 1],
                                                                                    start=(l == 0), stop=(l == 31)), reads=[("w1b", kind), ("peb", kind)], writes=["p_c"])
                P.op("dve", lambda e, kind=kind: e.tensor_tensor(out=cb[:], in0=p_c[:], in1=b1s[kind][:], op=ALU.add), reads=["p_c", ("b1s", kind)], writes=["cb"])
                for g in range(2):
                    gs = slice(64 * g, 64 * g + 64)
                    for n0 in range(0, NBP, 512):
                        nn = min(512, NBP - n0)
                        for hc in range(2):
                            for l in range(32):
                                rhs = raw[kind][gs, l + 16 * n0: l + 16 * (n0 + nn): 16]
                                P.op("pe", lambda e, hc=hc, l=l, rhs=rhs, gs=gs, w13=w13, nn=nn: e.matmul(
                                    p_h[:, 0:nn], lhsT=w13[gs, l, hc * 128:(hc + 1) * 128], rhs=rhs, start=(l == 0), stop=(l == 31)),
                                    reads=[("w1b", kind), ("raw", kind)], writes=["p_h"])
                            P.op("act", lambda e, hc=hc, nn=nn: e.activation(out=u[:, 0:nn], in_=p_h[:, 0:nn], func=AF.Identity, bias=cb[:, hc:hc + 1]),
                                 reads=["p_h", "cb"], writes=["u"])
                            P.op("act", lambda e, nn=nn: e.activation(out=u2[:, 0:nn], in_=u[:, 0:nn], func=AF.Square), reads=["u"], writes=["u2"])
                            P.op("dve", lambda e, nn=nn: e.tensor_scalar(out=u2[:, 0:nn], in0=u2[:, 0:nn], scalar1=0.044715, scalar2=1.0, op0=ALU.mult, op1=ALU.add),
                                 reads=["u2"], writes=["u2"])
                            P.op("dve", lambda e, nn=nn: e.tensor_tensor(out=u3[:, 0:nn], in0=u2[:, 0:nn], in1=u[:, 0:nn], op=ALU.mult), reads=["u2", "u"], writes=["u3"])
                            P.op("act", lambda e, nn=nn: e.activation(out=u3[:, 0:nn], in_=u3[:, 0:nn], func=AF.Sigmoid, scale=1.5957691216), reads=["u3"], writes=["u3"])
                            P.op("dve", lambda e, hc=hc, nn=nn: e.tensor_tensor(out=hid[hc][:, 0:nn], in0=u3[:, 0:nn], in1=u[:, 0:nn], op=ALU.mult),
                                 reads=["u3", "u"], writes=[("hid", hc)])
                        w23 = w2b[kind][:].rearrange("p (c d) -> p c d", c=2)
                        if kind == 0:
                            for hc in range(2):
                                P.op("pe", lambda e, hc=hc, gs=gs, nn=nn, w23=w23: e.matmul(p_o[gs, 0:nn], lhsT=w23[:, hc, :], rhs=hid[hc][:, 0:nn], start=(hc == 0), stop=(hc == 1)),
                                     reads=[("hid", 0), ("hid", 1), ("w2b", 0)], writes=["p_o"])
                            P.op("act", lambda e, gs=gs, n0=n0, nn=nn: e.copy(out=kcT[gs, n0:n0 + nn], in_=p_o[gs, 0:nn]), reads=["p_o"], writes=["kcT"])
                        else:
                            for c4 in range(nn // 128):
                                for hc in range(2):
                                    P.op("pe", lambda e, hc=hc, c4=c4, w23=w23: e.matmul(p_o[:, c4 * 64:(c4 + 1) * 64], lhsT=hid[hc][:, c4 * 128:(c4 + 1) * 128], rhs=w23[:, hc, :],
                                                                                        start=(hc == 0), stop=(hc == 1)), reads=[("hid", 0), ("hid", 1), ("w2b", 1)], writes=["p_o"])
                                P.op("act", lambda e, c4=c4, g=g, n0=n0: e.copy(out=vca4[:, n0 // 128 + c4, g, 0:64], in_=p_o[:, c4 * 64:(c4 + 1) * 64]),
                                     reads=["p_o"], writes=["vca"])
            P.emit()

        for st in _blk(cfg.stop >= 3):
            sb = lambda name, shape, dt=F32: st.enter_context(nc.sbuf_tensor(name, shape, dt))
            ps = lambda name, shape, dt=F32: st.enter_context(nc.psum_tensor(name, shape, dt))
            P = Prog(nc, SEM)
            wob = sb("wob", [128, 8 * 1024], BF16)
            wob3 = wob[:].rearrange("p (k n) -> p k n", k=8)
            stg = [sb("stgc%d" % i, [128, 1024]) for i in range(2)]
            for kc in range(8):
                P.dma("sp", stg[kc % 2][:], w_out[kc * 128:(kc + 1) * 128, :], writes=[("stg", kc % 2)])
                P.op("pool", lambda e, kc=kc: e.tensor_copy(out=wob3[:, kc, :], in_=stg[kc % 2][:]), reads=[("stg", kc % 2)], writes=["wob"])
            expd = sb("expd", [128, S], BF16); tri = sb("tri", [128, 1024], BF16)
            kbias = sb("kbias", [128, NW]); cbias = sb("cbias", [128, NCH])
            cpen = [sb("cpen%d" % i, [128, NCH * 512], BF16) for i in range(2)]
            selm = [sb("selm%d" % i, [128, 256]) for i in range(2)]
            xo = [sb("xo%d" % i, [128, 1024]) for i in range(2)]
            eb = [sb("eb%d" % i, [128, 512], BF16) for i in range(3)]
            imp = sb("imp", [128, 128]); impm = sb("impm", [128, 128]); r1 = sb("r1", [128, 128]); r2 = sb("r2", [128, 128])
            m8 = sb("m8", [128, 16]); seln = sb("seln", [128, 128], BF16); selT = sb("selT", [128, 512], BF16)
            den = sb("den", [128, 4]); coef = sb("coef", [128, 4])
            nsa = sb("nsa", [128, 512])
            cat = sb("cat", [128, 1024], BF16); catT = sb("catT", [128, 1024], BF16)
            x1t = [sb("x1t%d" % i, [128, 1024]) for i in range(2)]
            p_s = [ps("p_s%d" % i, [128, 512]) for i in range(2)]
            p_a = [ps("p_a%d" % i, [128, 512]) for i in range(4)]
            p_x = ps("p_x", [128, 1024])
            GA1 = sb("GA1", [128, 1024])
            bcast_rows(P, sb, [(p_s[0], ("p_s", 0)), (p_s[1], ("p_s", 1))], [(4, GA1, "GA1")])
            P.dma("sp", expd[:], tb["expand"], writes=["expd"])
            P.dma("sp", tri[:], tb["tri"], writes=["tri"])
            P.dma("sp", kbias[:], tb["keybias"], writes=["kbias"])
            P.dma("sp", cbias[:], tb["cmpbias"], writes=["cbias"])
            p_xb = p_x[:].bitcast(BF16)
            gts4 = gts[:].rearrange("p (i g h c) -> p i g h c", g=2, h=4, c=3)
            nsa4 = nsa[:].rearrange("p (g h d) -> p g h d", g=2, h=4)
            sc_i = [0]
            eb_i = [0]

            def attend(i, g, chunks, first_branch, br):
                W = chunks[0]["v"].shape[-1]
                nchk = len(chunks)
                qg = qTs[64 * g:64 * g + 64, i * 512:(i + 1) * 512]
                for ci, ch in enumerate(chunks):
                    pb = sc_i[0] % 2; sc_i[0] += 1
                    pst = p_s[pb]
                    npen = len(ch["pens"])
                    P.op("pe", lambda e, ch=ch, pst=pst, npen=npen: e.matmul(pst[:], lhsT=ch["lhsT"], rhs=qg, start=True, stop=(npen == 0)),
                         reads=ch["keys_r"] + ["qTs"], writes=[("p_s", pb)])
                    for pi, (pl, pr, pk) in enumerate(ch["pens"]):
                        P.op("pe", lambda e, pl=pl, pr=pr, pst=pst, last=(pi == npen - 1): e.matmul(
                            pst[:], lhsT=pl, rhs=pr, start=False, stop=last), reads=pk, writes=[("p_s", pb)])
                    ei = eb_i[0] % 3; eb_i[0] += 1
                    et = eb[ei]
                    if ch["bias"] is None:
                        P.op("act", lambda e, et=et, pst=pst: e.activation(out=et[:], in_=pst[:], func=AF.Exp, scale=0.125), reads=[("p_s", pb)], writes=[("eb", ei)])
                    else:
                        P.op("act", lambda e, et=et, pst=pst, ch=ch: e.activation(out=et[:], in_=pst[:], func=AF.Exp, scale=0.125, bias=ch["bias"]),
                             reads=[("p_s", pb), "kbias", "cbias"], writes=[("eb", ei)])
                    for h in range(4):
                        P.op("pe", lambda e, h=h, et=et, ch=ch, ci=ci: e.matmul(p_a[h][:, 0:W], lhsT=et[:, h * 128:(h + 1) * 128], rhs=ch["v"],
                                                                              start=(ci == 0), stop=(ci == nchk - 1)),
                             reads=[("eb", ei)] + ch["keys_v"], writes=[("p_a", h)])
                for h in range(4):
                    P.op("dve", lambda e, h=h: e.tensor_scalar(out=den[:, h:h + 1], in0=p_a[h][:, 64:65], scalar1=1e-30, scalar2=None, op0=ALU.max),
                         reads=[("p_a", h)], writes=[("den", h)])
                    P.op("dve", lambda e, h=h: e.reciprocal(out=den[:, h:h + 1], in_=den[:, h:h + 1]), reads=[("den", h)], writes=[("den", h)])
                    P.op("dve", lambda e, h=h: e.tensor_tensor(out=coef[:, h:h + 1], in0=den[:, h:h + 1], in1=gts4[:, i, g, h, br:br + 1], op=ALU.mult),
                         reads=[("den", h), "gts"], writes=[("coef", h)])
                    if first_branch:
                        P.op("dve", lambda e, h=h: e.tensor_scalar(out=nsa4[:, g, h, :], in0=p_a[h][:, 0:64], scalar1=coef[:, h:h + 1], scalar2=None, op0=ALU.mult),
                             reads=[("p_a", h), ("coef", h)], writes=[("nsa", g, h)])
                    else:
                        P.op("dve", lambda e, h=h: e.scalar_tensor_tensor(out=nsa4[:, g, h, :], in0=p_a[h][:, 0:64], scalar=coef[:, h:h + 1], in1=nsa4[:, g, h, :],
                                                                         op0=ALU.mult, op1=ALU.add),
                             reads=[("p_a", h), ("coef", h), ("nsa", g, h)], writes=[("nsa", g, h)])

            for i in range(NO):
                T = T0 + i
                b = i % 2
                P.dma("sp", cpen[b][:], tb["cmppen"][i * 128:(i + 1) * 128, :], writes=[("cpen", b)])
                P.dma("sp", selm[b][:], tb["selm"][i * 128:(i + 1) * 128, :], writes=[("selm", b)])
                P.dma("sp", xo[b][:], xw[T * 128:(T + 1) * 128, :], writes=[("xo", b)])
                for g in range(2):
                    gs = slice(64 * g, 64 * g + 64)
                    chunks = []
                    for c_ in range(NCH):
                        chunks.append(dict(lhsT=kcT[gs, c_ * 128:(c_ + 1) * 128], keys_r=["kcT"],
                                           pens=[(identb[:], cpen[b][:, c_ * 512:(c_ + 1) * 512], [("cpen", b), "identb"])],
                                           bias=cbias[:, c_:c_ + 1], v=vca4[:, c_, g, :], keys_v=["vca"]))
                    attend(i, g, chunks, True, 0)
                    for h in range(4):
                        if h == 0:
                            P.op("dve", lambda e: e.tensor_scalar(out=imp[:], in0=p_a[0][:, 65:193], scalar1=den[:, 0:1], scalar2=None, op0=ALU.mult),
                                 reads=[("p_a", 0), ("den", 0)], writes=["imp"])
                        else:
                            P.op("dve", lambda e, h=h: e.scalar_tensor_tensor(out=imp[:], in0=p_a[h][:, 65:193], scalar=den[:, h:h + 1], in1=imp[:], op0=ALU.mult, op1=ALU.add),
                                 reads=[("p_a", h), ("den", h), "imp"], writes=["imp"])
                    P.op("dve", lambda e, b=b: e.tensor_tensor(out=impm[:], in0=imp[:], in1=selm[b][:, 0:128], op=ALU.mult), reads=["imp", ("selm", b)], writes=["impm"])
                    P.op("dve", lambda e, b=b: e.tensor_tensor(out=impm[:], in0=impm[:], in1=selm[b][:, 128:256], op=ALU.add), reads=["impm", ("selm", b)], writes=["impm"])
                    P.op("dve", lambda e: e.max(out=m8[:, 0:8], in_=impm[:]), reads=["impm"], writes=["m8a"])
                    P.op("dve", lambda e: e.match_replace(out=r1[:], in_to_replace=m8[:, 0:8], in_values=impm[:], imm_value=-BIG), reads=["impm", "m8a"], writes=["r1"])
                    P.op("dve", lambda e: e.max(out=m8[:, 8:16], in_=r1[:]), reads=["r1"], writes=["m8b"])
                    P.op("dve", lambda e: e.match_replace(out=r2[:], in_to_replace=m8[:, 8:16], in_values=r1[:], imm_value=-BIG), reads=["r1", "m8b"], writes=["r2"])
                    P.op("dve", lambda e: e.tensor_tensor(out=r1[:], in0=impm[:], in1=r2[:], op=ALU.subtract), reads=["impm", "r2"], writes=["r1"])
                    P.op("dve", lambda e: e.tensor_scalar(out=seln[:], in0=r1[:], scalar1=0.0, scalar2=NEG, op0=ALU.is_le, op1=ALU.mult), reads=["r1"], writes=["seln"])
                    P.op("pe", lambda e: e.transpose(p_xb[:, 0:128], seln[:], identb[:]), reads=["seln"], writes=["p_x"])
                    P.op("dve", lambda e: e.tensor_copy(out=selT[:].rearrange("p (h t) -> p h t", h=4), in_=p_xb[:, 0:128].unsqueeze(1).to_broadcast([128, 4, 128])),
                         reads=["p_x"], writes=["selT"])
                    chunks = []
                    for c_ in range(T + 1):
                        pens = [(expd[:, c_ * 128:(c_ + 1) * 128], selT[:], ["expd", "selT"])]
                        if c_ == T:
                            pens.append((identb[:], tri[:, 0:512], ["tri", "identb"]))
                        chunks.append(dict(lhsT=ksT[gs, c_ * 128:(c_ + 1) * 128], keys_r=["ksT"], pens=pens, bias=None, v=vs4[:, c_, g, :], keys_v=["vs"]))
                    attend(i, g, chunks, False, 1)
                    chunks = []
                    for c_ in range(max(0, T - 4), T + 1):
                        wk = c_ - (NW - NWK)
                        pens = []
                        if c_ == T - 4:
                            pens.append((identb[:], tri[:, 512:1024], ["tri", "identb"]))
                        if c_ == T:
                            pens.append((identb[:], tri[:, 0:512], ["tri", "identb"]))
                        chunks.append(dict(lhsT=kwT[gs, wk * 128:(wk + 1) * 128], keys_r=["kwT"], pens=pens, bias=kbias[:, c_:c_ + 1], v=vw4[:, wk, g, :], keys_v=["vw"]))
                    attend(i, g, chunks, False, 2)
                P.op("act", lambda e, i=i: e.copy(out=cat[:, 0:512], in_=reto[:, i * 512:(i + 1) * 512]), reads=["reto"], writes=["cat_a"])
                P.op("act", lambda e: e.copy(out=cat[:, 512:1024], in_=nsa[:]), reads=[("nsa", g_, h_) for g_ in range(2) for h_ in range(4)], writes=["cat_b"])
                for kc in range(8):
                    P.op("pe", lambda e, kc=kc: e.transpose(p_xb[:, kc * 128:(kc + 1) * 128], cat[:, kc * 128:(kc + 1) * 128], identb[:]),
                         reads=["cat_a", "cat_b"], writes=["p_x"])
                P.op("act", lambda e: e.copy(out=catT[:], in_=p_xb[:, 0:1024]), reads=["p_x"], writes=["catT"])
                for half in range(2):
                    for kc in range(8):
                        P.op("pe", lambda e, kc=kc, half=half: e.matmul(p_x[:, half * 512:(half + 1) * 512], lhsT=catT[:, kc * 128:(kc + 1) * 128],
                                                                        rhs=wob3[:, kc, half * 512:(half + 1) * 512], start=(kc == 0), stop=(kc == 7)),
                             reads=["catT", "wob"], writes=["p_x"])
                P.op("dve", lambda e, b=b: e.tensor_tensor(out=x1t[b][:], in0=p_x[:], in1=GA1[:], op=ALU.mult), reads=["p_x", "GA1"], writes=[("x1t", b)])
                P.op("pool", lambda e, b=b: e.tensor_tensor(out=x1t[b][:], in0=x1t[b][:], in1=xo[b][:], op=ALU.add), reads=[("x1t", b), ("xo", b)], writes=[("x1t", b)])
                P.dma("sp", x1d[i * 128:(i + 1) * 128, :], x1t[b][:], reads=[("x1t", b)])
            P.emit()

        mid.close()
        for st in _blk(cfg.stop >= 4):
            sb = lambda name, shape, dt=F32: st.enter_context(nc.sbuf_tensor(name, shape, dt))
            ps = lambda name, shape, dt=F32: st.enter_context(nc.psum_tensor(name, shape, dt))
            P = Prog(nc, SEM)
            wqb = sb("wqb", [128, 8 * 2048], BF16)
            wqb3 = wqb[:].rearrange("p (k n) -> p k n", k=8)
            stg = [sb("stgd%d" % i, [128, 2048]) for i in range(2)]
            for kc in range(8):
                P.dma("sp", stg[kc % 2][:], w_pq[kc * 128:(kc + 1) * 128, :], writes=[("stg", kc % 2)])
                P.op("act", lambda e, kc=kc: e.copy(out=wqb3[:, kc, :], in_=stg[kc % 2][:]), reads=[("stg", kc % 2)], writes=["wqb"])
            A2R = sb("A2R", [128, 1024]); B2R = sb("B2R", [128, 1024]); GA2 = sb("GA2", [128, 1024]); GF = sb("GF", [128, 1024])
            P.dma("sp", GF[:], gfin.partition_broadcast(128), writes=["GF"])
            skb = sb("skb", [128, 2048], BF16)
            P.dma("sp", stg[0][:], skT, writes=[("stg", 0)])
            P.op("act", lambda e: e.copy(out=skb[:], in_=stg[0][:]), reads=[("stg", 0)], writes=["skb"])
            selrow = sb("selrow", [128, 128 * 128], BF16)
            P.dma("sp", selrow[:], tb["selrow"], writes=["selrow"])
            selrow3 = selrow[:].rearrange("p (t m) -> p t m", t=128)
            io16 = sb("io16", [128, 16])
            P.dma("sp", io16[:], tb["iota16"], writes=["io16"])
            io128 = sb("io128", [128, 128]); cm = [sb("cm%d" % i_, [128, 128]) for i_ in range(3)]
            P.dma("sp", io128[:], tb["iota128"], writes=["io128"])
            x1 = sb("x1", [128, 1024]); sq = sb("sqd", [128, 1024]); ss = sb("ssd", [128, 1]); rs = sb("rsd", [128, 1])
            tmod = sb("tmodd", [128, 1024]); h2b = sb("h2b", [128, 1024], BF16); h2T = sb("h2T", [128, 1024], BF16)
            qT = sb("qT", [128, 2048], BF16)
            s_sb = sb("s_sb", [128, 2048]); s2 = sb("s2", [128, 128])
            V16 = sb("V16", [128, 256]); I16 = sb("I16", [128, 256], U32); I16f = sb("I16f", [128, 256])
            cand = sb("cand", [128, 2048]); c2 = sb("c2", [128, 256])
            TV = sb("TV", [128, 128]); TJ = sb("TJ", [128, 128], U32); TA = sb("TA", [128, 128], U32); TBb = sb("TBb", [128, 128], U32)
            TAf = sb("TAf", [128, 128]); TBf = sb("TBf", [128, 128])
            oh = sb("oh", [128, 2048]); i0 = sb("i0", [128, 128]); i1 = sb("i1", [128, 128]); ef = sb("ef", [128, 128])
            ex = sb("ex", [128, 128]); sm = sb("sm", [128, 8]); Wt = sb("Wt", [128, 128])
            idxT = sb("idxT", [128, 128], U32); WT = sb("WT", [128, 128]); aT = sb("aT", [128, 128]); cT = sb("cT", [128, 128])
            g2 = sb("g2", [128, 128]); g3 = sb("g3", [128, 128])
            U = [sb("U%d" % i, [128, 1024]) for i in range(3)]
            jk = sq
            oT = tmod; x2 = sb("x2", [128, 1024]); yo = sb("yo", [128, 1024])
            p_q = ps("p_q", [128, 512]); p_s4 = ps("p_s4", [128, 2048]); p_hb = ps("p_hb", [128, 1024]); p_t = ps("p_t", [128, 512])
            bcast_rows(P, sb, [(p_q, "p_q"), (p_t, "p_t")], [(2, A2R, "A2R"), (3, B2R, "B2R"), (5, GA2, "GA2")])
            V4 = V16[:].rearrange("p (c k) -> p c k", k=16); I4 = I16[:].rearrange("p (c k) -> p c k", k=16)
            s3 = s_sb[:].rearrange("p (c n) -> p c n", n=128)
            for i in range(NO):
                P.dma("sp", x1[:], x1d[i * 128:(i + 1) * 128, :], writes=["x1"])
                P.op("act", lambda e: e.activation(out=sq[:], in_=x1[:], func=AF.Square), reads=["x1"], writes=["sq"])
                P.op("dve", lambda e: e.reduce_sum(out=ss[:], in_=sq[:], axis=AX.X), reads=["sq"], writes=["ss"])
                P.op("act", lambda e: e.activation(out=rs[:], in_=ss[:], func=AF.Sqrt, scale=1.0 / 1024, bias=epst[:]), reads=["ss"], writes=["rs"])
                P.op("dve", lambda e: e.reciprocal(out=rs[:], in_=rs[:]), reads=["rs"], writes=["rs"])
                P.op("dve", lambda e: e.scalar_tensor_tensor(out=tmod[:], in0=x1[:], scalar=rs[:], in1=A2R[:], op0=ALU.mult, op1=ALU.mult), reads=["x1", "rs", "A2R"], writes=["tmod"])
                P.op("pool", lambda e: e.tensor_tensor(out=h2b[:], in0=tmod[:], in1=B2R[:], op=ALU.add), reads=["tmod", "B2R"], writes=["h2b"])
                p_tb = p_t[:].bitcast(BF16)
                for kc in range(8):
                    P.op("pe", lambda e, kc=kc: e.transpose(p_tb[:, kc * 128:(kc + 1) * 128], h2b[:, kc * 128:(kc + 1) * 128], identb[:]), reads=["h2b"], writes=["p_t"])
                P.op("act", lambda e: e.copy(out=h2T[:], in_=p_tb[:, 0:1024]), reads=["p_t"], writes=["h2T"])
                for c4 in range(4):
                    for cq in range(4):
                        ch = c4 * 4 + cq
                        for kc in range(8):
                            P.op("pe", lambda e, ch=ch, cq=cq, kc=kc: e.matmul(p_q[:, cq * 128:(cq + 1) * 128], lhsT=wqb3[:, kc, ch * 128:(ch + 1) * 128],
                                                                              rhs=h2T[:, kc * 128:(kc + 1) * 128], start=(kc == 0), stop=(kc == 7)),
                                 reads=["wqb", "h2T"], writes=["p_q"])
                    P.op("act", lambda e, c4=c4: e.copy(out=qT[:, c4 * 512:(c4 + 1) * 512], in_=p_q[:]), reads=["p_q"], writes=[("qT", c4)])
                for ch in range(16):
                    P.op("pe", lambda e, ch=ch: e.matmul(p_s4[:, ch * 128:(ch + 1) * 128], lhsT=qT[:, ch * 128:(ch + 1) * 128], rhs=skb[:, ch * 128:(ch + 1) * 128],
                                                        start=True, stop=True), reads=[("qT", ch // 4), "skb"], writes=["p_s4"])
                for c4 in range(4):
                    P.op("act", lambda e, c4=c4: e.copy(out=s_sb[:, c4 * 512:(c4 + 1) * 512], in_=p_s4[:, c4 * 512:(c4 + 1) * 512]), reads=["p_s4"], writes=[("s_sb", c4)])
                for ch in range(16):
                    sk = ("s_sb", ch // 4)
                    P.op("dve", lambda e, ch=ch: e.max(out=V4[:, ch, 0:8], in_=s3[:, ch, :]), reads=[sk], writes=[("V", ch, 0)])
                    P.op("dve", lambda e, ch=ch: e.max_index(out=I4[:, ch, 0:8], in_max=V4[:, ch, 0:8], in_values=s3[:, ch, :]), reads=[sk, ("V", ch, 0)], writes=[("I", ch, 0)])
                    P.op("dve", lambda e, ch=ch: e.match_replace(out=s2[:], in_to_replace=V4[:, ch, 0:8], in_values=s3[:, ch, :], imm_value=-BIG), reads=[sk, ("V", ch, 0)], writes=["s2"])
                    P.op("dve", lambda e, ch=ch: e.max(out=V4[:, ch, 8:16], in_=s2[:]), reads=["s2"], writes=[("V", ch, 1)])
                    P.op("dve", lambda e, ch=ch: e.max_index(out=I4[:, ch, 8:16], in_max=V4[:, ch, 8:16], in_values=s2[:]), reads=["s2", ("V", ch, 1)], writes=[("I", ch, 1)])
                allV = [("V", ch, k_) for ch in range(16) for k_ in range(2)]
                allI = [("I", ch, k_) for ch in range(16) for k_ in range(2)]
                P.op("dve", lambda e: e.tensor_copy(out=I16f[:], in_=I16[:]), reads=allI, writes=["I16f"])
                cand4 = cand[:].rearrange("p (h a b) -> p h a b", a=16, b=16)
                V5 = V16[:].rearrange("p (h t k) -> p h t k", t=2, k=16)
                I5 = I16f[:].rearrange("p (h t k) -> p h t k", t=2, k=16)
                for hd in range(8):
                    P.op("dve", lambda e, hd=hd: e.tensor_tensor(out=cand4[:, hd], in0=V5[:, hd, 0, :].unsqueeze(2).to_broadcast([128, 16, 16]),
                                                                 in1=V5[:, hd, 1, :].unsqueeze(1).to_broadcast([128, 16, 16]), op=ALU.add),
                         reads=allV, writes=[("cand", hd)])
                    cd = cand[:, hd * 256:(hd + 1) * 256]
                    P.op("dve", lambda e, hd=hd, cd=cd: e.max(out=TV[:, hd * 16:hd * 16 + 8], in_=cd), reads=[("cand", hd)], writes=[("TV", hd, 0)])
                    P.op("dve", lambda e, hd=hd, cd=cd: e.max_index(out=TJ[:, hd * 16:hd * 16 + 8], in_max=TV[:, hd * 16:hd * 16 + 8], in_values=cd),
                         reads=[("cand", hd), ("TV", hd, 0)], writes=[("TJ", hd, 0)])
                    P.op("dve", lambda e, hd=hd, cd=cd: e.match_replace(out=c2[:], in_to_replace=TV[:, hd * 16:hd * 16 + 8], in_values=cd, imm_value=-BIG),
                         reads=[("cand", hd), ("TV", hd, 0)], writes=["c2"])
                    P.op("dve", lambda e, hd=hd: e.max(out=TV[:, hd * 16 + 8:hd * 16 + 16], in_=c2[:]), reads=["c2"], writes=[("TV", hd, 1)])
                    P.op("dve", lambda e, hd=hd: e.max_index(out=TJ[:, hd * 16 + 8:hd * 16 + 16], in_max=TV[:, hd * 16 + 8:hd * 16 + 16], in_values=c2[:]),
                         reads=["c2", ("TV", hd, 1)], writes=[("TJ", hd, 1)])
                allTV = [("TV", hd, k_) for hd in range(8) for k_ in range(2)]
                allTJ = [("TJ", hd, k_) for hd in range(8) for k_ in range(2)]
                TV3 = TV[:].rearrange("p (h k) -> p h k", k=16)
                ex3 = ex[:].rearrange("p (h k) -> p h k", k=16)
                P.op("dve", lambda e: e.tensor_tensor(out=ex3, in0=TV3, in1=TV3[:, :, 0:1].to_broadcast([128, 8, 16]), op=ALU.subtract), reads=allTV, writes=["ex"])
                P.op("act", lambda e: e.activation(out=ex[:], in_=ex[:], func=AF.Exp), reads=["ex"], writes=["ex"])
                P.op("dve", lambda e: e.tensor_reduce(out=sm[:], in_=ex3, axis=AX.X, op=ALU.add), reads=["ex"], writes=["sm"])
                P.op("dve", lambda e: e.reciprocal(out=sm[:], in_=sm[:]), reads=["sm"], writes=["sm"])
                P.op("dve", lambda e: e.tensor_tensor(out=Wt[:].rearrange("p (h k) -> p h k", k=16), in0=ex3, in1=sm[:].unsqueeze(2).to_broadcast([128, 8, 16]), op=ALU.mult),
                     reads=["ex", "sm"], writes=["Wt"])
                P.op("dve", lambda e: e.tensor_single_scalar(out=TA[:], in_=TJ[:], scalar=4, op=ALU.logical_shift_right), reads=allTJ, writes=["TA"])
                P.op("dve", lambda e: e.tensor_single_scalar(out=TBb[:], in_=TJ[:], scalar=15, op=ALU.bitwise_and), reads=allTJ, writes=["TB"])
                P.op("dve", lambda e: e.tensor_copy(out=TAf[:], in_=TA[:]), reads=["TA"], writes=["TAf"])
                P.op("dve", lambda e: e.tensor_copy(out=TBf[:], in_=TBb[:]), reads=["TB"], writes=["TBf"])
                oh4 = oh[:].rearrange("p (h k a) -> p h k a", k=16, a=16)
                for which, (tf, dst) in enumerate([(TAf, i0), (TBf, i1)]):
                    tf3 = tf[:].rearrange("p (h k) -> p h k", k=16)
                    P.op("dve", lambda e, tf3=tf3: e.tensor_tensor(out=oh4, in0=tf3.unsqueeze(3).to_broadcast([128, 8, 16, 16]),
                                                                  in1=io16[:].unsqueeze(1).unsqueeze(1).to_broadcast([128, 8, 16, 16]), op=ALU.is_equal),
                         reads=["TAf", "TBf", "io16"], writes=["oh"])
                    P.op("dve", lambda e, which=which: e.tensor_tensor(out=oh4, in0=oh4, in1=I5[:, :, which, :].unsqueeze(2).to_broadcast([128, 8, 16, 16]), op=ALU.mult),
                         reads=["oh", "I16f"], writes=["oh"])
                    P.op("dve", lambda e, dst=dst: e.tensor_reduce(out=dst[:], in_=oh4, axis=AX.X, op=ALU.add), reads=["oh"], writes=["i01_%d" % which])
                P.op("dve", lambda e: e.scalar_tensor_tensor(out=ef[:], in0=i0[:], scalar=128.0, in1=i1[:], op0=ALU.mult, op1=ALU.add), reads=["i01_0", "i01_1"], writes=["ef"])
                P.op("pe", lambda e: e.transpose(p_t[:, 0:128], ef[:], identf[:]), reads=["ef"], writes=["p_t"])
                P.op("pe", lambda e: e.transpose(p_t[:, 128:256], Wt[:], identf[:]), reads=["Wt"], writes=["p_t"])
                P.op("dve", lambda e: e.tensor_copy(out=idxT[:], in_=p_t[:, 0:128]), reads=["p_t"], writes=["idxT"])
                P.op("dve", lambda e: e.tensor_copy(out=WT[:], in_=p_t[:, 128:256]), reads=["p_t"], writes=["WT"])
                for t_ in range(128):
                    ub = U[t_ % 3]
                    P.op("pool", lambda e, ub=ub, t_=t_: e.indirect_dma_start(out=ub[:], out_offset=None, in_=pdown,
                                                                             in_offset=bass.IndirectOffsetOnAxis(ap=idxT[:, t_:t_ + 1], axis=0)),
                         reads=["idxT"], writes=[("U", t_ % 3)], dma=True)
                    for half in range(2):
                        P.op("pe", lambda e, t_=t_, half=half: e.matmul(p_hb[:, half * 512:(half + 1) * 512], lhsT=selrow3[:, t_, :], rhs=h2b[:, half * 512:(half + 1) * 512],
                                                                        start=True, stop=True), reads=["selrow", "h2b"], writes=["p_hb"])
                    P.op("dve", lambda e, ub=ub, t_=t_: e.tensor_tensor_reduce(out=jk[:], in0=ub[:], in1=p_hb[:], scale=1.0, scalar=0.0, op0=ALU.mult, op1=ALU.add,
                                                                              accum_out=aT[:, t_:t_ + 1]), reads=[("U", t_ % 3), "p_hb"], writes=["sq", "aT"])
                P.op("act", lambda e: e.activation(out=g2[:], in_=aT[:], func=AF.Square), reads=["aT"], writes=["g2"])
                P.op("dve", lambda e: e.tensor_scalar(out=g2[:], in0=g2[:], scalar1=0.044715, scalar2=1.0, op0=ALU.mult, op1=ALU.add), reads=["g2"], writes=["g2"])
                P.op("dve", lambda e: e.tensor_tensor(out=g3[:], in0=g2[:], in1=aT[:], op=ALU.mult), reads=["g2", "aT"], writes=["g3"])
                P.op("act", lambda e: e.activation(out=g3[:], in_=g3[:], func=AF.Sigmoid, scale=1.5957691216), reads=["g3"], writes=["g3"])
                P.op("dve", lambda e: e.tensor_tensor(out=g3[:], in0=g3[:], in1=aT[:], op=ALU.mult), reads=["g3", "aT"], writes=["g3"])
                P.op("dve", lambda e: e.tensor_tensor(out=cT[:], in0=g3[:], in1=WT[:], op=ALU.mult), reads=["g3", "WT"], writes=["cT"])
                for t_ in range(128):
                    ub = U[t_ % 3]
                    P.op("pool", lambda e, ub=ub, t_=t_: e.indirect_dma_start(out=ub[:], out_offset=None, in_=pup,
                                                                             in_offset=bass.IndirectOffsetOnAxis(ap=idxT[:, t_:t_ + 1], axis=0)),
                         reads=["idxT"], writes=[("U", t_ % 3)], dma=True)
                    cmt = cm[t_ % 3]
                    P.op("dve", lambda e, cmt=cmt, t_=t_: e.scalar_tensor_tensor(out=cmt[:], in0=io128[:], scalar=float(t_), in1=cT[:, t_:t_ + 1].to_broadcast([128, 128]),
                                                                                op0=ALU.is_equal, op1=ALU.mult), reads=["io128", "cT"], writes=[("cm", t_ % 3)])
                    for half in range(2):
                        P.op("pe", lambda e, ub=ub, cmt=cmt, t_=t_, half=half: e.matmul(p_s4[:, half * 512:(half + 1) * 512], lhsT=cmt[:],
                                                                                      rhs=ub[:, half * 512:(half + 1) * 512], start=(t_ == 0), stop=(t_ == 127)),
                             reads=[("U", t_ % 3), ("cm", t_ % 3)], writes=["p_s4"])
                P.op("dve", lambda e: e.tensor_tensor(out=x2[:], in0=p_s4[:, 0:1024], in1=GA2[:], op=ALU.mult), reads=["p_s4", "GA2"], writes=["x2"])
                P.op("pool", lambda e: e.tensor_tensor(out=x2[:], in0=x2[:], in1=x1[:], op=ALU.add), reads=["x2", "x1"], writes=["x2"])
                P.op("act", lambda e: e.activation(out=sq[:], in_=x2[:], func=AF.Square), reads=["x2"], writes=["sq"])
                P.op("dve", lambda e: e.reduce_sum(out=ss[:], in_=sq[:], axis=AX.X), reads=["sq"], writes=["ss"])
                P.op("act", lambda e: e.activation(out=rs[:], in_=ss[:], func=AF.Sqrt, scale=1.0 / 1024, bias=epst[:]), reads=["ss"], writes=["rs"])
                P.op("dve", lambda e: e.reciprocal(out=rs[:], in_=rs[:]), reads=["rs"], writes=["rs"])
                P.op("dve", lambda e: e.scalar_tensor_tensor(out=yo[:], in0=x2[:], scalar=rs[:], in1=GF[:], op0=ALU.mult, op1=ALU.mult), reads=["x2", "rs", "GF"], writes=["yo"])
                P.dma("sp", out[i * 128:(i + 1) * 128, :], yo[:], reads=["yo"])
            P.emit()
    return nc


def make_inputs(cfg, core, inp, n_per_batch=4, S_full=None):
    NW, NO = cfg.NW, cfg.NO
    b, j = core // n_per_batch, core % n_per_batch
    off = NO * 128 * (j + 1) - NW * 128
    x = inp["x"][b]
    xw = np.zeros((NW * 128, 1024), np.float32)
    lo = max(0, -off)
    xw[lo:] = x[off + lo: off + NW * 128]
    m = {"xw": xw}
    m["ccol"] = np.ascontiguousarray(inp["c"][b].reshape(8, 128).T)
    m["w_ada"] = inp["w_ada"][0]
    m["b_adaT"] = np.ascontiguousarray(inp["b_ada"][0].reshape(48, 128).T)
    m["gmixT"] = np.ascontiguousarray(inp["g_norm_mix"][0].reshape(8, 128).T)
    m["gffnT"] = np.ascontiguousarray(inp["g_norm_ffn"][0].reshape(8, 128).T)
    m["gfin"] = inp["g_norm_final"].reshape(1, 1024)
    m["w_in"] = np.ascontiguousarray(inp["w_in"][0][:, w_in_perm()])
    m["w_out"] = inp["w_out"][0]
    for nm, key in (("w1k", "w_cmp_k1"), ("w1v", "w_cmp_v1")):
        m[nm] = np.ascontiguousarray(inp[key][0].reshape(32, 64, 256).transpose(1, 0, 2).reshape(64, 32 * 256))
    m["peTk"] = np.ascontiguousarray(inp["pe_cmp_k"][0].T)
    m["peTv"] = np.ascontiguousarray(inp["pe_cmp_v"][0].T)
    m["b1k"] = np.ascontiguousarray(inp["b_cmp_k1"][0].reshape(2, 128).T)
    m["b1v"] = np.ascontiguousarray(inp["b_cmp_v1"][0].reshape(2, 128).T)
    m["w2k"] = np.ascontiguousarray(inp["w_cmp_k2"][0].reshape(2, 128, 64).transpose(1, 0, 2).reshape(128, 128))
    m["w2v"] = np.ascontiguousarray(inp["w_cmp_v2"][0].reshape(2, 128, 64).transpose(1, 0, 2).reshape(128, 128))
    m["w_pq"] = inp["w_peer_q"][0]
    m["skT"] = np.ascontiguousarray(inp["peer_subkeys"][0].reshape(16, 128, 128).transpose(2, 0, 1).reshape(128, 2048))
    m["pdown"] = inp["peer_down"][0]
    m["pup"] = inp["peer_up"][0]
    for k_, v_ in host_tables(cfg, off).items():
        m["t_" + k_] = v_
    return m


_NC_CACHE = {}


def kernel(**inputs):
    inp = {k: np.asarray(v) for k, v in inputs.items()}
    cfg = CFG()
    if "nc" not in _NC_CACHE:
        _NC_CACHE["nc"] = build(cfg)
        mybir.codegen_inst_isa_subclasses(_NC_CACHE["nc"])
    nc = _NC_CACHE["nc"]
    in_maps = [make_inputs(cfg, c, inp) for c in range(8)]
    res = run_bass_kernel_spmd(nc, in_maps, core_ids=list(range(8)))
    out = np.zeros((2, 8192, 1024), np.float32)
    for c in range(8):
        b, j = c // 4, c % 4
        out[b, j * 2048:(j + 1) * 2048] = res.results[c]["out"]
    return out
```

```python
import numpy as np
import ml_dtypes
from contextlib import ExitStack
import concourse.bass as bass
import concourse.mybir as mybir
from concourse.bass_utils import run_bass_kernel_spmd

F32 = mybir.dt.float32
BF16 = mybir.dt.bfloat16
U32 = mybir.dt.uint32
F32R = mybir.dt.float32r
ALU = mybir.AluOpType
AF = mybir.ActivationFunctionType
AX = mybir.AxisListType

import os
ROPE_ENG = os.environ.get("ROPE_ENG", "dve")
NEG = -30000.0
BIG = 1.0e30
COMPUTE = ("pe", "act", "dve", "pool")


class _Op:
    __slots__ = ("eng", "fn", "reads", "writes", "deps", "is_dma", "signal", "ev", "id")


class SemState:
    def __init__(self, nc, stack, n_dma_sems=24):
        self.n_dma_sems = n_dma_sems
        self.sems = {}
        for e in COMPUTE:
            self.sems[e] = stack.enter_context(nc.semaphore("s_" + e))
        for k in range(n_dma_sems):
            self.sems["d%d" % k] = stack.enter_context(nc.semaphore("s_d%d" % k))
        self.cnt = {e: 0 for e in COMPUTE}
        self.dma_cnt = [0] * n_dma_sems


class Prog:
    def __init__(self, nc, state):
        self.nc = nc
        self.state = state
        self.ops = []
        self.last_writer = {}
        self.readers = {}
        self.n_dma_sems = state.n_dma_sems
        self.dma_rr = 0
        self.sw_rr = 0
        self.dma_last = [None] * self.n_dma_sems
        self.dma_cnt = state.dma_cnt

    def op(self, eng, fn, reads=(), writes=(), dma=False):
        o = _Op()
        o.eng, o.fn, o.is_dma = eng, fn, dma
        o.reads, o.writes = tuple(reads), tuple(writes)
        o.deps = set()
        o.signal = False
        o.ev = None
        o.id = len(self.ops)
        for r in o.reads:
            w = self.last_writer.get(r)
            if w is not None:
                o.deps.add(w)
        for w_ in o.writes:
            w = self.last_writer.get(w_)
            if w is not None:
                o.deps.add(w)
            lastc = {}
            for rd in self.readers.get(w_, ()):
                p_ = self.ops[rd]
                if p_.is_dma:
                    o.deps.add(rd)
                else:
                    lastc[p_.eng] = rd
            o.deps.update(lastc.values())
        for r in o.reads:
            self.readers.setdefault(r, []).append(o.id)
        for w_ in o.writes:
            self.last_writer[w_] = o.id
            self.readers[w_] = []
        o.deps.discard(o.id)
        if dma:
            if eng == "pool":
                k = self.n_dma_sems - 4 + self.sw_rr
                self.sw_rr = (self.sw_rr + 1) % 4
            else:
                k = self.dma_rr
                self.dma_rr = (self.dma_rr + 1) % (self.n_dma_sems - 4)
            prev = self.dma_last[k]
            if prev is not None:
                o.deps.add(prev)
            self.dma_last[k] = o.id
            self.dma_cnt[k] += 16
            o.ev = ("d%d" % k, self.dma_cnt[k])
        self.ops.append(o)
        return o.id

    def dma(self, eng, out, in_, reads=(), writes=(), **kw):
        return self.op(eng, lambda e: e.dma_start(out=out, in_=in_, **kw), reads, writes, dma=True)

    def emit(self):
        nc, ops = self.nc, self.ops
        for o in ops:
            nd = set()
            for d in o.deps:
                p = ops[d]
                if p.is_dma or o.is_dma or p.eng != o.eng:
                    nd.add(d)
                elif o.eng != "pe":
                    nd.add(d)
            o.deps = nd
            for d in nd:
                if not ops[d].is_dma:
                    ops[d].signal = True
        cnt = self.state.cnt
        for o in ops:
            if not o.is_dma and o.signal:
                cnt[o.eng] += 1
                o.ev = (o.eng, cnt[o.eng])
        finals = [ops[i] for i in self.dma_last if i is not None]
        with ExitStack() as st:
            sems = self.state.sems
            block = st.enter_context(nc.Block())
            streams = {}
            for o in ops:
                streams.setdefault(o.eng, []).append(o)

            def run_stream(ename, e, final=False):
                waited = {}

                def wait(ev):
                    if waited.get(ev[0], 0) < ev[1]:
                        e.wait_ge(sems[ev[0]], ev[1])
                        waited[ev[0]] = ev[1]

                for o in streams.get(ename, []):
                    for d in sorted(o.deps):
                        wait(ops[d].ev)
                    ins = o.fn(e)
                    if o.is_dma:
                        ins.then_inc(sems[o.ev[0]], 16)
                    elif o.signal:
                        ins.then_inc(sems[o.eng], 1)
                if final:
                    for o in finals:
                        wait(o.ev)

            block.sync(lambda e: run_stream("sp", e, final=True))
            block.scalar(lambda e: run_stream("act", e))
            block.vector(lambda e: run_stream("dve", e))
            block.gpsimd(lambda e: run_stream("pool", e))
            block.tensor(lambda e: run_stream("pe", e))


def _blk(flag):
    if flag:
        with ExitStack() as st:
            yield st


class CFG:
    def __init__(self, NW=64, NO=16, NEXP=16384, stop=9):
        self.NW, self.NO, self.NEXP, self.stop = NW, NO, NEXP, stop
        self.astop = 99
        self.NB = NW * 8
        self.NCH = max(1, self.NB // 128)
        self.NWK = min(NW, NO + 4)
        self.NSB = NW * 2


LOGG = [float(np.log1p(-np.exp2(-5.0 - h))) for h in range(4)]


def host_tables(cfg, off):
    NW, NO, NB, NCH = cfg.NW, cfg.NO, cfg.NB, cfg.NCH
    S = NW * 128
    p = (off + np.arange(S)).astype(np.float32)
    t = {}
    inv128 = (10000.0 ** (-np.arange(0, 128, 2, dtype=np.float32) / 128)).astype(np.float32)
    inv64 = (10000.0 ** (-np.arange(0, 64, 2, dtype=np.float32) / 64)).astype(np.float32)
    a128 = p[:, None] * inv128[None]
    a64 = p[:, None] * inv64[None]
    t["rope"] = np.concatenate([np.cos(a128), np.sin(a128), np.cos(a64), np.sin(a64)], 1).astype(np.float32)
    valid_tile = ((off + 128 * np.arange(NW)) >= 0).astype(np.float32)
    n = np.arange(128, dtype=np.float32)
    sc = 128 ** -0.5
    zt = np.stack([np.exp((127 - n) * LOGG[h]) * sc for h in range(4)], 1)
    t["zeta"] = (zt[:, None, :] * valid_tile[None, :, None]).astype(np.float32).reshape(128, NW * 4)
    dm = np.zeros((128, 4, 128), np.float32)
    for h in range(4):
        d = n[None, :] - n[:, None]
        dm[:, h, :] = np.where(d >= 0, np.exp(np.where(d >= 0, d, 0) * LOGG[h]), 0.0) * sc
    t["dmat"] = dm.reshape(128, 512)
    xi = np.stack([np.exp((n + 1.0) * LOGG[h]) for h in range(4)], 0)
    t["xi"] = np.broadcast_to(xi[None], (128, 4, 128)).reshape(128, 512).astype(np.float32).copy()
    t["keybias"] = np.broadcast_to(np.where(valid_tile > 0, 0.0, NEG)[None], (128, NW)).astype(np.float32).copy()
    nb = np.arange(NCH * 128)
    cvalid = ((off + 16 * nb) >= 0) & (nb <= NB - 2)
    t["cmpbias"] = np.where(cvalid, 0.0, NEG).astype(np.float32).reshape(NCH, 128).T.copy()
    tq = (NW - NO) * 128 + np.arange(NO * 128)
    cp = np.where((16 * nb[:, None] + 31) <= tq[None, :], 0.0, NEG)
    cp = cp.reshape(NCH, 128, NO, 128).transpose(2, 1, 0, 3)
    cp = np.broadcast_to(cp[:, :, :, None, :], (NO, 128, NCH, 4, 128))
    t["cmppen"] = cp.astype(ml_dtypes.bfloat16).reshape(NO * 128, NCH * 512)
    k = np.arange(128)
    t["tri"] = np.concatenate([np.tile(np.where(k[:, None] <= k[None, :], 0.0, NEG), (1, 4)),
                               np.tile(np.where(k[:, None] > k[None, :], 0.0, NEG), (1, 4))], 1).astype(ml_dtypes.bfloat16)
    t["iota128"] = np.broadcast_to(np.arange(128, dtype=np.float32)[None], (128, 128)).copy()
    mb = np.arange(128)
    cur = tq // 64
    b0 = (-off) // 64
    validb = (mb[None, :] >= b0) & (mb[None, :] <= cur[:, None]) & (mb[None, :] < NW * 2)
    forced = ((mb[None, :] == b0) | (mb[None, :] == cur[:, None]) | (mb[None, :] == cur[:, None] - 1)) & validb
    m1 = (validb & ~forced).astype(np.float32)
    m2 = np.where(forced, 1e9, np.where(validb, 0.0, -BIG)).astype(np.float32)
    t["selm"] = np.concatenate([m1, m2], 1)
    key = np.arange(S)
    ex = np.zeros((128, S), np.float32)
    ex[key // 64, key] = 1.0
    t["expand"] = ex.astype(ml_dtypes.bfloat16)
    cs = 16 * nb
    ss = 64 * mb
    ov = np.clip(np.minimum(cs[:, None] + 32, ss[None, :] + 64) - np.maximum(cs[:, None], ss[None, :]), 0, None) / 16.0
    t["overlap"] = ov.reshape(NCH, 128, 128).transpose(1, 0, 2).reshape(128, NCH * 128).astype(ml_dtypes.bfloat16)
    t["ident"] = np.eye(128, dtype=np.float32)
    sel = np.zeros((128, 128, 128), np.float32)
    sel[k, k, :] = 1.0
    t["selrow"] = sel.reshape(128, 128 * 128).astype(ml_dtypes.bfloat16)
    t["iota16"] = np.broadcast_to(np.arange(16, dtype=np.float32)[None], (128, 16)).copy()
    return t


def w_in_perm():
    r = lambda a, b: list(range(a, b))
    return np.array(r(512, 1024) + r(1024, 1536) + r(2560, 2688) + r(2816, 2944) + r(3072, 3200)
                    + r(2688, 2816) + r(2944, 3072) + r(3200, 3328)
                    + r(0, 512) + r(1536, 2048)
                    + [2048 + (g * 4 + h) * 64 + d for h in range(4) for g in range(2) for d in range(64)]
                    + r(3328, 3352))


B_RK, B_RV, B_NK, B_NV, B_RQ, B_RG, B_NQ, B_GT = (0, 512), (512, 512), (1024, 384), (1408, 384), \
    (1792, 512), (2304, 512), (2816, 512), (3328, 24)


def build(cfg):
    NW, NO, NB, NCH, NWK = cfg.NW, cfg.NO, cfg.NB, cfg.NCH, cfg.NWK
    T0 = NW - NO
    S = NW * 128
    nc = bass.Bass("TRN2", target_bir_lowering=False)
    dram = lambda name, shape, dt=F32, kind="ExternalInput": nc.dram_tensor(name, shape, dt, kind=kind).ap()
    xw = dram("xw", [S, 1024])
    ccol = dram("ccol", [128, 8])
    w_ada = dram("w_ada", [1024, 6144])
    b_adaT = dram("b_adaT", [128, 48])
    gmixT = dram("gmixT", [128, 8])
    gffnT = dram("gffnT", [128, 8])
    gfin = dram("gfin", [1, 1024])
    w_in = dram("w_in", [1024, 3352])
    w_out = dram("w_out", [1024, 1024])
    w1k = dram("w1k", [64, 32 * 256])
    w1v = dram("w1v", [64, 32 * 256])
    peTk = dram("peTk", [64, 32])
    peTv = dram("peTv", [64, 32])
    b1k = dram("b1k", [128, 2])
    b1v = dram("b1v", [128, 2])
    w2k = dram("w2k", [128, 2 * 64])
    w2v = dram("w2v", [128, 2 * 64])
    w_pq = dram("w_pq", [1024, 2048])
    skT = dram("skT", [128, 2048])
    pdown = dram("pdown", [cfg.NEXP, 1024])
    pup = dram("pup", [cfg.NEXP, 1024])
    tb = {}
    for name, shape, dt in [("rope", [S, 192], F32), ("zeta", [128, NW * 4], F32), ("dmat", [128, 512], F32),
                            ("xi", [128, 512], F32), ("keybias", [128, NW], F32), ("cmpbias", [128, NCH], F32),
                            ("cmppen", [NO * 128, NCH * 512], BF16), ("tri", [128, 1024], BF16), ("iota128", [128, 128], F32),
                            ("selm", [NO * 128, 256], F32), ("expand", [128, S], BF16),
                            ("overlap", [128, NCH * 128], BF16), ("ident", [128, 128], F32),
                            ("selrow", [128, 128 * 128], BF16), ("iota16", [128, 16], F32)]:
        tb[name] = dram("t_" + name, shape, dt)
    out = dram("out", [NO * 128, 1024], kind="ExternalOutput")
    rawd = dram("rawd", [2, 128, S], BF16, kind="Internal")
    RAWLEN = max(S + 16, 16 * NCH * 128 + 32)
    x1d = dram("x1d", [NO * 128, 1024], kind="Internal")
    pdb = dram("pdb", [cfg.NEXP, 1024], BF16, kind="Internal")
    pub = dram("pub", [cfg.NEXP, 1024], BF16, kind="Internal")

    with ExitStack() as outer:
        sbo = lambda name, shape, dt=F32: outer.enter_context(nc.sbuf_tensor(name, shape, dt))
        SEM = SemState(nc, outer)
        vec = sbo("vec", [128, 48])

        _bc = [0]

        def bcast_rows(P, sb, pbrk, items):
            _bc[0] += 1
            onesf = sb("onesf%d" % _bc[0], [128, 128]); diag = [sb("diag%d_%d" % (i_, _bc[0]), [128, 128]) for i_ in range(2)]
            P.op("dve", lambda e: e.memset(onesf[:], 1.0), writes=["onesf"])
            n = 0
            for vi, dst, dkey in items:
                for half in range(2):
                    pb, pkey = pbrk[n % 2]
                    for q in range(4):
                        fc = half * 4 + q
                        dg = diag[q % 2]
                        P.op("dve", lambda e, dg=dg, vi=vi, fc=fc: e.tensor_scalar(out=dg[:], in0=identf[:], scalar1=vec[:, vi * 8 + fc:vi * 8 + fc + 1],
                                                                                  scalar2=None, op0=ALU.mult),
                             reads=["identf", "vec"], writes=[("diag", q % 2)])
                        P.op("pe", lambda e, dg=dg, pb=pb, q=q: e.matmul(pb[:, q * 128:(q + 1) * 128], lhsT=onesf[:], rhs=dg[:], start=True, stop=True),
                             reads=[("diag", q % 2), "onesf"], writes=[pkey])
                    P.op("act", lambda e, pb=pb, dst=dst, half=half: e.copy(out=dst[:, half * 512:(half + 1) * 512], in_=pb[:]),
                         reads=[pkey], writes=[dkey])
                    n += 1
        identf = sbo("identf", [128, 128]); identb = sbo("identb", [128, 128], BF16)
        epst = sbo("epst", [128, 1])
        mid = ExitStack()
        sbm = lambda name, shape, dt=F32: mid.enter_context(nc.sbuf_tensor(name, shape, dt))
        ksT = sbm("ksT", [128, S], BF16)
        vs = sbm("vs", [128, NW * 2 * 65], BF16)
        kwT = sbm("kwT", [128, NWK * 128], BF16)
        vw = sbm("vw", [128, NWK * 2 * 65], BF16)
        kcT = sbm("kcT", [128, NCH * 128], BF16)
        vca = sbm("vca", [128, NCH * 2 * 193], BF16)
        reto = sbm("reto", [128, NO * 512], BF16)
        qTs = sbm("qTs", [128, NO * 512], BF16)
        gts = sbm("gts", [128, NO * 24])
        vs4 = vs[:].rearrange("p (t g d) -> p t g d", g=2, d=65)
        vw4 = vw[:].rearrange("p (t g d) -> p t g d", g=2, d=65)
        vca4 = vca[:].rearrange("p (c g d) -> p c g d", g=2, d=193)

        for st in _blk(cfg.stop >= 0):
            sb = lambda name, shape, dt=F32: st.enter_context(nc.sbuf_tensor(name, shape, dt))
            ps = lambda name, shape, dt=F32: st.enter_context(nc.psum_tensor(name, shape, dt))
            P = Prog(nc, SEM)
            wad = [sb("wad%d" % i, [128, 8 * 1024]) for i in range(2)]
            cc = sb("cc", [128, 8]); sil = sb("sil", [128, 8])
            badT = sb("badT", [128, 48]); gm = sb("gm", [128, 8]); gf_ = sb("gf_", [128, 8])
            modT = sb("modT", [128, 48])
            pm = ps("pm", [128, 48])
            P.dma("sp", cc[:], ccol, writes=["cc"])
            P.dma("sp", badT[:], b_adaT, writes=["badT"])
            P.dma("sp", gm[:], gmixT, writes=["gm"])
            P.dma("sp", gf_[:], gffnT, writes=["gf_"])
            P.dma("sp", identf[:], tb["ident"], writes=["identf"])
            P.op("dve", lambda e: e.memset(epst[:], 1e-6), writes=["eps"])
            P.op("act", lambda e: e.activation(out=sil[:], in_=cc[:], func=AF.Silu), reads=["cc"], writes=["sil"])
            P.op("dve", lambda e: e.tensor_copy(out=identb[:], in_=identf[:]), reads=["identf"], writes=["identb"])
            for s in range(6):
                wt = wad[s % 2]
                wt3 = wt[:].rearrange("p (k n) -> p k n", k=8)
                for kc in range(8):
                    P.dma("sp" if kc % 2 == 0 else "act", wt3[:, kc, :], w_ada[kc * 128:(kc + 1) * 128, s * 1024:(s + 1) * 1024],
                          writes=[("wad", s % 2, kc)])
                for fc in range(8):
                    for kc in range(8):
                        P.op("pe", lambda e, fc=fc, kc=kc, wt3=wt3, s=s: e.matmul(
                            pm[:, s * 8 + fc:s * 8 + fc + 1], lhsT=wt3[:, kc, fc * 128:(fc + 1) * 128], rhs=sil[:, kc:kc + 1],
                            start=(kc == 0), stop=(kc == 7)), reads=[("wad", s % 2, kc), "sil"], writes=["pm"])
            P.op("dve", lambda e: e.tensor_tensor(out=modT[:], in0=pm[:], in1=badT[:], op=ALU.add), reads=["pm", "badT"], writes=["modT"])
            P.op("dve", lambda e: e.scalar_tensor_tensor(out=vec[:, 0:8], in0=modT[:, 8:16], scalar=1.0, in1=gm[:], op0=ALU.add, op1=ALU.mult),
                 reads=["modT", "gm"], writes=["vec"])
            P.op("dve", lambda e: e.scalar_tensor_tensor(out=vec[:, 16:24], in0=modT[:, 32:40], scalar=1.0, in1=gf_[:], op0=ALU.add, op1=ALU.mult),
                 reads=["modT", "gf_"], writes=["vec"])
            P.op("dve", lambda e: e.tensor_copy(out=vec[:, 8:16], in_=modT[:, 0:8]), reads=["modT"], writes=["vec"])
            P.op("dve", lambda e: e.tensor_copy(out=vec[:, 24:32], in_=modT[:, 24:32]), reads=["modT"], writes=["vec"])
            P.op("dve", lambda e: e.tensor_copy(out=vec[:, 32:40], in_=modT[:, 16:24]), reads=["modT"], writes=["vec"])
            P.op("dve", lambda e: e.tensor_copy(out=vec[:, 40:48], in_=modT[:, 40:48]), reads=["modT"], writes=["vec"])
            P.emit()

        for st in _blk(cfg.stop >= 1):
            sb = lambda name, shape, dt=F32: st.enter_context(nc.sbuf_tensor(name, shape, dt))
            ps = lambda name, shape, dt=F32: st.enter_context(nc.psum_tensor(name, shape, dt))
            P = Prog(nc, SEM)
            wib = sb("wib", [128, 8 * 3352], BF16)
            wib3 = wib[:].rearrange("p (k n) -> p k n", k=8)
            stg = [sb("stg%d" % i, [128, 838]) for i in range(2)]
            A1R = sb("A1R", [128, 1024]); B1R = sb("B1R", [128, 1024])
            n_ = 0
            for kc in range(8):
                for cq in range(4):
                    sl = slice(cq * 838, (cq + 1) * 838)
                    P.dma("sp", stg[n_ % 2][:], w_in[kc * 128:(kc + 1) * 128, sl], writes=[("stg", n_ % 2)])
                    if n_ % 2:
                        P.op("act", lambda e, kc=kc, sl=sl, n_=n_: e.copy(out=wib3[:, kc, sl], in_=stg[n_ % 2][:]), reads=[("stg", n_ % 2)], writes=["wib"])
                    else:
                        P.op("pool", lambda e, kc=kc, sl=sl, n_=n_: e.tensor_copy(out=wib3[:, kc, sl], in_=stg[n_ % 2][:]), reads=[("stg", n_ % 2)], writes=["wib"])
                    n_ += 1
            xt = [sb("xt%d" % i, [128, 1024]) for i in range(2)]
            sq = sb("sq", [128, 1024])
            ss = sb("ss", [128, 2]); rs = sb("rs", [128, 2])
            tmod = sb("tmod", [128, 1024])
            hb = sb("hb", [128, 1024], BF16)
            hT = [sb("hT%d" % i, [128, 1024], BF16) for i in range(2)]
            rp = [sb("rp%d" % i, [128, 192]) for i in range(2)]
            pj = [sb("pj%d" % i, [128, 512]) for i in range(3)]
            ra = sb("ra", [128, 256]); rb_ = sb("rb_", [128, 256]); rc_ = sb("rc_", [128, 256]); rd_ = sb("rd_", [128, 256])
            ktok = sb("ktok", [128, 512], BF16)
            qtok = sb("qtok", [128, 512], BF16)
            vtok = sb("vtok", [128, 512], BF16)
            vz = sb("vz", [128, 512], BF16)
            nk = sb("nk", [128, 384], BF16)
            nv = sb("nv", [128, 384], BF16)
            nq = sb("nq", [128, 512], BF16)
            Sst = sb("Sst", [128, 512]); Sb = sb("Sb", [128, 512], BF16)
            zt = sb("zt", [128, NW * 4]); dmat = sb("dmat", [128, 512]); xit = sb("xit", [128, 512])
            kTr = sb("kTr", [128, 512], BF16); qTr = sb("qTr", [128, 512], BF16); qxT = sb("qxT", [128, 512], BF16)
            pT = sb("pT", [128, 512], BF16)
            yv = sb("yv", [128, 512]); sg = sb("sg", [128, 512])
            st6 = sb("st6", [128, 4 * 6]); mv = sb("mv", [128, 4 * 2]); rstd4 = sb("rstd4", [128, 4])
            rawst = [sb("rawst%d" % i, [128, 256], BF16) for i in range(2)]
            p_tr = ps("p_tr", [128, 1024], BF16)
            p_mm = [ps("p_mm%d" % i, [128, 512]) for i in range(3)]
            p_kv = ps("p_kv", [128, 512])
            p_sc = ps("p_sc", [128, 512])
            p_y = ps("p_y", [128, 512])
            bcast_rows(P, sb, [(p_y, "p_y"), (p_sc, "p_sc")], [(0, A1R, "A1R"), (1, B1R, "B1R")])
            P.dma("sp", zt[:], tb["zeta"], writes=["zt"])
            P.dma("sp", dmat[:], tb["dmat"], writes=["dmat"])
            P.dma("sp", xit[:], tb["xi"], writes=["xit"])
            P.op("dve", lambda e: e.memset(Sst[:], 0.0), writes=["Sst"])
            P.op("dve", lambda e: e.memset(Sb[:], 0.0), writes=["Sb"])
            P.op("pool", lambda e: e.memset(vs[:], 1.0), writes=["vs"])
            P.op("pool", lambda e: e.memset(vw[:], 1.0), writes=["vw"])

            def rope(src, dst, H, D, cos, sin, keys_r, key_w):
                if os.environ.get("SKIP_ROPE"):
                    return
                h2 = D // 2
                s3 = src.rearrange("p (h d) -> p h d", h=H)
                d3 = dst.rearrange("p (h d) -> p h d", h=H)
                x1, x2 = s3[:, :, 0:h2], s3[:, :, h2:D]
                cb = cos.unsqueeze(1).to_broadcast([128, H, h2])
                sbb = sin.unsqueeze(1).to_broadcast([128, H, h2])
                n_ = H * h2
                v = lambda t_: t_[:, 0:n_].rearrange("p (h d) -> p h d", h=H)
                P.op("dve", lambda e: e.tensor_tensor(out=v(ra), in0=x1, in1=cb, op=ALU.mult), reads=keys_r, writes=["ra"])
                P.op(ROPE_ENG, lambda e: e.tensor_tensor(out=v(rb_), in0=x2, in1=sbb, op=ALU.mult), reads=keys_r, writes=["rb"])
                P.op("dve", lambda e: e.tensor_tensor(out=d3[:, :, 0:h2], in0=v(ra), in1=v(rb_), op=ALU.subtract), reads=["ra", "rb"], writes=[key_w + "_lo"])
                P.op(ROPE_ENG, lambda e: e.tensor_tensor(out=v(rc_), in0=x2, in1=cb, op=ALU.mult), reads=keys_r, writes=["rc"])
                P.op("dve", lambda e: e.tensor_tensor(out=v(rd_), in0=x1, in1=sbb, op=ALU.mult), reads=keys_r, writes=["rd"])
                P.op(ROPE_ENG, lambda e: e.tensor_tensor(out=d3[:, :, h2:D], in0=v(rc_), in1=v(rd_), op=ALU.add), reads=["rc", "rd"], writes=[key_w + "_hi"])

            def proj(blk, pdst, pkey, hTt, hkey):
                c0, w = blk
                for kc in range(8):
                    P.op("pe", lambda e, kc=kc: e.matmul(pdst[:, 0:w], lhsT=hTt[:, kc * 128:(kc + 1) * 128], rhs=wib3[:, kc, c0:c0 + w],
                                                        start=(kc == 0), stop=(kc == 7)), reads=[hkey, "wib"], writes=[pkey])

            for T in range(NW if cfg.astop >= 1 else 0):
                b = T % 2
                own = T >= T0
                i = T - T0
                P.dma("sp", xt[b][:], xw[T * 128:(T + 1) * 128, :], writes=[("xt", b)])
                P.dma("sp", rp[b][:], tb["rope"][T * 128:(T + 1) * 128, :], writes=[("rp", b)])
                P.op("act", lambda e, b=b: e.activation(out=sq[:], in_=xt[b][:], func=AF.Square), reads=[("xt", b)], writes=["sq"])
                P.op("dve", lambda e, b=b: e.reduce_sum(out=ss[:, b:b + 1], in_=sq[:], axis=AX.X), reads=["sq"], writes=[("ss", b)])
                P.op("act", lambda e, b=b: e.activation(out=rs[:, b:b + 1], in_=ss[:, b:b + 1], func=AF.Sqrt, scale=1.0 / 1024, bias=epst[:]),
                     reads=[("ss", b), "eps"], writes=[("rs", b)])
                P.op("dve", lambda e, b=b: e.reciprocal(out=rs[:, b:b + 1], in_=rs[:, b:b + 1]), reads=[("rs", b)], writes=[("rs", b)])
                P.op("dve", lambda e, b=b: e.scalar_tensor_tensor(out=tmod[:], in0=xt[b][:], scalar=rs[:, b:b + 1], in1=A1R[:], op0=ALU.mult, op1=ALU.mult),
                     reads=[("xt", b), ("rs", b), "A1R"], writes=["tmod"])
                P.op("pool", lambda e: e.tensor_tensor(out=hb[:], in0=tmod[:], in1=B1R[:], op=ALU.add), reads=["tmod", "B1R"], writes=["hb"])
                for kc in range(8):
                    P.op("pe", lambda e, kc=kc: e.transpose(p_tr[:, kc * 128:(kc + 1) * 128], hb[:, kc * 128:(kc + 1) * 128], identb[:]),
                         reads=["hb"], writes=["p_tr"])
                P.op("act", lambda e, b=b: e.copy(out=hT[b][:], in_=p_tr[:]), reads=["p_tr"], writes=[("hT", b)])
                hkey = ("hT", b)
                cos128, sin128, cos64, sin64 = rp[b][:, 0:64], rp[b][:, 64:128], rp[b][:, 128:160], rp[b][:, 160:192]
                if cfg.astop < 2:
                    continue
                proj(B_RK, p_mm[0], ("p_mm", 0), hT[b], hkey)
                P.op("act", lambda e: e.copy(out=pj[0][:], in_=p_mm[0][:]), reads=[("p_mm", 0)], writes=[("pj", 0)])
                rope(pj[0][:], ktok[:], 4, 128, cos128, sin128, [("pj", 0), ("rp", b)], "ktok")
                proj(B_RV, p_mm[1], ("p_mm", 1), hT[b], hkey)
                P.op("act", lambda e: e.copy(out=vtok[:], in_=p_mm[1][:]), reads=[("p_mm", 1)], writes=["vtok"])
                for h in range(0 if os.environ.get("SKIP_VZ") else 4):
                    P.op("dve", lambda e, h=h, T=T: e.tensor_scalar(out=vz[:, h * 128:(h + 1) * 128], in0=vtok[:, h * 128:(h + 1) * 128],
                                                                    scalar1=zt[:, T * 4 + h:T * 4 + h + 1], scalar2=None, op0=ALU.mult),
                         reads=["vtok", "zt"], writes=[("vz", h)])
                if cfg.astop < 2.2:
                    continue
                proj(B_NK, p_mm[2], ("p_mm", 2), hT[b], hkey)
                P.op("act", lambda e: e.copy(out=pj[2][:, 0:384], in_=p_mm[2][:, 0:384]), reads=[("p_mm", 2)], writes=[("pj", 2)])
                if cfg.astop < 2.5:
                    continue
                rope(pj[2][:, 0:384], nk[:], 6, 64, cos64, sin64, [("pj", 2), ("rp", b)], "nk")
                if cfg.astop < 2.8:
                    continue
                proj(B_NV, p_mm[0], ("p_mm", 0), hT[b], hkey)
                P.op("act", lambda e: e.copy(out=nv[:], in_=p_mm[0][:, 0:384]), reads=[("p_mm", 0)], writes=["nv"])
                if cfg.astop < 4:
                    continue
                P.op("dve", lambda e, T=T: e.tensor_copy(out=vs4[:, T, :, 0:64], in_=nv[:, 128:256].rearrange("p (g d) -> p g d", g=2)),
                     reads=["nv"], writes=["vs"])
                wk = T - (NW - NWK)
                if wk >= 0:
                    P.op("dve", lambda e, wk=wk: e.tensor_copy(out=vw4[:, wk, :, 0:64], in_=nv[:, 256:384].rearrange("p (g d) -> p g d", g=2)),
                         reads=["nv"], writes=["vw"])
                srcs = [nk[:, 0:128], nk[:, 128:256], nk[:, 256:384], nv[:, 0:128]]
                for q, s_ in enumerate(srcs):
                    P.op("pe", lambda e, q=q, s_=s_: e.transpose(p_tr[:, q * 128:(q + 1) * 128], s_, identb[:]),
                         reads=["nk_lo", "nk_hi", "nv"], writes=["p_tr"])
                P.op("act", lambda e, T=T: e.copy(out=ksT[:, T * 128:(T + 1) * 128], in_=p_tr[:, 128:256]), reads=["p_tr"], writes=["ksT"])
                if wk >= 0:
                    P.op("act", lambda e, wk=wk: e.copy(out=kwT[:, wk * 128:(wk + 1) * 128], in_=p_tr[:, 256:384]), reads=["p_tr"], writes=["kwT"])
                rw = rawst[T % 2]
                P.op("act", lambda e, rw=rw: e.copy(out=rw[:, 0:128], in_=p_tr[:, 0:128]), reads=["p_tr"], writes=[("rawst", T % 2)])
                P.op("act", lambda e, rw=rw: e.copy(out=rw[:, 128:256], in_=p_tr[:, 384:512]), reads=["p_tr"], writes=[("rawst", T % 2)])
                for kind in range(2):
                    P.dma("sp", rawd[kind, :, T * 128:(T + 1) * 128], rw[:, kind * 128:(kind + 1) * 128], reads=[("rawst", T % 2)])
                if cfg.astop < 5:
                    continue
                if own and cfg.astop >= 6:
                    proj(B_RQ, p_mm[1], ("p_mm", 1), hT[b], hkey)
                    P.op("act", lambda e: e.copy(out=pj[1][:], in_=p_mm[1][:]), reads=[("p_mm", 1)], writes=[("pj", 1)])
                    rope(pj[1][:], qtok[:], 4, 128, cos128, sin128, [("pj", 1), ("rp", b)], "qtok")
                    for h in range(4):
                        P.op("pe", lambda e, h=h: e.transpose(p_tr[:, h * 128:(h + 1) * 128], ktok[:, h * 128:(h + 1) * 128], identb[:]),
                             reads=["ktok_lo", "ktok_hi"], writes=["p_tr"])
                    P.op("act", lambda e: e.copy(out=kTr[:], in_=p_tr[:, 0:512]), reads=["p_tr"], writes=["kTr"])
                    for h in range(4):
                        P.op("pe", lambda e, h=h: e.transpose(p_tr[:, 512 + h * 128:512 + (h + 1) * 128], qtok[:, h * 128:(h + 1) * 128], identb[:]),
                             reads=["qtok_lo", "qtok_hi"], writes=["p_tr"])
                    P.op("act", lambda e: e.copy(out=qTr[:], in_=p_tr[:, 512:1024]), reads=["p_tr"], writes=["qTr"])
                    P.op("dve", lambda e: e.tensor_tensor(out=qxT[:], in0=qTr[:], in1=xit[:], op=ALU.mult), reads=["qTr", "xit"], writes=["qxT"])
                    for h in range(4):
                        hs = slice(h * 128, (h + 1) * 128)
                        P.op("pe", lambda e, hs=hs: e.matmul(p_sc[:, hs], lhsT=kTr[:, hs], rhs=qTr[:, hs], start=True, stop=True),
                             reads=["kTr", "qTr"], writes=["p_sc"])
                    P.op("dve", lambda e: e.tensor_tensor(out=pT[:], in0=p_sc[:], in1=dmat[:], op=ALU.mult), reads=["p_sc", "dmat"], writes=["pT"])
                    for h in range(4):
                        hs = slice(h * 128, (h + 1) * 128)
                        P.op("pe", lambda e, hs=hs: e.matmul(p_y[:, hs], lhsT=pT[:, hs], rhs=vtok[:, hs], start=True, stop=False),
                             reads=["pT", "vtok"], writes=["p_y"])
                        P.op("pe", lambda e, hs=hs: e.matmul(p_y[:, hs], lhsT=qxT[:, hs], rhs=Sb[:, hs], start=False, stop=True),
                             reads=["qxT", "Sb"], writes=["p_y"])
                    P.op("act", lambda e: e.copy(out=yv[:], in_=p_y[:]), reads=["p_y"], writes=["yv"])
                    for h in range(4):
                        P.op("dve", lambda e, h=h: e.bn_stats(out=st6[:, h * 6:(h + 1) * 6], in_=yv[:, h * 128:(h + 1) * 128]), reads=["yv"], writes=[("st6", h)])
                        P.op("dve", lambda e, h=h: e.bn_aggr(out=mv[:, h * 2:(h + 1) * 2], in_=st6[:, h * 6:(h + 1) * 6]), reads=[("st6", h)], writes=[("mv", h)])
                    mv3 = mv[:].rearrange("p (h t) -> p h t", t=2)
                    P.op("act", lambda e: e.activation(out=rstd4[:], in_=mv3[:, :, 1], func=AF.Sqrt, bias=epst[:]),
                         reads=[("mv", 0), ("mv", 1), ("mv", 2), ("mv", 3), "eps"], writes=["rstd4"])
                    P.op("dve", lambda e: e.reciprocal(out=rstd4[:], in_=rstd4[:]), reads=["rstd4"], writes=["rstd4"])
                    proj(B_RG, p_mm[2], ("p_mm", 2), hT[b], hkey)
                    P.op("act", lambda e: e.activation(out=sg[:], in_=p_mm[2][:], func=AF.Silu), reads=[("p_mm", 2)], writes=["sg"])
                    for h in range(4):
                        hs = slice(h * 128, (h + 1) * 128)
                        P.op("dve", lambda e, h=h, hs=hs: e.tensor_scalar(out=yv[:, hs], in0=yv[:, hs], scalar1=mv[:, 2 * h:2 * h + 1], scalar2=rstd4[:, h:h + 1],
                                                                         op0=ALU.subtract, op1=ALU.mult),
                             reads=["yv", ("mv", h), "rstd4"], writes=[("yn", h)])
                        P.op("pool", lambda e, h=h, hs=hs, i=i: e.tensor_tensor(out=reto[:, i * 512 + h * 128:i * 512 + (h + 1) * 128], in0=yv[:, hs], in1=sg[:, hs], op=ALU.mult),
                             reads=[("yn", h), "sg"], writes=["reto"])
                    proj(B_NQ, p_mm[0], ("p_mm", 0), hT[b], hkey)
                    P.op("act", lambda e: e.copy(out=pj[0][:], in_=p_mm[0][:]), reads=[("p_mm", 0)], writes=[("pj", 0)])
                    rope(pj[0][:], nq[:], 8, 64, cos64, sin64, [("pj", 0), ("rp", b)], "nq")
                    for h in range(4):
                        P.op("pe", lambda e, h=h: e.transpose(p_tr[:, h * 128:(h + 1) * 128], nq[:, h * 128:(h + 1) * 128], identb[:]),
                             reads=["nq_lo", "nq_hi"], writes=["p_tr"])
                    P.op("act", lambda e, i=i: e.copy(out=qTs[:, i * 512:(i + 1) * 512], in_=p_tr[:, 0:512]), reads=["p_tr"], writes=["qTs"])
                    proj(B_GT, p_mm[1], ("p_mm", 1), hT[b], hkey)
                    P.op("act", lambda e, i=i: e.activation(out=gts[:, i * 24:(i + 1) * 24], in_=p_mm[1][:, 0:24], func=AF.Sigmoid),
                         reads=[("p_mm", 1)], writes=["gts"])
                for h in range(4):
                    hs = slice(h * 128, (h + 1) * 128)
                    P.op("pe", lambda e, hs=hs: e.matmul(p_kv[:, hs], lhsT=ktok[:, hs], rhs=vz[:, hs], start=True, stop=True),
                         reads=["ktok_lo", "ktok_hi", ("vz", 0), ("vz", 1), ("vz", 2), ("vz", 3)], writes=["p_kv"])
                for h in range(4):
                    hs = slice(h * 128, (h + 1) * 128)
                    P.op("dve", lambda e, h=h, hs=hs: e.scalar_tensor_tensor(out=Sst[:, hs], in0=Sst[:, hs], scalar=float(np.exp(128 * LOGG[h])), in1=p_kv[:, hs],
                                                                            op0=ALU.mult, op1=ALU.add), reads=["p_kv", "Sst"], writes=["Sst"])
                P.op("act", lambda e: e.copy(out=Sb[:], in_=Sst[:]), reads=["Sst"], writes=["Sb"])
            P.emit()

        for st in _blk(cfg.stop >= 2):
            sb = lambda name, shape, dt=F32: st.enter_context(nc.sbuf_tensor(name, shape, dt))
            ps = lambda name, shape, dt=F32: st.enter_context(nc.psum_tensor(name, shape, dt))
            P = Prog(nc, SEM)
            NBP = NCH * 128
            raw = [sb("raw%d" % k, [128, RAWLEN], BF16) for k in range(2)]
            w1s = sb("w1s", [128, 32 * 256]); w1b = [sb("w1b%d" % k, [128, 32 * 256], BF16) for k in range(2)]
            pes = sb("pes", [128, 32]); peb = [sb("peb%d" % k, [128, 32], BF16) for k in range(2)]
            b1s = [sb("b1s%d" % k, [128, 2]) for k in range(2)]
            w2s = sb("w2s", [128, 128]); w2b = [sb("w2b%d" % k, [128, 128], BF16) for k in range(2)]
            cb = sb("cb", [128, 2])
            u = sb("u", [128, 512]); u2 = sb("u2", [128, 512]); u3 = sb("u3", [128, 512])
            hid = [sb("hid%d" % i, [128, 512], BF16) for i in range(2)]
            ovl = sb("ovl", [128, NCH * 128], BF16)
            p_h = ps("p_h", [128, 512]); p_c = ps("p_c", [128, 2]); p_o = ps("p_o", [128, 512])
            P.dma("sp", ovl[:], tb["overlap"], writes=["ovl"])
            P.op("pool", lambda e: e.memset(vca[:], 1.0), writes=["vca"])
            for cc_ in range(NCH):
                for g in range(2):
                    P.op("pool", lambda e, cc_=cc_, g=g: e.tensor_copy(out=vca4[:, cc_, g, 65:193], in_=ovl[:, cc_ * 128:(cc_ + 1) * 128]), reads=["ovl"], writes=["vca"])
            for kind, (w1d, ped, b1d, w2d) in enumerate([(w1k, peTk, b1k, w2k), (w1v, peTv, b1v, w2v)]):
                P.op("pool", lambda e, kind=kind: e.memset(raw[kind][:], 0.0), writes=[("raw", kind)])
                P.dma("sp", raw[kind][:, 0:S], rawd[kind], writes=[("raw", kind)])
                for half in range(2):
                    P.dma("sp", w1s[half * 64:(half + 1) * 64, :], w1d, writes=["w1s"])
                    P.dma("sp", pes[half * 64:(half + 1) * 64, :], ped, writes=["pes"])
                P.op("act", lambda e, kind=kind: e.copy(out=w1b[kind][:], in_=w1s[:]), reads=["w1s"], writes=[("w1b", kind)])
                P.op("dve", lambda e, kind=kind: e.tensor_copy(out=peb[kind][:], in_=pes[:]), reads=["pes"], writes=[("peb", kind)])
                P.dma("sp", b1s[kind][:], b1d, writes=[("b1s", kind)])
                P.dma("sp", w2s[:], w2d, writes=["w2s"])
                P.op("dve", lambda e, kind=kind: e.tensor_copy(out=w2b[kind][:], in_=w2s[:]), reads=["w2s"], writes=[("w2b", kind)])
                w13 = w1b[kind][:].rearrange("p (l n) -> p l n", l=32)
                for hc in range(2):
                    for l in range(32):
                        P.op("pe", lambda e, hc=hc, l=l, w13=w13, kind=kind: e.matmul(p_c[:, hc:hc + 1], lhsT=w13[0:64, l, hc * 128:(hc + 1) * 128], rhs=peb[kind][0:64, l:l + 1],
                                                                                    start=(l == 0), stop=(l == 31)), reads=[("w1b", kind), ("peb", kind)], writes=["p_c"])
                P.op("dve", lambda e, kind=kind: e.tensor_tensor(out=cb[:], in0=p_c[:], in1=b1s[kind][:], op=ALU.add), reads=["p_c", ("b1s", kind)], writes=["cb"])
                for g in range(2):
                    gs = slice(64 * g, 64 * g + 64)
                    for n0 in range(0, NBP, 512):
                        nn = min(512, NBP - n0)
                        for hc in range(2):
                            for l in range(32):
                                rhs = raw[kind][gs, l + 16 * n0: l + 16 * (n0 + nn): 16]
                                P.op("pe", lambda e, hc=hc, l=l, rhs=rhs, gs=gs, w13=w13, nn=nn: e.matmul(
                                    p_h[:, 0:nn], lhsT=w13[gs, l, hc * 128:(hc + 1) * 128], rhs=rhs, start=(l == 0), stop=(l == 31)),
                                    reads=[("w1b", kind), ("raw", kind)], writes=["p_h"])
                            P.op("act", lambda e, hc=hc, nn=nn: e.activation(out=u[:, 0:nn], in_=p_h[:, 0:nn], func=AF.Identity, bias=cb[:, hc:hc + 1]),
                                 reads=["p_h", "cb"], writes=["u"])
                            P.op("act", lambda e, nn=nn: e.activation(out=u2[:, 0:nn], in_=u[:, 0:nn], func=AF.Square), reads=["u"], writes=["u2"])
                            P.op("dve", lambda e, nn=nn: e.tensor_scalar(out=u2[:, 0:nn], in0=u2[:, 0:nn], scalar1=0.044715, scalar2=1.0, op0=ALU.mult, op1=ALU.add),
                                 reads=["u2"], writes=["u2"])
                            P.op("dve", lambda e, nn=nn: e.tensor_tensor(out=u3[:, 0:nn], in0=u2[:, 0:nn], in1=u[:, 0:nn], op=ALU.mult), reads=["u2", "u"], writes=["u3"])
                            P.op("act", lambda e, nn=nn: e.activation(out=u3[:, 0:nn], in_=u3[:, 0:nn], func=AF.Sigmoid, scale=1.5957691216), reads=["u3"], writes=["u3"])
                            P.op("dve", lambda e, hc=hc, nn=nn: e.tensor_tensor(out=hid[hc][:, 0:nn], in0=u3[:, 0:nn], in1=u[:, 0:nn], op=ALU.mult),
                                 reads=["u3", "u"], writes=[("hid", hc)])
                        w23 = w2b[kind][:].rearrange("p (c d) -> p c d", c=2)
                        if kind == 0:
                            for hc in range(2):
                                P.op("pe", lambda e, hc=hc, gs=gs, nn=nn, w23=w23: e.matmul(p_o[gs, 0:nn], lhsT=w23[:, hc, :], rhs=hid[hc][:, 0:nn], start=(hc == 0), stop=(hc == 1)),
                                     reads=[("hid", 0), ("hid", 1), ("w2b", 0)], writes=["p_o"])
                            P.op("act", lambda e, gs=gs, n0=n0, nn=nn: e.copy(out=kcT[gs, n0:n0 + nn], in_=p_o[gs, 0:nn]), reads=["p_o"], writes=["kcT"])
                        else:
                            for c4 in range(nn // 128):
                                for hc in range(2):
                                    P.op("pe", lambda e, hc=hc, c4=c4, w23=w23: e.matmul(p_o[:, c4 * 64:(c4 + 1) * 64], lhsT=hid[hc][:, c4 * 128:(c4 + 1) * 128], rhs=w23[:, hc, :],
                                                                                        start=(hc == 0), stop=(hc == 1)), reads=[("hid", 0), ("hid", 1), ("w2b", 1)], writes=["p_o"])
                                P.op("act", lambda e, c4=c4, g=g, n0=n0: e.copy(out=vca4[:, n0 // 128 + c4, g, 0:64], in_=p_o[:, c4 * 64:(c4 + 1) * 64]),
                                     reads=["p_o"], writes=["vca"])
            P.emit()

        for st in _blk(cfg.stop >= 3):
            sb = lambda name, shape, dt=F32: st.enter_context(nc.sbuf_tensor(name, shape, dt))
            ps = lambda name, shape, dt=F32: st.enter_context(nc.psum_tensor(name, shape, dt))
            P = Prog(nc, SEM)
            wob = sb("wob", [128, 8 * 1024], BF16)
            wob3 = wob[:].rearrange("p (k n) -> p k n", k=8)
            stg = [sb("stgc%d" % i, [128, 1024]) for i in range(2)]
            for kc in range(8):
                P.dma("sp", stg[kc % 2][:], w_out[kc * 128:(kc + 1) * 128, :], writes=[("stg", kc % 2)])
                P.op("pool", lambda e, kc=kc: e.tensor_copy(out=wob3[:, kc, :], in_=stg[kc % 2][:]), reads=[("stg", kc % 2)], writes=["wob"])
            expd = sb("expd", [128, S], BF16); tri = sb("tri", [128, 1024], BF16)
            kbias = sb("kbias", [128, NW]); cbias = sb("cbias", [128, NCH])
            cpen = [sb("cpen%d" % i, [128, NCH * 512], BF16) for i in range(2)]
            selm = [sb("selm%d" % i, [128, 256]) for i in range(2)]
            xo = [sb("xo%d" % i, [128, 1024]) for i in range(2)]
            eb = [sb("eb%d" % i, [128, 512], BF16) for i in range(3)]
            imp = sb("imp", [128, 128]); impm = sb("impm", [128, 128]); r1 = sb("r1", [128, 128]); r2 = sb("r2", [128, 128])
            m8 = sb("m8", [128, 16]); seln = sb("seln", [128, 128], BF16); selT = sb("selT", [128, 512], BF16)
            den = sb("den", [128, 4]); coef = sb("coef", [128, 4])
            nsa = sb("nsa", [128, 512])
            cat = sb("cat", [128, 1024], BF16); catT = sb("catT", [128, 1024], BF16)
            x1t = [sb("x1t%d" % i, [128, 1024]) for i in range(2)]
            p_s = [ps("p_s%d" % i, [128, 512]) for i in range(2)]
            p_a = [ps("p_a%d" % i, [128, 512]) for i in range(4)]
            p_x = ps("p_x", [128, 1024])
            GA1 = sb("GA1", [128, 1024])
            bcast_rows(P, sb, [(p_s[0], ("p_s", 0)), (p_s[1], ("p_s", 1))], [(4, GA1, "GA1")])
            P.dma("sp", expd[:], tb["expand"], writes=["expd"])
            P.dma("sp", tri[:], tb["tri"], writes=["tri"])
            P.dma("sp", kbias[:], tb["keybias"], writes=["kbias"])
            P.dma("sp", cbias[:], tb["cmpbias"], writes=["cbias"])
            p_xb = p_x[:].bitcast(BF16)
            gts4 = gts[:].rearrange("p (i g h c) -> p i g h c", g=2, h=4, c=3)
            nsa4 = nsa[:].rearrange("p (g h d) -> p g h d", g=2, h=4)
            sc_i = [0]
            eb_i = [0]

            def attend(i, g, chunks, first_branch, br):
                W = chunks[0]["v"].shape[-1]
                nchk = len(chunks)
                qg = qTs[64 * g:64 * g + 64, i * 512:(i + 1) * 512]
                pbs = []

                def stage_s(ci):
                    ch = chunks[ci]
                    pb = sc_i[0] % 2; sc_i[0] += 1
                    pbs.append(pb)
                    pst = p_s[pb]
                    npen = len(ch["pens"])
                    P.op("pe", lambda e, ch=ch, pst=pst, npen=npen: e.matmul(pst[:], lhsT=ch["lhsT"], rhs=qg, start=True, stop=(npen == 0)),
                         reads=ch["keys_r"] + ["qTs"], writes=[("p_s", pb)])
                    for pi, (pl, pr, pk) in enumerate(ch["pens"]):
                        P.op("pe", lambda e, pl=pl, pr=pr, pst=pst, last=(pi == npen - 1): e.matmul(
                            pst[:], lhsT=pl, rhs=pr, start=False, stop=last), reads=pk, writes=[("p_s", pb)])

                def stage_ev(ci):
                    ch = chunks[ci]
                    pb = pbs[ci]
                    pst = p_s[pb]
                    ei = eb_i[0] % 3; eb_i[0] += 1
                    et = eb[ei]
                    if ch["bias"] is None:
                        P.op("act", lambda e, et=et, pst=pst: e.activation(out=et[:], in_=pst[:], func=AF.Exp, scale=0.125), reads=[("p_s", pb)], writes=[("eb", ei)])
                    else:
                        P.op("act", lambda e, et=et, pst=pst, ch=ch: e.activation(out=et[:], in_=pst[:], func=AF.Exp, scale=0.125, bias=ch["bias"]),
                             reads=[("p_s", pb), "kbias", "cbias"], writes=[("eb", ei)])
                    for h in range(4):
                        P.op("pe", lambda e, h=h, et=et, ch=ch, ci=ci: e.matmul(p_a[h][:, 0:W], lhsT=et[:, h * 128:(h + 1) * 128], rhs=ch["v"],
                                                                              start=(ci == 0), stop=(ci == nchk - 1)),
                             reads=[("eb", ei)] + ch["keys_v"], writes=[("p_a", h)])

                stage_s(0)
                for ci in range(nchk):
                    if ci + 1 < nchk:
                        stage_s(ci + 1)
                    stage_ev(ci)
                for h in range(4):
                    P.op("dve", lambda e, h=h: e.tensor_scalar(out=den[:, h:h + 1], in0=p_a[h][:, 64:65], scalar1=1e-30, scalar2=None, op0=ALU.max),
                         reads=[("p_a", h)], writes=[("den", h)])
                    P.op("dve", lambda e, h=h: e.reciprocal(out=den[:, h:h + 1], in_=den[:, h:h + 1]), reads=[("den", h)], writes=[("den", h)])
                    P.op("dve", lambda e, h=h: e.tensor_tensor(out=coef[:, h:h + 1], in0=den[:, h:h + 1], in1=gts4[:, i, g, h, br:br + 1], op=ALU.mult),
                         reads=[("den", h), "gts"], writes=[("coef", h)])
                    if first_branch:
                        P.op("dve", lambda e, h=h: e.tensor_scalar(out=nsa4[:, g, h, :], in0=p_a[h][:, 0:64], scalar1=coef[:, h:h + 1], scalar2=None, op0=ALU.mult),
                             reads=[("p_a", h), ("coef", h)], writes=[("nsa", g, h)])
                    else:
                        P.op("dve", lambda e, h=h: e.scalar_tensor_tensor(out=nsa4[:, g, h, :], in0=p_a[h][:, 0:64], scalar=coef[:, h:h + 1], in1=nsa4[:, g, h, :],
                                                                         op0=ALU.mult, op1=ALU.add),
                             reads=[("p_a", h), ("coef", h), ("nsa", g, h)], writes=[("nsa", g, h)])

            for i in range(NO):
                T = T0 + i
                b = i % 2
                P.dma("sp", cpen[b][:], tb["cmppen"][i * 128:(i + 1) * 128, :], writes=[("cpen", b)])
                P.dma("sp", selm[b][:], tb["selm"][i * 128:(i + 1) * 128, :], writes=[("selm", b)])
                P.dma("sp", xo[b][:], xw[T * 128:(T + 1) * 128, :], writes=[("xo", b)])
                for g in range(2):
                    gs = slice(64 * g, 64 * g + 64)
                    chunks = []
                    for c_ in range(NCH):
                        chunks.append(dict(lhsT=kcT[gs, c_ * 128:(c_ + 1) * 128], keys_r=["kcT"],
                                           pens=[(identb[:], cpen[b][:, c_ * 512:(c_ + 1) * 512], [("cpen", b), "identb"])],
                                           bias=cbias[:, c_:c_ + 1], v=vca4[:, c_, g, :], keys_v=["vca"]))
                    attend(i, g, chunks, True, 0)
                    for h in range(4):
                        if h == 0:
                            P.op("dve", lambda e: e.tensor_scalar(out=imp[:], in0=p_a[0][:, 65:193], scalar1=den[:, 0:1], scalar2=None, op0=ALU.mult),
                                 reads=[("p_a", 0), ("den", 0)], writes=["imp"])
                        else:
                            P.op("dve", lambda e, h=h: e.scalar_tensor_tensor(out=imp[:], in0=p_a[h][:, 65:193], scalar=den[:, h:h + 1], in1=imp[:], op0=ALU.mult, op1=ALU.add),
                                 reads=[("p_a", h), ("den", h), "imp"], writes=["imp"])
                    P.op("dve", lambda e, b=b: e.tensor_tensor(out=impm[:], in0=imp[:], in1=selm[b][:, 0:128], op=ALU.mult), reads=["imp", ("selm", b)], writes=["impm"])
                    P.op("dve", lambda e, b=b: e.tensor_tensor(out=impm[:], in0=impm[:], in1=selm[b][:, 128:256], op=ALU.add), reads=["impm", ("selm", b)], writes=["impm"])
                    P.op("dve", lambda e: e.max(out=m8[:, 0:8], in_=impm[:]), reads=["impm"], writes=["m8a"])
                    P.op("dve", lambda e: e.match_replace(out=r1[:], in_to_replace=m8[:, 0:8], in_values=impm[:], imm_value=-BIG), reads=["impm", "m8a"], writes=["r1"])
                    P.op("dve", lambda e: e.max(out=m8[:, 8:16], in_=r1[:]), reads=["r1"], writes=["m8b"])
                    P.op("dve", lambda e: e.match_replace(out=r2[:], in_to_replace=m8[:, 8:16], in_values=r1[:], imm_value=-BIG), reads=["r1", "m8b"], writes=["r2"])
                    P.op("dve", lambda e: e.tensor_tensor(out=r1[:], in0=impm[:], in1=r2[:], op=ALU.subtract), reads=["impm", "r2"], writes=["r1"])
                    P.op("dve", lambda e: e.tensor_scalar(out=seln[:], in0=r1[:], scalar1=0.0, scalar2=NEG, op0=ALU.is_le, op1=ALU.mult), reads=["r1"], writes=["seln"])
                    P.op("pe", lambda e: e.transpose(p_xb[:, 0:128], seln[:], identb[:]), reads=["seln"], writes=["p_x"])
                    P.op("dve", lambda e: e.tensor_copy(out=selT[:].rearrange("p (h t) -> p h t", h=4), in_=p_xb[:, 0:128].unsqueeze(1).to_broadcast([128, 4, 128])),
                         reads=["p_x"], writes=["selT"])
                    chunks = []
                    for c_ in range(T + 1):
                        pens = [(expd[:, c_ * 128:(c_ + 1) * 128], selT[:], ["expd", "selT"])]
                        if c_ == T:
                            pens.append((identb[:], tri[:, 0:512], ["tri", "identb"]))
                        chunks.append(dict(lhsT=ksT[gs, c_ * 128:(c_ + 1) * 128], keys_r=["ksT"], pens=pens, bias=None, v=vs4[:, c_, g, :], keys_v=["vs"]))
                    attend(i, g, chunks, False, 1)
                    chunks = []
                    for c_ in range(max(0, T - 4), T + 1):
                        wk = c_ - (NW - NWK)
                        pens = []
                        if c_ == T - 4:
                            pens.append((identb[:], tri[:, 512:1024], ["tri", "identb"]))
                        if c_ == T:
                            pens.append((identb[:], tri[:, 0:512], ["tri", "identb"]))
                        chunks.append(dict(lhsT=kwT[gs, wk * 128:(wk + 1) * 128], keys_r=["kwT"], pens=pens, bias=kbias[:, c_:c_ + 1], v=vw4[:, wk, g, :], keys_v=["vw"]))
                    attend(i, g, chunks, False, 2)
                P.op("act", lambda e, i=i: e.copy(out=cat[:, 0:512], in_=reto[:, i * 512:(i + 1) * 512]), reads=["reto"], writes=["cat_a"])
                P.op("act", lambda e: e.copy(out=cat[:, 512:1024], in_=nsa[:]), reads=[("nsa", g_, h_) for g_ in range(2) for h_ in range(4)], writes=["cat_b"])
                for kc in range(8):
                    P.op("pe", lambda e, kc=kc: e.transpose(p_xb[:, kc * 128:(kc + 1) * 128], cat[:, kc * 128:(kc + 1) * 128], identb[:]),
                         reads=["cat_a", "cat_b"], writes=["p_x"])
                P.op("act", lambda e: e.copy(out=catT[:], in_=p_xb[:, 0:1024]), reads=["p_x"], writes=["catT"])
                for half in range(2):
                    for kc in range(8):
                        P.op("pe", lambda e, kc=kc, half=half: e.matmul(p_x[:, half * 512:(half + 1) * 512], lhsT=catT[:, kc * 128:(kc + 1) * 128],
                                                                        rhs=wob3[:, kc, half * 512:(half + 1) * 512], start=(kc == 0), stop=(kc == 7)),
                             reads=["catT", "wob"], writes=["p_x"])
                P.op("dve", lambda e, b=b: e.tensor_tensor(out=x1t[b][:], in0=p_x[:], in1=GA1[:], op=ALU.mult), reads=["p_x", "GA1"], writes=[("x1t", b)])
                P.op("pool", lambda e, b=b: e.tensor_tensor(out=x1t[b][:], in0=x1t[b][:], in1=xo[b][:], op=ALU.add), reads=[("x1t", b), ("xo", b)], writes=[("x1t", b)])
                P.dma("sp", x1d[i * 128:(i + 1) * 128, :], x1t[b][:], reads=[("x1t", b)])
            P.emit()

        mid.close()
        for st in _blk(cfg.stop >= 4):
            sb = lambda name, shape, dt=F32: st.enter_context(nc.sbuf_tensor(name, shape, dt))
            P = Prog(nc, SEM)
            RP = 8
            NCK = cfg.NEXP // (128 * RP)
            cin = [sb("cin%d" % i_, [128, RP * 1024]) for i_ in range(3)]
            cob = [sb("cob%d" % i_, [128, RP * 1024], BF16) for i_ in range(3)]
            n_ = 0
            for src, dst in ((pdown, pdb), (pup, pub)):
                srcv = src.rearrange("(c p j) d -> c p (j d)", p=128, j=RP)
                dstv = dst.rearrange("(c p j) d -> c p (j d)", p=128, j=RP)
                for c_ in range(NCK):
                    k_ = n_ % 3
                    P.dma("sp", cin[k_][:], srcv[c_], writes=[("cin", k_)])
                    eng = ("act", "dve", "pool")[n_ % 3]
                    if eng == "act":
                        P.op("act", lambda e, k_=k_: e.copy(out=cob[k_][:], in_=cin[k_][:]), reads=[("cin", k_)], writes=[("cob", k_)])
                    else:
                        P.op(eng, lambda e, k_=k_: e.tensor_copy(out=cob[k_][:], in_=cin[k_][:]), reads=[("cin", k_)], writes=[("cob", k_)])
                    P.dma("act", dstv[c_], cob[k_][:], reads=[("cob", k_)])
                    n_ += 1
            P.emit()
        for st in _blk(cfg.stop >= 4):
            sb = lambda name, shape, dt=F32: st.enter_context(nc.sbuf_tensor(name, shape, dt))
            ps = lambda name, shape, dt=F32: st.enter_context(nc.psum_tensor(name, shape, dt))
            P = Prog(nc, SEM)
            wqb = sb("wqb", [128, 8 * 2048], BF16)
            wqb3 = wqb[:].rearrange("p (k n) -> p k n", k=8)
            stg = [sb("stgd%d" % i, [128, 2048]) for i in range(2)]
            for kc in range(8):
                P.dma("sp", stg[kc % 2][:], w_pq[kc * 128:(kc + 1) * 128, :], writes=[("stg", kc % 2)])
                P.op("act", lambda e, kc=kc: e.copy(out=wqb3[:, kc, :], in_=stg[kc % 2][:]), reads=[("stg", kc % 2)], writes=["wqb"])
            A2R = sb("A2R", [128, 1024]); B2R = sb("B2R", [128, 1024]); GA2 = sb("GA2", [128, 1024]); GF = sb("GF", [128, 1024])
            P.dma("sp", GF[:], gfin.partition_broadcast(128), writes=["GF"])
            skb = sb("skb", [128, 2048], BF16)
            P.dma("sp", stg[0][:], skT, writes=[("stg", 0)])
            P.op("act", lambda e: e.copy(out=skb[:], in_=stg[0][:]), reads=[("stg", 0)], writes=["skb"])
            selrow = sb("selrow", [128, 128 * 128], BF16)
            P.dma("sp", selrow[:], tb["selrow"], writes=["selrow"])
            selrow3 = selrow[:].rearrange("p (t m) -> p t m", t=128)
            io16 = sb("io16", [128, 16])
            P.dma("sp", io16[:], tb["iota16"], writes=["io16"])
            io128 = sb("io128", [128, 128]); cm = [sb("cm%d" % i_, [128, 128], BF16) for i_ in range(3)]
            P.dma("sp", io128[:], tb["iota128"], writes=["io128"])
            x1 = sb("x1", [128, 1024]); sq = sb("sqd", [128, 1024]); ss = sb("ssd", [128, 1]); rs = sb("rsd", [128, 1])
            tmod = sb("tmodd", [128, 1024]); h2b = sb("h2b", [128, 1024], BF16); h2T = sb("h2T", [128, 1024], BF16)
            qT = sb("qT", [128, 2048], BF16)
            s_sb = sb("s_sb", [128, 2048]); s2 = sb("s2", [128, 128])
            V16 = sb("V16", [128, 256]); I16 = sb("I16", [128, 256], U32); I16f = sb("I16f", [128, 256])
            cand = sb("cand", [128, 2048]); c2 = sb("c2", [128, 256])
            TV = sb("TV", [128, 128]); TJ = sb("TJ", [128, 128], U32); TA = sb("TA", [128, 128], U32); TBb = sb("TBb", [128, 128], U32)
            TAf = sb("TAf", [128, 128]); TBf = sb("TBf", [128, 128])
            oh = sb("oh", [128, 2048]); i0 = sb("i0", [128, 128]); i1 = sb("i1", [128, 128]); ef = sb("ef", [128, 128])
            ex = sb("ex", [128, 128]); sm = sb("sm", [128, 8]); Wt = sb("Wt", [128, 128])
            idxT = sb("idxT", [128, 128], U32); WT = sb("WT", [128, 128]); aT = sb("aT", [128, 128]); cT = sb("cT", [128, 128])
            g2 = sb("g2", [128, 128]); g3 = sb("g3", [128, 128])
            NU = 6
            U = [sb("U%d" % i, [128, 1024], BF16) for i in range(NU)]
            jk = sq
            oT = tmod; x2 = sb("x2", [128, 1024]); yo = sb("yo", [128, 1024])
            p_q = ps("p_q", [128, 512]); p_s4 = ps("p_s4", [128, 2048]); p_hb = ps("p_hb", [128, 1024]); p_t = ps("p_t", [128, 512])
            bcast_rows(P, sb, [(p_q, "p_q"), (p_t, "p_t")], [(2, A2R, "A2R"), (3, B2R, "B2R"), (5, GA2, "GA2")])
            V4 = V16[:].rearrange("p (c k) -> p c k", k=16); I4 = I16[:].rearrange("p (c k) -> p c k", k=16)
            s3 = s_sb[:].rearrange("p (c n) -> p c n", n=128)
            for i in range(NO):
                P.dma("sp", x1[:], x1d[i * 128:(i + 1) * 128, :], writes=["x1"])
                P.op("act", lambda e: e.activation(out=sq[:], in_=x1[:], func=AF.Square), reads=["x1"], writes=["sq"])
                P.op("dve", lambda e: e.reduce_sum(out=ss[:], in_=sq[:], axis=AX.X), reads=["sq"], writes=["ss"])
                P.op("act", lambda e: e.activation(out=rs[:], in_=ss[:], func=AF.Sqrt, scale=1.0 / 1024, bias=epst[:]), reads=["ss"], writes=["rs"])
                P.op("dve", lambda e: e.reciprocal(out=rs[:], in_=rs[:]), reads=["rs"], writes=["rs"])
                P.op("dve", lambda e: e.scalar_tensor_tensor(out=tmod[:], in0=x1[:], scalar=rs[:], in1=A2R[:], op0=ALU.mult, op1=ALU.mult), reads=["x1", "rs", "A2R"], writes=["tmod"])
                P.op("pool", lambda e: e.tensor_tensor(out=h2b[:], in0=tmod[:], in1=B2R[:], op=ALU.add), reads=["tmod", "B2R"], writes=["h2b"])
                p_tb = p_t[:].bitcast(BF16)
                for kc in range(8):
                    P.op("pe", lambda e, kc=kc: e.transpose(p_tb[:, kc * 128:(kc + 1) * 128], h2b[:, kc * 128:(kc + 1) * 128], identb[:]), reads=["h2b"], writes=["p_t"])
                P.op("act", lambda e: e.copy(out=h2T[:], in_=p_tb[:, 0:1024]), reads=["p_t"], writes=["h2T"])
                for c4 in range(4):
                    for cq in range(4):
                        ch = c4 * 4 + cq
                        for kc in range(8):
                            P.op("pe", lambda e, ch=ch, cq=cq, kc=kc: e.matmul(p_q[:, cq * 128:(cq + 1) * 128], lhsT=wqb3[:, kc, ch * 128:(ch + 1) * 128],
                                                                              rhs=h2T[:, kc * 128:(kc + 1) * 128], start=(kc == 0), stop=(kc == 7)),
                                 reads=["wqb", "h2T"], writes=["p_q"])
                    P.op("act", lambda e, c4=c4: e.copy(out=qT[:, c4 * 512:(c4 + 1) * 512], in_=p_q[:]), reads=["p_q"], writes=[("qT", c4)])
                for ch in range(16):
                    P.op("pe", lambda e, ch=ch: e.matmul(p_s4[:, ch * 128:(ch + 1) * 128], lhsT=qT[:, ch * 128:(ch + 1) * 128], rhs=skb[:, ch * 128:(ch + 1) * 128],
                                                        start=True, stop=True), reads=[("qT", ch // 4), "skb"], writes=[("p_s4", ch // 8)])
                for c4 in range(4):
                    P.op("act", lambda e, c4=c4: e.copy(out=s_sb[:, c4 * 512:(c4 + 1) * 512], in_=p_s4[:, c4 * 512:(c4 + 1) * 512]), reads=[("p_s4", c4 // 2)], writes=[("s_sb", c4)])
                for ch in range(16):
                    sk = ("s_sb", ch // 4)
                    P.op("dve", lambda e, ch=ch: e.max(out=V4[:, ch, 0:8], in_=s3[:, ch, :]), reads=[sk], writes=[("V", ch, 0)])
                    P.op("dve", lambda e, ch=ch: e.max_index(out=I4[:, ch, 0:8], in_max=V4[:, ch, 0:8], in_values=s3[:, ch, :]), reads=[sk, ("V", ch, 0)], writes=[("I", ch, 0)])
                    P.op("dve", lambda e, ch=ch: e.match_replace(out=s2[:], in_to_replace=V4[:, ch, 0:8], in_values=s3[:, ch, :], imm_value=-BIG), reads=[sk, ("V", ch, 0)], writes=["s2"])
                    P.op("dve", lambda e, ch=ch: e.max(out=V4[:, ch, 8:16], in_=s2[:]), reads=["s2"], writes=[("V", ch, 1)])
                    P.op("dve", lambda e, ch=ch: e.max_index(out=I4[:, ch, 8:16], in_max=V4[:, ch, 8:16], in_values=s2[:]), reads=["s2", ("V", ch, 1)], writes=[("I", ch, 1)])
                allV = [("V", ch, k_) for ch in range(16) for k_ in range(2)]
                allI = [("I", ch, k_) for ch in range(16) for k_ in range(2)]
                P.op("dve", lambda e: e.tensor_copy(out=I16f[:], in_=I16[:]), reads=allI, writes=["I16f"])
                cand4 = cand[:].rearrange("p (h a b) -> p h a b", a=16, b=16)
                V5 = V16[:].rearrange("p (h t k) -> p h t k", t=2, k=16)
                I5 = I16f[:].rearrange("p (h t k) -> p h t k", t=2, k=16)
                for hd in range(8):
                    P.op("dve", lambda e, hd=hd: e.tensor_tensor(out=cand4[:, hd], in0=V5[:, hd, 0, :].unsqueeze(2).to_broadcast([128, 16, 16]),
                                                                 in1=V5[:, hd, 1, :].unsqueeze(1).to_broadcast([128, 16, 16]), op=ALU.add),
                         reads=allV, writes=[("cand", hd)])
                    cd = cand[:, hd * 256:(hd + 1) * 256]
                    P.op("dve", lambda e, hd=hd, cd=cd: e.max(out=TV[:, hd * 16:hd * 16 + 8], in_=cd), reads=[("cand", hd)], writes=[("TV", hd, 0)])
                    P.op("dve", lambda e, hd=hd, cd=cd: e.max_index(out=TJ[:, hd * 16:hd * 16 + 8], in_max=TV[:, hd * 16:hd * 16 + 8], in_values=cd),
                         reads=[("cand", hd), ("TV", hd, 0)], writes=[("TJ", hd, 0)])
                    P.op("dve", lambda e, hd=hd, cd=cd: e.match_replace(out=c2[:], in_to_replace=TV[:, hd * 16:hd * 16 + 8], in_values=cd, imm_value=-BIG),
                         reads=[("cand", hd), ("TV", hd, 0)], writes=["c2"])
                    P.op("dve", lambda e, hd=hd: e.max(out=TV[:, hd * 16 + 8:hd * 16 + 16], in_=c2[:]), reads=["c2"], writes=[("TV", hd, 1)])
                    P.op("dve", lambda e, hd=hd: e.max_index(out=TJ[:, hd * 16 + 8:hd * 16 + 16], in_max=TV[:, hd * 16 + 8:hd * 16 + 16], in_values=c2[:]),
                         reads=["c2", ("TV", hd, 1)], writes=[("TJ", hd, 1)])
                allTV = [("TV", hd, k_) for hd in range(8) for k_ in range(2)]
                allTJ = [("TJ", hd, k_) for hd in range(8) for k_ in range(2)]
                TV3 = TV[:].rearrange("p (h k) -> p h k", k=16)
                ex3 = ex[:].rearrange("p (h k) -> p h k", k=16)
                P.op("dve", lambda e: e.tensor_tensor(out=ex3, in0=TV3, in1=TV3[:, :, 0:1].to_broadcast([128, 8, 16]), op=ALU.subtract), reads=allTV, writes=["ex"])
                P.op("act", lambda e: e.activation(out=ex[:], in_=ex[:], func=AF.Exp), reads=["ex"], writes=["ex"])
                P.op("dve", lambda e: e.tensor_reduce(out=sm[:], in_=ex3, axis=AX.X, op=ALU.add), reads=["ex"], writes=["sm"])
                P.op("dve", lambda e: e.reciprocal(out=sm[:], in_=sm[:]), reads=["sm"], writes=["sm"])
                P.op("dve", lambda e: e.tensor_tensor(out=Wt[:].rearrange("p (h k) -> p h k", k=16), in0=ex3, in1=sm[:].unsqueeze(2).to_broadcast([128, 8, 16]), op=ALU.mult),
                     reads=["ex", "sm"], writes=["Wt"])
                P.op("dve", lambda e: e.tensor_single_scalar(out=TA[:], in_=TJ[:], scalar=4, op=ALU.logical_shift_right), reads=allTJ, writes=["TA"])
                P.op("dve", lambda e: e.tensor_single_scalar(out=TBb[:], in_=TJ[:], scalar=15, op=ALU.bitwise_and), reads=allTJ, writes=["TB"])
                P.op("dve", lambda e: e.tensor_copy(out=TAf[:], in_=TA[:]), reads=["TA"], writes=["TAf"])
                P.op("dve", lambda e: e.tensor_copy(out=TBf[:], in_=TBb[:]), reads=["TB"], writes=["TBf"])
                oh4 = oh[:].rearrange("p (h k a) -> p h k a", k=16, a=16)
                for which, (tf, dst) in enumerate([(TAf, i0), (TBf, i1)]):
                    tf3 = tf[:].rearrange("p (h k) -> p h k", k=16)
                    P.op("dve", lambda e, tf3=tf3: e.tensor_tensor(out=oh4, in0=tf3.unsqueeze(3).to_broadcast([128, 8, 16, 16]),
                                                                  in1=io16[:].unsqueeze(1).unsqueeze(1).to_broadcast([128, 8, 16, 16]), op=ALU.is_equal),
                         reads=["TAf", "TBf", "io16"], writes=["oh"])
                    P.op("dve", lambda e, which=which: e.tensor_tensor(out=oh4, in0=oh4, in1=I5[:, :, which, :].unsqueeze(2).to_broadcast([128, 8, 16, 16]), op=ALU.mult),
                         reads=["oh", "I16f"], writes=["oh"])
                    P.op("dve", lambda e, dst=dst: e.tensor_reduce(out=dst[:], in_=oh4, axis=AX.X, op=ALU.add), reads=["oh"], writes=["i01_%d" % which])
                P.op("dve", lambda e: e.scalar_tensor_tensor(out=ef[:], in0=i0[:], scalar=128.0, in1=i1[:], op0=ALU.mult, op1=ALU.add), reads=["i01_0", "i01_1"], writes=["ef"])
                P.op("pe", lambda e: e.transpose(p_t[:, 0:128], ef[:], identf[:]), reads=["ef"], writes=["p_t"])
                P.op("pe", lambda e: e.transpose(p_t[:, 128:256], Wt[:], identf[:]), reads=["Wt"], writes=["p_t"])
                P.op("dve", lambda e: e.tensor_copy(out=idxT[:], in_=p_t[:, 0:128]), reads=["p_t"], writes=["idxT"])
                P.op("dve", lambda e: e.tensor_copy(out=WT[:], in_=p_t[:, 128:256]), reads=["p_t"], writes=["WT"])
                for t_ in range(128):
                    ub = U[t_ % NU]
                    P.op("pool", lambda e, ub=ub, t_=t_: e.indirect_dma_start(out=ub[:], out_offset=None, in_=pdb,
                                                                             in_offset=bass.IndirectOffsetOnAxis(ap=idxT[:, t_:t_ + 1], axis=0)),
                         reads=["idxT"], writes=[("U", t_ % NU)], dma=True)
                    hbt, hbk = (p_hb[:], "p_hb") if t_ % 2 == 0 else (p_s4[:, 1024:2048], ("p_s4", 1))
                    for half in range(2):
                        P.op("pe", lambda e, t_=t_, half=half, hbt=hbt: e.matmul(hbt[:, half * 512:(half + 1) * 512], lhsT=selrow3[:, t_, :], rhs=h2b[:, half * 512:(half + 1) * 512],
                                                                                 start=True, stop=True), reads=["selrow", "h2b"], writes=[hbk])
                    P.op("dve", lambda e, ub=ub, t_=t_, hbt=hbt: e.tensor_tensor_reduce(out=jk[:], in0=ub[:], in1=hbt, scale=1.0, scalar=0.0, op0=ALU.mult, op1=ALU.add,
                                                                                       accum_out=aT[:, t_:t_ + 1]), reads=[("U", t_ % NU), hbk], writes=["sq", "aT"])
                P.op("act", lambda e: e.activation(out=g2[:], in_=aT[:], func=AF.Square), reads=["aT"], writes=["g2"])
                P.op("dve", lambda e: e.tensor_scalar(out=g2[:], in0=g2[:], scalar1=0.044715, scalar2=1.0, op0=ALU.mult, op1=ALU.add), reads=["g2"], writes=["g2"])
                P.op("dve", lambda e: e.tensor_tensor(out=g3[:], in0=g2[:], in1=aT[:], op=ALU.mult), reads=["g2", "aT"], writes=["g3"])
                P.op("act", lambda e: e.activation(out=g3[:], in_=g3[:], func=AF.Sigmoid, scale=1.5957691216), reads=["g3"], writes=["g3"])
                P.op("dve", lambda e: e.tensor_tensor(out=g3[:], in0=g3[:], in1=aT[:], op=ALU.mult), reads=["g3", "aT"], writes=["g3"])
                P.op("dve", lambda e: e.tensor_tensor(out=cT[:], in0=g3[:], in1=WT[:], op=ALU.mult), reads=["g3", "WT"], writes=["cT"])
                for t_ in range(128):
                    ub = U[t_ % NU]
                    P.op("pool", lambda e, ub=ub, t_=t_: e.indirect_dma_start(out=ub[:], out_offset=None, in_=pub,
                                                                             in_offset=bass.IndirectOffsetOnAxis(ap=idxT[:, t_:t_ + 1], axis=0)),
                         reads=["idxT"], writes=[("U", t_ % NU)], dma=True)
                    cmt = cm[t_ % 3]
                    P.op("dve", lambda e, cmt=cmt, t_=t_: e.scalar_tensor_tensor(out=cmt[:], in0=io128[:], scalar=float(t_), in1=cT[:, t_:t_ + 1].to_broadcast([128, 128]),
                                                                                op0=ALU.is_equal, op1=ALU.mult), reads=["io128", "cT"], writes=[("cm", t_ % 3)])
                    for half in range(2):
                        P.op("pe", lambda e, ub=ub, cmt=cmt, t_=t_, half=half: e.matmul(p_s4[:, half * 512:(half + 1) * 512], lhsT=cmt[:],
                                                                                      rhs=ub[:, half * 512:(half + 1) * 512], start=(t_ == 0), stop=(t_ == 127)),
                             reads=[("U", t_ % NU), ("cm", t_ % 3)], writes=[("p_s4", 0)])
                P.op("dve", lambda e: e.tensor_tensor(out=x2[:], in0=p_s4[:, 0:1024], in1=GA2[:], op=ALU.mult), reads=[("p_s4", 0), "GA2"], writes=["x2"])
                P.op("pool", lambda e: e.tensor_tensor(out=x2[:], in0=x2[:], in1=x1[:], op=ALU.add), reads=["x2", "x1"], writes=["x2"])
                P.op("act", lambda e: e.activation(out=sq[:], in_=x2[:], func=AF.Square), reads=["x2"], writes=["sq"])
                P.op("dve", lambda e: e.reduce_sum(out=ss[:], in_=sq[:], axis=AX.X), reads=["sq"], writes=["ss"])
                P.op("act", lambda e: e.activation(out=rs[:], in_=ss[:], func=AF.Sqrt, scale=1.0 / 1024, bias=epst[:]), reads=["ss"], writes=["rs"])
                P.op("dve", lambda e: e.reciprocal(out=rs[:], in_=rs[:]), reads=["rs"], writes=["rs"])
                P.op("dve", lambda e: e.scalar_tensor_tensor(out=yo[:], in0=x2[:], scalar=rs[:], in1=GF[:], op0=ALU.mult, op1=ALU.mult), reads=["x2", "rs", "GF"], writes=["yo"])
                P.dma("sp", out[i * 128:(i + 1) * 128, :], yo[:], reads=["yo"])
            P.emit()
    return nc


def make_inputs(cfg, core, inp, n_per_batch=4, S_full=None):
    NW, NO = cfg.NW, cfg.NO
    b, j = core // n_per_batch, core % n_per_batch
    off = NO * 128 * (j + 1) - NW * 128
    x = inp["x"][b]
    xw = np.zeros((NW * 128, 1024), np.float32)
    lo = max(0, -off)
    xw[lo:] = x[off + lo: off + NW * 128]
    m = {"xw": xw}
    m["ccol"] = np.ascontiguousarray(inp["c"][b].reshape(8, 128).T)
    m["w_ada"] = inp["w_ada"][0]
    m["b_adaT"] = np.ascontiguousarray(inp["b_ada"][0].reshape(48, 128).T)
    m["gmixT"] = np.ascontiguousarray(inp["g_norm_mix"][0].reshape(8, 128).T)
    m["gffnT"] = np.ascontiguousarray(inp["g_norm_ffn"][0].reshape(8, 128).T)
    m["gfin"] = inp["g_norm_final"].reshape(1, 1024)
    m["w_in"] = np.ascontiguousarray(inp["w_in"][0][:, w_in_perm()])
    m["w_out"] = inp["w_out"][0]
    for nm, key in (("w1k", "w_cmp_k1"), ("w1v", "w_cmp_v1")):
        m[nm] = np.ascontiguousarray(inp[key][0].reshape(32, 64, 256).transpose(1, 0, 2).reshape(64, 32 * 256))
    m["peTk"] = np.ascontiguousarray(inp["pe_cmp_k"][0].T)
    m["peTv"] = np.ascontiguousarray(inp["pe_cmp_v"][0].T)
    m["b1k"] = np.ascontiguousarray(inp["b_cmp_k1"][0].reshape(2, 128).T)
    m["b1v"] = np.ascontiguousarray(inp["b_cmp_v1"][0].reshape(2, 128).T)
    m["w2k"] = np.ascontiguousarray(inp["w_cmp_k2"][0].reshape(2, 128, 64).transpose(1, 0, 2).reshape(128, 128))
    m["w2v"] = np.ascontiguousarray(inp["w_cmp_v2"][0].reshape(2, 128, 64).transpose(1, 0, 2).reshape(128, 128))
    m["w_pq"] = inp["w_peer_q"][0]
    m["skT"] = np.ascontiguousarray(inp["peer_subkeys"][0].reshape(16, 128, 128).transpose(2, 0, 1).reshape(128, 2048))
    m["pdown"] = inp["peer_down"][0]
    m["pup"] = inp["peer_up"][0]
    for k_, v_ in host_tables(cfg, off).items():
        m["t_" + k_] = v_
    return m


_NC_CACHE = {}


def kernel(**inputs):
    inp = {k: np.asarray(v) for k, v in inputs.items()}
    cfg = CFG()
    if "nc" not in _NC_CACHE:
        _NC_CACHE["nc"] = build(cfg)
        mybir.codegen_inst_isa_subclasses(_NC_CACHE["nc"])
    nc = _NC_CACHE["nc"]
    in_maps = [make_inputs(cfg, c, inp) for c in range(8)]
    res = run_bass_kernel_spmd(nc, in_maps, core_ids=list(range(8)))
    out = np.zeros((2, 8192, 1024), np.float32)
    for c in range(8):
        b, j = c // 4, c % 4
        out[b, j * 2048:(j + 1) * 2048] = res.results[c]["out"]
    return out
```

```python
import numpy as np
import ml_dtypes
from contextlib import ExitStack
import concourse.bass as bass
import concourse.mybir as mybir
from concourse.bass_utils import run_bass_kernel_spmd

F32 = mybir.dt.float32
BF16 = mybir.dt.bfloat16
U32 = mybir.dt.uint32
F32R = mybir.dt.float32r
ALU = mybir.AluOpType
AF = mybir.ActivationFunctionType
AX = mybir.AxisListType

import os
ROPE_ENG = os.environ.get("ROPE_ENG", "dve")
NEG = -30000.0
BIG = 1.0e30
COMPUTE = ("pe", "act", "dve", "pool")


class _Op:
    __slots__ = ("eng", "fn", "reads", "writes", "deps", "is_dma", "signal", "ev", "id")


class SemState:
    def __init__(self, nc, stack, n_dma_sems=24):
        self.n_dma_sems = n_dma_sems
        self.sems = {}
        for e in COMPUTE:
            self.sems[e] = stack.enter_context(nc.semaphore("s_" + e))
        for k in range(n_dma_sems):
            self.sems["d%d" % k] = stack.enter_context(nc.semaphore("s_d%d" % k))
        self.cnt = {e: 0 for e in COMPUTE}
        self.dma_cnt = [0] * n_dma_sems


class Prog:
    def __init__(self, nc, state):
        self.nc = nc
        self.state = state
        self.ops = []
        self.last_writer = {}
        self.readers = {}
        self.n_dma_sems = state.n_dma_sems
        self.dma_rr = 0
        self.sw_rr = 0
        self.dma_last = [None] * self.n_dma_sems
        self.dma_cnt = state.dma_cnt

    def op(self, eng, fn, reads=(), writes=(), dma=False):
        o = _Op()
        o.eng, o.fn, o.is_dma = eng, fn, dma
        o.reads, o.writes = tuple(reads), tuple(writes)
        o.deps = set()
        o.signal = False
        o.ev = None
        o.id = len(self.ops)
        for r in o.reads:
            w = self.last_writer.get(r)
            if w is not None:
                o.deps.add(w)
        for w_ in o.writes:
            w = self.last_writer.get(w_)
            if w is not None:
                o.deps.add(w)
            lastc = {}
            for rd in self.readers.get(w_, ()):
                p_ = self.ops[rd]
                if p_.is_dma:
                    o.deps.add(rd)
                else:
                    lastc[p_.eng] = rd
            o.deps.update(lastc.values())
        for r in o.reads:
            self.readers.setdefault(r, []).append(o.id)
        for w_ in o.writes:
            self.last_writer[w_] = o.id
            self.readers[w_] = []
        o.deps.discard(o.id)
        if dma:
            if eng == "pool":
                k = self.n_dma_sems - 4 + self.sw_rr
                self.sw_rr = (self.sw_rr + 1) % 4
            else:
                k = self.dma_rr
                self.dma_rr = (self.dma_rr + 1) % (self.n_dma_sems - 4)
            prev = self.dma_last[k]
            if prev is not None:
                o.deps.add(prev)
            self.dma_last[k] = o.id
            self.dma_cnt[k] += 16
            o.ev = ("d%d" % k, self.dma_cnt[k])
        self.ops.append(o)
        return o.id

    def dma(self, eng, out, in_, reads=(), writes=(), **kw):
        return self.op(eng, lambda e: e.dma_start(out=out, in_=in_, **kw), reads, writes, dma=True)

    def emit(self):
        nc, ops = self.nc, self.ops
        for o in ops:
            nd = set()
            for d in o.deps:
                p = ops[d]
                if p.is_dma or o.is_dma or p.eng != o.eng:
                    nd.add(d)
                elif o.eng != "pe":
                    nd.add(d)
            o.deps = nd
            for d in nd:
                if not ops[d].is_dma:
                    ops[d].signal = True
        cnt = self.state.cnt
        for o in ops:
            if not o.is_dma and o.signal:
                cnt[o.eng] += 1
                o.ev = (o.eng, cnt[o.eng])
        finals = [ops[i] for i in self.dma_last if i is not None]
        with ExitStack() as st:
            sems = self.state.sems
            block = st.enter_context(nc.Block())
            streams = {}
            for o in ops:
                streams.setdefault(o.eng, []).append(o)

            def run_stream(ename, e, final=False):
                waited = {}

                def wait(ev):
                    if waited.get(ev[0], 0) < ev[1]:
                        e.wait_ge(sems[ev[0]], ev[1])
                        waited[ev[0]] = ev[1]

                for o in streams.get(ename, []):
                    for d in sorted(o.deps):
                        wait(ops[d].ev)
                    ins = o.fn(e)
                    if o.is_dma:
                        ins.then_inc(sems[o.ev[0]], 16)
                    elif o.signal:
                        ins.then_inc(sems[o.eng], 1)
                if final:
                    for o in finals:
                        wait(o.ev)

            block.sync(lambda e: run_stream("sp", e, final=True))
            block.scalar(lambda e: run_stream("act", e))
            block.vector(lambda e: run_stream("dve", e))
            block.gpsimd(lambda e: run_stream("pool", e))
            block.tensor(lambda e: run_stream("pe", e))


def _blk(flag):
    if flag:
        with ExitStack() as st:
            yield st


class CFG:
    def __init__(self, NW=64, NO=16, NEXP=16384, stop=9):
        self.NW, self.NO, self.NEXP, self.stop = NW, NO, NEXP, stop
        self.astop = 99
        self.NB = NW * 8
        self.NCH = max(1, self.NB // 128)
        self.NWK = min(NW, NO + 4)
        self.NSB = NW * 2


LOGG = [float(np.log1p(-np.exp2(-5.0 - h))) for h in range(4)]


def host_tables(cfg, off):
    NW, NO, NB, NCH = cfg.NW, cfg.NO, cfg.NB, cfg.NCH
    S = NW * 128
    p = (off + np.arange(S)).astype(np.float32)
    t = {}
    inv128 = (10000.0 ** (-np.arange(0, 128, 2, dtype=np.float32) / 128)).astype(np.float32)
    inv64 = (10000.0 ** (-np.arange(0, 64, 2, dtype=np.float32) / 64)).astype(np.float32)
    a128 = p[:, None] * inv128[None]
    a64 = p[:, None] * inv64[None]
    t["rope"] = np.concatenate([np.cos(a128), np.sin(a128), np.cos(a64), np.sin(a64)], 1).astype(np.float32)
    valid_tile = ((off + 128 * np.arange(NW)) >= 0).astype(np.float32)
    n = np.arange(128, dtype=np.float32)
    sc = 128 ** -0.5
    zt = np.stack([np.exp((127 - n) * LOGG[h]) * sc for h in range(4)], 1)
    t["zeta"] = (zt[:, None, :] * valid_tile[None, :, None]).astype(np.float32).reshape(128, NW * 4)
    dm = np.zeros((128, 4, 128), np.float32)
    for h in range(4):
        d = n[None, :] - n[:, None]
        dm[:, h, :] = np.where(d >= 0, np.exp(np.where(d >= 0, d, 0) * LOGG[h]), 0.0) * sc
    t["dmat"] = dm.reshape(128, 512)
    xi = np.stack([np.exp((n + 1.0) * LOGG[h]) for h in range(4)], 0)
    t["xi"] = np.broadcast_to(xi[None], (128, 4, 128)).reshape(128, 512).astype(np.float32).copy()
    t["keybias"] = np.broadcast_to(np.where(valid_tile > 0, 0.0, NEG)[None], (128, NW)).astype(np.float32).copy()
    nb = np.arange(NCH * 128)
    cvalid = ((off + 16 * nb) >= 0) & (nb <= NB - 2)
    t["cmpbias"] = np.where(cvalid, 0.0, NEG).astype(np.float32).reshape(NCH, 128).T.copy()
    tq = (NW - NO) * 128 + np.arange(NO * 128)
    cp = np.where((16 * nb[:, None] + 31) <= tq[None, :], 0.0, NEG)
    cp = cp.reshape(NCH, 128, NO, 128).transpose(2, 1, 0, 3)
    cp = np.broadcast_to(cp[:, :, :, None, :], (NO, 128, NCH, 4, 128))
    t["cmppen"] = cp.astype(ml_dtypes.bfloat16).reshape(NO * 128, NCH * 512)
    k = np.arange(128)
    t["tri"] = np.concatenate([np.tile(np.where(k[:, None] <= k[None, :], 0.0, NEG), (1, 4)),
                               np.tile(np.where(k[:, None] > k[None, :], 0.0, NEG), (1, 4))], 1).astype(ml_dtypes.bfloat16)
    t["iota128"] = np.broadcast_to(np.arange(128, dtype=np.float32)[None], (128, 128)).copy()
    mb = np.arange(128)
    cur = tq // 64
    b0 = (-off) // 64
    validb = (mb[None, :] >= b0) & (mb[None, :] <= cur[:, None]) & (mb[None, :] < NW * 2)
    forced = ((mb[None, :] == b0) | (mb[None, :] == cur[:, None]) | (mb[None, :] == cur[:, None] - 1)) & validb
    m1 = (validb & ~forced).astype(np.float32)
    m2 = np.where(forced, 1e9, np.where(validb, 0.0, -BIG)).astype(np.float32)
    t["selm"] = np.concatenate([m1, m2], 1)
    key = np.arange(S)
    ex = np.zeros((128, S), np.float32)
    ex[key // 64, key] = 1.0
    t["expand"] = ex.astype(ml_dtypes.bfloat16)
    cs = 16 * nb
    ss = 64 * mb
    ov = np.clip(np.minimum(cs[:, None] + 32, ss[None, :] + 64) - np.maximum(cs[:, None], ss[None, :]), 0, None) / 16.0
    t["overlap"] = ov.reshape(NCH, 128, 128).transpose(1, 0, 2).reshape(128, NCH * 128).astype(ml_dtypes.bfloat16)
    t["ident"] = np.eye(128, dtype=np.float32)
    sel = np.zeros((128, 128, 128), np.float32)
    sel[k, k, :] = 1.0
    t["selrow"] = sel.reshape(128, 128 * 128).astype(ml_dtypes.bfloat16)
    t["iota16"] = np.broadcast_to(np.arange(16, dtype=np.float32)[None], (128, 16)).copy()
    return t


def w_in_perm():
    r = lambda a, b: list(range(a, b))
    return np.array(r(512, 1024) + r(1024, 1536) + r(2560, 2688) + r(2816, 2944) + r(3072, 3200)
                    + r(2688, 2816) + r(2944, 3072) + r(3200, 3328)
                    + r(0, 512) + r(1536, 2048)
                    + [2048 + (g * 4 + h) * 64 + d for h in range(4) for g in range(2) for d in range(64)]
                    + r(3328, 3352))


B_RK, B_RV, B_NK, B_NV, B_RQ, B_RG, B_NQ, B_GT = (0, 512), (512, 512), (1024, 384), (1408, 384), \
    (1792, 512), (2304, 512), (2816, 512), (3328, 24)


def build(cfg):
    NW, NO, NB, NCH, NWK = cfg.NW, cfg.NO, cfg.NB, cfg.NCH, cfg.NWK
    T0 = NW - NO
    S = NW * 128
    nc = bass.Bass("TRN2", target_bir_lowering=False)
    dram = lambda name, shape, dt=F32, kind="ExternalInput": nc.dram_tensor(name, shape, dt, kind=kind).ap()
    xw = dram("xw", [S, 1024])
    ccol = dram("ccol", [128, 8])
    w_ada = dram("w_ada", [1024, 6144])
    b_adaT = dram("b_adaT", [128, 48])
    gmixT = dram("gmixT", [128, 8])
    gffnT = dram("gffnT", [128, 8])
    gfin = dram("gfin", [1, 1024])
    w_in = dram("w_in", [1024, 3352])
    w_out = dram("w_out", [1024, 1024])
    w1k = dram("w1k", [64, 32 * 256])
    w1v = dram("w1v", [64, 32 * 256])
    peTk = dram("peTk", [64, 32])
    peTv = dram("peTv", [64, 32])
    b1k = dram("b1k", [128, 2])
    b1v = dram("b1v", [128, 2])
    w2k = dram("w2k", [128, 2 * 64])
    w2v = dram("w2v", [128, 2 * 64])
    w_pq = dram("w_pq", [1024, 2048])
    skT = dram("skT", [128, 2048])
    pdown = dram("pdown", [cfg.NEXP, 1024])
    pup = dram("pup", [cfg.NEXP, 1024])
    tb = {}
    for name, shape, dt in [("rope", [S, 192], F32), ("zeta", [128, NW * 4], F32), ("dmat", [128, 512], F32),
                            ("xi", [128, 512], F32), ("keybias", [128, NW], F32), ("cmpbias", [128, NCH], F32),
                            ("cmppen", [NO * 128, NCH * 512], BF16), ("tri", [128, 1024], BF16), ("iota128", [128, 128], F32),
                            ("selm", [NO * 128, 256], F32), ("expand", [128, S], BF16),
                            ("overlap", [128, NCH * 128], BF16), ("ident", [128, 128], F32),
                            ("selrow", [128, 128 * 128], BF16), ("iota16", [128, 16], F32)]:
        tb[name] = dram("t_" + name, shape, dt)
    out = dram("out", [NO * 128, 1024], kind="ExternalOutput")
    rawd = dram("rawd", [2, 128, S], BF16, kind="Internal")
    RAWLEN = max(S + 16, 16 * NCH * 128 + 32)
    x1d = dram("x1d", [NO * 128, 1024], kind="Internal")
    pdb = dram("pdb", [cfg.NEXP, 1024], BF16, kind="Internal")
    pub = dram("pub", [cfg.NEXP, 1024], BF16, kind="Internal")

    with ExitStack() as outer:
        sbo = lambda name, shape, dt=F32: outer.enter_context(nc.sbuf_tensor(name, shape, dt))
        SEM = SemState(nc, outer)
        vec = sbo("vec", [128, 48])

        _bc = [0]

        def bcast_rows(P, sb, pbrk, items):
            _bc[0] += 1
            onesf = sb("onesf%d" % _bc[0], [128, 128]); diag = [sb("diag%d_%d" % (i_, _bc[0]), [128, 128]) for i_ in range(2)]
            P.op("dve", lambda e: e.memset(onesf[:], 1.0), writes=["onesf"])
            n = 0
            for vi, dst, dkey in items:
                for half in range(2):
                    pb, pkey = pbrk[n % 2]
                    for q in range(4):
                        fc = half * 4 + q
                        dg = diag[q % 2]
                        P.op("dve", lambda e, dg=dg, vi=vi, fc=fc: e.tensor_scalar(out=dg[:], in0=identf[:], scalar1=vec[:, vi * 8 + fc:vi * 8 + fc + 1],
                                                                                  scalar2=None, op0=ALU.mult),
                             reads=["identf", "vec"], writes=[("diag", q % 2)])
                        P.op("pe", lambda e, dg=dg, pb=pb, q=q: e.matmul(pb[:, q * 128:(q + 1) * 128], lhsT=onesf[:], rhs=dg[:], start=True, stop=True),
                             reads=[("diag", q % 2), "onesf"], writes=[pkey])
                    P.op("act", lambda e, pb=pb, dst=dst, half=half: e.copy(out=dst[:, half * 512:(half + 1) * 512], in_=pb[:]),
                         reads=[pkey], writes=[dkey])
                    n += 1
        identf = sbo("identf", [128, 128]); identb = sbo("identb", [128, 128], BF16)
        epst = sbo("epst", [128, 1])
        mid = ExitStack()
        sbm = lambda name, shape, dt=F32: mid.enter_context(nc.sbuf_tensor(name, shape, dt))
        ksT = sbm("ksT", [128, S], BF16)
        vs = sbm("vs", [128, NW * 2 * 65], BF16)
        kwT = sbm("kwT", [128, NWK * 128], BF16)
        vw = sbm("vw", [128, NWK * 2 * 65], BF16)
        kcT = sbm("kcT", [128, NCH * 128], BF16)
        vca = sbm("vca", [128, NCH * 2 * 193], BF16)
        reto = sbm("reto", [128, NO * 512], BF16)
        qTs = sbm("qTs", [128, NO * 512], BF16)
        gts = sbm("gts", [128, NO * 24])
        vs4 = vs[:].rearrange("p (t g d) -> p t g d", g=2, d=65)
        vw4 = vw[:].rearrange("p (t g d) -> p t g d", g=2, d=65)
        vca4 = vca[:].rearrange("p (c g d) -> p c g d", g=2, d=193)

        for st in _blk(cfg.stop >= 0):
            sb = lambda name, shape, dt=F32: st.enter_context(nc.sbuf_tensor(name, shape, dt))
            ps = lambda name, shape, dt=F32: st.enter_context(nc.psum_tensor(name, shape, dt))
            P = Prog(nc, SEM)
            wad = [sb("wad%d" % i, [128, 8 * 1024]) for i in range(2)]
            cc = sb("cc", [128, 8]); sil = sb("sil", [128, 8])
            badT = sb("badT", [128, 48]); gm = sb("gm", [128, 8]); gf_ = sb("gf_", [128, 8])
            modT = sb("modT", [128, 48])
            pm = ps("pm", [128, 48])
            P.dma("sp", cc[:], ccol, writes=["cc"])
            P.dma("sp", badT[:], b_adaT, writes=["badT"])
            P.dma("sp", gm[:], gmixT, writes=["gm"])
            P.dma("sp", gf_[:], gffnT, writes=["gf_"])
            P.dma("sp", identf[:], tb["ident"], writes=["identf"])
            P.op("dve", lambda e: e.memset(epst[:], 1e-6), writes=["eps"])
            P.op("act", lambda e: e.activation(out=sil[:], in_=cc[:], func=AF.Silu), reads=["cc"], writes=["sil"])
            P.op("dve", lambda e: e.tensor_copy(out=identb[:], in_=identf[:]), reads=["identf"], writes=["identb"])
            for s in range(6):
                wt = wad[s % 2]
                wt3 = wt[:].rearrange("p (k n) -> p k n", k=8)
                for kc in range(8):
                    P.dma("sp" if kc % 2 == 0 else "act", wt3[:, kc, :], w_ada[kc * 128:(kc + 1) * 128, s * 1024:(s + 1) * 1024],
                          writes=[("wad", s % 2, kc)])
                for fc in range(8):
                    for kc in range(8):
                        P.op("pe", lambda e, fc=fc, kc=kc, wt3=wt3, s=s: e.matmul(
                            pm[:, s * 8 + fc:s * 8 + fc + 1], lhsT=wt3[:, kc, fc * 128:(fc + 1) * 128], rhs=sil[:, kc:kc + 1],
                            start=(kc == 0), stop=(kc == 7)), reads=[("wad", s % 2, kc), "sil"], writes=["pm"])
            P.op("dve", lambda e: e.tensor_tensor(out=modT[:], in0=pm[:], in1=badT[:], op=ALU.add), reads=["pm", "badT"], writes=["modT"])
            P.op("dve", lambda e: e.scalar_tensor_tensor(out=vec[:, 0:8], in0=modT[:, 8:16], scalar=1.0, in1=gm[:], op0=ALU.add, op1=ALU.mult),
                 reads=["modT", "gm"], writes=["vec"])
            P.op("dve", lambda e: e.scalar_tensor_tensor(out=vec[:, 16:24], in0=modT[:, 32:40], scalar=1.0, in1=gf_[:], op0=ALU.add, op1=ALU.mult),
                 reads=["modT", "gf_"], writes=["vec"])
            P.op("dve", lambda e: e.tensor_copy(out=vec[:, 8:16], in_=modT[:, 0:8]), reads=["modT"], writes=["vec"])
            P.op("dve", lambda e: e.tensor_copy(out=vec[:, 24:32], in_=modT[:, 24:32]), reads=["modT"], writes=["vec"])
            P.op("dve", lambda e: e.tensor_copy(out=vec[:, 32:40], in_=modT[:, 16:24]), reads=["modT"], writes=["vec"])
            P.op("dve", lambda e: e.tensor_copy(out=vec[:, 40:48], in_=modT[:, 40:48]), reads=["modT"], writes=["vec"])
            P.emit()

        for st in _blk(cfg.stop >= 1):
            sb = lambda name, shape, dt=F32: st.enter_context(nc.sbuf_tensor(name, shape, dt))
            ps = lambda name, shape, dt=F32: st.enter_context(nc.psum_tensor(name, shape, dt))
            P = Prog(nc, SEM)
            wib = sb("wib", [128, 8 * 3352], BF16)
            wib3 = wib[:].rearrange("p (k n) -> p k n", k=8)
            stg = [sb("stg%d" % i, [128, 838]) for i in range(2)]
            A1R = sb("A1R", [128, 1024]); B1R = sb("B1R", [128, 1024])
            n_ = 0
            for kc in range(8):
                for cq in range(4):
                    sl = slice(cq * 838, (cq + 1) * 838)
                    P.dma("sp", stg[n_ % 2][:], w_in[kc * 128:(kc + 1) * 128, sl], writes=[("stg", n_ % 2)])
                    if n_ % 2:
                        P.op("act", lambda e, kc=kc, sl=sl, n_=n_: e.copy(out=wib3[:, kc, sl], in_=stg[n_ % 2][:]), reads=[("stg", n_ % 2)], writes=["wib"])
                    else:
                        P.op("pool", lambda e, kc=kc, sl=sl, n_=n_: e.tensor_copy(out=wib3[:, kc, sl], in_=stg[n_ % 2][:]), reads=[("stg", n_ % 2)], writes=["wib"])
                    n_ += 1
            xt = [sb("xt%d" % i, [128, 1024]) for i in range(2)]
            sq = sb("sq", [128, 1024])
            ss = sb("ss", [128, 2]); rs = sb("rs", [128, 2])
            tmod = sb("tmod", [128, 1024])
            hb = sb("hb", [128, 1024], BF16)
            hT = [sb("hT%d" % i, [128, 1024], BF16) for i in range(2)]
            rp = [sb("rp%d" % i, [128, 192]) for i in range(2)]
            pj = [sb("pj%d" % i, [128, 512]) for i in range(3)]
            ra = sb("ra", [128, 256]); rb_ = sb("rb_", [128, 256]); rc_ = sb("rc_", [128, 256]); rd_ = sb("rd_", [128, 256])
            ktok = sb("ktok", [128, 512], BF16)
            qtok = sb("qtok", [128, 512], BF16)
            vtok = sb("vtok", [128, 512], BF16)
            vz = sb("vz", [128, 512], BF16)
            nk = sb("nk", [128, 384], BF16)
            nv = sb("nv", [128, 384], BF16)
            nq = sb("nq", [128, 512], BF16)
            Sst = sb("Sst", [128, 512]); Sb = sb("Sb", [128, 512], BF16)
            zt = sb("zt", [128, NW * 4]); dmat = sb("dmat", [128, 512]); xit = sb("xit", [128, 512])
            kTr = sb("kTr", [128, 512], BF16); qTr = sb("qTr", [128, 512], BF16); qxT = sb("qxT", [128, 512], BF16)
            pT = sb("pT", [128, 512], BF16)
            yv = sb("yv", [128, 512]); sg = sb("sg", [128, 512])
            st6 = sb("st6", [128, 4 * 6]); mv = sb("mv", [128, 4 * 2]); rstd4 = sb("rstd4", [128, 4])
            rawst = [sb("rawst%d" % i, [128, 256], BF16) for i in range(2)]
            p_tr = ps("p_tr", [128, 1024], BF16)
            p_mm = [ps("p_mm%d" % i, [128, 512]) for i in range(3)]
            p_kv = ps("p_kv", [128, 512])
            p_sc = ps("p_sc", [128, 512])
            p_y = ps("p_y", [128, 512])
            bcast_rows(P, sb, [(p_y, "p_y"), (p_sc, "p_sc")], [(0, A1R, "A1R"), (1, B1R, "B1R")])
            P.dma("sp", zt[:], tb["zeta"], writes=["zt"])
            P.dma("sp", dmat[:], tb["dmat"], writes=["dmat"])
            P.dma("sp", xit[:], tb["xi"], writes=["xit"])
            P.op("dve", lambda e: e.memset(Sst[:], 0.0), writes=["Sst"])
            P.op("dve", lambda e: e.memset(Sb[:], 0.0), writes=["Sb"])
            P.op("pool", lambda e: e.memset(vs[:], 1.0), writes=["vs"])
            P.op("pool", lambda e: e.memset(vw[:], 1.0), writes=["vw"])

            def rope(src, dst, H, D, cos, sin, keys_r, key_w):
                if os.environ.get("SKIP_ROPE"):
                    return
                h2 = D // 2
                s3 = src.rearrange("p (h d) -> p h d", h=H)
                d3 = dst.rearrange("p (h d) -> p h d", h=H)
                x1, x2 = s3[:, :, 0:h2], s3[:, :, h2:D]
                cb = cos.unsqueeze(1).to_broadcast([128, H, h2])
                sbb = sin.unsqueeze(1).to_broadcast([128, H, h2])
                n_ = H * h2
                v = lambda t_: t_[:, 0:n_].rearrange("p (h d) -> p h d", h=H)
                P.op("dve", lambda e: e.tensor_tensor(out=v(ra), in0=x1, in1=cb, op=ALU.mult), reads=keys_r, writes=["ra"])
                P.op(ROPE_ENG, lambda e: e.tensor_tensor(out=v(rb_), in0=x2, in1=sbb, op=ALU.mult), reads=keys_r, writes=["rb"])
                P.op("dve", lambda e: e.tensor_tensor(out=d3[:, :, 0:h2], in0=v(ra), in1=v(rb_), op=ALU.subtract), reads=["ra", "rb"], writes=[key_w + "_lo"])
                P.op(ROPE_ENG, lambda e: e.tensor_tensor(out=v(rc_), in0=x2, in1=cb, op=ALU.mult), reads=keys_r, writes=["rc"])
                P.op("dve", lambda e: e.tensor_tensor(out=v(rd_), in0=x1, in1=sbb, op=ALU.mult), reads=keys_r, writes=["rd"])
                P.op(ROPE_ENG, lambda e: e.tensor_tensor(out=d3[:, :, h2:D], in0=v(rc_), in1=v(rd_), op=ALU.add), reads=["rc", "rd"], writes=[key_w + "_hi"])

            def proj(blk, pdst, pkey, hTt, hkey):
                c0, w = blk
                for kc in range(8):
                    P.op("pe", lambda e, kc=kc: e.matmul(pdst[:, 0:w], lhsT=hTt[:, kc * 128:(kc + 1) * 128], rhs=wib3[:, kc, c0:c0 + w],
                                                        start=(kc == 0), stop=(kc == 7)), reads=[hkey, "wib"], writes=[pkey])

            for T in range(NW if cfg.astop >= 1 else 0):
                b = T % 2
                own = T >= T0
                i = T - T0
                P.dma("sp", xt[b][:], xw[T * 128:(T + 1) * 128, :], writes=[("xt", b)])
                P.dma("sp", rp[b][:], tb["rope"][T * 128:(T + 1) * 128, :], writes=[("rp", b)])
                P.op("act", lambda e, b=b: e.activation(out=sq[:], in_=xt[b][:], func=AF.Square), reads=[("xt", b)], writes=["sq"])
                P.op("dve", lambda e, b=b: e.reduce_sum(out=ss[:, b:b + 1], in_=sq[:], axis=AX.X), reads=["sq"], writes=[("ss", b)])
                P.op("act", lambda e, b=b: e.activation(out=rs[:, b:b + 1], in_=ss[:, b:b + 1], func=AF.Sqrt, scale=1.0 / 1024, bias=epst[:]),
                     reads=[("ss", b), "eps"], writes=[("rs", b)])
                P.op("dve", lambda e, b=b: e.reciprocal(out=rs[:, b:b + 1], in_=rs[:, b:b + 1]), reads=[("rs", b)], writes=[("rs", b)])
                P.op("dve", lambda e, b=b: e.scalar_tensor_tensor(out=tmod[:], in0=xt[b][:], scalar=rs[:, b:b + 1], in1=A1R[:], op0=ALU.mult, op1=ALU.mult),
                     reads=[("xt", b), ("rs", b), "A1R"], writes=["tmod"])
                P.op("pool", lambda e: e.tensor_tensor(out=hb[:], in0=tmod[:], in1=B1R[:], op=ALU.add), reads=["tmod", "B1R"], writes=["hb"])
                for kc in range(8):
                    P.op("pe", lambda e, kc=kc: e.transpose(p_tr[:, kc * 128:(kc + 1) * 128], hb[:, kc * 128:(kc + 1) * 128], identb[:]),
                         reads=["hb"], writes=["p_tr"])
                P.op("act", lambda e, b=b: e.copy(out=hT[b][:], in_=p_tr[:]), reads=["p_tr"], writes=[("hT", b)])
                hkey = ("hT", b)
                cos128, sin128, cos64, sin64 = rp[b][:, 0:64], rp[b][:, 64:128], rp[b][:, 128:160], rp[b][:, 160:192]
                if cfg.astop < 2:
                    continue
                proj(B_RK, p_mm[0], ("p_mm", 0), hT[b], hkey)
                P.op("act", lambda e: e.copy(out=pj[0][:], in_=p_mm[0][:]), reads=[("p_mm", 0)], writes=[("pj", 0)])
                rope(pj[0][:], ktok[:], 4, 128, cos128, sin128, [("pj", 0), ("rp", b)], "ktok")
                proj(B_RV, p_mm[1], ("p_mm", 1), hT[b], hkey)
                P.op("act", lambda e: e.copy(out=vtok[:], in_=p_mm[1][:]), reads=[("p_mm", 1)], writes=["vtok"])
                for h in range(0 if os.environ.get("SKIP_VZ") else 4):
                    P.op("dve", lambda e, h=h, T=T: e.tensor_scalar(out=vz[:, h * 128:(h + 1) * 128], in0=vtok[:, h * 128:(h + 1) * 128],
                                                                    scalar1=zt[:, T * 4 + h:T * 4 + h + 1], scalar2=None, op0=ALU.mult),
                         reads=["vtok", "zt"], writes=[("vz", h)])
                if cfg.astop < 2.2:
                    continue
                proj(B_NK, p_mm[2], ("p_mm", 2), hT[b], hkey)
                P.op("act", lambda e: e.copy(out=pj[2][:, 0:384], in_=p_mm[2][:, 0:384]), reads=[("p_mm", 2)], writes=[("pj", 2)])
                if cfg.astop < 2.5:
                    continue
                rope(pj[2][:, 0:384], nk[:], 6, 64, cos64, sin64, [("pj", 2), ("rp", b)], "nk")
                if cfg.astop < 2.8:
                    continue
                proj(B_NV, p_mm[0], ("p_mm", 0), hT[b], hkey)
                P.op("act", lambda e: e.copy(out=nv[:], in_=p_mm[0][:, 0:384]), reads=[("p_mm", 0)], writes=["nv"])
                if cfg.astop < 4:
                    continue
                P.op("dve", lambda e, T=T: e.tensor_copy(out=vs4[:, T, :, 0:64], in_=nv[:, 128:256].rearrange("p (g d) -> p g d", g=2)),
                     reads=["nv"], writes=["vs"])
                wk = T - (NW - NWK)
                if wk >= 0:
                    P.op("dve", lambda e, wk=wk: e.tensor_copy(out=vw4[:, wk, :, 0:64], in_=nv[:, 256:384].rearrange("p (g d) -> p g d", g=2)),
                         reads=["nv"], writes=["vw"])
                srcs = [nk[:, 0:128], nk[:, 128:256], nk[:, 256:384], nv[:, 0:128]]
                for q, s_ in enumerate(srcs):
                    P.op("pe", lambda e, q=q, s_=s_: e.transpose(p_tr[:, q * 128:(q + 1) * 128], s_, identb[:]),
                         reads=["nk_lo", "nk_hi", "nv"], writes=["p_tr"])
                P.op("act", lambda e, T=T: e.copy(out=ksT[:, T * 128:(T + 1) * 128], in_=p_tr[:, 128:256]), reads=["p_tr"], writes=["ksT"])
                if wk >= 0:
                    P.op("act", lambda e, wk=wk: e.copy(out=kwT[:, wk * 128:(wk + 1) * 128], in_=p_tr[:, 256:384]), reads=["p_tr"], writes=["kwT"])
                rw = rawst[T % 2]
                P.op("act", lambda e, rw=rw: e.copy(out=rw[:, 0:128], in_=p_tr[:, 0:128]), reads=["p_tr"], writes=[("rawst", T % 2)])
                P.op("act", lambda e, rw=rw: e.copy(out=rw[:, 128:256], in_=p_tr[:, 384:512]), reads=["p_tr"], writes=[("rawst", T % 2)])
                for kind in range(2):
                    P.dma("sp", rawd[kind, :, T * 128:(T + 1) * 128], rw[:, kind * 128:(kind + 1) * 128], reads=[("rawst", T % 2)])
                if cfg.astop < 5:
                    continue
                if own and cfg.astop >= 6:
                    proj(B_RQ, p_mm[1], ("p_mm", 1), hT[b], hkey)
                    P.op("act", lambda e: e.copy(out=pj[1][:], in_=p_mm[1][:]), reads=[("p_mm", 1)], writes=[("pj", 1)])
                    rope(pj[1][:], qtok[:], 4, 128, cos128, sin128, [("pj", 1), ("rp", b)], "qtok")
                    for h in range(4):
                        P.op("pe", lambda e, h=h: e.transpose(p_tr[:, h * 128:(h + 1) * 128], ktok[:, h * 128:(h + 1) * 128], identb[:]),
                             reads=["ktok_lo", "ktok_hi"], writes=["p_tr"])
                    P.op("act", lambda e: e.copy(out=kTr[:], in_=p_tr[:, 0:512]), reads=["p_tr"], writes=["kTr"])
                    for h in range(4):
                        P.op("pe", lambda e, h=h: e.transpose(p_tr[:, 512 + h * 128:512 + (h + 1) * 128], qtok[:, h * 128:(h + 1) * 128], identb[:]),
                             reads=["qtok_lo", "qtok_hi"], writes=["p_tr"])
                    P.op("act", lambda e: e.copy(out=qTr[:], in_=p_tr[:, 512:1024]), reads=["p_tr"], writes=["qTr"])
                    P.op("dve", lambda e: e.tensor_tensor(out=qxT[:], in0=qTr[:], in1=xit[:], op=ALU.mult), reads=["qTr", "xit"], writes=["qxT"])
                    for h in range(4):
                        hs = slice(h * 128, (h + 1) * 128)
                        P.op("pe", lambda e, hs=hs: e.matmul(p_sc[:, hs], lhsT=kTr[:, hs], rhs=qTr[:, hs], start=True, stop=True),
                             reads=["kTr", "qTr"], writes=["p_sc"])
                    P.op("dve", lambda e: e.tensor_tensor(out=pT[:], in0=p_sc[:], in1=dmat[:], op=ALU.mult), reads=["p_sc", "dmat"], writes=["pT"])
                    for h in range(4):
                        hs = slice(h * 128, (h + 1) * 128)
                        P.op("pe", lambda e, hs=hs: e.matmul(p_y[:, hs], lhsT=pT[:, hs], rhs=vtok[:, hs], start=True, stop=False),
                             reads=["pT", "vtok"], writes=["p_y"])
                        P.op("pe", lambda e, hs=hs: e.matmul(p_y[:, hs], lhsT=qxT[:, hs], rhs=Sb[:, hs], start=False, stop=True),
                             reads=["qxT", "Sb"], writes=["p_y"])
                    P.op("act", lambda e: e.copy(out=yv[:], in_=p_y[:]), reads=["p_y"], writes=["yv"])
                    for h in range(4):
                        P.op("dve", lambda e, h=h: e.bn_stats(out=st6[:, h * 6:(h + 1) * 6], in_=yv[:, h * 128:(h + 1) * 128]), reads=["yv"], writes=[("st6", h)])
                        P.op("dve", lambda e, h=h: e.bn_aggr(out=mv[:, h * 2:(h + 1) * 2], in_=st6[:, h * 6:(h + 1) * 6]), reads=[("st6", h)], writes=[("mv", h)])
                    mv3 = mv[:].rearrange("p (h t) -> p h t", t=2)
                    P.op("act", lambda e: e.activation(out=rstd4[:], in_=mv3[:, :, 1], func=AF.Sqrt, bias=epst[:]),
                         reads=[("mv", 0), ("mv", 1), ("mv", 2), ("mv", 3), "eps"], writes=["rstd4"])
                    P.op("dve", lambda e: e.reciprocal(out=rstd4[:], in_=rstd4[:]), reads=["rstd4"], writes=["rstd4"])
                    proj(B_RG, p_mm[2], ("p_mm", 2), hT[b], hkey)
                    P.op("act", lambda e: e.activation(out=sg[:], in_=p_mm[2][:], func=AF.Silu), reads=[("p_mm", 2)], writes=["sg"])
                    for h in range(4):
                        hs = slice(h * 128, (h + 1) * 128)
                        P.op("dve", lambda e, h=h, hs=hs: e.tensor_scalar(out=yv[:, hs], in0=yv[:, hs], scalar1=mv[:, 2 * h:2 * h + 1], scalar2=rstd4[:, h:h + 1],
                                                                         op0=ALU.subtract, op1=ALU.mult),
                             reads=["yv", ("mv", h), "rstd4"], writes=[("yn", h)])
                        P.op("pool", lambda e, h=h, hs=hs, i=i: e.tensor_tensor(out=reto[:, i * 512 + h * 128:i * 512 + (h + 1) * 128], in0=yv[:, hs], in1=sg[:, hs], op=ALU.mult),
                             reads=[("yn", h), "sg"], writes=["reto"])
                    proj(B_NQ, p_mm[0], ("p_mm", 0), hT[b], hkey)
                    P.op("act", lambda e: e.copy(out=pj[0][:], in_=p_mm[0][:]), reads=[("p_mm", 0)], writes=[("pj", 0)])
                    rope(pj[0][:], nq[:], 8, 64, cos64, sin64, [("pj", 0), ("rp", b)], "nq")
                    for h in range(4):
                        P.op("pe", lambda e, h=h: e.transpose(p_tr[:, h * 128:(h + 1) * 128], nq[:, h * 128:(h + 1) * 128], identb[:]),
                             reads=["nq_lo", "nq_hi"], writes=["p_tr"])
                    P.op("act", lambda e, i=i: e.copy(out=qTs[:, i * 512:(i + 1) * 512], in_=p_tr[:, 0:512]), reads=["p_tr"], writes=["qTs"])
                    proj(B_GT, p_mm[1], ("p_mm", 1), hT[b], hkey)
                    P.op("act", lambda e, i=i: e.activation(out=gts[:, i * 24:(i + 1) * 24], in_=p_mm[1][:, 0:24], func=AF.Sigmoid),
                         reads=[("p_mm", 1)], writes=["gts"])
                for h in range(4):
                    hs = slice(h * 128, (h + 1) * 128)
                    P.op("pe", lambda e, hs=hs: e.matmul(p_kv[:, hs], lhsT=ktok[:, hs], rhs=vz[:, hs], start=True, stop=True),
                         reads=["ktok_lo", "ktok_hi", ("vz", 0), ("vz", 1), ("vz", 2), ("vz", 3)], writes=["p_kv"])
                for h in range(4):
                    hs = slice(h * 128, (h + 1) * 128)
                    P.op("dve", lambda e, h=h, hs=hs: e.scalar_tensor_tensor(out=Sst[:, hs], in0=Sst[:, hs], scalar=float(np.exp(128 * LOGG[h])), in1=p_kv[:, hs],
                                                                            op0=ALU.mult, op1=ALU.add), reads=["p_kv", "Sst"], writes=["Sst"])
                P.op("act", lambda e: e.copy(out=Sb[:], in_=Sst[:]), reads=["Sst"], writes=["Sb"])
            P.emit()

        for st in _blk(cfg.stop >= 2):
            sb = lambda name, shape, dt=F32: st.enter_context(nc.sbuf_tensor(name, shape, dt))
            ps = lambda name, shape, dt=F32: st.enter_context(nc.psum_tensor(name, shape, dt))
            P = Prog(nc, SEM)
            NBP = NCH * 128
            raw = [sb("raw%d" % k, [128, RAWLEN], BF16) for k in range(2)]
            w1s = sb("w1s", [128, 32 * 256]); w1b = [sb("w1b%d" % k, [128, 32 * 256], BF16) for k in range(2)]
            pes = sb("pes", [128, 32]); peb = [sb("peb%d" % k, [128, 32], BF16) for k in range(2)]
            b1s = [sb("b1s%d" % k, [128, 2]) for k in range(2)]
            w2s = sb("w2s", [128, 128]); w2b = [sb("w2b%d" % k, [128, 128], BF16) for k in range(2)]
            cb = sb("cb", [128, 2])
            u = sb("u", [128, 512]); u2 = sb("u2", [128, 512]); u3 = sb("u3", [128, 512])
            hid = [sb("hid%d" % i, [128, 512], BF16) for i in range(2)]
            ovl = sb("ovl", [128, NCH * 128], BF16)
            p_h = ps("p_h", [128, 512]); p_c = ps("p_c", [128, 2]); p_o = ps("p_o", [128, 512])
            P.dma("sp", ovl[:], tb["overlap"], writes=["ovl"])
            P.op("pool", lambda e: e.memset(vca[:], 1.0), writes=["vca"])
            for cc_ in range(NCH):
                for g in range(2):
                    P.op("pool", lambda e, cc_=cc_, g=g: e.tensor_copy(out=vca4[:, cc_, g, 65:193], in_=ovl[:, cc_ * 128:(cc_ + 1) * 128]), reads=["ovl"], writes=["vca"])
            for kind, (w1d, ped, b1d, w2d) in enumerate([(w1k, peTk, b1k, w2k), (w1v, peTv, b1v, w2v)]):
                P.op("pool", lambda e, kind=kind: e.memset(raw[kind][:], 0.0), writes=[("raw", kind)])
                P.dma("sp", raw[kind][:, 0:S], rawd[kind], writes=[("raw", kind)])
                for half in range(2):
                    P.dma("sp", w1s[half * 64:(half + 1) * 64, :], w1d, writes=["w1s"])
                    P.dma("sp", pes[half * 64:(half + 1) * 64, :], ped, writes=["pes"])
                P.op("act", lambda e, kind=kind: e.copy(out=w1b[kind][:], in_=w1s[:]), reads=["w1s"], writes=[("w1b", kind)])
                P.op("dve", lambda e, kind=kind: e.tensor_copy(out=peb[kind][:], in_=pes[:]), reads=["pes"], writes=[("peb", kind)])
                P.dma("sp", b1s[kind][:], b1d, writes=[("b1s", kind)])
                P.dma("sp", w2s[:], w2d, writes=["w2s"])
                P.op("dve", lambda e, kind=kind: e.tensor_copy(out=w2b[kind][:], in_=w2s[:]), reads=["w2s"], writes=[("w2b", kind)])
                w13 = w1b[kind][:].rearrange("p (l n) -> p l n", l=32)
                for hc in range(2):
                    for l in range(32):
                        P.op("pe", lambda e, hc=hc, l=l, w13=w13, kind=kind: e.matmul(p_c[:, hc:hc + 1], lhsT=w13[0:64, l, hc * 128:(hc + 1) * 128], rhs=peb[kind][0:64, l:l + 1],
                                                                                    start=(l == 0), stop=(l == 31)), reads=[("w1b", kind), ("peb", kind)], writes=["p_c"])
                P.op("dve", lambda e, kind=kind: e.tensor_tensor(out=cb[:], in0=p_c[:], in1=b1s[kind][:], op=ALU.add), reads=["p_c", ("b1s", kind)], writes=["cb"])
                for g in range(2):
                    gs = slice(64 * g, 64 * g + 64)
                    for n0 in range(0, NBP, 512):
                        nn = min(512, NBP - n0)
                        for hc in range(2):
                            for l in range(32):
                                rhs = raw[kind][gs, l + 16 * n0: l + 16 * (n0 + nn): 16]
                                P.op("pe", lambda e, hc=hc, l=l, rhs=rhs, gs=gs, w13=w13, nn=nn: e.matmul(
                                    p_h[:, 0:nn], lhsT=w13[gs, l, hc * 128:(hc + 1) * 128], rhs=rhs, start=(l == 0), stop=(l == 31)),
                                    reads=[("w1b", kind), ("raw", kind)], writes=["p_h"])
                            P.op("act", lambda e, hc=hc, nn=nn: e.activation(out=u[:, 0:nn], in_=p_h[:, 0:nn], func=AF.Identity, bias=cb[:, hc:hc + 1]),
                                 reads=["p_h", "cb"], writes=["u"])
                            P.op("act", lambda e, nn=nn: e.activation(out=u2[:, 0:nn], in_=u[:, 0:nn], func=AF.Square), reads=["u"], writes=["u2"])
                            P.op("dve", lambda e, nn=nn: e.tensor_scalar(out=u2[:, 0:nn], in0=u2[:, 0:nn], scalar1=0.044715, scalar2=1.0, op0=ALU.mult, op1=ALU.add),
                                 reads=["u2"], writes=["u2"])
                            P.op("dve", lambda e, nn=nn: e.tensor_tensor(out=u3[:, 0:nn], in0=u2[:, 0:nn], in1=u[:, 0:nn], op=ALU.mult), reads=["u2", "u"], writes=["u3"])
                            P.op("act", lambda e, nn=nn: e.activation(out=u3[:, 0:nn], in_=u3[:, 0:nn], func=AF.Sigmoid, scale=1.5957691216), reads=["u3"], writes=["u3"])
                            P.op("dve", lambda e, hc=hc, nn=nn: e.tensor_tensor(out=hid[hc][:, 0:nn], in0=u3[:, 0:nn], in1=u[:, 0:nn], op=ALU.mult),
                                 reads=["u3", "u"], writes=[("hid", hc)])
                        w23 = w2b[kind][:].rearrange("p (c d) -> p c d", c=2)
                        if kind == 0:
                            for hc in range(2):
                                P.op("pe", lambda e, hc=hc, gs=gs, nn=nn, w23=w23: e.matmul(p_o[gs, 0:nn], lhsT=w23[:, hc, :], rhs=hid[hc][:, 0:nn], start=(hc == 0), stop=(hc == 1)),
                                     reads=[("hid", 0), ("hid", 1), ("w2b", 0)], writes=["p_o"])
                            P.op("act", lambda e, gs=gs, n0=n0, nn=nn: e.copy(out=kcT[gs, n0:n0 + nn], in_=p_o[gs, 0:nn]), reads=["p_o"], writes=["kcT"])
                        else:
                            for c4 in range(nn // 128):
                                for hc in range(2):
                                    P.op("pe", lambda e, hc=hc, c4=c4, w23=w23: e.matmul(p_o[:, c4 * 64:(c4 + 1) * 64], lhsT=hid[hc][:, c4 * 128:(c4 + 1) * 128], rhs=w23[:, hc, :],
                                                                                        start=(hc == 0), stop=(hc == 1)), reads=[("hid", 0), ("hid", 1), ("w2b", 1)], writes=["p_o"])
                                P.op("act", lambda e, c4=c4, g=g, n0=n0: e.copy(out=vca4[:, n0 // 128 + c4, g, 0:64], in_=p_o[:, c4 * 64:(c4 + 1) * 64]),
                                     reads=["p_o"], writes=["vca"])
            P.emit()

        for st in _blk(cfg.stop >= 3):
            sb = lambda name, shape, dt=F32: st.enter_context(nc.sbuf_tensor(name, shape, dt))
            ps = lambda name, shape, dt=F32: st.enter_context(nc.psum_tensor(name, shape, dt))
            P = Prog(nc, SEM)
            wob = sb("wob", [128, 8 * 1024], BF16)
            wob3 = wob[:].rearrange("p (k n) -> p k n", k=8)
            stg = [sb("stgc%d" % i, [128, 1024]) for i in range(2)]
            for kc in range(8):
                P.dma("sp", stg[kc % 2][:], w_out[kc * 128:(kc + 1) * 128, :], writes=[("stg", kc % 2)])
                P.op("pool", lambda e, kc=kc: e.tensor_copy(out=wob3[:, kc, :], in_=stg[kc % 2][:]), reads=[("stg", kc % 2)], writes=["wob"])
            expd = sb("expd", [128, S], BF16); tri = sb("tri", [128, 1024], BF16)
            kbias = sb("kbias", [128, NW]); cbias = sb("cbias", [128, NCH])
            cpen = [sb("cpen%d" % i, [128, NCH * 512], BF16) for i in range(2)]
            selm = [sb("selm%d" % i, [128, 256]) for i in range(2)]
            xo = [sb("xo%d" % i, [128, 1024]) for i in range(2)]
            eb = [sb("eb%d" % i, [128, 512], BF16) for i in range(3)]
            imp = sb("imp", [128, 128]); impm = sb("impm", [128, 128]); r1 = sb("r1", [128, 128]); r2 = sb("r2", [128, 128])
            m8 = sb("m8", [128, 16]); seln = sb("seln", [128, 128], BF16); selT = sb("selT", [128, 512], BF16)
            den = sb("den", [128, 4]); coef = sb("coef", [128, 4])
            nsa = sb("nsa", [128, 512])
            cat = sb("cat", [128, 1024], BF16); catT = sb("catT", [128, 1024], BF16)
            x1t = [sb("x1t%d" % i, [128, 1024]) for i in range(2)]
            p_s = [ps("p_s%d" % i, [128, 512]) for i in range(2)]
            p_a = [ps("p_a%d" % i, [128, 512]) for i in range(4)]
            p_x = ps("p_x", [128, 1024])
            GA1 = sb("GA1", [128, 1024])
            bcast_rows(P, sb, [(p_s[0], ("p_s", 0)), (p_s[1], ("p_s", 1))], [(4, GA1, "GA1")])
            RP = 2
            NCK = cfg.NEXP // (128 * RP)
            cin = [sb("cin%d" % i_, [128, RP * 1024]) for i_ in range(2)]
            cob = [sb("cob%d" % i_, [128, RP * 1024], BF16) for i_ in range(2)]
            cast_jobs = []
            for src_, dst_ in ((pdown, pdb), (pup, pub)):
                srcv = src_.rearrange("(c p j) d -> c p (j d)", p=128, j=RP)
                dstv = dst_.rearrange("(c p j) d -> c p (j d)", p=128, j=RP)
                for c_ in range(NCK):
                    cast_jobs.append((srcv[c_], dstv[c_]))
            cast_n = [0]

            def emit_casts(n):
                for _ in range(n):
                    if cast_n[0] >= len(cast_jobs):
                        return
                    src1, dst1 = cast_jobs[cast_n[0]]
                    k_ = cast_n[0] % 2
                    P.dma("sp", cin[k_][:], src1, writes=[("cin", k_)])
                    eng = "pool" if cast_n[0] % 2 else "dve"
                    P.op(eng, lambda e, k_=k_: e.tensor_copy(out=cob[k_][:], in_=cin[k_][:]), reads=[("cin", k_)], writes=[("cob", k_)])
                    P.dma("sp", dst1, cob[k_][:], reads=[("cob", k_)])
                    cast_n[0] += 1

            P.dma("sp", expd[:], tb["expand"], writes=["expd"])
            P.dma("sp", tri[:], tb["tri"], writes=["tri"])
            P.dma("sp", kbias[:], tb["keybias"], writes=["kbias"])
            P.dma("sp", cbias[:], tb["cmpbias"], writes=["cbias"])
            p_xb = p_x[:].bitcast(BF16)
            gts4 = gts[:].rearrange("p (i g h c) -> p i g h c", g=2, h=4, c=3)
            nsa4 = nsa[:].rearrange("p (g h d) -> p g h d", g=2, h=4)
            sc_i = [0]
            eb_i = [0]

            def attend(i, g, chunks, first_branch, br):
                W = chunks[0]["v"].shape[-1]
                nchk = len(chunks)
                qg = qTs[64 * g:64 * g + 64, i * 512:(i + 1) * 512]
                pbs = []

                def stage_s(ci):
                    ch = chunks[ci]
                    pb = sc_i[0] % 2; sc_i[0] += 1
                    pbs.append(pb)
                    pst = p_s[pb]
                    npen = len(ch["pens"])
                    P.op("pe", lambda e, ch=ch, pst=pst, npen=npen: e.matmul(pst[:], lhsT=ch["lhsT"], rhs=qg, start=True, stop=(npen == 0)),
                         reads=ch["keys_r"] + ["qTs"], writes=[("p_s", pb)])
                    for pi, (pl, pr, pk) in enumerate(ch["pens"]):
                        P.op("pe", lambda e, pl=pl, pr=pr, pst=pst, last=(pi == npen - 1): e.matmul(
                            pst[:], lhsT=pl, rhs=pr, start=False, stop=last), reads=pk, writes=[("p_s", pb)])

                def stage_ev(ci):
                    ch = chunks[ci]
                    pb = pbs[ci]
                    pst = p_s[pb]
                    ei = eb_i[0] % 3; eb_i[0] += 1
                    et = eb[ei]
                    if ch["bias"] is None:
                        P.op("act", lambda e, et=et, pst=pst: e.activation(out=et[:], in_=pst[:], func=AF.Exp, scale=0.125), reads=[("p_s", pb)], writes=[("eb", ei)])
                    else:
                        P.op("act", lambda e, et=et, pst=pst, ch=ch: e.activation(out=et[:], in_=pst[:], func=AF.Exp, scale=0.125, bias=ch["bias"]),
                             reads=[("p_s", pb), "kbias", "cbias"], writes=[("eb", ei)])
                    for h in range(4):
                        P.op("pe", lambda e, h=h, et=et, ch=ch, ci=ci: e.matmul(p_a[h][:, 0:W], lhsT=et[:, h * 128:(h + 1) * 128], rhs=ch["v"],
                                                                              start=(ci == 0), stop=(ci == nchk - 1)),
                             reads=[("eb", ei)] + ch["keys_v"], writes=[("p_a", h)])

                stage_s(0)
                for ci in range(nchk):
                    if ci + 1 < nchk:
                        stage_s(ci + 1)
                    stage_ev(ci)
                for h in range(4):
                    P.op("dve", lambda e, h=h: e.tensor_scalar(out=den[:, h:h + 1], in0=p_a[h][:, 64:65], scalar1=1e-30, scalar2=None, op0=ALU.max),
                         reads=[("p_a", h)], writes=[("den", h)])
                    P.op("dve", lambda e, h=h: e.reciprocal(out=den[:, h:h + 1], in_=den[:, h:h + 1]), reads=[("den", h)], writes=[("den", h)])
                    P.op("dve", lambda e, h=h: e.tensor_tensor(out=coef[:, h:h + 1], in0=den[:, h:h + 1], in1=gts4[:, i, g, h, br:br + 1], op=ALU.mult),
                         reads=[("den", h), "gts"], writes=[("coef", h)])
                    if first_branch:
                        P.op("dve", lambda e, h=h: e.tensor_scalar(out=nsa4[:, g, h, :], in0=p_a[h][:, 0:64], scalar1=coef[:, h:h + 1], scalar2=None, op0=ALU.mult),
                             reads=[("p_a", h), ("coef", h)], writes=[("nsa", g, h)])
                    else:
                        P.op("dve", lambda e, h=h: e.scalar_tensor_tensor(out=nsa4[:, g, h, :], in0=p_a[h][:, 0:64], scalar=coef[:, h:h + 1], in1=nsa4[:, g, h, :],
                                                                         op0=ALU.mult, op1=ALU.add),
                             reads=[("p_a", h), ("coef", h), ("nsa", g, h)], writes=[("nsa", g, h)])

            CPT = -(-len(cast_jobs) // (NO * 6))
            for i in range(NO):
                T = T0 + i
                b = i % 2
                P.dma("sp", cpen[b][:], tb["cmppen"][i * 128:(i + 1) * 128, :], writes=[("cpen", b)])
                P.dma("sp", selm[b][:], tb["selm"][i * 128:(i + 1) * 128, :], writes=[("selm", b)])
                P.dma("sp", xo[b][:], xw[T * 128:(T + 1) * 128, :], writes=[("xo", b)])
                for g in range(2):
                    gs = slice(64 * g, 64 * g + 64)
                    chunks = []
                    for c_ in range(NCH):
                        chunks.append(dict(lhsT=kcT[gs, c_ * 128:(c_ + 1) * 128], keys_r=["kcT"],
                                           pens=[(identb[:], cpen[b][:, c_ * 512:(c_ + 1) * 512], [("cpen", b), "identb"])],
                                           bias=cbias[:, c_:c_ + 1], v=vca4[:, c_, g, :], keys_v=["vca"]))
                    emit_casts(CPT)
                    attend(i, g, chunks, True, 0)
                    for h in range(4):
                        if h == 0:
                            P.op("dve", lambda e: e.tensor_scalar(out=imp[:], in0=p_a[0][:, 65:193], scalar1=den[:, 0:1], scalar2=None, op0=ALU.mult),
                                 reads=[("p_a", 0), ("den", 0)], writes=["imp"])
                        else:
                            P.op("dve", lambda e, h=h: e.scalar_tensor_tensor(out=imp[:], in0=p_a[h][:, 65:193], scalar=den[:, h:h + 1], in1=imp[:], op0=ALU.mult, op1=ALU.add),
                                 reads=[("p_a", h), ("den", h), "imp"], writes=["imp"])
                    P.op("dve", lambda e, b=b: e.tensor_tensor(out=impm[:], in0=imp[:], in1=selm[b][:, 0:128], op=ALU.mult), reads=["imp", ("selm", b)], writes=["impm"])
                    P.op("dve", lambda e, b=b: e.tensor_tensor(out=impm[:], in0=impm[:], in1=selm[b][:, 128:256], op=ALU.add), reads=["impm", ("selm", b)], writes=["impm"])
                    P.op("dve", lambda e: e.max(out=m8[:, 0:8], in_=impm[:]), reads=["impm"], writes=["m8a"])
                    P.op("dve", lambda e: e.match_replace(out=r1[:], in_to_replace=m8[:, 0:8], in_values=impm[:], imm_value=-BIG), reads=["impm", "m8a"], writes=["r1"])
                    P.op("dve", lambda e: e.max(out=m8[:, 8:16], in_=r1[:]), reads=["r1"], writes=["m8b"])
                    P.op("dve", lambda e: e.match_replace(out=r2[:], in_to_replace=m8[:, 8:16], in_values=r1[:], imm_value=-BIG), reads=["r1", "m8b"], writes=["r2"])
                    P.op("dve", lambda e: e.tensor_tensor(out=r1[:], in0=impm[:], in1=r2[:], op=ALU.subtract), reads=["impm", "r2"], writes=["r1"])
                    P.op("dve", lambda e: e.tensor_scalar(out=seln[:], in0=r1[:], scalar1=0.0, scalar2=NEG, op0=ALU.is_le, op1=ALU.mult), reads=["r1"], writes=["seln"])
                    P.op("pe", lambda e: e.transpose(p_xb[:, 0:128], seln[:], identb[:]), reads=["seln"], writes=["p_x"])
                    P.op("dve", lambda e: e.tensor_copy(out=selT[:].rearrange("p (h t) -> p h t", h=4), in_=p_xb[:, 0:128].unsqueeze(1).to_broadcast([128, 4, 128])),
                         reads=["p_x"], writes=["selT"])
                    chunks = []
                    for c_ in range(T + 1):
                        pens = [(expd[:, c_ * 128:(c_ + 1) * 128], selT[:], ["expd", "selT"])]
                        if c_ == T:
                            pens.append((identb[:], tri[:, 0:512], ["tri", "identb"]))
                        chunks.append(dict(lhsT=ksT[gs, c_ * 128:(c_ + 1) * 128], keys_r=["ksT"], pens=pens, bias=None, v=vs4[:, c_, g, :], keys_v=["vs"]))
                    emit_casts(CPT)
                    attend(i, g, chunks, False, 1)
                    chunks = []
                    for c_ in range(max(0, T - 4), T + 1):
                        wk = c_ - (NW - NWK)
                        pens = []
                        if c_ == T - 4:
                            pens.append((identb[:], tri[:, 512:1024], ["tri", "identb"]))
                        if c_ == T:
                            pens.append((identb[:], tri[:, 0:512], ["tri", "identb"]))
                        chunks.append(dict(lhsT=kwT[gs, wk * 128:(wk + 1) * 128], keys_r=["kwT"], pens=pens, bias=kbias[:, c_:c_ + 1], v=vw4[:, wk, g, :], keys_v=["vw"]))
                    emit_casts(CPT)
                    attend(i, g, chunks, False, 2)
                P.op("act", lambda e, i=i: e.copy(out=cat[:, 0:512], in_=reto[:, i * 512:(i + 1) * 512]), reads=["reto"], writes=["cat_a"])
                P.op("act", lambda e: e.copy(out=cat[:, 512:1024], in_=nsa[:]), reads=[("nsa", g_, h_) for g_ in range(2) for h_ in range(4)], writes=["cat_b"])
                for kc in range(8):
                    P.op("pe", lambda e, kc=kc: e.transpose(p_xb[:, kc * 128:(kc + 1) * 128], cat[:, kc * 128:(kc + 1) * 128], identb[:]),
                         reads=["cat_a", "cat_b"], writes=["p_x"])
                P.op("act", lambda e: e.copy(out=catT[:], in_=p_xb[:, 0:1024]), reads=["p_x"], writes=["catT"])
                for half in range(2):
                    for kc in range(8):
                        P.op("pe", lambda e, kc=kc, half=half: e.matmul(p_x[:, half * 512:(half + 1) * 512], lhsT=catT[:, kc * 128:(kc + 1) * 128],
                                                                        rhs=wob3[:, kc, half * 512:(half + 1) * 512], start=(kc == 0), stop=(kc == 7)),
                             reads=["catT", "wob"], writes=["p_x"])
                P.op("dve", lambda e, b=b: e.tensor_tensor(out=x1t[b][:], in0=p_x[:], in1=GA1[:], op=ALU.mult), reads=["p_x", "GA1"], writes=[("x1t", b)])
                P.op("pool", lambda e, b=b: e.tensor_tensor(out=x1t[b][:], in0=x1t[b][:], in1=xo[b][:], op=ALU.add), reads=[("x1t", b), ("xo", b)], writes=[("x1t", b)])
                P.dma("sp", x1d[i * 128:(i + 1) * 128, :], x1t[b][:], reads=[("x1t", b)])
            emit_casts(len(cast_jobs))
            P.emit()

        mid.close()
        for st in _blk(cfg.stop >= 4):
            sb = lambda name, shape, dt=F32: st.enter_context(nc.sbuf_tensor(name, shape, dt))
            ps = lambda name, shape, dt=F32: st.enter_context(nc.psum_tensor(name, shape, dt))
            P = Prog(nc, SEM)
            wqb = sb("wqb", [128, 8 * 2048], BF16)
            wqb3 = wqb[:].rearrange("p (k n) -> p k n", k=8)
            stg = [sb("stgd%d" % i, [128, 2048]) for i in range(2)]
            for kc in range(8):
                P.dma("sp", stg[kc % 2][:], w_pq[kc * 128:(kc + 1) * 128, :], writes=[("stg", kc % 2)])
                P.op("act", lambda e, kc=kc: e.copy(out=wqb3[:, kc, :], in_=stg[kc % 2][:]), reads=[("stg", kc % 2)], writes=["wqb"])
            A2R = sb("A2R", [128, 1024]); B2R = sb("B2R", [128, 1024]); GA2 = sb("GA2", [128, 1024]); GF = sb("GF", [128, 1024])
            P.dma("sp", GF[:], gfin.partition_broadcast(128), writes=["GF"])
            skb = sb("skb", [128, 2048], BF16)
            P.dma("sp", stg[0][:], skT, writes=[("stg", 0)])
            P.op("act", lambda e: e.copy(out=skb[:], in_=stg[0][:]), reads=[("stg", 0)], writes=["skb"])
            selrow = sb("selrow", [128, 128 * 128], BF16)
            P.dma("sp", selrow[:], tb["selrow"], writes=["selrow"])
            selrow3 = selrow[:].rearrange("p (t m) -> p t m", t=128)
            io16 = sb("io16", [128, 16])
            P.dma("sp", io16[:], tb["iota16"], writes=["io16"])
            io128 = sb("io128", [128, 128]); cm = [sb("cm%d" % i_, [128, 128], BF16) for i_ in range(3)]
            P.dma("sp", io128[:], tb["iota128"], writes=["io128"])
            x1 = sb("x1", [128, 1024]); sq = sb("sqd", [128, 1024]); ss = sb("ssd", [128, 1]); rs = sb("rsd", [128, 1])
            tmod = sb("tmodd", [128, 1024]); h2b = sb("h2b", [128, 1024], BF16); h2T = sb("h2T", [128, 1024], BF16)
            qT = sb("qT", [128, 2048], BF16)
            s_sb = sb("s_sb", [128, 2048]); s2 = sb("s2", [128, 128])
            V16 = sb("V16", [128, 256]); I16 = sb("I16", [128, 256], U32); I16f = sb("I16f", [128, 256])
            cand = sb("cand", [128, 2048]); c2 = sb("c2", [128, 256])
            TV = sb("TV", [128, 128]); TJ = sb("TJ", [128, 128], U32); TA = sb("TA", [128, 128], U32); TBb = sb("TBb", [128, 128], U32)
            TAf = sb("TAf", [128, 128]); TBf = sb("TBf", [128, 128])
            oh = sb("oh", [128, 2048]); i0 = sb("i0", [128, 128]); i1 = sb("i1", [128, 128]); ef = sb("ef", [128, 128])
            ex = sb("ex", [128, 128]); sm = sb("sm", [128, 8]); Wt = sb("Wt", [128, 128])
            idxT = sb("idxT", [128, 128], U32); WT = sb("WT", [128, 128]); aT = sb("aT", [128, 128]); cT = sb("cT", [128, 128])
            g2 = sb("g2", [128, 128]); g3 = sb("g3", [128, 128])
            NU = 6
            U = [sb("U%d" % i, [128, 1024], BF16) for i in range(NU)]
            jk = sq
            oT = tmod; x2 = sb("x2", [128, 1024]); yo = sb("yo", [128, 1024])
            p_q = ps("p_q", [128, 512]); p_s4 = ps("p_s4", [128, 2048]); p_hb = ps("p_hb", [128, 1024]); p_t = ps("p_t", [128, 512])
            bcast_rows(P, sb, [(p_q, "p_q"), (p_t, "p_t")], [(2, A2R, "A2R"), (3, B2R, "B2R"), (5, GA2, "GA2")])
            V4 = V16[:].rearrange("p (c k) -> p c k", k=16); I4 = I16[:].rearrange("p (c k) -> p c k", k=16)
            s3 = s_sb[:].rearrange("p (c n) -> p c n", n=128)
            for i in range(NO):
                P.dma("sp", x1[:], x1d[i * 128:(i + 1) * 128, :], writes=["x1"])
                P.op("act", lambda e: e.activation(out=sq[:], in_=x1[:], func=AF.Square), reads=["x1"], writes=["sq"])
                P.op("dve", lambda e: e.reduce_sum(out=ss[:], in_=sq[:], axis=AX.X), reads=["sq"], writes=["ss"])
                P.op("act", lambda e: e.activation(out=rs[:], in_=ss[:], func=AF.Sqrt, scale=1.0 / 1024, bias=epst[:]), reads=["ss"], writes=["rs"])
                P.op("dve", lambda e: e.reciprocal(out=rs[:], in_=rs[:]), reads=["rs"], writes=["rs"])
                P.op("dve", lambda e: e.scalar_tensor_tensor(out=tmod[:], in0=x1[:], scalar=rs[:], in1=A2R[:], op0=ALU.mult, op1=ALU.mult), reads=["x1", "rs", "A2R"], writes=["tmod"])
                P.op("pool", lambda e: e.tensor_tensor(out=h2b[:], in0=tmod[:], in1=B2R[:], op=ALU.add), reads=["tmod", "B2R"], writes=["h2b"])
                p_tb = p_t[:].bitcast(BF16)
                for kc in range(8):
                    P.op("pe", lambda e, kc=kc: e.transpose(p_tb[:, kc * 128:(kc + 1) * 128], h2b[:, kc * 128:(kc + 1) * 128], identb[:]), reads=["h2b"], writes=["p_t"])
                P.op("act", lambda e: e.copy(out=h2T[:], in_=p_tb[:, 0:1024]), reads=["p_t"], writes=["h2T"])
                for c4 in range(4):
                    for cq in range(4):
                        ch = c4 * 4 + cq
                        for kc in range(8):
                            P.op("pe", lambda e, ch=ch, cq=cq, kc=kc: e.matmul(p_q[:, cq * 128:(cq + 1) * 128], lhsT=wqb3[:, kc, ch * 128:(ch + 1) * 128],
                                                                              rhs=h2T[:, kc * 128:(kc + 1) * 128], start=(kc == 0), stop=(kc == 7)),
                                 reads=["wqb", "h2T"], writes=["p_q"])
                    P.op("act", lambda e, c4=c4: e.copy(out=qT[:, c4 * 512:(c4 + 1) * 512], in_=p_q[:]), reads=["p_q"], writes=[("qT", c4)])
                for ch in range(16):
                    P.op("pe", lambda e, ch=ch: e.matmul(p_s4[:, ch * 128:(ch + 1) * 128], lhsT=qT[:, ch * 128:(ch + 1) * 128], rhs=skb[:, ch * 128:(ch + 1) * 128],
                                                        start=True, stop=True), reads=[("qT", ch // 4), "skb"], writes=[("p_s4", ch // 8)])
                for c4 in range(4):
                    P.op("act", lambda e, c4=c4: e.copy(out=s_sb[:, c4 * 512:(c4 + 1) * 512], in_=p_s4[:, c4 * 512:(c4 + 1) * 512]), reads=[("p_s4", c4 // 2)], writes=[("s_sb", c4)])
                for ch in range(16):
                    sk = ("s_sb", ch // 4)
                    P.op("dve", lambda e, ch=ch: e.max(out=V4[:, ch, 0:8], in_=s3[:, ch, :]), reads=[sk], writes=[("V", ch, 0)])
                    P.op("dve", lambda e, ch=ch: e.max_index(out=I4[:, ch, 0:8], in_max=V4[:, ch, 0:8], in_values=s3[:, ch, :]), reads=[sk, ("V", ch, 0)], writes=[("I", ch, 0)])
                    P.op("dve", lambda e, ch=ch: e.match_replace(out=s2[:], in_to_replace=V4[:, ch, 0:8], in_values=s3[:, ch, :], imm_value=-BIG), reads=[sk, ("V", ch, 0)], writes=["s2"])
                    P.op("dve", lambda e, ch=ch: e.max(out=V4[:, ch, 8:16], in_=s2[:]), reads=["s2"], writes=[("V", ch, 1)])
                    P.op("dve", lambda e, ch=ch: e.max_index(out=I4[:, ch, 8:16], in_max=V4[:, ch, 8:16], in_values=s2[:]), reads=["s2", ("V", ch, 1)], writes=[("I", ch, 1)])
                allV = [("V", ch, k_) for ch in range(16) for k_ in range(2)]
                allI = [("I", ch, k_) for ch in range(16) for k_ in range(2)]
                P.op("dve", lambda e: e.tensor_copy(out=I16f[:], in_=I16[:]), reads=allI, writes=["I16f"])
                cand4 = cand[:].rearrange("p (h a b) -> p h a b", a=16, b=16)
                V5 = V16[:].rearrange("p (h t k) -> p h t k", t=2, k=16)
                I5 = I16f[:].rearrange("p (h t k) -> p h t k", t=2, k=16)
                for hd in range(8):
                    P.op("dve", lambda e, hd=hd: e.tensor_tensor(out=cand4[:, hd], in0=V5[:, hd, 0, :].unsqueeze(2).to_broadcast([128, 16, 16]),
                                                                 in1=V5[:, hd, 1, :].unsqueeze(1).to_broadcast([128, 16, 16]), op=ALU.add),
                         reads=allV, writes=[("cand", hd)])
                    cd = cand[:, hd * 256:(hd + 1) * 256]
                    P.op("dve", lambda e, hd=hd, cd=cd: e.max(out=TV[:, hd * 16:hd * 16 + 8], in_=cd), reads=[("cand", hd)], writes=[("TV", hd, 0)])
                    P.op("dve", lambda e, hd=hd, cd=cd: e.max_index(out=TJ[:, hd * 16:hd * 16 + 8], in_max=TV[:, hd * 16:hd * 16 + 8], in_values=cd),
                         reads=[("cand", hd), ("TV", hd, 0)], writes=[("TJ", hd, 0)])
                    P.op("dve", lambda e, hd=hd, cd=cd: e.match_replace(out=c2[:], in_to_replace=TV[:, hd * 16:hd * 16 + 8], in_values=cd, imm_value=-BIG),
                         reads=[("cand", hd), ("TV", hd, 0)], writes=["c2"])
                    P.op("dve", lambda e, hd=hd: e.max(out=TV[:, hd * 16 + 8:hd * 16 + 16], in_=c2[:]), reads=["c2"], writes=[("TV", hd, 1)])
                    P.op("dve", lambda e, hd=hd: e.max_index(out=TJ[:, hd * 16 + 8:hd * 16 + 16], in_max=TV[:, hd * 16 + 8:hd * 16 + 16], in_values=c2[:]),
                         reads=["c2", ("TV", hd, 1)], writes=[("TJ", hd, 1)])
                allTV = [("TV", hd, k_) for hd in range(8) for k_ in range(2)]
                allTJ = [("TJ", hd, k_) for hd in range(8) for k_ in range(2)]
                TV3 = TV[:].rearrange("p (h k) -> p h k", k=16)
                ex3 = ex[:].rearrange("p (h k) -> p h k", k=16)
                P.op("dve", lambda e: e.tensor_tensor(out=ex3, in0=TV3, in1=TV3[:, :, 0:1].to_broadcast([128, 8, 16]), op=ALU.subtract), reads=allTV, writes=["ex"])
                P.op("act", lambda e: e.activation(out=ex[:], in_=ex[:], func=AF.Exp), reads=["ex"], writes=["ex"])
                P.op("dve", lambda e: e.tensor_reduce(out=sm[:], in_=ex3, axis=AX.X, op=ALU.add), reads=["ex"], writes=["sm"])
                P.op("dve", lambda e: e.reciprocal(out=sm[:], in_=sm[:]), reads=["sm"], writes=["sm"])
                P.op("dve", lambda e: e.tensor_tensor(out=Wt[:].rearrange("p (h k) -> p h k", k=16), in0=ex3, in1=sm[:].unsqueeze(2).to_broadcast([128, 8, 16]), op=ALU.mult),
                     reads=["ex", "sm"], writes=["Wt"])
                P.op("dve", lambda e: e.tensor_single_scalar(out=TA[:], in_=TJ[:], scalar=4, op=ALU.logical_shift_right), reads=allTJ, writes=["TA"])
                P.op("dve", lambda e: e.tensor_single_scalar(out=TBb[:], in_=TJ[:], scalar=15, op=ALU.bitwise_and), reads=allTJ, writes=["TB"])
                P.op("dve", lambda e: e.tensor_copy(out=TAf[:], in_=TA[:]), reads=["TA"], writes=["TAf"])
                P.op("dve", lambda e: e.tensor_copy(out=TBf[:], in_=TBb[:]), reads=["TB"], writes=["TBf"])
                oh4 = oh[:].rearrange("p (h k a) -> p h k a", k=16, a=16)
                for which, (tf, dst) in enumerate([(TAf, i0), (TBf, i1)]):
                    tf3 = tf[:].rearrange("p (h k) -> p h k", k=16)
                    P.op("dve", lambda e, tf3=tf3: e.tensor_tensor(out=oh4, in0=tf3.unsqueeze(3).to_broadcast([128, 8, 16, 16]),
                                                                  in1=io16[:].unsqueeze(1).unsqueeze(1).to_broadcast([128, 8, 16, 16]), op=ALU.is_equal),
                         reads=["TAf", "TBf", "io16"], writes=["oh"])
                    P.op("dve", lambda e, which=which: e.tensor_tensor(out=oh4, in0=oh4, in1=I5[:, :, which, :].unsqueeze(2).to_broadcast([128, 8, 16, 16]), op=ALU.mult),
                         reads=["oh", "I16f"], writes=["oh"])
                    P.op("dve", lambda e, dst=dst: e.tensor_reduce(out=dst[:], in_=oh4, axis=AX.X, op=ALU.add), reads=["oh"], writes=["i01_%d" % which])
                P.op("dve", lambda e: e.scalar_tensor_tensor(out=ef[:], in0=i0[:], scalar=128.0, in1=i1[:], op0=ALU.mult, op1=ALU.add), reads=["i01_0", "i01_1"], writes=["ef"])
                P.op("pe", lambda e: e.transpose(p_t[:, 0:128], ef[:], identf[:]), reads=["ef"], writes=["p_t"])
                P.op("pe", lambda e: e.transpose(p_t[:, 128:256], Wt[:], identf[:]), reads=["Wt"], writes=["p_t"])
                P.op("dve", lambda e: e.tensor_copy(out=idxT[:], in_=p_t[:, 0:128]), reads=["p_t"], writes=["idxT"])
                P.op("dve", lambda e: e.tensor_copy(out=WT[:], in_=p_t[:, 128:256]), reads=["p_t"], writes=["WT"])
                for t_ in range(128):
                    ub = U[t_ % NU]
                    P.op("pool", lambda e, ub=ub, t_=t_: e.indirect_dma_start(out=ub[:], out_offset=None, in_=pdb,
                                                                             in_offset=bass.IndirectOffsetOnAxis(ap=idxT[:, t_:t_ + 1], axis=0)),
                         reads=["idxT"], writes=[("U", t_ % NU)], dma=True)
                    hbt, hbk = (p_hb[:], "p_hb") if t_ % 2 == 0 else (p_s4[:, 1024:2048], ("p_s4", 1))
                    for half in range(2):
                        P.op("pe", lambda e, t_=t_, half=half, hbt=hbt: e.matmul(hbt[:, half * 512:(half + 1) * 512], lhsT=selrow3[:, t_, :], rhs=h2b[:, half * 512:(half + 1) * 512],
                                                                                 start=True, stop=True), reads=["selrow", "h2b"], writes=[hbk])
                    P.op("dve", lambda e, ub=ub, t_=t_, hbt=hbt: e.tensor_tensor_reduce(out=jk[:], in0=ub[:], in1=hbt, scale=1.0, scalar=0.0, op0=ALU.mult, op1=ALU.add,
                                                                                       accum_out=aT[:, t_:t_ + 1]), reads=[("U", t_ % NU), hbk], writes=["sq", "aT"])
                P.op("act", lambda e: e.activation(out=g2[:], in_=aT[:], func=AF.Square), reads=["aT"], writes=["g2"])
                P.op("dve", lambda e: e.tensor_scalar(out=g2[:], in0=g2[:], scalar1=0.044715, scalar2=1.0, op0=ALU.mult, op1=ALU.add), reads=["g2"], writes=["g2"])
                P.op("dve", lambda e: e.tensor_tensor(out=g3[:], in0=g2[:], in1=aT[:], op=ALU.mult), reads=["g2", "aT"], writes=["g3"])
                P.op("act", lambda e: e.activation(out=g3[:], in_=g3[:], func=AF.Sigmoid, scale=1.5957691216), reads=["g3"], writes=["g3"])
                P.op("dve", lambda e: e.tensor_tensor(out=g3[:], in0=g3[:], in1=aT[:], op=ALU.mult), reads=["g3", "aT"], writes=["g3"])
                P.op("dve", lambda e: e.tensor_tensor(out=cT[:], in0=g3[:], in1=WT[:], op=ALU.mult), reads=["g3", "WT"], writes=["cT"])
                for t_ in range(128):
                    ub = U[t_ % NU]
                    P.op("pool", lambda e, ub=ub, t_=t_: e.indirect_dma_start(out=ub[:], out_offset=None, in_=pub,
                                                                             in_offset=bass.IndirectOffsetOnAxis(ap=idxT[:, t_:t_ + 1], axis=0)),
                         reads=["idxT"], writes=[("U", t_ % NU)], dma=True)
                    cmt = cm[t_ % 3]
                    P.op("dve", lambda e, cmt=cmt, t_=t_: e.scalar_tensor_tensor(out=cmt[:], in0=io128[:], scalar=float(t_), in1=cT[:, t_:t_ + 1].to_broadcast([128, 128]),
                                                                                op0=ALU.is_equal, op1=ALU.mult), reads=["io128", "cT"], writes=[("cm", t_ % 3)])
                    for half in range(2):
                        P.op("pe", lambda e, ub=ub, cmt=cmt, t_=t_, half=half: e.matmul(p_s4[:, half * 512:(half + 1) * 512], lhsT=cmt[:],
                                                                                      rhs=ub[:, half * 512:(half + 1) * 512], start=(t_ == 0), stop=(t_ == 127)),
                             reads=[("U", t_ % NU), ("cm", t_ % 3)], writes=[("p_s4", 0)])
                P.op("dve", lambda e: e.tensor_tensor(out=x2[:], in0=p_s4[:, 0:1024], in1=GA2[:], op=ALU.mult), reads=[("p_s4", 0), "GA2"], writes=["x2"])
                P.op("pool", lambda e: e.tensor_tensor(out=x2[:], in0=x2[:], in1=x1[:], op=ALU.add), reads=["x2", "x1"], writes=["x2"])
                P.op("act", lambda e: e.activation(out=sq[:], in_=x2[:], func=AF.Square), reads=["x2"], writes=["sq"])
                P.op("dve", lambda e: e.reduce_sum(out=ss[:], in_=sq[:], axis=AX.X), reads=["sq"], writes=["ss"])
                P.op("act", lambda e: e.activation(out=rs[:], in_=ss[:], func=AF.Sqrt, scale=1.0 / 1024, bias=epst[:]), reads=["ss"], writes=["rs"])
                P.op("dve", lambda e: e.reciprocal(out=rs[:], in_=rs[:]), reads=["rs"], writes=["rs"])
                P.op("dve", lambda e: e.scalar_tensor_tensor(out=yo[:], in0=x2[:], scalar=rs[:], in1=GF[:], op0=ALU.mult, op1=ALU.mult), reads=["x2", "rs", "GF"], writes=["yo"])
                P.dma("sp", out[i * 128:(i + 1) * 128, :], yo[:], reads=["yo"])
            P.emit()
    return nc


def make_inputs(cfg, core, inp, n_per_batch=4, S_full=None):
    NW, NO = cfg.NW, cfg.NO
    b, j = core // n_per_batch, core % n_per_batch
    off = NO * 128 * (j + 1) - NW * 128
    x = inp["x"][b]
    xw = np.zeros((NW * 128, 1024), np.float32)
    lo = max(0, -off)
    xw[lo:] = x[off + lo: off + NW * 128]
    m = {"xw": xw}
    m["ccol"] = np.ascontiguousarray(inp["c"][b].reshape(8, 128).T)
    m["w_ada"] = inp["w_ada"][0]
    m["b_adaT"] = np.ascontiguousarray(inp["b_ada"][0].reshape(48, 128).T)
    m["gmixT"] = np.ascontiguousarray(inp["g_norm_mix"][0].reshape(8, 128).T)
    m["gffnT"] = np.ascontiguousarray(inp["g_norm_ffn"][0].reshape(8, 128).T)
    m["gfin"] = inp["g_norm_final"].reshape(1, 1024)
    m["w_in"] = np.ascontiguousarray(inp["w_in"][0][:, w_in_perm()])
    m["w_out"] = inp["w_out"][0]
    for nm, key in (("w1k", "w_cmp_k1"), ("w1v", "w_cmp_v1")):
        m[nm] = np.ascontiguousarray(inp[key][0].reshape(32, 64, 256).transpose(1, 0, 2).reshape(64, 32 * 256))
    m["peTk"] = np.ascontiguousarray(inp["pe_cmp_k"][0].T)
    m["peTv"] = np.ascontiguousarray(inp["pe_cmp_v"][0].T)
    m["b1k"] = np.ascontiguousarray(inp["b_cmp_k1"][0].reshape(2, 128).T)
    m["b1v"] = np.ascontiguousarray(inp["b_cmp_v1"][0].reshape(2, 128).T)
    m["w2k"] = np.ascontiguousarray(inp["w_cmp_k2"][0].reshape(2, 128, 64).transpose(1, 0, 2).reshape(128, 128))
    m["w2v"] = np.ascontiguousarray(inp["w_cmp_v2"][0].reshape(2, 128, 64).transpose(1, 0, 2).reshape(128, 128))
    m["w_pq"] = inp["w_peer_q"][0]
    m["skT"] = np.ascontiguousarray(inp["peer_subkeys"][0].reshape(16, 128, 128).transpose(2, 0, 1).reshape(128, 2048))
    m["pdown"] = inp["peer_down"][0]
    m["pup"] = inp["peer_up"][0]
    for k_, v_ in host_tables(cfg, off).items():
        m["t_" + k_] = v_
    return m


_NC_CACHE = {}


def kernel(**inputs):
    inp = {k: np.asarray(v) for k, v in inputs.items()}
    cfg = CFG()
    if "nc" not in _NC_CACHE:
        _NC_CACHE["nc"] = build(cfg)
        mybir.codegen_inst_isa_subclasses(_NC_CACHE["nc"])
    nc = _NC_CACHE["nc"]
    in_maps = [make_inputs(cfg, c, inp) for c in range(8)]
    res = run_bass_kernel_spmd(nc, in_maps, core_ids=list(range(8)))
    out = np.zeros((2, 8192, 1024), np.float32)
    for c in range(8):
        b, j = c // 4, c % 4
        out[b, j * 2048:(j + 1) * 2048] = res.results[c]["out"]
    return out
```

```python
import numpy as np
import ml_dtypes
from contextlib import ExitStack
import concourse.bass as bass
import concourse.mybir as mybir
from concourse.bass_utils import run_bass_kernel_spmd

F32 = mybir.dt.float32
BF16 = mybir.dt.bfloat16
U32 = mybir.dt.uint32
F32R = mybir.dt.float32r
ALU = mybir.AluOpType
AF = mybir.ActivationFunctionType
AX = mybir.AxisListType

import os
ROPE_ENG = os.environ.get("ROPE_ENG", "dve")
NEG = -30000.0
BIG = 1.0e30
COMPUTE = ("pe", "act", "dve", "pool")
NSW = 8


class _Op:
    __slots__ = ("eng", "fn", "reads", "writes", "deps", "is_dma", "signal", "ev", "id")


class SemState:
    def __init__(self, nc, stack, n_dma_sems=28):
        self.n_dma_sems = n_dma_sems
        self.sems = {}
        for e in COMPUTE:
            self.sems[e] = stack.enter_context(nc.semaphore("s_" + e))
        for k in range(n_dma_sems):
            self.sems["d%d" % k] = stack.enter_context(nc.semaphore("s_d%d" % k))
        self.cnt = {e: 0 for e in COMPUTE}
        self.dma_cnt = [0] * n_dma_sems


class Prog:
    def __init__(self, nc, state):
        self.nc = nc
        self.state = state
        self.ops = []
        self.last_writer = {}
        self.readers = {}
        self.n_dma_sems = state.n_dma_sems
        self.dma_rr = 0
        self.sw_rr = 0
        self.dma_last = [None] * self.n_dma_sems
        self.dma_cnt = state.dma_cnt

    def op(self, eng, fn, reads=(), writes=(), dma=False):
        o = _Op()
        o.eng, o.fn, o.is_dma = eng, fn, dma
        o.reads, o.writes = tuple(reads), tuple(writes)
        o.deps = set()
        o.signal = False
        o.ev = None
        o.id = len(self.ops)
        for r in o.reads:
            w = self.last_writer.get(r)
            if w is not None:
                o.deps.add(w)
        for w_ in o.writes:
            w = self.last_writer.get(w_)
            if w is not None:
                o.deps.add(w)
            lastc = {}
            for rd in self.readers.get(w_, ()):
                p_ = self.ops[rd]
                if p_.is_dma:
                    o.deps.add(rd)
                else:
                    lastc[p_.eng] = rd
            o.deps.update(lastc.values())
        for r in o.reads:
            self.readers.setdefault(r, []).append(o.id)
        for w_ in o.writes:
            self.last_writer[w_] = o.id
            self.readers[w_] = []
        o.deps.discard(o.id)
        if dma:
            if eng == "pool":
                k = self.n_dma_sems - NSW + self.sw_rr
                self.sw_rr = (self.sw_rr + 1) % NSW
            else:
                k = self.dma_rr
                self.dma_rr = (self.dma_rr + 1) % (self.n_dma_sems - NSW)
            prev = self.dma_last[k]
            if prev is not None:
                o.deps.add(prev)
            self.dma_last[k] = o.id
            self.dma_cnt[k] += 16
            o.ev = ("d%d" % k, self.dma_cnt[k])
        self.ops.append(o)
        return o.id

    def dma(self, eng, out, in_, reads=(), writes=(), **kw):
        return self.op(eng, lambda e: e.dma_start(out=out, in_=in_, **kw), reads, writes, dma=True)

    def emit(self):
        nc, ops = self.nc, self.ops
        for o in ops:
            nd = set()
            for d in o.deps:
                p = ops[d]
                if p.is_dma or o.is_dma or p.eng != o.eng:
                    nd.add(d)
                elif o.eng != "pe":
                    nd.add(d)
            o.deps = nd
            for d in nd:
                if not ops[d].is_dma:
                    ops[d].signal = True
        cnt = self.state.cnt
        for o in ops:
            if not o.is_dma and o.signal:
                cnt[o.eng] += 1
                o.ev = (o.eng, cnt[o.eng])
        finals = [ops[i] for i in self.dma_last if i is not None]
        with ExitStack() as st:
            sems = self.state.sems
            block = st.enter_context(nc.Block())
            streams = {}
            for o in ops:
                streams.setdefault(o.eng, []).append(o)

            def run_stream(ename, e, final=False):
                waited = {}

                def wait(ev):
                    if waited.get(ev[0], 0) < ev[1]:
                        e.wait_ge(sems[ev[0]], ev[1])
                        waited[ev[0]] = ev[1]

                for o in streams.get(ename, []):
                    for d in sorted(o.deps):
                        wait(ops[d].ev)
                    ins = o.fn(e)
                    if o.is_dma:
                        ins.then_inc(sems[o.ev[0]], 16)
                    elif o.signal:
                        ins.then_inc(sems[o.eng], 1)
                if final:
                    for o in finals:
                        wait(o.ev)

            block.sync(lambda e: run_stream("sp", e, final=True))
            block.scalar(lambda e: run_stream("act", e))
            block.vector(lambda e: run_stream("dve", e))
            block.gpsimd(lambda e: run_stream("pool", e))
            block.tensor(lambda e: run_stream("pe", e))


def _blk(flag):
    if flag:
        with ExitStack() as st:
            yield st


class CFG:
    def __init__(self, NW=64, NO=16, NEXP=16384, stop=9):
        self.NW, self.NO, self.NEXP, self.stop = NW, NO, NEXP, stop
        self.astop = 99
        self.NB = NW * 8
        self.NCH = max(1, self.NB // 128)
        self.NWK = min(NW, NO + 4)
        self.NSB = NW * 2


LOGG = [float(np.log1p(-np.exp2(-5.0 - h))) for h in range(4)]


def host_tables(cfg, off):
    NW, NO, NB, NCH = cfg.NW, cfg.NO, cfg.NB, cfg.NCH
    S = NW * 128
    p = (off + np.arange(S)).astype(np.float32)
    t = {}
    inv128 = (10000.0 ** (-np.arange(0, 128, 2, dtype=np.float32) / 128)).astype(np.float32)
    inv64 = (10000.0 ** (-np.arange(0, 64, 2, dtype=np.float32) / 64)).astype(np.float32)
    a128 = p[:, None] * inv128[None]
    a64 = p[:, None] * inv64[None]
    t["rope"] = np.concatenate([np.cos(a128), np.sin(a128), np.cos(a64), np.sin(a64)], 1).astype(np.float32)
    valid_tile = ((off + 128 * np.arange(NW)) >= 0).astype(np.float32)
    n = np.arange(128, dtype=np.float32)
    sc = 128 ** -0.5
    zt = np.stack([np.exp((127 - n) * LOGG[h]) * sc for h in range(4)], 1)
    t["zeta"] = (zt[:, None, :] * valid_tile[None, :, None]).astype(np.float32).reshape(128, NW * 4)
    dm = np.zeros((128, 4, 128), np.float32)
    for h in range(4):
        d = n[None, :] - n[:, None]
        dm[:, h, :] = np.where(d >= 0, np.exp(np.where(d >= 0, d, 0) * LOGG[h]), 0.0) * sc
    t["dmat"] = dm.reshape(128, 512)
    xi = np.stack([np.exp((n + 1.0) * LOGG[h]) for h in range(4)], 0)
    t["xi"] = np.broadcast_to(xi[None], (128, 4, 128)).reshape(128, 512).astype(np.float32).copy()
    t["keybias"] = np.broadcast_to(np.where(valid_tile > 0, 0.0, NEG)[None], (128, NW)).astype(np.float32).copy()
    nb = np.arange(NCH * 128)
    cvalid = ((off + 16 * nb) >= 0) & (nb <= NB - 2)
    t["cmpbias"] = np.where(cvalid, 0.0, NEG).astype(np.float32).reshape(NCH, 128).T.copy()
    tq = (NW - NO) * 128 + np.arange(NO * 128)
    cp = np.where((16 * nb[:, None] + 31) <= tq[None, :], 0.0, NEG)
    cp = cp.reshape(NCH, 128, NO, 128).transpose(2, 1, 0, 3)
    cp = np.broadcast_to(cp[:, :, :, None, :], (NO, 128, NCH, 4, 128))
    t["cmppen"] = cp.astype(ml_dtypes.bfloat16).reshape(NO * 128, NCH * 512)
    k = np.arange(128)
    t["tri"] = np.concatenate([np.tile(np.where(k[:, None] <= k[None, :], 0.0, NEG), (1, 4)),
                               np.tile(np.where(k[:, None] > k[None, :], 0.0, NEG), (1, 4))], 1).astype(ml_dtypes.bfloat16)
    t["iota128"] = np.broadcast_to(np.arange(128, dtype=np.float32)[None], (128, 128)).copy()
    mb = np.arange(128)
    cur = tq // 64
    b0 = (-off) // 64
    validb = (mb[None, :] >= b0) & (mb[None, :] <= cur[:, None]) & (mb[None, :] < NW * 2)
    forced = ((mb[None, :] == b0) | (mb[None, :] == cur[:, None]) | (mb[None, :] == cur[:, None] - 1)) & validb
    m1 = (validb & ~forced).astype(np.float32)
    m2 = np.where(forced, 1e9, np.where(validb, 0.0, -BIG)).astype(np.float32)
    t["selm"] = np.concatenate([m1, m2], 1)
    key = np.arange(S)
    ex = np.zeros((128, S), np.float32)
    ex[key // 64, key] = 1.0
    t["expand"] = ex.astype(ml_dtypes.bfloat16)
    cs = 16 * nb
    ss = 64 * mb
    ov = np.clip(np.minimum(cs[:, None] + 32, ss[None, :] + 64) - np.maximum(cs[:, None], ss[None, :]), 0, None) / 16.0
    t["overlap"] = ov.reshape(NCH, 128, 128).transpose(1, 0, 2).reshape(128, NCH * 128).astype(ml_dtypes.bfloat16)
    t["ident"] = np.eye(128, dtype=np.float32)
    sel = np.zeros((128, 128, 128), np.float32)
    sel[k, k, :] = 1.0
    t["selrow"] = sel.reshape(128, 128 * 128).astype(ml_dtypes.bfloat16)
    t["iota16"] = np.broadcast_to(np.arange(16, dtype=np.float32)[None], (128, 16)).copy()
    return t


def w_in_perm():
    r = lambda a, b: list(range(a, b))
    return np.array(r(512, 1024) + r(1024, 1536) + r(2560, 2688) + r(2816, 2944) + r(3072, 3200)
                    + r(2688, 2816) + r(2944, 3072) + r(3200, 3328)
                    + r(0, 512) + r(1536, 2048)
                    + [2048 + (g * 4 + h) * 64 + d for h in range(4) for g in range(2) for d in range(64)]
                    + r(3328, 3352))


B_RK, B_RV, B_NK, B_NV, B_RQ, B_RG, B_NQ, B_GT = (0, 512), (512, 512), (1024, 384), (1408, 384), \
    (1792, 512), (2304, 512), (2816, 512), (3328, 24)


def build(cfg):
    NW, NO, NB, NCH, NWK = cfg.NW, cfg.NO, cfg.NB, cfg.NCH, cfg.NWK
    T0 = NW - NO
    S = NW * 128
    nc = bass.Bass("TRN2", target_bir_lowering=False)
    dram = lambda name, shape, dt=F32, kind="ExternalInput": nc.dram_tensor(name, shape, dt, kind=kind).ap()
    xw = dram("xw", [S, 1024])
    ccol = dram("ccol", [128, 8])
    w_ada = dram("w_ada", [1024, 6144])
    b_adaT = dram("b_adaT", [128, 48])
    gmixT = dram("gmixT", [128, 8])
    gffnT = dram("gffnT", [128, 8])
    gfin = dram("gfin", [1, 1024])
    w_in = dram("w_in", [1024, 3352])
    w_out = dram("w_out", [1024, 1024])
    w1k = dram("w1k", [64, 32 * 256])
    w1v = dram("w1v", [64, 32 * 256])
    peTk = dram("peTk", [64, 32])
    peTv = dram("peTv", [64, 32])
    b1k = dram("b1k", [128, 2])
    b1v = dram("b1v", [128, 2])
    w2k = dram("w2k", [128, 2 * 64])
    w2v = dram("w2v", [128, 2 * 64])
    w_pq = dram("w_pq", [1024, 2048])
    skT = dram("skT", [128, 2048])
    pdown = dram("pdown", [cfg.NEXP, 1024])
    pup = dram("pup", [cfg.NEXP, 1024])
    tb = {}
    for name, shape, dt in [("rope", [S, 192], F32), ("zeta", [128, NW * 4], F32), ("dmat", [128, 512], F32),
                            ("xi", [128, 512], F32), ("keybias", [128, NW], F32), ("cmpbias", [128, NCH], F32),
                            ("cmppen", [NO * 128, NCH * 512], BF16), ("tri", [128, 1024], BF16), ("iota128", [128, 128], F32),
                            ("selm", [NO * 128, 256], F32), ("expand", [128, S], BF16),
                            ("overlap", [128, NCH * 128], BF16), ("ident", [128, 128], F32),
                            ("selrow", [128, 128 * 128], BF16), ("iota16", [128, 16], F32)]:
        tb[name] = dram("t_" + name, shape, dt)
    out = dram("out", [NO * 128, 1024], kind="ExternalOutput")
    rawd = dram("rawd", [2, 128, S], BF16, kind="Internal")
    RAWLEN = max(S + 16, 16 * NCH * 128 + 32)
    x1d = dram("x1d", [NO * 128, 1024], kind="Internal")
    pdb = dram("pdb", [cfg.NEXP, 1024], BF16, kind="Internal")
    pub = dram("pub", [cfg.NEXP, 1024], BF16, kind="Internal")

    with ExitStack() as outer:
        sbo = lambda name, shape, dt=F32: outer.enter_context(nc.sbuf_tensor(name, shape, dt))
        SEM = SemState(nc, outer)
        vec = sbo("vec", [128, 48])

        _bc = [0]

        def bcast_rows(P, sb, pbrk, items):
            _bc[0] += 1
            onesf = sb("onesf%d" % _bc[0], [128, 128]); diag = [sb("diag%d_%d" % (i_, _bc[0]), [128, 128]) for i_ in range(2)]
            P.op("dve", lambda e: e.memset(onesf[:], 1.0), writes=["onesf"])
            n = 0
            for vi, dst, dkey in items:
                for half in range(2):
                    pb, pkey = pbrk[n % 2]
                    for q in range(4):
                        fc = half * 4 + q
                        dg = diag[q % 2]
                        P.op("dve", lambda e, dg=dg, vi=vi, fc=fc: e.tensor_scalar(out=dg[:], in0=identf[:], scalar1=vec[:, vi * 8 + fc:vi * 8 + fc + 1],
                                                                                  scalar2=None, op0=ALU.mult),
                             reads=["identf", "vec"], writes=[("diag", q % 2)])
                        P.op("pe", lambda e, dg=dg, pb=pb, q=q: e.matmul(pb[:, q * 128:(q + 1) * 128], lhsT=onesf[:], rhs=dg[:], start=True, stop=True),
                             reads=[("diag", q % 2), "onesf"], writes=[pkey])
                    P.op("act", lambda e, pb=pb, dst=dst, half=half: e.copy(out=dst[:, half * 512:(half + 1) * 512], in_=pb[:]),
                         reads=[pkey], writes=[dkey])
                    n += 1
        identf = sbo("identf", [128, 128]); identb = sbo("identb", [128, 128], BF16)
        epst = sbo("epst", [128, 1])
        mid = ExitStack()
        sbm = lambda name, shape, dt=F32: mid.enter_context(nc.sbuf_tensor(name, shape, dt))
        ksT = sbm("ksT", [128, S], BF16)
        vs = sbm("vs", [128, NW * 2 * 65], BF16)
        kwT = sbm("kwT", [128, NWK * 128], BF16)
        vw = sbm("vw", [128, NWK * 2 * 65], BF16)
        kcT = sbm("kcT", [128, NCH * 128], BF16)
        vca = sbm("vca", [128, NCH * 2 * 193], BF16)
        reto = sbm("reto", [128, NO * 512], BF16)
        qTs = sbm("qTs", [128, NO * 512], BF16)
        gts = sbm("gts", [128, NO * 24])
        vs4 = vs[:].rearrange("p (t g d) -> p t g d", g=2, d=65)
        vw4 = vw[:].rearrange("p (t g d) -> p t g d", g=2, d=65)
        vca4 = vca[:].rearrange("p (c g d) -> p c g d", g=2, d=193)

        for st in _blk(cfg.stop >= 0):
            sb = lambda name, shape, dt=F32: st.enter_context(nc.sbuf_tensor(name, shape, dt))
            ps = lambda name, shape, dt=F32: st.enter_context(nc.psum_tensor(name, shape, dt))
            P = Prog(nc, SEM)
            wad = [sb("wad%d" % i, [128, 8 * 1024]) for i in range(2)]
            cc = sb("cc", [128, 8]); sil = sb("sil", [128, 8])
            badT = sb("badT", [128, 48]); gm = sb("gm", [128, 8]); gf_ = sb("gf_", [128, 8])
            modT = sb("modT", [128, 48])
            pm = ps("pm", [128, 48])
            P.dma("sp", cc[:], ccol, writes=["cc"])
            P.dma("sp", badT[:], b_adaT, writes=["badT"])
            P.dma("sp", gm[:], gmixT, writes=["gm"])
            P.dma("sp", gf_[:], gffnT, writes=["gf_"])
            P.dma("sp", identf[:], tb["ident"], writes=["identf"])
            P.op("dve", lambda e: e.memset(epst[:], 1e-6), writes=["eps"])
            P.op("act", lambda e: e.activation(out=sil[:], in_=cc[:], func=AF.Silu), reads=["cc"], writes=["sil"])
            P.op("dve", lambda e: e.tensor_copy(out=identb[:], in_=identf[:]), reads=["identf"], writes=["identb"])
            for s in range(6):
                wt = wad[s % 2]
                wt3 = wt[:].rearrange("p (k n) -> p k n", k=8)
                for kc in range(8):
                    P.dma("sp" if kc % 2 == 0 else "act", wt3[:, kc, :], w_ada[kc * 128:(kc + 1) * 128, s * 1024:(s + 1) * 1024],
                          writes=[("wad", s % 2, kc)])
                for fc in range(8):
                    for kc in range(8):
                        P.op("pe", lambda e, fc=fc, kc=kc, wt3=wt3, s=s: e.matmul(
                            pm[:, s * 8 + fc:s * 8 + fc + 1], lhsT=wt3[:, kc, fc * 128:(fc + 1) * 128], rhs=sil[:, kc:kc + 1],
                            start=(kc == 0), stop=(kc == 7)), reads=[("wad", s % 2, kc), "sil"], writes=["pm"])
            P.op("dve", lambda e: e.tensor_tensor(out=modT[:], in0=pm[:], in1=badT[:], op=ALU.add), reads=["pm", "badT"], writes=["modT"])
            P.op("dve", lambda e: e.scalar_tensor_tensor(out=vec[:, 0:8], in0=modT[:, 8:16], scalar=1.0, in1=gm[:], op0=ALU.add, op1=ALU.mult),
                 reads=["modT", "gm"], writes=["vec"])
            P.op("dve", lambda e: e.scalar_tensor_tensor(out=vec[:, 16:24], in0=modT[:, 32:40], scalar=1.0, in1=gf_[:], op0=ALU.add, op1=ALU.mult),
                 reads=["modT", "gf_"], writes=["vec"])
            P.op("dve", lambda e: e.tensor_copy(out=vec[:, 8:16], in_=modT[:, 0:8]), reads=["modT"], writes=["vec"])
            P.op("dve", lambda e: e.tensor_copy(out=vec[:, 24:32], in_=modT[:, 24:32]), reads=["modT"], writes=["vec"])
            P.op("dve", lambda e: e.tensor_copy(out=vec[:, 32:40], in_=modT[:, 16:24]), reads=["modT"], writes=["vec"])
            P.op("dve", lambda e: e.tensor_copy(out=vec[:, 40:48], in_=modT[:, 40:48]), reads=["modT"], writes=["vec"])
            P.emit()

        for st in _blk(cfg.stop >= 1):
            sb = lambda name, shape, dt=F32: st.enter_context(nc.sbuf_tensor(name, shape, dt))
            ps = lambda name, shape, dt=F32: st.enter_context(nc.psum_tensor(name, shape, dt))
            P = Prog(nc, SEM)
            wib = sb("wib", [128, 8 * 3352], BF16)
            wib3 = wib[:].rearrange("p (k n) -> p k n", k=8)
            stg = [sb("stg%d" % i, [128, 838]) for i in range(2)]
            A1R = sb("A1R", [128, 1024]); B1R = sb("B1R", [128, 1024])
            n_ = 0
            for kc in range(8):
                for cq in range(4):
                    sl = slice(cq * 838, (cq + 1) * 838)
                    P.dma("sp", stg[n_ % 2][:], w_in[kc * 128:(kc + 1) * 128, sl], writes=[("stg", n_ % 2)])
                    if n_ % 2:
                        P.op("act", lambda e, kc=kc, sl=sl, n_=n_: e.copy(out=wib3[:, kc, sl], in_=stg[n_ % 2][:]), reads=[("stg", n_ % 2)], writes=["wib"])
                    else:
                        P.op("pool", lambda e, kc=kc, sl=sl, n_=n_: e.tensor_copy(out=wib3[:, kc, sl], in_=stg[n_ % 2][:]), reads=[("stg", n_ % 2)], writes=["wib"])
                    n_ += 1
            xt = [sb("xt%d" % i, [128, 1024]) for i in range(2)]
            sq = sb("sq", [128, 1024])
            ss = sb("ss", [128, 2]); rs = sb("rs", [128, 2])
            tmod = sb("tmod", [128, 1024])
            hb = sb("hb", [128, 1024], BF16)
            hT = [sb("hT%d" % i, [128, 1024], BF16) for i in range(2)]
            rp = [sb("rp%d" % i, [128, 192]) for i in range(2)]
            pj = [sb("pj%d" % i, [128, 512]) for i in range(3)]
            ra = sb("ra", [128, 256]); rb_ = sb("rb_", [128, 256]); rc_ = sb("rc_", [128, 256]); rd_ = sb("rd_", [128, 256])
            ktok = sb("ktok", [128, 512], BF16)
            qtok = sb("qtok", [128, 512], BF16)
            vtok = sb("vtok", [128, 512], BF16)
            vz = sb("vz", [128, 512], BF16)
            nk = sb("nk", [128, 384], BF16)
            nv = sb("nv", [128, 384], BF16)
            nq = sb("nq", [128, 512], BF16)
            Sst = sb("Sst", [128, 512]); Sb = sb("Sb", [128, 512], BF16)
            zt = sb("zt", [128, NW * 4]); dmat = sb("dmat", [128, 512]); xit = sb("xit", [128, 512])
            kTr = sb("kTr", [128, 512], BF16); qTr = sb("qTr", [128, 512], BF16); qxT = sb("qxT", [128, 512], BF16)
            pT = sb("pT", [128, 512], BF16)
            yv = sb("yv", [128, 512]); sg = sb("sg", [128, 512])
            st6 = sb("st6", [128, 4 * 6]); mv = sb("mv", [128, 4 * 2]); rstd4 = sb("rstd4", [128, 4])
            rawst = [sb("rawst%d" % i, [128, 256], BF16) for i in range(2)]
            p_tr = ps("p_tr", [128, 1024], BF16)
            p_mm = [ps("p_mm%d" % i, [128, 512]) for i in range(3)]
            p_kv = ps("p_kv", [128, 512])
            p_sc = ps("p_sc", [128, 512])
            p_y = ps("p_y", [128, 512])
            bcast_rows(P, sb, [(p_y, "p_y"), (p_sc, "p_sc")], [(0, A1R, "A1R"), (1, B1R, "B1R")])
            P.dma("sp", zt[:], tb["zeta"], writes=["zt"])
            P.dma("sp", dmat[:], tb["dmat"], writes=["dmat"])
            P.dma("sp", xit[:], tb["xi"], writes=["xit"])
            P.op("dve", lambda e: e.memset(Sst[:], 0.0), writes=["Sst"])
            P.op("dve", lambda e: e.memset(Sb[:], 0.0), writes=["Sb"])
            P.op("pool", lambda e: e.memset(vs[:], 1.0), writes=["vs"])
            P.op("pool", lambda e: e.memset(vw[:], 1.0), writes=["vw"])

            def rope(src, dst, H, D, cos, sin, keys_r, key_w):
                if os.environ.get("SKIP_ROPE"):
                    return
                h2 = D // 2
                s3 = src.rearrange("p (h d) -> p h d", h=H)
                d3 = dst.rearrange("p (h d) -> p h d", h=H)
                x1, x2 = s3[:, :, 0:h2], s3[:, :, h2:D]
                cb = cos.unsqueeze(1).to_broadcast([128, H, h2])
                sbb = sin.unsqueeze(1).to_broadcast([128, H, h2])
                n_ = H * h2
                v = lambda t_: t_[:, 0:n_].rearrange("p (h d) -> p h d", h=H)
                P.op("dve", lambda e: e.tensor_tensor(out=v(ra), in0=x1, in1=cb, op=ALU.mult), reads=keys_r, writes=["ra"])
                P.op(ROPE_ENG, lambda e: e.tensor_tensor(out=v(rb_), in0=x2, in1=sbb, op=ALU.mult), reads=keys_r, writes=["rb"])
                P.op("dve", lambda e: e.tensor_tensor(out=d3[:, :, 0:h2], in0=v(ra), in1=v(rb_), op=ALU.subtract), reads=["ra", "rb"], writes=[key_w + "_lo"])
                P.op(ROPE_ENG, lambda e: e.tensor_tensor(out=v(rc_), in0=x2, in1=cb, op=ALU.mult), reads=keys_r, writes=["rc"])
                P.op("dve", lambda e: e.tensor_tensor(out=v(rd_), in0=x1, in1=sbb, op=ALU.mult), reads=keys_r, writes=["rd"])
                P.op(ROPE_ENG, lambda e: e.tensor_tensor(out=d3[:, :, h2:D], in0=v(rc_), in1=v(rd_), op=ALU.add), reads=["rc", "rd"], writes=[key_w + "_hi"])

            def proj(blk, pdst, pkey, hTt, hkey):
                c0, w = blk
                for kc in range(8):
                    P.op("pe", lambda e, kc=kc: e.matmul(pdst[:, 0:w], lhsT=hTt[:, kc * 128:(kc + 1) * 128], rhs=wib3[:, kc, c0:c0 + w],
                                                        start=(kc == 0), stop=(kc == 7)), reads=[hkey, "wib"], writes=[pkey])

            for T in range(NW if cfg.astop >= 1 else 0):
                b = T % 2
                own = T >= T0
                i = T - T0
                P.dma("sp", xt[b][:], xw[T * 128:(T + 1) * 128, :], writes=[("xt", b)])
                P.dma("sp", rp[b][:], tb["rope"][T * 128:(T + 1) * 128, :], writes=[("rp", b)])
                P.op("act", lambda e, b=b: e.activation(out=sq[:], in_=xt[b][:], func=AF.Square), reads=[("xt", b)], writes=["sq"])
                P.op("dve", lambda e, b=b: e.reduce_sum(out=ss[:, b:b + 1], in_=sq[:], axis=AX.X), reads=["sq"], writes=[("ss", b)])
                P.op("act", lambda e, b=b: e.activation(out=rs[:, b:b + 1], in_=ss[:, b:b + 1], func=AF.Sqrt, scale=1.0 / 1024, bias=epst[:]),
                     reads=[("ss", b), "eps"], writes=[("rs", b)])
                P.op("dve", lambda e, b=b: e.reciprocal(out=rs[:, b:b + 1], in_=rs[:, b:b + 1]), reads=[("rs", b)], writes=[("rs", b)])
                P.op("dve", lambda e, b=b: e.scalar_tensor_tensor(out=tmod[:], in0=xt[b][:], scalar=rs[:, b:b + 1], in1=A1R[:], op0=ALU.mult, op1=ALU.mult),
                     reads=[("xt", b), ("rs", b), "A1R"], writes=["tmod"])
                P.op("pool", lambda e: e.tensor_tensor(out=hb[:], in0=tmod[:], in1=B1R[:], op=ALU.add), reads=["tmod", "B1R"], writes=["hb"])
                for kc in range(8):
                    P.op("pe", lambda e, kc=kc: e.transpose(p_tr[:, kc * 128:(kc + 1) * 128], hb[:, kc * 128:(kc + 1) * 128], identb[:]),
                         reads=["hb"], writes=["p_tr"])
                P.op("act", lambda e, b=b: e.copy(out=hT[b][:], in_=p_tr[:]), reads=["p_tr"], writes=[("hT", b)])
                hkey = ("hT", b)
                cos128, sin128, cos64, sin64 = rp[b][:, 0:64], rp[b][:, 64:128], rp[b][:, 128:160], rp[b][:, 160:192]
                if cfg.astop < 2:
                    continue
                proj(B_RK, p_mm[0], ("p_mm", 0), hT[b], hkey)
                P.op("act", lambda e: e.copy(out=pj[0][:], in_=p_mm[0][:]), reads=[("p_mm", 0)], writes=[("pj", 0)])
                rope(pj[0][:], ktok[:], 4, 128, cos128, sin128, [("pj", 0), ("rp", b)], "ktok")
                proj(B_RV, p_mm[1], ("p_mm", 1), hT[b], hkey)
                P.op("act", lambda e: e.copy(out=vtok[:], in_=p_mm[1][:]), reads=[("p_mm", 1)], writes=["vtok"])
                for h in range(0 if os.environ.get("SKIP_VZ") else 4):
                    P.op("dve", lambda e, h=h, T=T: e.tensor_scalar(out=vz[:, h * 128:(h + 1) * 128], in0=vtok[:, h * 128:(h + 1) * 128],
                                                                    scalar1=zt[:, T * 4 + h:T * 4 + h + 1], scalar2=None, op0=ALU.mult),
                         reads=["vtok", "zt"], writes=[("vz", h)])
                if cfg.astop < 2.2:
                    continue
                proj(B_NK, p_mm[2], ("p_mm", 2), hT[b], hkey)
                P.op("act", lambda e: e.copy(out=pj[2][:, 0:384], in_=p_mm[2][:, 0:384]), reads=[("p_mm", 2)], writes=[("pj", 2)])
                if cfg.astop < 2.5:
                    continue
                rope(pj[2][:, 0:384], nk[:], 6, 64, cos64, sin64, [("pj", 2), ("rp", b)], "nk")
                if cfg.astop < 2.8:
                    continue
                proj(B_NV, p_mm[0], ("p_mm", 0), hT[b], hkey)
                P.op("act", lambda e: e.copy(out=nv[:], in_=p_mm[0][:, 0:384]), reads=[("p_mm", 0)], writes=["nv"])
                if cfg.astop < 4:
                    continue
                P.op("dve", lambda e, T=T: e.tensor_copy(out=vs4[:, T, :, 0:64], in_=nv[:, 128:256].rearrange("p (g d) -> p g d", g=2)),
                     reads=["nv"], writes=["vs"])
                wk = T - (NW - NWK)
                if wk >= 0:
                    P.op("dve", lambda e, wk=wk: e.tensor_copy(out=vw4[:, wk, :, 0:64], in_=nv[:, 256:384].rearrange("p (g d) -> p g d", g=2)),
                         reads=["nv"], writes=["vw"])
                srcs = [nk[:, 0:128], nk[:, 128:256], nk[:, 256:384], nv[:, 0:128]]
                for q, s_ in enumerate(srcs):
                    P.op("pe", lambda e, q=q, s_=s_: e.transpose(p_tr[:, q * 128:(q + 1) * 128], s_, identb[:]),
                         reads=["nk_lo", "nk_hi", "nv"], writes=["p_tr"])
                P.op("act", lambda e, T=T: e.copy(out=ksT[:, T * 128:(T + 1) * 128], in_=p_tr[:, 128:256]), reads=["p_tr"], writes=["ksT"])
                if wk >= 0:
                    P.op("act", lambda e, wk=wk: e.copy(out=kwT[:, wk * 128:(wk + 1) * 128], in_=p_tr[:, 256:384]), reads=["p_tr"], writes=["kwT"])
                rw = rawst[T % 2]
                P.op("act", lambda e, rw=rw: e.copy(out=rw[:, 0:128], in_=p_tr[:, 0:128]), reads=["p_tr"], writes=[("rawst", T % 2)])
                P.op("act", lambda e, rw=rw: e.copy(out=rw[:, 128:256], in_=p_tr[:, 384:512]), reads=["p_tr"], writes=[("rawst", T % 2)])
                for kind in range(2):
                    P.dma("sp", rawd[kind, :, T * 128:(T + 1) * 128], rw[:, kind * 128:(kind + 1) * 128], reads=[("rawst", T % 2)])
                if cfg.astop < 5:
                    continue
                if own and cfg.astop >= 6:
                    proj(B_RQ, p_mm[1], ("p_mm", 1), hT[b], hkey)
                    P.op("act", lambda e: e.copy(out=pj[1][:], in_=p_mm[1][:]), reads=[("p_mm", 1)], writes=[("pj", 1)])
                    rope(pj[1][:], qtok[:], 4, 128, cos128, sin128, [("pj", 1), ("rp", b)], "qtok")
                    for h in range(4):
                        P.op("pe", lambda e, h=h: e.transpose(p_tr[:, h * 128:(h + 1) * 128], ktok[:, h * 128:(h + 1) * 128], identb[:]),
                             reads=["ktok_lo", "ktok_hi"], writes=["p_tr"])
                    P.op("act", lambda e: e.copy(out=kTr[:], in_=p_tr[:, 0:512]), reads=["p_tr"], writes=["kTr"])
                    for h in range(4):
                        P.op("pe", lambda e, h=h: e.transpose(p_tr[:, 512 + h * 128:512 + (h + 1) * 128], qtok[:, h * 128:(h + 1) * 128], identb[:]),
                             reads=["qtok_lo", "qtok_hi"], writes=["p_tr"])
                    P.op("act", lambda e: e.copy(out=qTr[:], in_=p_tr[:, 512:1024]), reads=["p_tr"], writes=["qTr"])
                    P.op("dve", lambda e: e.tensor_tensor(out=qxT[:], in0=qTr[:], in1=xit[:], op=ALU.mult), reads=["qTr", "xit"], writes=["qxT"])
                    for h in range(4):
                        hs = slice(h * 128, (h + 1) * 128)
                        P.op("pe", lambda e, hs=hs: e.matmul(p_sc[:, hs], lhsT=kTr[:, hs], rhs=qTr[:, hs], start=True, stop=True),
                             reads=["kTr", "qTr"], writes=["p_sc"])
                    P.op("dve", lambda e: e.tensor_tensor(out=pT[:], in0=p_sc[:], in1=dmat[:], op=ALU.mult), reads=["p_sc", "dmat"], writes=["pT"])
                    for h in range(4):
                        hs = slice(h * 128, (h + 1) * 128)
                        P.op("pe", lambda e, hs=hs: e.matmul(p_y[:, hs], lhsT=pT[:, hs], rhs=vtok[:, hs], start=True, stop=False),
                             reads=["pT", "vtok"], writes=["p_y"])
                        P.op("pe", lambda e, hs=hs: e.matmul(p_y[:, hs], lhsT=qxT[:, hs], rhs=Sb[:, hs], start=False, stop=True),
                             reads=["qxT", "Sb"], writes=["p_y"])
                    P.op("act", lambda e: e.copy(out=yv[:], in_=p_y[:]), reads=["p_y"], writes=["yv"])
                    for h in range(4):
                        P.op("dve", lambda e, h=h: e.bn_stats(out=st6[:, h * 6:(h + 1) * 6], in_=yv[:, h * 128:(h + 1) * 128]), reads=["yv"], writes=[("st6", h)])
                        P.op("dve", lambda e, h=h: e.bn_aggr(out=mv[:, h * 2:(h + 1) * 2], in_=st6[:, h * 6:(h + 1) * 6]), reads=[("st6", h)], writes=[("mv", h)])
                    mv3 = mv[:].rearrange("p (h t) -> p h t", t=2)
                    P.op("act", lambda e: e.activation(out=rstd4[:], in_=mv3[:, :, 1], func=AF.Sqrt, bias=epst[:]),
                         reads=[("mv", 0), ("mv", 1), ("mv", 2), ("mv", 3), "eps"], writes=["rstd4"])
                    P.op("dve", lambda e: e.reciprocal(out=rstd4[:], in_=rstd4[:]), reads=["rstd4"], writes=["rstd4"])
                    proj(B_RG, p_mm[2], ("p_mm", 2), hT[b], hkey)
                    P.op("act", lambda e: e.activation(out=sg[:], in_=p_mm[2][:], func=AF.Silu), reads=[("p_mm", 2)], writes=["sg"])
                    for h in range(4):
                        hs = slice(h * 128, (h + 1) * 128)
                        P.op("dve", lambda e, h=h, hs=hs: e.tensor_scalar(out=yv[:, hs], in0=yv[:, hs], scalar1=mv[:, 2 * h:2 * h + 1], scalar2=rstd4[:, h:h + 1],
                                                                         op0=ALU.subtract, op1=ALU.mult),
                             reads=["yv", ("mv", h), "rstd4"], writes=[("yn", h)])
                        P.op("pool", lambda e, h=h, hs=hs, i=i: e.tensor_tensor(out=reto[:, i * 512 + h * 128:i * 512 + (h + 1) * 128], in0=yv[:, hs], in1=sg[:, hs], op=ALU.mult),
                             reads=[("yn", h), "sg"], writes=["reto"])
                    proj(B_NQ, p_mm[0], ("p_mm", 0), hT[b], hkey)
                    P.op("act", lambda e: e.copy(out=pj[0][:], in_=p_mm[0][:]), reads=[("p_mm", 0)], writes=[("pj", 0)])
                    rope(pj[0][:], nq[:], 8, 64, cos64, sin64, [("pj", 0), ("rp", b)], "nq")
                    for h in range(4):
                        P.op("pe", lambda e, h=h: e.transpose(p_tr[:, h * 128:(h + 1) * 128], nq[:, h * 128:(h + 1) * 128], identb[:]),
                             reads=["nq_lo", "nq_hi"], writes=["p_tr"])
                    P.op("act", lambda e, i=i: e.copy(out=qTs[:, i * 512:(i + 1) * 512], in_=p_tr[:, 0:512]), reads=["p_tr"], writes=["qTs"])
                    proj(B_GT, p_mm[1], ("p_mm", 1), hT[b], hkey)
                    P.op("act", lambda e, i=i: e.activation(out=gts[:, i * 24:(i + 1) * 24], in_=p_mm[1][:, 0:24], func=AF.Sigmoid),
                         reads=[("p_mm", 1)], writes=["gts"])
                for h in range(4):
                    hs = slice(h * 128, (h + 1) * 128)
                    P.op("pe", lambda e, hs=hs: e.matmul(p_kv[:, hs], lhsT=ktok[:, hs], rhs=vz[:, hs], start=True, stop=True),
                         reads=["ktok_lo", "ktok_hi", ("vz", 0), ("vz", 1), ("vz", 2), ("vz", 3)], writes=["p_kv"])
                for h in range(4):
                    hs = slice(h * 128, (h + 1) * 128)
                    P.op("dve", lambda e, h=h, hs=hs: e.scalar_tensor_tensor(out=Sst[:, hs], in0=Sst[:, hs], scalar=float(np.exp(128 * LOGG[h])), in1=p_kv[:, hs],
                                                                            op0=ALU.mult, op1=ALU.add), reads=["p_kv", "Sst"], writes=["Sst"])
                P.op("act", lambda e: e.copy(out=Sb[:], in_=Sst[:]), reads=["Sst"], writes=["Sb"])
            P.emit()

        for st in _blk(cfg.stop >= 2):
            sb = lambda name, shape, dt=F32: st.enter_context(nc.sbuf_tensor(name, shape, dt))
            ps = lambda name, shape, dt=F32: st.enter_context(nc.psum_tensor(name, shape, dt))
            P = Prog(nc, SEM)
            NBP = NCH * 128
            raw = [sb("raw%d" % k, [128, RAWLEN], BF16) for k in range(2)]
            w1s = sb("w1s", [128, 32 * 256]); w1b = [sb("w1b%d" % k, [128, 32 * 256], BF16) for k in range(2)]
            pes = sb("pes", [128, 32]); peb = [sb("peb%d" % k, [128, 32], BF16) for k in range(2)]
            b1s = [sb("b1s%d" % k, [128, 2]) for k in range(2)]
            w2s = sb("w2s", [128, 128]); w2b = [sb("w2b%d" % k, [128, 128], BF16) for k in range(2)]
            cb = sb("cb", [128, 2])
            u = sb("u", [128, 512]); u2 = sb("u2", [128, 512]); u3 = sb("u3", [128, 512])
            hid = [sb("hid%d" % i, [128, 512], BF16) for i in range(2)]
            ovl = sb("ovl", [128, NCH * 128], BF16)
            p_h = ps("p_h", [128, 512]); p_c = ps("p_c", [128, 2]); p_o = ps("p_o", [128, 512])
            P.dma("sp", ovl[:], tb["overlap"], writes=["ovl"])
            P.op("pool", lambda e: e.memset(vca[:], 1.0), writes=["vca"])
            for cc_ in range(NCH):
                for g in range(2):
                    P.op("pool", lambda e, cc_=cc_, g=g: e.tensor_copy(out=vca4[:, cc_, g, 65:193], in_=ovl[:, cc_ * 128:(cc_ + 1) * 128]), reads=["ovl"], writes=["vca"])
            for kind, (w1d, ped, b1d, w2d) in enumerate([(w1k, peTk, b1k, w2k), (w1v, peTv, b1v, w2v)]):
                P.op("pool", lambda e, kind=kind: e.memset(raw[kind][:], 0.0), writes=[("raw", kind)])
                P.dma("sp", raw[kind][:, 0:S], rawd[kind], writes=[("raw", kind)])
                for half in range(2):
                    P.dma("sp", w1s[half * 64:(half + 1) * 64, :], w1d, writes=["w1s"])
                    P.dma("sp", pes[half * 64:(half + 1) * 64, :], ped, writes=["pes"])
                P.op("act", lambda e, kind=kind: e.copy(out=w1b[kind][:], in_=w1s[:]), reads=["w1s"], writes=[("w1b", kind)])
                P.op("dve", lambda e, kind=kind: e.tensor_copy(out=peb[kind][:], in_=pes[:]), reads=["pes"], writes=[("peb", kind)])
                P.dma("sp", b1s[kind][:], b1d, writes=[("b1s", kind)])
                P.dma("sp", w2s[:], w2d, writes=["w2s"])
                P.op("dve", lambda e, kind=kind: e.tensor_copy(out=w2b[kind][:], in_=w2s[:]), reads=["w2s"], writes=[("w2b", kind)])
                w13 = w1b[kind][:].rearrange("p (l n) -> p l n", l=32)
                for hc in range(2):
                    for l in range(32):
                        P.op("pe", lambda e, hc=hc, l=l, w13=w13, kind=kind: e.matmul(p_c[:, hc:hc + 1], lhsT=w13[0:64, l, hc * 128:(hc + 1) * 128], rhs=peb[kind][0:64, l:l + 1],
                                                                                    start=(l == 0), stop=(l == 31)), reads=[("w1b", kind), ("peb", kind)], writes=["p_c"])
                P.op("dve", lambda e, kind=kind: e.tensor_tensor(out=cb[:], in0=p_c[:], in1=b1s[kind][:], op=ALU.add), reads=["p_c", ("b1s", kind)], writes=["cb"])
                for g in range(2):
                    gs = slice(64 * g, 64 * g + 64)
                    for n0 in range(0, NBP, 512):
                        nn = min(512, NBP - n0)
                        for hc in range(2):
                            for l in range(32):
                                rhs = raw[kind][gs, l + 16 * n0: l + 16 * (n0 + nn): 16]
                                P.op("pe", lambda e, hc=hc, l=l, rhs=rhs, gs=gs, w13=w13, nn=nn: e.matmul(
                                    p_h[:, 0:nn], lhsT=w13[gs, l, hc * 128:(hc + 1) * 128], rhs=rhs, start=(l == 0), stop=(l == 31)),
                                    reads=[("w1b", kind), ("raw", kind)], writes=["p_h"])
                            P.op("act", lambda e, hc=hc, nn=nn: e.activation(out=u[:, 0:nn], in_=p_h[:, 0:nn], func=AF.Identity, bias=cb[:, hc:hc + 1]),
                                 reads=["p_h", "cb"], writes=["u"])
                            P.op("act", lambda e, nn=nn: e.activation(out=u2[:, 0:nn], in_=u[:, 0:nn], func=AF.Square), reads=["u"], writes=["u2"])
                            P.op("dve", lambda e, nn=nn: e.tensor_scalar(out=u2[:, 0:nn], in0=u2[:, 0:nn], scalar1=0.044715, scalar2=1.0, op0=ALU.mult, op1=ALU.add),
                                 reads=["u2"], writes=["u2"])
                            P.op("dve", lambda e, nn=nn: e.tensor_tensor(out=u3[:, 0:nn], in0=u2[:, 0:nn], in1=u[:, 0:nn], op=ALU.mult), reads=["u2", "u"], writes=["u3"])
                            P.op("act", lambda e, nn=nn: e.activation(out=u3[:, 0:nn], in_=u3[:, 0:nn], func=AF.Sigmoid, scale=1.5957691216), reads=["u3"], writes=["u3"])
                            P.op("dve", lambda e, hc=hc, nn=nn: e.tensor_tensor(out=hid[hc][:, 0:nn], in0=u3[:, 0:nn], in1=u[:, 0:nn], op=ALU.mult),
                                 reads=["u3", "u"], writes=[("hid", hc)])
                        w23 = w2b[kind][:].rearrange("p (c d) -> p c d", c=2)
                        if kind == 0:
                            for hc in range(2):
                                P.op("pe", lambda e, hc=hc, gs=gs, nn=nn, w23=w23: e.matmul(p_o[gs, 0:nn], lhsT=w23[:, hc, :], rhs=hid[hc][:, 0:nn], start=(hc == 0), stop=(hc == 1)),
                                     reads=[("hid", 0), ("hid", 1), ("w2b", 0)], writes=["p_o"])
                            P.op("act", lambda e, gs=gs, n0=n0, nn=nn: e.copy(out=kcT[gs, n0:n0 + nn], in_=p_o[gs, 0:nn]), reads=["p_o"], writes=["kcT"])
                        else:
                            for c4 in range(nn // 128):
                                for hc in range(2):
                                    P.op("pe", lambda e, hc=hc, c4=c4, w23=w23: e.matmul(p_o[:, c4 * 64:(c4 + 1) * 64], lhsT=hid[hc][:, c4 * 128:(c4 + 1) * 128], rhs=w23[:, hc, :],
                                                                                        start=(hc == 0), stop=(hc == 1)), reads=[("hid", 0), ("hid", 1), ("w2b", 1)], writes=["p_o"])
                                P.op("act", lambda e, c4=c4, g=g, n0=n0: e.copy(out=vca4[:, n0 // 128 + c4, g, 0:64], in_=p_o[:, c4 * 64:(c4 + 1) * 64]),
                                     reads=["p_o"], writes=["vca"])
            P.emit()

        for st in _blk(cfg.stop >= 3):
            sb = lambda name, shape, dt=F32: st.enter_context(nc.sbuf_tensor(name, shape, dt))
            ps = lambda name, shape, dt=F32: st.enter_context(nc.psum_tensor(name, shape, dt))
            P = Prog(nc, SEM)
            wob = sb("wob", [128, 8 * 1024], BF16)
            wob3 = wob[:].rearrange("p (k n) -> p k n", k=8)
            stg = [sb("stgc%d" % i, [128, 1024]) for i in range(2)]
            for kc in range(8):
                P.dma("sp", stg[kc % 2][:], w_out[kc * 128:(kc + 1) * 128, :], writes=[("stg", kc % 2)])
                P.op("pool", lambda e, kc=kc: e.tensor_copy(out=wob3[:, kc, :], in_=stg[kc % 2][:]), reads=[("stg", kc % 2)], writes=["wob"])
            expd = sb("expd", [128, S], BF16); tri = sb("tri", [128, 1024], BF16)
            kbias = sb("kbias", [128, NW]); cbias = sb("cbias", [128, NCH])
            cpen = [sb("cpen%d" % i, [128, NCH * 512], BF16) for i in range(2)]
            selm = [sb("selm%d" % i, [128, 256]) for i in range(2)]
            xo = [sb("xo%d" % i, [128, 1024]) for i in range(2)]
            eb = [sb("eb%d" % i, [128, 512], BF16) for i in range(3)]
            imp = sb("imp", [128, 128]); impm = sb("impm", [128, 128]); r1 = sb("r1", [128, 128]); r2 = sb("r2", [128, 128])
            m8 = sb("m8", [128, 16]); seln = sb("seln", [128, 128], BF16); selT = sb("selT", [128, 512], BF16)
            den = sb("den", [128, 4]); coef = sb("coef", [128, 4])
            nsa = sb("nsa", [128, 512])
            cat = sb("cat", [128, 1024], BF16); catT = sb("catT", [128, 1024], BF16)
            x1t = [sb("x1t%d" % i, [128, 1024]) for i in range(2)]
            p_s = [ps("p_s%d" % i, [128, 512]) for i in range(2)]
            p_a = [ps("p_a%d" % i, [128, 512]) for i in range(4)]
            p_x = ps("p_x", [128, 1024])
            GA1 = sb("GA1", [128, 1024])
            bcast_rows(P, sb, [(p_s[0], ("p_s", 0)), (p_s[1], ("p_s", 1))], [(4, GA1, "GA1")])
            RP = 2
            NCK = cfg.NEXP // (128 * RP)
            cin = [sb("cin%d" % i_, [128, RP * 1024]) for i_ in range(2)]
            cob = [sb("cob%d" % i_, [128, RP * 1024], BF16) for i_ in range(2)]
            cast_jobs = []
            for src_, dst_ in ((pdown, pdb), (pup, pub)):
                srcv = src_.rearrange("(c p j) d -> c p (j d)", p=128, j=RP)
                dstv = dst_.rearrange("(c p j) d -> c p (j d)", p=128, j=RP)
                for c_ in range(NCK):
                    cast_jobs.append((srcv[c_], dstv[c_]))
            cast_n = [0]

            def emit_casts(n):
                for _ in range(n):
                    if cast_n[0] >= len(cast_jobs):
                        return
                    src1, dst1 = cast_jobs[cast_n[0]]
                    k_ = cast_n[0] % 2
                    P.dma("sp", cin[k_][:], src1, writes=[("cin", k_)])
                    eng = "pool" if cast_n[0] % 2 else "dve"
                    P.op(eng, lambda e, k_=k_: e.tensor_copy(out=cob[k_][:], in_=cin[k_][:]), reads=[("cin", k_)], writes=[("cob", k_)])
                    P.dma("sp", dst1, cob[k_][:], reads=[("cob", k_)])
                    cast_n[0] += 1

            P.dma("sp", expd[:], tb["expand"], writes=["expd"])
            P.dma("sp", tri[:], tb["tri"], writes=["tri"])
            P.dma("sp", kbias[:], tb["keybias"], writes=["kbias"])
            P.dma("sp", cbias[:], tb["cmpbias"], writes=["cbias"])
            p_xb = p_x[:].bitcast(BF16)
            gts4 = gts[:].rearrange("p (i g h c) -> p i g h c", g=2, h=4, c=3)
            nsa4 = nsa[:].rearrange("p (g h d) -> p g h d", g=2, h=4)
            sc_i = [0]
            eb_i = [0]

            def attend(i, g, chunks, first_branch, br):
                W = chunks[0]["v"].shape[-1]
                nchk = len(chunks)
                qg = qTs[64 * g:64 * g + 64, i * 512:(i + 1) * 512]
                pbs = []

                def stage_s(ci):
                    ch = chunks[ci]
                    pb = sc_i[0] % 2; sc_i[0] += 1
                    pbs.append(pb)
                    pst = p_s[pb]
                    npen = len(ch["pens"])
                    P.op("pe", lambda e, ch=ch, pst=pst, npen=npen: e.matmul(pst[:], lhsT=ch["lhsT"], rhs=qg, start=True, stop=(npen == 0)),
                         reads=ch["keys_r"] + ["qTs"], writes=[("p_s", pb)])
                    for pi, (pl, pr, pk) in enumerate(ch["pens"]):
                        P.op("pe", lambda e, pl=pl, pr=pr, pst=pst, last=(pi == npen - 1): e.matmul(
                            pst[:], lhsT=pl, rhs=pr, start=False, stop=last), reads=pk, writes=[("p_s", pb)])

                def stage_ev(ci):
                    ch = chunks[ci]
                    pb = pbs[ci]
                    pst = p_s[pb]
                    ei = eb_i[0] % 3; eb_i[0] += 1
                    et = eb[ei]
                    if ch["bias"] is None:
                        P.op("act", lambda e, et=et, pst=pst: e.activation(out=et[:], in_=pst[:], func=AF.Exp, scale=0.125), reads=[("p_s", pb)], writes=[("eb", ei)])
                    else:
                        P.op("act", lambda e, et=et, pst=pst, ch=ch: e.activation(out=et[:], in_=pst[:], func=AF.Exp, scale=0.125, bias=ch["bias"]),
                             reads=[("p_s", pb), "kbias", "cbias"], writes=[("eb", ei)])
                    for h in range(4):
                        P.op("pe", lambda e, h=h, et=et, ch=ch, ci=ci: e.matmul(p_a[h][:, 0:W], lhsT=et[:, h * 128:(h + 1) * 128], rhs=ch["v"],
                                                                              start=(ci == 0), stop=(ci == nchk - 1)),
                             reads=[("eb", ei)] + ch["keys_v"], writes=[("p_a", h)])

                stage_s(0)
                for ci in range(nchk):
                    if ci + 1 < nchk:
                        stage_s(ci + 1)
                    stage_ev(ci)
                for h in range(4):
                    P.op("dve", lambda e, h=h: e.tensor_scalar(out=den[:, h:h + 1], in0=p_a[h][:, 64:65], scalar1=1e-30, scalar2=None, op0=ALU.max),
                         reads=[("p_a", h)], writes=[("den", h)])
                    P.op("dve", lambda e, h=h: e.reciprocal(out=den[:, h:h + 1], in_=den[:, h:h + 1]), reads=[("den", h)], writes=[("den", h)])
                    P.op("dve", lambda e, h=h: e.tensor_tensor(out=coef[:, h:h + 1], in0=den[:, h:h + 1], in1=gts4[:, i, g, h, br:br + 1], op=ALU.mult),
                         reads=[("den", h), "gts"], writes=[("coef", h)])
                    if first_branch:
                        P.op("dve", lambda e, h=h: e.tensor_scalar(out=nsa4[:, g, h, :], in0=p_a[h][:, 0:64], scalar1=coef[:, h:h + 1], scalar2=None, op0=ALU.mult),
                             reads=[("p_a", h), ("coef", h)], writes=[("nsa", g, h)])
                    else:
                        P.op("dve", lambda e, h=h: e.scalar_tensor_tensor(out=nsa4[:, g, h, :], in0=p_a[h][:, 0:64], scalar=coef[:, h:h + 1], in1=nsa4[:, g, h, :],
                                                                         op0=ALU.mult, op1=ALU.add),
                             reads=[("p_a", h), ("coef", h), ("nsa", g, h)], writes=[("nsa", g, h)])

            CPT = -(-len(cast_jobs) // (NO * 6))
            for i in range(NO):
                T = T0 + i
                b = i % 2
                P.dma("sp", cpen[b][:], tb["cmppen"][i * 128:(i + 1) * 128, :], writes=[("cpen", b)])
                P.dma("sp", selm[b][:], tb["selm"][i * 128:(i + 1) * 128, :], writes=[("selm", b)])
                P.dma("sp", xo[b][:], xw[T * 128:(T + 1) * 128, :], writes=[("xo", b)])
                for g in range(2):
                    gs = slice(64 * g, 64 * g + 64)
                    chunks = []
                    for c_ in range(NCH):
                        chunks.append(dict(lhsT=kcT[gs, c_ * 128:(c_ + 1) * 128], keys_r=["kcT"],
                                           pens=[(identb[:], cpen[b][:, c_ * 512:(c_ + 1) * 512], [("cpen", b), "identb"])],
                                           bias=cbias[:, c_:c_ + 1], v=vca4[:, c_, g, :], keys_v=["vca"]))
                    emit_casts(CPT)
                    attend(i, g, chunks, True, 0)
                    for h in range(4):
                        if h == 0:
                            P.op("dve", lambda e: e.tensor_scalar(out=imp[:], in0=p_a[0][:, 65:193], scalar1=den[:, 0:1], scalar2=None, op0=ALU.mult),
                                 reads=[("p_a", 0), ("den", 0)], writes=["imp"])
                        else:
                            P.op("dve", lambda e, h=h: e.scalar_tensor_tensor(out=imp[:], in0=p_a[h][:, 65:193], scalar=den[:, h:h + 1], in1=imp[:], op0=ALU.mult, op1=ALU.add),
                                 reads=[("p_a", h), ("den", h), "imp"], writes=["imp"])
                    P.op("dve", lambda e, b=b: e.tensor_tensor(out=impm[:], in0=imp[:], in1=selm[b][:, 0:128], op=ALU.mult), reads=["imp", ("selm", b)], writes=["impm"])
                    P.op("dve", lambda e, b=b: e.tensor_tensor(out=impm[:], in0=impm[:], in1=selm[b][:, 128:256], op=ALU.add), reads=["impm", ("selm", b)], writes=["impm"])
                    P.op("dve", lambda e: e.max(out=m8[:, 0:8], in_=impm[:]), reads=["impm"], writes=["m8a"])
                    P.op("dve", lambda e: e.match_replace(out=r1[:], in_to_replace=m8[:, 0:8], in_values=impm[:], imm_value=-BIG), reads=["impm", "m8a"], writes=["r1"])
                    P.op("dve", lambda e: e.max(out=m8[:, 8:16], in_=r1[:]), reads=["r1"], writes=["m8b"])
                    P.op("dve", lambda e: e.match_replace(out=r2[:], in_to_replace=m8[:, 8:16], in_values=r1[:], imm_value=-BIG), reads=["r1", "m8b"], writes=["r2"])
                    P.op("dve", lambda e: e.tensor_tensor(out=r1[:], in0=impm[:], in1=r2[:], op=ALU.subtract), reads=["impm", "r2"], writes=["r1"])
                    P.op("dve", lambda e: e.tensor_scalar(out=seln[:], in0=r1[:], scalar1=0.0, scalar2=NEG, op0=ALU.is_le, op1=ALU.mult), reads=["r1"], writes=["seln"])
                    P.op("pe", lambda e: e.transpose(p_xb[:, 0:128], seln[:], identb[:]), reads=["seln"], writes=["p_x"])
                    P.op("dve", lambda e: e.tensor_copy(out=selT[:].rearrange("p (h t) -> p h t", h=4), in_=p_xb[:, 0:128].unsqueeze(1).to_broadcast([128, 4, 128])),
                         reads=["p_x"], writes=["selT"])
                    chunks = []
                    for c_ in range(T + 1):
                        pens = [(expd[:, c_ * 128:(c_ + 1) * 128], selT[:], ["expd", "selT"])]
                        if c_ == T:
                            pens.append((identb[:], tri[:, 0:512], ["tri", "identb"]))
                        chunks.append(dict(lhsT=ksT[gs, c_ * 128:(c_ + 1) * 128], keys_r=["ksT"], pens=pens, bias=None, v=vs4[:, c_, g, :], keys_v=["vs"]))
                    emit_casts(CPT)
                    attend(i, g, chunks, False, 1)
                    chunks = []
                    for c_ in range(max(0, T - 4), T + 1):
                        wk = c_ - (NW - NWK)
                        pens = []
                        if c_ == T - 4:
                            pens.append((identb[:], tri[:, 512:1024], ["tri", "identb"]))
                        if c_ == T:
                            pens.append((identb[:], tri[:, 0:512], ["tri", "identb"]))
                        chunks.append(dict(lhsT=kwT[gs, wk * 128:(wk + 1) * 128], keys_r=["kwT"], pens=pens, bias=kbias[:, c_:c_ + 1], v=vw4[:, wk, g, :], keys_v=["vw"]))
                    emit_casts(CPT)
                    attend(i, g, chunks, False, 2)
                P.op("act", lambda e, i=i: e.copy(out=cat[:, 0:512], in_=reto[:, i * 512:(i + 1) * 512]), reads=["reto"], writes=["cat_a"])
                P.op("act", lambda e: e.copy(out=cat[:, 512:1024], in_=nsa[:]), reads=[("nsa", g_, h_) for g_ in range(2) for h_ in range(4)], writes=["cat_b"])
                for kc in range(8):
                    P.op("pe", lambda e, kc=kc: e.transpose(p_xb[:, kc * 128:(kc + 1) * 128], cat[:, kc * 128:(kc + 1) * 128], identb[:]),
                         reads=["cat_a", "cat_b"], writes=["p_x"])
                P.op("act", lambda e: e.copy(out=catT[:], in_=p_xb[:, 0:1024]), reads=["p_x"], writes=["catT"])
                for half in range(2):
                    for kc in range(8):
                        P.op("pe", lambda e, kc=kc, half=half: e.matmul(p_x[:, half * 512:(half + 1) * 512], lhsT=catT[:, kc * 128:(kc + 1) * 128],
                                                                        rhs=wob3[:, kc, half * 512:(half + 1) * 512], start=(kc == 0), stop=(kc == 7)),
                             reads=["catT", "wob"], writes=["p_x"])
                P.op("dve", lambda e, b=b: e.tensor_tensor(out=x1t[b][:], in0=p_x[:], in1=GA1[:], op=ALU.mult), reads=["p_x", "GA1"], writes=[("x1t", b)])
                P.op("pool", lambda e, b=b: e.tensor_tensor(out=x1t[b][:], in0=x1t[b][:], in1=xo[b][:], op=ALU.add), reads=[("x1t", b), ("xo", b)], writes=[("x1t", b)])
                P.dma("sp", x1d[i * 128:(i + 1) * 128, :], x1t[b][:], reads=[("x1t", b)])
            emit_casts(len(cast_jobs))
            P.emit()

        mid.close()
        for st in _blk(cfg.stop >= 4):
            sb = lambda name, shape, dt=F32: st.enter_context(nc.sbuf_tensor(name, shape, dt))
            ps = lambda name, shape, dt=F32: st.enter_context(nc.psum_tensor(name, shape, dt))
            P = Prog(nc, SEM)
            wqb = sb("wqb", [128, 8 * 2048], BF16)
            wqb3 = wqb[:].rearrange("p (k n) -> p k n", k=8)
            stg = [sb("stgd%d" % i, [128, 2048]) for i in range(2)]
            for kc in range(8):
                P.dma("sp", stg[kc % 2][:], w_pq[kc * 128:(kc + 1) * 128, :], writes=[("stg", kc % 2)])
                P.op("act", lambda e, kc=kc: e.copy(out=wqb3[:, kc, :], in_=stg[kc % 2][:]), reads=[("stg", kc % 2)], writes=["wqb"])
            A2R = sb("A2R", [128, 1024]); B2R = sb("B2R", [128, 1024]); GA2 = sb("GA2", [128, 1024]); GF = sb("GF", [128, 1024])
            P.dma("sp", GF[:], gfin.partition_broadcast(128), writes=["GF"])
            skb = sb("skb", [128, 2048], BF16)
            P.dma("sp", stg[0][:], skT, writes=[("stg", 0)])
            P.op("act", lambda e: e.copy(out=skb[:], in_=stg[0][:]), reads=[("stg", 0)], writes=["skb"])
            selrow = sb("selrow", [128, 128 * 128], BF16)
            P.dma("sp", selrow[:], tb["selrow"], writes=["selrow"])
            selrow3 = selrow[:].rearrange("p (t m) -> p t m", t=128)
            io16 = sb("io16", [128, 16])
            P.dma("sp", io16[:], tb["iota16"], writes=["io16"])
            io128 = sb("io128", [128, 128]); cm = [sb("cm%d" % i_, [128, 128], BF16) for i_ in range(3)]
            P.dma("sp", io128[:], tb["iota128"], writes=["io128"])
            x1 = sb("x1", [128, 1024]); sq = sb("sqd", [128, 1024]); ss = sb("ssd", [128, 1]); rs = sb("rsd", [128, 1])
            tmod = sb("tmodd", [128, 1024]); h2b = sb("h2b", [128, 1024], BF16); h2T = sb("h2T", [128, 1024], BF16)
            qT = sb("qT", [128, 2048], BF16)
            s_sb = sb("s_sb", [128, 2048]); s2 = sb("s2", [128, 128])
            V16 = sb("V16", [128, 256]); I16 = sb("I16", [128, 256], U32); I16f = sb("I16f", [128, 256])
            cand = sb("cand", [128, 2048]); c2 = sb("c2", [128, 256])
            TV = sb("TV", [128, 128]); TJ = sb("TJ", [128, 128], U32); TA = sb("TA", [128, 128], U32); TBb = sb("TBb", [128, 128], U32)
            TAf = sb("TAf", [128, 128]); TBf = sb("TBf", [128, 128])
            oh = sb("oh", [128, 2048]); i0 = sb("i0", [128, 128]); i1 = sb("i1", [128, 128]); ef = sb("ef", [128, 128])
            ex = sb("ex", [128, 128]); sm = sb("sm", [128, 8]); Wt = sb("Wt", [128, 128])
            idxT = sb("idxT", [128, 128], U32); WT = sb("WT", [128, 128]); aT = sb("aT", [128, 128]); cT = sb("cT", [128, 128])
            g2 = sb("g2", [128, 128]); g3 = sb("g3", [128, 128])
            NU = 8
            U = [sb("U%d" % i, [128, 1024], BF16) for i in range(NU)]
            jk = sq
            oT = tmod; x2 = sb("x2", [128, 1024]); yo = sb("yo", [128, 1024])
            p_q = ps("p_q", [128, 512]); p_s4 = ps("p_s4", [128, 2048]); p_hb = ps("p_hb", [128, 1024]); p_t = ps("p_t", [128, 512])
            bcast_rows(P, sb, [(p_q, "p_q"), (p_t, "p_t")], [(2, A2R, "A2R"), (3, B2R, "B2R"), (5, GA2, "GA2")])
            V4 = V16[:].rearrange("p (c k) -> p c k", k=16); I4 = I16[:].rearrange("p (c k) -> p c k", k=16)
            s3 = s_sb[:].rearrange("p (c n) -> p c n", n=128)
            for i in range(NO):
                P.dma("sp", x1[:], x1d[i * 128:(i + 1) * 128, :], writes=["x1"])
                P.op("act", lambda e: e.activation(out=sq[:], in_=x1[:], func=AF.Square), reads=["x1"], writes=["sq"])
                P.op("dve", lambda e: e.reduce_sum(out=ss[:], in_=sq[:], axis=AX.X), reads=["sq"], writes=["ss"])
                P.op("act", lambda e: e.activation(out=rs[:], in_=ss[:], func=AF.Sqrt, scale=1.0 / 1024, bias=epst[:]), reads=["ss"], writes=["rs"])
                P.op("dve", lambda e: e.reciprocal(out=rs[:], in_=rs[:]), reads=["rs"], writes=["rs"])
                P.op("dve", lambda e: e.scalar_tensor_tensor(out=tmod[:], in0=x1[:], scalar=rs[:], in1=A2R[:], op0=ALU.mult, op1=ALU.mult), reads=["x1", "rs", "A2R"], writes=["tmod"])
                P.op("pool", lambda e: e.tensor_tensor(out=h2b[:], in0=tmod[:], in1=B2R[:], op=ALU.add), reads=["tmod", "B2R"], writes=["h2b"])
                p_tb = p_t[:].bitcast(BF16)
                for kc in range(8):
                    P.op("pe", lambda e, kc=kc: e.transpose(p_tb[:, kc * 128:(kc + 1) * 128], h2b[:, kc * 128:(kc + 1) * 128], identb[:]), reads=["h2b"], writes=["p_t"])
                P.op("act", lambda e: e.copy(out=h2T[:], in_=p_tb[:, 0:1024]), reads=["p_t"], writes=["h2T"])
                for c4 in range(4):
                    for cq in range(4):
                        ch = c4 * 4 + cq
                        for kc in range(8):
                            P.op("pe", lambda e, ch=ch, cq=cq, kc=kc: e.matmul(p_q[:, cq * 128:(cq + 1) * 128], lhsT=wqb3[:, kc, ch * 128:(ch + 1) * 128],
                                                                              rhs=h2T[:, kc * 128:(kc + 1) * 128], start=(kc == 0), stop=(kc == 7)),
                                 reads=["wqb", "h2T"], writes=["p_q"])
                    P.op("act", lambda e, c4=c4: e.copy(out=qT[:, c4 * 512:(c4 + 1) * 512], in_=p_q[:]), reads=["p_q"], writes=[("qT", c4)])
                for ch in range(16):
                    P.op("pe", lambda e, ch=ch: e.matmul(p_s4[:, ch * 128:(ch + 1) * 128], lhsT=qT[:, ch * 128:(ch + 1) * 128], rhs=skb[:, ch * 128:(ch + 1) * 128],
                                                        start=True, stop=True), reads=[("qT", ch // 4), "skb"], writes=[("p_s4", ch // 8)])
                for c4 in range(4):
                    P.op("act", lambda e, c4=c4: e.copy(out=s_sb[:, c4 * 512:(c4 + 1) * 512], in_=p_s4[:, c4 * 512:(c4 + 1) * 512]), reads=[("p_s4", c4 // 2)], writes=[("s_sb", c4)])
                for ch in range(16):
                    sk = ("s_sb", ch // 4)
                    P.op("dve", lambda e, ch=ch: e.max(out=V4[:, ch, 0:8], in_=s3[:, ch, :]), reads=[sk], writes=[("V", ch, 0)])
                    P.op("dve", lambda e, ch=ch: e.max_index(out=I4[:, ch, 0:8], in_max=V4[:, ch, 0:8], in_values=s3[:, ch, :]), reads=[sk, ("V", ch, 0)], writes=[("I", ch, 0)])
                    P.op("dve", lambda e, ch=ch: e.match_replace(out=s2[:], in_to_replace=V4[:, ch, 0:8], in_values=s3[:, ch, :], imm_value=-BIG), reads=[sk, ("V", ch, 0)], writes=["s2"])
                    P.op("dve", lambda e, ch=ch: e.max(out=V4[:, ch, 8:16], in_=s2[:]), reads=["s2"], writes=[("V", ch, 1)])
                    P.op("dve", lambda e, ch=ch: e.max_index(out=I4[:, ch, 8:16], in_max=V4[:, ch, 8:16], in_values=s2[:]), reads=["s2", ("V", ch, 1)], writes=[("I", ch, 1)])
                allV = [("V", ch, k_) for ch in range(16) for k_ in range(2)]
                allI = [("I", ch, k_) for ch in range(16) for k_ in range(2)]
                P.op("dve", lambda e: e.tensor_copy(out=I16f[:], in_=I16[:]), reads=allI, writes=["I16f"])
                cand4 = cand[:].rearrange("p (h a b) -> p h a b", a=16, b=16)
                V5 = V16[:].rearrange("p (h t k) -> p h t k", t=2, k=16)
                I5 = I16f[:].rearrange("p (h t k) -> p h t k", t=2, k=16)
                for hd in range(8):
                    P.op("dve", lambda e, hd=hd: e.tensor_tensor(out=cand4[:, hd], in0=V5[:, hd, 0, :].unsqueeze(2).to_broadcast([128, 16, 16]),
                                                                 in1=V5[:, hd, 1, :].unsqueeze(1).to_broadcast([128, 16, 16]), op=ALU.add),
                         reads=allV, writes=[("cand", hd)])
                    cd = cand[:, hd * 256:(hd + 1) * 256]
                    P.op("dve", lambda e, hd=hd, cd=cd: e.max(out=TV[:, hd * 16:hd * 16 + 8], in_=cd), reads=[("cand", hd)], writes=[("TV", hd, 0)])
                    P.op("dve", lambda e, hd=hd, cd=cd: e.max_index(out=TJ[:, hd * 16:hd * 16 + 8], in_max=TV[:, hd * 16:hd * 16 + 8], in_values=cd),
                         reads=[("cand", hd), ("TV", hd, 0)], writes=[("TJ", hd, 0)])
                    P.op("dve", lambda e, hd=hd, cd=cd: e.match_replace(out=c2[:], in_to_replace=TV[:, hd * 16:hd * 16 + 8], in_values=cd, imm_value=-BIG),
                         reads=[("cand", hd), ("TV", hd, 0)], writes=["c2"])
                    P.op("dve", lambda e, hd=hd: e.max(out=TV[:, hd * 16 + 8:hd * 16 + 16], in_=c2[:]), reads=["c2"], writes=[("TV", hd, 1)])
                    P.op("dve", lambda e, hd=hd: e.max_index(out=TJ[:, hd * 16 + 8:hd * 16 + 16], in_max=TV[:, hd * 16 + 8:hd * 16 + 16], in_values=c2[:]),
                         reads=["c2", ("TV", hd, 1)], writes=[("TJ", hd, 1)])
                allTV = [("TV", hd, k_) for hd in range(8) for k_ in range(2)]
                allTJ = [("TJ", hd, k_) for hd in range(8) for k_ in range(2)]
                TV3 = TV[:].rearrange("p (h k) -> p h k", k=16)
                ex3 = ex[:].rearrange("p (h k) -> p h k", k=16)
                P.op("dve", lambda e: e.tensor_tensor(out=ex3, in0=TV3, in1=TV3[:, :, 0:1].to_broadcast([128, 8, 16]), op=ALU.subtract), reads=allTV, writes=["ex"])
                P.op("act", lambda e: e.activation(out=ex[:], in_=ex[:], func=AF.Exp), reads=["ex"], writes=["ex"])
                P.op("dve", lambda e: e.tensor_reduce(out=sm[:], in_=ex3, axis=AX.X, op=ALU.add), reads=["ex"], writes=["sm"])
                P.op("dve", lambda e: e.reciprocal(out=sm[:], in_=sm[:]), reads=["sm"], writes=["sm"])
                P.op("dve", lambda e: e.tensor_tensor(out=Wt[:].rearrange("p (h k) -> p h k", k=16), in0=ex3, in1=sm[:].unsqueeze(2).to_broadcast([128, 8, 16]), op=ALU.mult),
                     reads=["ex", "sm"], writes=["Wt"])
                P.op("dve", lambda e: e.tensor_single_scalar(out=TA[:], in_=TJ[:], scalar=4, op=ALU.logical_shift_right), reads=allTJ, writes=["TA"])
                P.op("dve", lambda e: e.tensor_single_scalar(out=TBb[:], in_=TJ[:], scalar=15, op=ALU.bitwise_and), reads=allTJ, writes=["TB"])
                P.op("dve", lambda e: e.tensor_copy(out=TAf[:], in_=TA[:]), reads=["TA"], writes=["TAf"])
                P.op("dve", lambda e: e.tensor_copy(out=TBf[:], in_=TBb[:]), reads=["TB"], writes=["TBf"])
                oh4 = oh[:].rearrange("p (h k a) -> p h k a", k=16, a=16)
                for which, (tf, dst) in enumerate([(TAf, i0), (TBf, i1)]):
                    tf3 = tf[:].rearrange("p (h k) -> p h k", k=16)
                    P.op("dve", lambda e, tf3=tf3: e.tensor_tensor(out=oh4, in0=tf3.unsqueeze(3).to_broadcast([128, 8, 16, 16]),
                                                                  in1=io16[:].unsqueeze(1).unsqueeze(1).to_broadcast([128, 8, 16, 16]), op=ALU.is_equal),
                         reads=["TAf", "TBf", "io16"], writes=["oh"])
                    P.op("dve", lambda e, which=which: e.tensor_tensor(out=oh4, in0=oh4, in1=I5[:, :, which, :].unsqueeze(2).to_broadcast([128, 8, 16, 16]), op=ALU.mult),
                         reads=["oh", "I16f"], writes=["oh"])
                    P.op("dve", lambda e, dst=dst: e.tensor_reduce(out=dst[:], in_=oh4, axis=AX.X, op=ALU.add), reads=["oh"], writes=["i01_%d" % which])
                P.op("dve", lambda e: e.scalar_tensor_tensor(out=ef[:], in0=i0[:], scalar=128.0, in1=i1[:], op0=ALU.mult, op1=ALU.add), reads=["i01_0", "i01_1"], writes=["ef"])
                P.op("pe", lambda e: e.transpose(p_t[:, 0:128], ef[:], identf[:]), reads=["ef"], writes=["p_t"])
                P.op("pe", lambda e: e.transpose(p_t[:, 128:256], Wt[:], identf[:]), reads=["Wt"], writes=["p_t"])
                P.op("dve", lambda e: e.tensor_copy(out=idxT[:], in_=p_t[:, 0:128]), reads=["p_t"], writes=["idxT"])
                P.op("dve", lambda e: e.tensor_copy(out=WT[:], in_=p_t[:, 128:256]), reads=["p_t"], writes=["WT"])
                for t_ in range(128):
                    ub = U[t_ % NU]
                    P.op("pool", lambda e, ub=ub, t_=t_: e.indirect_dma_start(out=ub[:], out_offset=None, in_=pdb,
                                                                             in_offset=bass.IndirectOffsetOnAxis(ap=idxT[:, t_:t_ + 1], axis=0)),
                         reads=["idxT"], writes=[("U", t_ % NU)], dma=True)
                    hbt, hbk = (p_hb[:], "p_hb") if t_ % 2 == 0 else (p_s4[:, 1024:2048], ("p_s4", 1))
                    for half in range(2):
                        P.op("pe", lambda e, t_=t_, half=half, hbt=hbt: e.matmul(hbt[:, half * 512:(half + 1) * 512], lhsT=selrow3[:, t_, :], rhs=h2b[:, half * 512:(half + 1) * 512],
                                                                                 start=True, stop=True), reads=["selrow", "h2b"], writes=[hbk])
                    P.op("dve", lambda e, ub=ub, t_=t_, hbt=hbt: e.tensor_tensor_reduce(out=jk[:], in0=ub[:], in1=hbt, scale=1.0, scalar=0.0, op0=ALU.mult, op1=ALU.add,
                                                                                       accum_out=aT[:, t_:t_ + 1]), reads=[("U", t_ % NU), hbk], writes=["sq", "aT"])
                P.op("act", lambda e: e.activation(out=g2[:], in_=aT[:], func=AF.Square), reads=["aT"], writes=["g2"])
                P.op("dve", lambda e: e.tensor_scalar(out=g2[:], in0=g2[:], scalar1=0.044715, scalar2=1.0, op0=ALU.mult, op1=ALU.add), reads=["g2"], writes=["g2"])
                P.op("dve", lambda e: e.tensor_tensor(out=g3[:], in0=g2[:], in1=aT[:], op=ALU.mult), reads=["g2", "aT"], writes=["g3"])
                P.op("act", lambda e: e.activation(out=g3[:], in_=g3[:], func=AF.Sigmoid, scale=1.5957691216), reads=["g3"], writes=["g3"])
                P.op("dve", lambda e: e.tensor_tensor(out=g3[:], in0=g3[:], in1=aT[:], op=ALU.mult), reads=["g3", "aT"], writes=["g3"])
                P.op("dve", lambda e: e.tensor_tensor(out=cT[:], in0=g3[:], in1=WT[:], op=ALU.mult), reads=["g3", "WT"], writes=["cT"])
                for t_ in range(128):
                    ub = U[t_ % NU]
                    P.op("pool", lambda e, ub=ub, t_=t_: e.indirect_dma_start(out=ub[:], out_offset=None, in_=pub,
                                                                             in_offset=bass.IndirectOffsetOnAxis(ap=idxT[:, t_:t_ + 1], axis=0)),
                         reads=["idxT"], writes=[("U", t_ % NU)], dma=True)
                    cmt = cm[t_ % 3]
                    P.op("dve", lambda e, cmt=cmt, t_=t_: e.scalar_tensor_tensor(out=cmt[:], in0=io128[:], scalar=float(t_), in1=cT[:, t_:t_ + 1].to_broadcast([128, 128]),
                                                                                op0=ALU.is_equal, op1=ALU.mult), reads=["io128", "cT"], writes=[("cm", t_ % 3)])
                    for half in range(2):
                        P.op("pe", lambda e, ub=ub, cmt=cmt, t_=t_, half=half: e.matmul(p_s4[:, half * 512:(half + 1) * 512], lhsT=cmt[:],
                                                                                      rhs=ub[:, half * 512:(half + 1) * 512], start=(t_ == 0), stop=(t_ == 127)),
                             reads=[("U", t_ % NU), ("cm", t_ % 3)], writes=[("p_s4", 0)])
                P.op("dve", lambda e: e.tensor_tensor(out=x2[:], in0=p_s4[:, 0:1024], in1=GA2[:], op=ALU.mult), reads=[("p_s4", 0), "GA2"], writes=["x2"])
                P.op("pool", lambda e: e.tensor_tensor(out=x2[:], in0=x2[:], in1=x1[:], op=ALU.add), reads=["x2", "x1"], writes=["x2"])
                P.op("act", lambda e: e.activation(out=sq[:], in_=x2[:], func=AF.Square), reads=["x2"], writes=["sq"])
                P.op("dve", lambda e: e.reduce_sum(out=ss[:], in_=sq[:], axis=AX.X), reads=["sq"], writes=["ss"])
                P.op("act", lambda e: e.activation(out=rs[:], in_=ss[:], func=AF.Sqrt, scale=1.0 / 1024, bias=epst[:]), reads=["ss"], writes=["rs"])
                P.op("dve", lambda e: e.reciprocal(out=rs[:], in_=rs[:]), reads=["rs"], writes=["rs"])
                P.op("dve", lambda e: e.scalar_tensor_tensor(out=yo[:], in0=x2[:], scalar=rs[:], in1=GF[:], op0=ALU.mult, op1=ALU.mult), reads=["x2", "rs", "GF"], writes=["yo"])
                P.dma("sp", out[i * 128:(i + 1) * 128, :], yo[:], reads=["yo"])
            P.emit()
    return nc


def make_inputs(cfg, core, inp, n_per_batch=4, S_full=None):
    NW, NO = cfg.NW, cfg.NO
    b, j = core // n_per_batch, core % n_per_batch
    off = NO * 128 * (j + 1) - NW * 128
    x = inp["x"][b]
    xw = np.zeros((NW * 128, 1024), np.float32)
    lo = max(0, -off)
    xw[lo:] = x[off + lo: off + NW * 128]
    m = {"xw": xw}
    m["ccol"] = np.ascontiguousarray(inp["c"][b].reshape(8, 128).T)
    m["w_ada"] = inp["w_ada"][0]
    m["b_adaT"] = np.ascontiguousarray(inp["b_ada"][0].reshape(48, 128).T)
    m["gmixT"] = np.ascontiguousarray(inp["g_norm_mix"][0].reshape(8, 128).T)
    m["gffnT"] = np.ascontiguousarray(inp["g_norm_ffn"][0].reshape(8, 128).T)
    m["gfin"] = inp["g_norm_final"].reshape(1, 1024)
    m["w_in"] = np.ascontiguousarray(inp["w_in"][0][:, w_in_perm()])
    m["w_out"] = inp["w_out"][0]
    for nm, key in (("w1k", "w_cmp_k1"), ("w1v", "w_cmp_v1")):
        m[nm] = np.ascontiguousarray(inp[key][0].reshape(32, 64, 256).transpose(1, 0, 2).reshape(64, 32 * 256))
    m["peTk"] = np.ascontiguousarray(inp["pe_cmp_k"][0].T)
    m["peTv"] = np.ascontiguousarray(inp["pe_cmp_v"][0].T)
    m["b1k"] = np.ascontiguousarray(inp["b_cmp_k1"][0].reshape(2, 128).T)
    m["b1v"] = np.ascontiguousarray(inp["b_cmp_v1"][0].reshape(2, 128).T)
    m["w2k"] = np.ascontiguousarray(inp["w_cmp_k2"][0].reshape(2, 128, 64).transpose(1, 0, 2).reshape(128, 128))
    m["w2v"] = np.ascontiguousarray(inp["w_cmp_v2"][0].reshape(2, 128, 64).transpose(1, 0, 2).reshape(128, 128))
    m["w_pq"] = inp["w_peer_q"][0]
    m["skT"] = np.ascontiguousarray(inp["peer_subkeys"][0].reshape(16, 128, 128).transpose(2, 0, 1).reshape(128, 2048))
    m["pdown"] = inp["peer_down"][0]
    m["pup"] = inp["peer_up"][0]
    for k_, v_ in host_tables(cfg, off).items():
        m["t_" + k_] = v_
    return m


_NC_CACHE = {}


def kernel(**inputs):
    inp = {k: np.asarray(v) for k, v in inputs.items()}
    cfg = CFG()
    if "nc" not in _NC_CACHE:
        _NC_CACHE["nc"] = build(cfg)
        mybir.codegen_inst_isa_subclasses(_NC_CACHE["nc"])
    nc = _NC_CACHE["nc"]
    in_maps = [make_inputs(cfg, c, inp) for c in range(8)]
    res = run_bass_kernel_spmd(nc, in_maps, core_ids=list(range(8)))
    out = np.zeros((2, 8192, 1024), np.float32)
    for c in range(8):
        b, j = c // 4, c % 4
        out[b, j * 2048:(j + 1) * 2048] = res.results[c]["out"]
    return out
```

```python
import numpy as np
import ml_dtypes
from contextlib import ExitStack
import concourse.bass as bass
import concourse.mybir as mybir
from concourse.bass_utils import run_bass_kernel_spmd

F32 = mybir.dt.float32
BF16 = mybir.dt.bfloat16
U32 = mybir.dt.uint32
F32R = mybir.dt.float32r
ALU = mybir.AluOpType
AF = mybir.ActivationFunctionType
AX = mybir.AxisListType

import os
ROPE_ENG = os.environ.get("ROPE_ENG", "dve")
NEG = -30000.0
BIG = 1.0e30
COMPUTE = ("pe", "act", "dve", "pool")
NSW = 8


class _Op:
    __slots__ = ("eng", "fn", "reads", "writes", "deps", "is_dma", "signal", "ev", "id")


class SemState:
    def __init__(self, nc, stack, n_dma_sems=28):
        self.n_dma_sems = n_dma_sems
        self.sems = {}
        for e in COMPUTE:
            self.sems[e] = stack.enter_context(nc.semaphore("s_" + e))
        for k in range(n_dma_sems):
            self.sems["d%d" % k] = stack.enter_context(nc.semaphore("s_d%d" % k))
        self.cnt = {e: 0 for e in COMPUTE}
        self.dma_cnt = [0] * n_dma_sems


class Prog:
    def __init__(self, nc, state):
        self.nc = nc
        self.state = state
        self.ops = []
        self.last_writer = {}
        self.readers = {}
        self.n_dma_sems = state.n_dma_sems
        self.dma_rr = 0
        self.sw_rr = 0
        self.dma_last = [None] * self.n_dma_sems
        self.dma_cnt = state.dma_cnt

    def op(self, eng, fn, reads=(), writes=(), dma=False):
        o = _Op()
        o.eng, o.fn, o.is_dma = eng, fn, dma
        o.reads, o.writes = tuple(reads), tuple(writes)
        o.deps = set()
        o.signal = False
        o.ev = None
        o.id = len(self.ops)
        for r in o.reads:
            w = self.last_writer.get(r)
            if w is not None:
                o.deps.add(w)
        for w_ in o.writes:
            w = self.last_writer.get(w_)
            if w is not None:
                o.deps.add(w)
            lastc = {}
            for rd in self.readers.get(w_, ()):
                p_ = self.ops[rd]
                if p_.is_dma:
                    o.deps.add(rd)
                else:
                    lastc[p_.eng] = rd
            o.deps.update(lastc.values())
        for r in o.reads:
            self.readers.setdefault(r, []).append(o.id)
        for w_ in o.writes:
            self.last_writer[w_] = o.id
            self.readers[w_] = []
        o.deps.discard(o.id)
        if dma:
            if eng == "pool":
                k = self.n_dma_sems - NSW + self.sw_rr
                self.sw_rr = (self.sw_rr + 1) % NSW
            else:
                k = self.dma_rr
                self.dma_rr = (self.dma_rr + 1) % (self.n_dma_sems - NSW)
            prev = self.dma_last[k]
            if prev is not None:
                o.deps.add(prev)
            self.dma_last[k] = o.id
            self.dma_cnt[k] += 16
            o.ev = ("d%d" % k, self.dma_cnt[k])
        self.ops.append(o)
        return o.id

    def dma(self, eng, out, in_, reads=(), writes=(), **kw):
        return self.op(eng, lambda e: e.dma_start(out=out, in_=in_, **kw), reads, writes, dma=True)

    def emit(self):
        nc, ops = self.nc, self.ops
        for o in ops:
            nd = set()
            for d in o.deps:
                p = ops[d]
                if p.is_dma or o.is_dma or p.eng != o.eng:
                    nd.add(d)
                elif o.eng != "pe":
                    nd.add(d)
            o.deps = nd
            for d in nd:
                if not ops[d].is_dma:
                    ops[d].signal = True
        cnt = self.state.cnt
        for o in ops:
            if not o.is_dma and o.signal:
                cnt[o.eng] += 1
                o.ev = (o.eng, cnt[o.eng])
        finals = [ops[i] for i in self.dma_last if i is not None]
        with ExitStack() as st:
            sems = self.state.sems
            block = st.enter_context(nc.Block())
            streams = {}
            for o in ops:
                streams.setdefault(o.eng, []).append(o)

            def run_stream(ename, e, final=False):
                waited = {}

                def wait(ev):
                    if waited.get(ev[0], 0) < ev[1]:
                        e.wait_ge(sems[ev[0]], ev[1])
                        waited[ev[0]] = ev[1]

                for o in streams.get(ename, []):
                    for d in sorted(o.deps):
                        wait(ops[d].ev)
                    ins = o.fn(e)
                    if o.is_dma:
                        ins.then_inc(sems[o.ev[0]], 16)
                    elif o.signal:
                        ins.then_inc(sems[o.eng], 1)
                if final:
                    for o in finals:
                        wait(o.ev)

            block.sync(lambda e: run_stream("sp", e, final=True))
            block.scalar(lambda e: run_stream("act", e))
            block.vector(lambda e: run_stream("dve", e))
            block.gpsimd(lambda e: run_stream("pool", e))
            block.tensor(lambda e: run_stream("pe", e))


def _blk(flag):
    if flag:
        with ExitStack() as st:
            yield st


class CFG:
    def __init__(self, NW=64, NO=16, NEXP=16384, stop=9):
        self.NW, self.NO, self.NEXP, self.stop = NW, NO, NEXP, stop
        self.astop = 99
        self.NB = NW * 8
        self.NCH = max(1, self.NB // 128)
        self.NWK = min(NW, NO + 4)
        self.NSB = NW * 2


LOGG = [float(np.log1p(-np.exp2(-5.0 - h))) for h in range(4)]


def host_tables(cfg, off):
    NW, NO, NB, NCH = cfg.NW, cfg.NO, cfg.NB, cfg.NCH
    S = NW * 128
    p = (off + np.arange(S)).astype(np.float32)
    t = {}
    inv128 = (10000.0 ** (-np.arange(0, 128, 2, dtype=np.float32) / 128)).astype(np.float32)
    inv64 = (10000.0 ** (-np.arange(0, 64, 2, dtype=np.float32) / 64)).astype(np.float32)
    a128 = p[:, None] * inv128[None]
    a64 = p[:, None] * inv64[None]
    t["rope"] = np.concatenate([np.cos(a128), np.sin(a128), np.cos(a64), np.sin(a64)], 1).astype(np.float32)
    valid_tile = ((off + 128 * np.arange(NW)) >= 0).astype(np.float32)
    n = np.arange(128, dtype=np.float32)
    sc = 128 ** -0.5
    zt = np.stack([np.exp((127 - n) * LOGG[h]) * sc for h in range(4)], 1)
    t["zeta"] = (zt[:, None, :] * valid_tile[None, :, None]).astype(np.float32).reshape(128, NW * 4)
    dm = np.zeros((128, 4, 128), np.float32)
    for h in range(4):
        d = n[None, :] - n[:, None]
        dm[:, h, :] = np.where(d >= 0, np.exp(np.where(d >= 0, d, 0) * LOGG[h]), 0.0) * sc
    t["dmat"] = dm.reshape(128, 512)
    xi = np.stack([np.exp((n + 1.0) * LOGG[h]) for h in range(4)], 0)
    t["xi"] = np.broadcast_to(xi[None], (128, 4, 128)).reshape(128, 512).astype(np.float32).copy()
    t["keybias"] = np.broadcast_to(np.where(valid_tile > 0, 0.0, NEG)[None], (128, NW)).astype(np.float32).copy()
    nb = np.arange(NCH * 128)
    cvalid = ((off + 16 * nb) >= 0) & (nb <= NB - 2)
    t["cmpbias"] = np.where(cvalid, 0.0, NEG).astype(np.float32).reshape(NCH, 128).T.copy()
    tq = (NW - NO) * 128 + np.arange(NO * 128)
    cp = np.where((16 * nb[:, None] + 31) <= tq[None, :], 0.0, NEG)
    cp = cp.reshape(NCH, 128, NO, 128).transpose(2, 1, 0, 3)
    cp = np.broadcast_to(cp[:, :, :, None, :], (NO, 128, NCH, 4, 128))
    t["cmppen"] = cp.astype(ml_dtypes.bfloat16).reshape(NO * 128, NCH * 512)
    k = np.arange(128)
    t["tri"] = np.concatenate([np.tile(np.where(k[:, None] <= k[None, :], 0.0, NEG), (1, 4)),
                               np.tile(np.where(k[:, None] > k[None, :], 0.0, NEG), (1, 4))], 1).astype(ml_dtypes.bfloat16)
    t["iota128"] = np.broadcast_to(np.arange(128, dtype=np.float32)[None], (128, 128)).copy()
    mb = np.arange(128)
    cur = tq // 64
    b0 = (-off) // 64
    validb = (mb[None, :] >= b0) & (mb[None, :] <= cur[:, None]) & (mb[None, :] < NW * 2)
    forced = ((mb[None, :] == b0) | (mb[None, :] == cur[:, None]) | (mb[None, :] == cur[:, None] - 1)) & validb
    m1 = (validb & ~forced).astype(np.float32)
    m2 = np.where(forced, 1e9, np.where(validb, 0.0, -BIG)).astype(np.float32)
    t["selm"] = np.concatenate([m1, m2], 1)
    key = np.arange(S)
    ex = np.zeros((128, S), np.float32)
    ex[key // 64, key] = 1.0
    t["expand"] = ex.astype(ml_dtypes.bfloat16)
    cs = 16 * nb
    ss = 64 * mb
    ov = np.clip(np.minimum(cs[:, None] + 32, ss[None, :] + 64) - np.maximum(cs[:, None], ss[None, :]), 0, None) / 16.0
    t["overlap"] = ov.reshape(NCH, 128, 128).transpose(1, 0, 2).reshape(128, NCH * 128).astype(ml_dtypes.bfloat16)
    t["ident"] = np.eye(128, dtype=np.float32)
    sel = np.zeros((128, 128, 128), np.float32)
    sel[k, k, :] = 1.0
    t["selrow"] = sel.reshape(128, 128 * 128).astype(ml_dtypes.bfloat16)
    t["iota16"] = np.broadcast_to(np.arange(16, dtype=np.float32)[None], (128, 16)).copy()
    return t


def w_in_perm():
    r = lambda a, b: list(range(a, b))
    return np.array(r(512, 1024) + r(1024, 1536) + r(2560, 2688) + r(2816, 2944) + r(3072, 3200)
                    + r(2688, 2816) + r(2944, 3072) + r(3200, 3328)
                    + r(0, 512) + r(1536, 2048)
                    + [2048 + (g * 4 + h) * 64 + d for h in range(4) for g in range(2) for d in range(64)]
                    + r(3328, 3352))


B_RK, B_RV, B_NK, B_NV, B_RQ, B_RG, B_NQ, B_GT = (0, 512), (512, 512), (1024, 384), (1408, 384), \
    (1792, 512), (2304, 512), (2816, 512), (3328, 24)


def build(cfg):
    NW, NO, NB, NCH, NWK = cfg.NW, cfg.NO, cfg.NB, cfg.NCH, cfg.NWK
    T0 = NW - NO
    S = NW * 128
    nc = bass.Bass("TRN2", target_bir_lowering=False)
    dram = lambda name, shape, dt=F32, kind="ExternalInput": nc.dram_tensor(name, shape, dt, kind=kind).ap()
    xw = dram("xw", [S, 1024])
    ccol = dram("ccol", [128, 8])
    w_ada = dram("w_ada", [1024, 6144])
    b_adaT = dram("b_adaT", [128, 48])
    gmixT = dram("gmixT", [128, 8])
    gffnT = dram("gffnT", [128, 8])
    gfin = dram("gfin", [1, 1024])
    w_in = dram("w_in", [1024, 3352])
    w_out = dram("w_out", [1024, 1024])
    w1k = dram("w1k", [64, 32 * 256])
    w1v = dram("w1v", [64, 32 * 256])
    peTk = dram("peTk", [64, 32])
    peTv = dram("peTv", [64, 32])
    b1k = dram("b1k", [128, 2])
    b1v = dram("b1v", [128, 2])
    w2k = dram("w2k", [128, 2 * 64])
    w2v = dram("w2v", [128, 2 * 64])
    w_pq = dram("w_pq", [1024, 2048])
    skT = dram("skT", [128, 2048])
    pdown = dram("pdown", [cfg.NEXP, 1024])
    pup = dram("pup", [cfg.NEXP, 1024])
    tb = {}
    for name, shape, dt in [("rope", [S, 192], F32), ("zeta", [128, NW * 4], F32), ("dmat", [128, 512], F32),
                            ("xi", [128, 512], F32), ("keybias", [128, NW], F32), ("cmpbias", [128, NCH], F32),
                            ("cmppen", [NO * 128, NCH * 512], BF16), ("tri", [128, 1024], BF16), ("iota128", [128, 128], F32),
                            ("selm", [NO * 128, 256], F32), ("expand", [128, S], BF16),
                            ("overlap", [128, NCH * 128], BF16), ("ident", [128, 128], F32),
                            ("selrow", [128, 128 * 128], BF16), ("iota16", [128, 16], F32)]:
        tb[name] = dram("t_" + name, shape, dt)
    out = dram("out", [NO * 128, 1024], kind="ExternalOutput")
    rawd = dram("rawd", [2, 128, S], BF16, kind="Internal")
    RAWLEN = max(S + 16, 16 * NCH * 128 + 32)
    x1d = dram("x1d", [NO * 128, 1024], kind="Internal")
    pdb = dram("pdb", [cfg.NEXP, 1024], BF16, kind="Internal")
    pub = dram("pub", [cfg.NEXP, 1024], BF16, kind="Internal")

    with ExitStack() as outer:
        sbo = lambda name, shape, dt=F32: outer.enter_context(nc.sbuf_tensor(name, shape, dt))
        SEM = SemState(nc, outer)
        vec = sbo("vec", [128, 48])

        _bc = [0]

        def bcast_rows(P, sb, pbrk, items):
            _bc[0] += 1
            onesf = sb("onesf%d" % _bc[0], [128, 128]); diag = [sb("diag%d_%d" % (i_, _bc[0]), [128, 128]) for i_ in range(2)]
            P.op("dve", lambda e: e.memset(onesf[:], 1.0), writes=["onesf"])
            n = 0
            for vi, dst, dkey in items:
                for half in range(2):
                    pb, pkey = pbrk[n % 2]
                    for q in range(4):
                        fc = half * 4 + q
                        dg = diag[q % 2]
                        P.op("dve", lambda e, dg=dg, vi=vi, fc=fc: e.tensor_scalar(out=dg[:], in0=identf[:], scalar1=vec[:, vi * 8 + fc:vi * 8 + fc + 1],
                                                                                  scalar2=None, op0=ALU.mult),
                             reads=["identf", "vec"], writes=[("diag", q % 2)])
                        P.op("pe", lambda e, dg=dg, pb=pb, q=q: e.matmul(pb[:, q * 128:(q + 1) * 128], lhsT=onesf[:], rhs=dg[:], start=True, stop=True),
                             reads=[("diag", q % 2), "onesf"], writes=[pkey])
                    P.op("act", lambda e, pb=pb, dst=dst, half=half: e.copy(out=dst[:, half * 512:(half + 1) * 512], in_=pb[:]),
                         reads=[pkey], writes=[dkey])
                    n += 1
        identf = sbo("identf", [128, 128]); identb = sbo("identb", [128, 128], BF16)
        epst = sbo("epst", [128, 1])
        mid = ExitStack()
        sbm = lambda name, shape, dt=F32: mid.enter_context(nc.sbuf_tensor(name, shape, dt))
        ksT = sbm("ksT", [128, S], BF16)
        vs = sbm("vs", [128, NW * 2 * 65], BF16)
        kwT = sbm("kwT", [128, NWK * 128], BF16)
        vw = sbm("vw", [128, NWK * 2 * 65], BF16)
        kcT = sbm("kcT", [128, NCH * 128], BF16)
        vca = sbm("vca", [128, NCH * 2 * 193], BF16)
        reto = sbm("reto", [128, NO * 512], BF16)
        qTs = sbm("qTs", [128, NO * 512], BF16)
        gts = sbm("gts", [128, NO * 24])
        vs4 = vs[:].rearrange("p (t g d) -> p t g d", g=2, d=65)
        vw4 = vw[:].rearrange("p (t g d) -> p t g d", g=2, d=65)
        vca4 = vca[:].rearrange("p (c g d) -> p c g d", g=2, d=193)

        for st in _blk(cfg.stop >= 0):
            sb = lambda name, shape, dt=F32: st.enter_context(nc.sbuf_tensor(name, shape, dt))
            ps = lambda name, shape, dt=F32: st.enter_context(nc.psum_tensor(name, shape, dt))
            P = Prog(nc, SEM)
            wad = [sb("wad%d" % i, [128, 8 * 1024]) for i in range(2)]
            cc = sb("cc", [128, 8]); sil = sb("sil", [128, 8])
            badT = sb("badT", [128, 48]); gm = sb("gm", [128, 8]); gf_ = sb("gf_", [128, 8])
            modT = sb("modT", [128, 48])
            pm = ps("pm", [128, 48])
            P.dma("sp", cc[:], ccol, writes=["cc"])
            P.dma("sp", badT[:], b_adaT, writes=["badT"])
            P.dma("sp", gm[:], gmixT, writes=["gm"])
            P.dma("sp", gf_[:], gffnT, writes=["gf_"])
            P.dma("sp", identf[:], tb["ident"], writes=["identf"])
            P.op("dve", lambda e: e.memset(epst[:], 1e-6), writes=["eps"])
            P.op("act", lambda e: e.activation(out=sil[:], in_=cc[:], func=AF.Silu), reads=["cc"], writes=["sil"])
            P.op("dve", lambda e: e.tensor_copy(out=identb[:], in_=identf[:]), reads=["identf"], writes=["identb"])
            for s in range(6):
                wt = wad[s % 2]
                wt3 = wt[:].rearrange("p (k n) -> p k n", k=8)
                for kc in range(8):
                    P.dma("sp" if kc % 2 == 0 else "act", wt3[:, kc, :], w_ada[kc * 128:(kc + 1) * 128, s * 1024:(s + 1) * 1024],
                          writes=[("wad", s % 2, kc)])
                for fc in range(8):
                    for kc in range(8):
                        P.op("pe", lambda e, fc=fc, kc=kc, wt3=wt3, s=s: e.matmul(
                            pm[:, s * 8 + fc:s * 8 + fc + 1], lhsT=wt3[:, kc, fc * 128:(fc + 1) * 128], rhs=sil[:, kc:kc + 1],
                            start=(kc == 0), stop=(kc == 7)), reads=[("wad", s % 2, kc), "sil"], writes=["pm"])
            P.op("dve", lambda e: e.tensor_tensor(out=modT[:], in0=pm[:], in1=badT[:], op=ALU.add), reads=["pm", "badT"], writes=["modT"])
            P.op("dve", lambda e: e.scalar_tensor_tensor(out=vec[:, 0:8], in0=modT[:, 8:16], scalar=1.0, in1=gm[:], op0=ALU.add, op1=ALU.mult),
                 reads=["modT", "gm"], writes=["vec"])
            P.op("dve", lambda e: e.scalar_tensor_tensor(out=vec[:, 16:24], in0=modT[:, 32:40], scalar=1.0, in1=gf_[:], op0=ALU.add, op1=ALU.mult),
                 reads=["modT", "gf_"], writes=["vec"])
            P.op("dve", lambda e: e.tensor_copy(out=vec[:, 8:16], in_=modT[:, 0:8]), reads=["modT"], writes=["vec"])
            P.op("dve", lambda e: e.tensor_copy(out=vec[:, 24:32], in_=modT[:, 24:32]), reads=["modT"], writes=["vec"])
            P.op("dve", lambda e: e.tensor_copy(out=vec[:, 32:40], in_=modT[:, 16:24]), reads=["modT"], writes=["vec"])
            P.op("dve", lambda e: e.tensor_copy(out=vec[:, 40:48], in_=modT[:, 40:48]), reads=["modT"], writes=["vec"])
            P.emit()

        for st in _blk(cfg.stop >= 1):
            sb = lambda name, shape, dt=F32: st.enter_context(nc.sbuf_tensor(name, shape, dt))
            ps = lambda name, shape, dt=F32: st.enter_context(nc.psum_tensor(name, shape, dt))
            P = Prog(nc, SEM)
            wib = sb("wib", [128, 8 * 3352], BF16)
            wib3 = wib[:].rearrange("p (k n) -> p k n", k=8)
            stg = [sb("stg%d" % i, [128, 838]) for i in range(2)]
            A1R = sb("A1R", [128, 1024]); B1R = sb("B1R", [128, 1024])
            n_ = 0
            for kc in range(8):
                for cq in range(4):
                    sl = slice(cq * 838, (cq + 1) * 838)
                    P.dma("sp", stg[n_ % 2][:], w_in[kc * 128:(kc + 1) * 128, sl], writes=[("stg", n_ % 2)])
                    if n_ % 2:
                        P.op("act", lambda e, kc=kc, sl=sl, n_=n_: e.copy(out=wib3[:, kc, sl], in_=stg[n_ % 2][:]), reads=[("stg", n_ % 2)], writes=["wib"])
                    else:
                        P.op("pool", lambda e, kc=kc, sl=sl, n_=n_: e.tensor_copy(out=wib3[:, kc, sl], in_=stg[n_ % 2][:]), reads=[("stg", n_ % 2)], writes=["wib"])
                    n_ += 1
            xt = [sb("xt%d" % i, [128, 1024]) for i in range(2)]
            sq = sb("sq", [128, 1024])
            ss = sb("ss", [128, 2]); rs = sb("rs", [128, 2])
            tmod = sb("tmod", [128, 1024])
            hb = sb("hb", [128, 1024], BF16)
            hT = [sb("hT%d" % i, [128, 1024], BF16) for i in range(2)]
            rp = [sb("rp%d" % i, [128, 192]) for i in range(2)]
            pj = [sb("pj%d" % i, [128, 512]) for i in range(3)]
            ra = sb("ra", [128, 256]); rb_ = sb("rb_", [128, 256]); rc_ = sb("rc_", [128, 256]); rd_ = sb("rd_", [128, 256])
            ktok = sb("ktok", [128, 512], BF16)
            qtok = sb("qtok", [128, 512], BF16)
            vtok = sb("vtok", [128, 512], BF16)
            vz = sb("vz", [128, 512], BF16)
            nk = sb("nk", [128, 384], BF16)
            nv = sb("nv", [128, 384], BF16)
            nq = sb("nq", [128, 512], BF16)
            Sst = sb("Sst", [128, 512]); Sb = sb("Sb", [128, 512], BF16)
            zt = sb("zt", [128, NW * 4]); dmat = sb("dmat", [128, 512]); xit = sb("xit", [128, 512])
            kTr = sb("kTr", [128, 512], BF16); qTr = sb("qTr", [128, 512], BF16); qxT = sb("qxT", [128, 512], BF16)
            pT = sb("pT", [128, 512], BF16)
            yv = sb("yv", [128, 512]); sg = sb("sg", [128, 512])
            st6 = sb("st6", [128, 4 * 6]); mv = sb("mv", [128, 4 * 2]); rstd4 = sb("rstd4", [128, 4])
            rawst = [sb("rawst%d" % i, [128, 256], BF16) for i in range(2)]
            p_tr = ps("p_tr", [128, 1024], BF16)
            p_mm = [ps("p_mm%d" % i, [128, 512]) for i in range(3)]
            p_kv = ps("p_kv", [128, 512])
            p_sc = ps("p_sc", [128, 512])
            p_y = ps("p_y", [128, 512])
            bcast_rows(P, sb, [(p_y, "p_y"), (p_sc, "p_sc")], [(0, A1R, "A1R"), (1, B1R, "B1R")])
            P.dma("sp", zt[:], tb["zeta"], writes=["zt"])
            P.dma("sp", dmat[:], tb["dmat"], writes=["dmat"])
            P.dma("sp", xit[:], tb["xi"], writes=["xit"])
            P.op("dve", lambda e: e.memset(Sst[:], 0.0), writes=["Sst"])
            P.op("dve", lambda e: e.memset(Sb[:], 0.0), writes=["Sb"])
            P.op("pool", lambda e: e.memset(vs[:], 1.0), writes=["vs"])
            P.op("pool", lambda e: e.memset(vw[:], 1.0), writes=["vw"])

            def rope(src, dst, H, D, cos, sin, keys_r, key_w):
                if os.environ.get("SKIP_ROPE"):
                    return
                h2 = D // 2
                s3 = src.rearrange("p (h d) -> p h d", h=H)
                d3 = dst.rearrange("p (h d) -> p h d", h=H)
                x1, x2 = s3[:, :, 0:h2], s3[:, :, h2:D]
                cb = cos.unsqueeze(1).to_broadcast([128, H, h2])
                sbb = sin.unsqueeze(1).to_broadcast([128, H, h2])
                n_ = H * h2
                v = lambda t_: t_[:, 0:n_].rearrange("p (h d) -> p h d", h=H)
                P.op("dve", lambda e: e.tensor_tensor(out=v(ra), in0=x1, in1=cb, op=ALU.mult), reads=keys_r, writes=["ra"])
                P.op(ROPE_ENG, lambda e: e.tensor_tensor(out=v(rb_), in0=x2, in1=sbb, op=ALU.mult), reads=keys_r, writes=["rb"])
                P.op("dve", lambda e: e.tensor_tensor(out=d3[:, :, 0:h2], in0=v(ra), in1=v(rb_), op=ALU.subtract), reads=["ra", "rb"], writes=[key_w + "_lo"])
                P.op(ROPE_ENG, lambda e: e.tensor_tensor(out=v(rc_), in0=x2, in1=cb, op=ALU.mult), reads=keys_r, writes=["rc"])
                P.op("dve", lambda e: e.tensor_tensor(out=v(rd_), in0=x1, in1=sbb, op=ALU.mult), reads=keys_r, writes=["rd"])
                P.op(ROPE_ENG, lambda e: e.tensor_tensor(out=d3[:, :, h2:D], in0=v(rc_), in1=v(rd_), op=ALU.add), reads=["rc", "rd"], writes=[key_w + "_hi"])

            def proj(blk, pdst, pkey, hTt, hkey):
                c0, w = blk
                for kc in range(8):
                    P.op("pe", lambda e, kc=kc: e.matmul(pdst[:, 0:w], lhsT=hTt[:, kc * 128:(kc + 1) * 128], rhs=wib3[:, kc, c0:c0 + w],
                                                        start=(kc == 0), stop=(kc == 7)), reads=[hkey, "wib"], writes=[pkey])

            for T in range(NW if cfg.astop >= 1 else 0):
                b = T % 2
                own = T >= T0
                i = T - T0
                P.dma("sp", xt[b][:], xw[T * 128:(T + 1) * 128, :], writes=[("xt", b)])
                P.dma("sp", rp[b][:], tb["rope"][T * 128:(T + 1) * 128, :], writes=[("rp", b)])
                P.op("act", lambda e, b=b: e.activation(out=sq[:], in_=xt[b][:], func=AF.Square), reads=[("xt", b)], writes=["sq"])
                P.op("dve", lambda e, b=b: e.reduce_sum(out=ss[:, b:b + 1], in_=sq[:], axis=AX.X), reads=["sq"], writes=[("ss", b)])
                P.op("act", lambda e, b=b: e.activation(out=rs[:, b:b + 1], in_=ss[:, b:b + 1], func=AF.Sqrt, scale=1.0 / 1024, bias=epst[:]),
                     reads=[("ss", b), "eps"], writes=[("rs", b)])
                P.op("dve", lambda e, b=b: e.reciprocal(out=rs[:, b:b + 1], in_=rs[:, b:b + 1]), reads=[("rs", b)], writes=[("rs", b)])
                P.op("dve", lambda e, b=b: e.scalar_tensor_tensor(out=tmod[:], in0=xt[b][:], scalar=rs[:, b:b + 1], in1=A1R[:], op0=ALU.mult, op1=ALU.mult),
                     reads=[("xt", b), ("rs", b), "A1R"], writes=["tmod"])
                P.op("pool", lambda e: e.tensor_tensor(out=hb[:], in0=tmod[:], in1=B1R[:], op=ALU.add), reads=["tmod", "B1R"], writes=["hb"])
                for kc in range(8):
                    P.op("pe", lambda e, kc=kc: e.transpose(p_tr[:, kc * 128:(kc + 1) * 128], hb[:, kc * 128:(kc + 1) * 128], identb[:]),
                         reads=["hb"], writes=["p_tr"])
                P.op("act", lambda e, b=b: e.copy(out=hT[b][:], in_=p_tr[:]), reads=["p_tr"], writes=[("hT", b)])
                hkey = ("hT", b)
                cos128, sin128, cos64, sin64 = rp[b][:, 0:64], rp[b][:, 64:128], rp[b][:, 128:160], rp[b][:, 160:192]
                if cfg.astop < 2:
                    continue
                proj(B_RK, p_mm[0], ("p_mm", 0), hT[b], hkey)
                P.op("act", lambda e: e.copy(out=pj[0][:], in_=p_mm[0][:]), reads=[("p_mm", 0)], writes=[("pj", 0)])
                rope(pj[0][:], ktok[:], 4, 128, cos128, sin128, [("pj", 0), ("rp", b)], "ktok")
                proj(B_RV, p_mm[1], ("p_mm", 1), hT[b], hkey)
                P.op("act", lambda e: e.copy(out=vtok[:], in_=p_mm[1][:]), reads=[("p_mm", 1)], writes=["vtok"])
                for h in range(0 if os.environ.get("SKIP_VZ") else 4):
                    P.op("dve", lambda e, h=h, T=T: e.tensor_scalar(out=vz[:, h * 128:(h + 1) * 128], in0=vtok[:, h * 128:(h + 1) * 128],
                                                                    scalar1=zt[:, T * 4 + h:T * 4 + h + 1], scalar2=None, op0=ALU.mult),
                         reads=["vtok", "zt"], writes=[("vz", h)])
                if cfg.astop < 2.2:
                    continue
                proj(B_NK, p_mm[2], ("p_mm", 2), hT[b], hkey)
                P.op("act", lambda e: e.copy(out=pj[2][:, 0:384], in_=p_mm[2][:, 0:384]), reads=[("p_mm", 2)], writes=[("pj", 2)])
                if cfg.astop < 2.5:
                    continue
                rope(pj[2][:, 0:384], nk[:], 6, 64, cos64, sin64, [("pj", 2), ("rp", b)], "nk")
                if cfg.astop < 2.8:
                    continue
                proj(B_NV, p_mm[0], ("p_mm", 0), hT[b], hkey)
                P.op("act", lambda e: e.copy(out=nv[:], in_=p_mm[0][:, 0:384]), reads=[("p_mm", 0)], writes=["nv"])
                if cfg.astop < 4:
                    continue
                P.op("dve", lambda e, T=T: e.tensor_copy(out=vs4[:, T, :, 0:64], in_=nv[:, 128:256].rearrange("p (g d) -> p g d", g=2)),
                     reads=["nv"], writes=["vs"])
                wk = T - (NW - NWK)
                if wk >= 0:
                    P.op("dve", lambda e, wk=wk: e.tensor_copy(out=vw4[:, wk, :, 0:64], in_=nv[:, 256:384].rearrange("p (g d) -> p g d", g=2)),
                         reads=["nv"], writes=["vw"])
                srcs = [nk[:, 0:128], nk[:, 128:256], nk[:, 256:384], nv[:, 0:128]]
                for q, s_ in enumerate(srcs):
                    P.op("pe", lambda e, q=q, s_=s_: e.transpose(p_tr[:, q * 128:(q + 1) * 128], s_, identb[:]),
                         reads=["nk_lo", "nk_hi", "nv"], writes=["p_tr"])
                P.op("act", lambda e, T=T: e.copy(out=ksT[:, T * 128:(T + 1) * 128], in_=p_tr[:, 128:256]), reads=["p_tr"], writes=["ksT"])
                if wk >= 0:
                    P.op("act", lambda e, wk=wk: e.copy(out=kwT[:, wk * 128:(wk + 1) * 128], in_=p_tr[:, 256:384]), reads=["p_tr"], writes=["kwT"])
                rw = rawst[T % 2]
                P.op("act", lambda e, rw=rw: e.copy(out=rw[:, 0:128], in_=p_tr[:, 0:128]), reads=["p_tr"], writes=[("rawst", T % 2)])
                P.op("act", lambda e, rw=rw: e.copy(out=rw[:, 128:256], in_=p_tr[:, 384:512]), reads=["p_tr"], writes=[("rawst", T % 2)])
                for kind in range(2):
                    P.dma("sp", rawd[kind, :, T * 128:(T + 1) * 128], rw[:, kind * 128:(kind + 1) * 128], reads=[("rawst", T % 2)])
                if cfg.astop < 5:
                    continue
                if own and cfg.astop >= 6:
                    proj(B_RQ, p_mm[1], ("p_mm", 1), hT[b], hkey)
                    P.op("act", lambda e: e.copy(out=pj[1][:], in_=p_mm[1][:]), reads=[("p_mm", 1)], writes=[("pj", 1)])
                    rope(pj[1][:], qtok[:], 4, 128, cos128, sin128, [("pj", 1), ("rp", b)], "qtok")
                    for h in range(4):
                        P.op("pe", lambda e, h=h: e.transpose(p_tr[:, h * 128:(h + 1) * 128], ktok[:, h * 128:(h + 1) * 128], identb[:]),
                             reads=["ktok_lo", "ktok_hi"], writes=["p_tr"])
                    P.op("act", lambda e: e.copy(out=kTr[:], in_=p_tr[:, 0:512]), reads=["p_tr"], writes=["kTr"])
                    for h in range(4):
                        P.op("pe", lambda e, h=h: e.transpose(p_tr[:, 512 + h * 128:512 + (h + 1) * 128], qtok[:, h * 128:(h + 1) * 128], identb[:]),
                             reads=["qtok_lo", "qtok_hi"], writes=["p_tr"])
                    P.op("act", lambda e: e.copy(out=qTr[:], in_=p_tr[:, 512:1024]), reads=["p_tr"], writes=["qTr"])
                    P.op("dve", lambda e: e.tensor_tensor(out=qxT[:], in0=qTr[:], in1=xit[:], op=ALU.mult), reads=["qTr", "xit"], writes=["qxT"])
                    for h in range(4):
                        hs = slice(h * 128, (h + 1) * 128)
                        P.op("pe", lambda e, hs=hs: e.matmul(p_sc[:, hs], lhsT=kTr[:, hs], rhs=qTr[:, hs], start=True, stop=True),
                             reads=["kTr", "qTr"], writes=["p_sc"])
                    P.op("dve", lambda e: e.tensor_tensor(out=pT[:], in0=p_sc[:], in1=dmat[:], op=ALU.mult), reads=["p_sc", "dmat"], writes=["pT"])
                    for h in range(4):
                        hs = slice(h * 128, (h + 1) * 128)
                        P.op("pe", lambda e, hs=hs: e.matmul(p_y[:, hs], lhsT=pT[:, hs], rhs=vtok[:, hs], start=True, stop=False),
                             reads=["pT", "vtok"], writes=["p_y"])
                        P.op("pe", lambda e, hs=hs: e.matmul(p_y[:, hs], lhsT=qxT[:, hs], rhs=Sb[:, hs], start=False, stop=True),
                             reads=["qxT", "Sb"], writes=["p_y"])
                    P.op("act", lambda e: e.copy(out=yv[:], in_=p_y[:]), reads=["p_y"], writes=["yv"])
                    for h in range(4):
                        P.op("dve", lambda e, h=h: e.bn_stats(out=st6[:, h * 6:(h + 1) * 6], in_=yv[:, h * 128:(h + 1) * 128]), reads=["yv"], writes=[("st6", h)])
                        P.op("dve", lambda e, h=h: e.bn_aggr(out=mv[:, h * 2:(h + 1) * 2], in_=st6[:, h * 6:(h + 1) * 6]), reads=[("st6", h)], writes=[("mv", h)])
                    mv3 = mv[:].rearrange("p (h t) -> p h t", t=2)
                    P.op("act", lambda e: e.activation(out=rstd4[:], in_=mv3[:, :, 1], func=AF.Sqrt, bias=epst[:]),
                         reads=[("mv", 0), ("mv", 1), ("mv", 2), ("mv", 3), "eps"], writes=["rstd4"])
                    P.op("dve", lambda e: e.reciprocal(out=rstd4[:], in_=rstd4[:]), reads=["rstd4"], writes=["rstd4"])
                    proj(B_RG, p_mm[2], ("p_mm", 2), hT[b], hkey)
                    P.op("act", lambda e: e.activation(out=sg[:], in_=p_mm[2][:], func=AF.Silu), reads=[("p_mm", 2)], writes=["sg"])
                    for h in range(4):
                        hs = slice(h * 128, (h + 1) * 128)
                        P.op("dve", lambda e, h=h, hs=hs: e.tensor_scalar(out=yv[:, hs], in0=yv[:, hs], scalar1=mv[:, 2 * h:2 * h + 1], scalar2=rstd4[:, h:h + 1],
                                                                         op0=ALU.subtract, op1=ALU.mult),
                             reads=["yv", ("mv", h), "rstd4"], writes=[("yn", h)])
                        P.op("pool", lambda e, h=h, hs=hs, i=i: e.tensor_tensor(out=reto[:, i * 512 + h * 128:i * 512 + (h + 1) * 128], in0=yv[:, hs], in1=sg[:, hs], op=ALU.mult),
                             reads=[("yn", h), "sg"], writes=["reto"])
                    proj(B_NQ, p_mm[0], ("p_mm", 0), hT[b], hkey)
                    P.op("act", lambda e: e.copy(out=pj[0][:], in_=p_mm[0][:]), reads=[("p_mm", 0)], writes=[("pj", 0)])
                    rope(pj[0][:], nq[:], 8, 64, cos64, sin64, [("pj", 0), ("rp", b)], "nq")
                    for h in range(4):
                        P.op("pe", lambda e, h=h: e.transpose(p_tr[:, h * 128:(h + 1) * 128], nq[:, h * 128:(h + 1) * 128], identb[:]),
                             reads=["nq_lo", "nq_hi"], writes=["p_tr"])
                    P.op("act", lambda e, i=i: e.copy(out=qTs[:, i * 512:(i + 1) * 512], in_=p_tr[:, 0:512]), reads=["p_tr"], writes=["qTs"])
                    proj(B_GT, p_mm[1], ("p_mm", 1), hT[b], hkey)
                    P.op("act", lambda e, i=i: e.activation(out=gts[:, i * 24:(i + 1) * 24], in_=p_mm[1][:, 0:24], func=AF.Sigmoid),
                         reads=[("p_mm", 1)], writes=["gts"])
                for h in range(4):
                    hs = slice(h * 128, (h + 1) * 128)
                    P.op("pe", lambda e, hs=hs: e.matmul(p_kv[:, hs], lhsT=ktok[:, hs], rhs=vz[:, hs], start=True, stop=True),
                         reads=["ktok_lo", "ktok_hi", ("vz", 0), ("vz", 1), ("vz", 2), ("vz", 3)], writes=["p_kv"])
                for h in range(4):
                    hs = slice(h * 128, (h + 1) * 128)
                    P.op("dve", lambda e, h=h, hs=hs: e.scalar_tensor_tensor(out=Sst[:, hs], in0=Sst[:, hs], scalar=float(np.exp(128 * LOGG[h])), in1=p_kv[:, hs],
                                                                            op0=ALU.mult, op1=ALU.add), reads=["p_kv", "Sst"], writes=["Sst"])
                P.op("act", lambda e: e.copy(out=Sb[:], in_=Sst[:]), reads=["Sst"], writes=["Sb"])
            P.emit()

        for st in _blk(cfg.stop >= 2):
            sb = lambda name, shape, dt=F32: st.enter_context(nc.sbuf_tensor(name, shape, dt))
            ps = lambda name, shape, dt=F32: st.enter_context(nc.psum_tensor(name, shape, dt))
            P = Prog(nc, SEM)
            NBP = NCH * 128
            raw = [sb("raw%d" % k, [128, RAWLEN], BF16) for k in range(2)]
            w1s = sb("w1s", [128, 32 * 256]); w1b = [sb("w1b%d" % k, [128, 32 * 256], BF16) for k in range(2)]
            pes = sb("pes", [128, 32]); peb = [sb("peb%d" % k, [128, 32], BF16) for k in range(2)]
            b1s = [sb("b1s%d" % k, [128, 2]) for k in range(2)]
            w2s = sb("w2s", [128, 128]); w2b = [sb("w2b%d" % k, [128, 128], BF16) for k in range(2)]
            cb = sb("cb", [128, 2])
            u = sb("u", [128, 512]); u2 = sb("u2", [128, 512]); u3 = sb("u3", [128, 512])
            hid = [sb("hid%d" % i, [128, 512], BF16) for i in range(2)]
            ovl = sb("ovl", [128, NCH * 128], BF16)
            p_h = ps("p_h", [128, 512]); p_c = ps("p_c", [128, 2]); p_o = ps("p_o", [128, 512])
            P.dma("sp", ovl[:], tb["overlap"], writes=["ovl"])
            P.op("pool", lambda e: e.memset(vca[:], 1.0), writes=["vca"])
            for cc_ in range(NCH):
                for g in range(2):
                    P.op("pool", lambda e, cc_=cc_, g=g: e.tensor_copy(out=vca4[:, cc_, g, 65:193], in_=ovl[:, cc_ * 128:(cc_ + 1) * 128]), reads=["ovl"], writes=["vca"])
            for kind, (w1d, ped, b1d, w2d) in enumerate([(w1k, peTk, b1k, w2k), (w1v, peTv, b1v, w2v)]):
                P.op("pool", lambda e, kind=kind: e.memset(raw[kind][:], 0.0), writes=[("raw", kind)])
                P.dma("sp", raw[kind][:, 0:S], rawd[kind], writes=[("raw", kind)])
                for half in range(2):
                    P.dma("sp", w1s[half * 64:(half + 1) * 64, :], w1d, writes=["w1s"])
                    P.dma("sp", pes[half * 64:(half + 1) * 64, :], ped, writes=["pes"])
                P.op("act", lambda e, kind=kind: e.copy(out=w1b[kind][:], in_=w1s[:]), reads=["w1s"], writes=[("w1b", kind)])
                P.op("dve", lambda e, kind=kind: e.tensor_copy(out=peb[kind][:], in_=pes[:]), reads=["pes"], writes=[("peb", kind)])
                P.dma("sp", b1s[kind][:], b1d, writes=[("b1s", kind)])
                P.dma("sp", w2s[:], w2d, writes=["w2s"])
                P.op("dve", lambda e, kind=kind: e.tensor_copy(out=w2b[kind][:], in_=w2s[:]), reads=["w2s"], writes=[("w2b", kind)])
                w13 = w1b[kind][:].rearrange("p (l n) -> p l n", l=32)
                for hc in range(2):
                    for l in range(32):
                        P.op("pe", lambda e, hc=hc, l=l, w13=w13, kind=kind: e.matmul(p_c[:, hc:hc + 1], lhsT=w13[0:64, l, hc * 128:(hc + 1) * 128], rhs=peb[kind][0:64, l:l + 1],
                                                                                    start=(l == 0), stop=(l == 31)), reads=[("w1b", kind), ("peb", kind)], writes=["p_c"])
                P.op("dve", lambda e, kind=kind: e.tensor_tensor(out=cb[:], in0=p_c[:], in1=b1s[kind][:], op=ALU.add), reads=["p_c", ("b1s", kind)], writes=["cb"])
                for g in range(2):
                    gs = slice(64 * g, 64 * g + 64)
                    for n0 in range(0, NBP, 512):
                        nn = min(512, NBP - n0)
                        for hc in range(2):
                            for l in range(32):
                                rhs = raw[kind][gs, l + 16 * n0: l + 16 * (n0 + nn): 16]
                                P.op("pe", lambda e, hc=hc, l=l, rhs=rhs, gs=gs, w13=w13, nn=nn: e.matmul(
                                    p_h[:, 0:nn], lhsT=w13[gs, l, hc * 128:(hc + 1) * 128], rhs=rhs, start=(l == 0), stop=(l == 31)),
                                    reads=[("w1b", kind), ("raw", kind)], writes=["p_h"])
                            P.op("act", lambda e, hc=hc, nn=nn: e.activation(out=u[:, 0:nn], in_=p_h[:, 0:nn], func=AF.Identity, bias=cb[:, hc:hc + 1]),
                                 reads=["p_h", "cb"], writes=["u"])
                            P.op("act", lambda e, nn=nn: e.activation(out=u2[:, 0:nn], in_=u[:, 0:nn], func=AF.Square), reads=["u"], writes=["u2"])
                            P.op("dve", lambda e, nn=nn: e.tensor_scalar(out=u2[:, 0:nn], in0=u2[:, 0:nn], scalar1=0.044715, scalar2=1.0, op0=ALU.mult, op1=ALU.add),
                                 reads=["u2"], writes=["u2"])
                            P.op("dve", lambda e, nn=nn: e.tensor_tensor(out=u3[:, 0:nn], in0=u2[:, 0:nn], in1=u[:, 0:nn], op=ALU.mult), reads=["u2", "u"], writes=["u3"])
                            P.op("act", lambda e, nn=nn: e.activation(out=u3[:, 0:nn], in_=u3[:, 0:nn], func=AF.Sigmoid, scale=1.5957691216), reads=["u3"], writes=["u3"])
                            P.op("dve", lambda e, hc=hc, nn=nn: e.tensor_tensor(out=hid[hc][:, 0:nn], in0=u3[:, 0:nn], in1=u[:, 0:nn], op=ALU.mult),
                                 reads=["u3", "u"], writes=[("hid", hc)])
                        w23 = w2b[kind][:].rearrange("p (c d) -> p c d", c=2)
                        if kind == 0:
                            for hc in range(2):
                                P.op("pe", lambda e, hc=hc, gs=gs, nn=nn, w23=w23: e.matmul(p_o[gs, 0:nn], lhsT=w23[:, hc, :], rhs=hid[hc][:, 0:nn], start=(hc == 0), stop=(hc == 1)),
                                     reads=[("hid", 0), ("hid", 1), ("w2b", 0)], writes=["p_o"])
                            P.op("act", lambda e, gs=gs, n0=n0, nn=nn: e.copy(out=kcT[gs, n0:n0 + nn], in_=p_o[gs, 0:nn]), reads=["p_o"], writes=["kcT"])
                        else:
                            for c4 in range(nn // 128):
                                for hc in range(2):
                                    P.op("pe", lambda e, hc=hc, c4=c4, w23=w23: e.matmul(p_o[:, c4 * 64:(c4 + 1) * 64], lhsT=hid[hc][:, c4 * 128:(c4 + 1) * 128], rhs=w23[:, hc, :],
                                                                                        start=(hc == 0), stop=(hc == 1)), reads=[("hid", 0), ("hid", 1), ("w2b", 1)], writes=["p_o"])
                                P.op("act", lambda e, c4=c4, g=g, n0=n0: e.copy(out=vca4[:, n0 // 128 + c4, g, 0:64], in_=p_o[:, c4 * 64:(c4 + 1) * 64]),
                                     reads=["p_o"], writes=["vca"])
            P.emit()

        for st in _blk(cfg.stop >= 3):
            sb = lambda name, shape, dt=F32: st.enter_context(nc.sbuf_tensor(name, shape, dt))
            ps = lambda name, shape, dt=F32: st.enter_context(nc.psum_tensor(name, shape, dt))
            P = Prog(nc, SEM)
            wob = sb("wob", [128, 8 * 1024], BF16)
            wob3 = wob[:].rearrange("p (k n) -> p k n", k=8)
            stg = [sb("stgc%d" % i, [128, 1024]) for i in range(2)]
            for kc in range(8):
                P.dma("sp", stg[kc % 2][:], w_out[kc * 128:(kc + 1) * 128, :], writes=[("stg", kc % 2)])
                P.op("pool", lambda e, kc=kc: e.tensor_copy(out=wob3[:, kc, :], in_=stg[kc % 2][:]), reads=[("stg", kc % 2)], writes=["wob"])
            expd = sb("expd", [128, S], BF16); tri = sb("tri", [128, 1024], BF16)
            kbias = sb("kbias", [128, NW]); cbias = sb("cbias", [128, NCH])
            cpen = [sb("cpen%d" % i, [128, NCH * 512], BF16) for i in range(2)]
            selm = [sb("selm%d" % i, [128, 256]) for i in range(2)]
            xo = [sb("xo%d" % i, [128, 1024]) for i in range(2)]
            eb = [sb("eb%d" % i, [128, 512], BF16) for i in range(4)]
            imp = sb("imp", [128, 128]); impm = sb("impm", [128, 128]); r1 = sb("r1", [128, 128]); r2 = sb("r2", [128, 128])
            m8 = sb("m8", [128, 16]); seln = sb("seln", [128, 128], BF16); selT = sb("selT", [128, 512], BF16)
            den = sb("den", [128, 4]); coef = sb("coef", [128, 4])
            nsa = sb("nsa", [128, 512])
            cat = sb("cat", [128, 1024], BF16); catT = sb("catT", [128, 1024], BF16)
            x1t = [sb("x1t%d" % i, [128, 1024]) for i in range(2)]
            p_s = [ps("p_s%d" % i, [128, 512]) for i in range(2)]
            p_a = [ps("p_a%d" % i, [128, 512]) for i in range(4)]
            p_x = ps("p_x", [128, 1024])
            GA1 = sb("GA1", [128, 1024])
            bcast_rows(P, sb, [(p_s[0], ("p_s", 0)), (p_s[1], ("p_s", 1))], [(4, GA1, "GA1")])
            RP = 2
            NCK = cfg.NEXP // (128 * RP)
            cin = [sb("cin%d" % i_, [128, RP * 1024]) for i_ in range(2)]
            cob = [sb("cob%d" % i_, [128, RP * 1024], BF16) for i_ in range(2)]
            cast_jobs = []
            for src_, dst_ in ((pdown, pdb), (pup, pub)):
                srcv = src_.rearrange("(c p j) d -> c p (j d)", p=128, j=RP)
                dstv = dst_.rearrange("(c p j) d -> c p (j d)", p=128, j=RP)
                for c_ in range(NCK):
                    cast_jobs.append((srcv[c_], dstv[c_]))
            cast_n = [0]

            def emit_casts(n):
                for _ in range(n):
                    if cast_n[0] >= len(cast_jobs):
                        return
                    src1, dst1 = cast_jobs[cast_n[0]]
                    k_ = cast_n[0] % 2
                    P.dma("sp", cin[k_][:], src1, writes=[("cin", k_)])
                    eng = "pool" if cast_n[0] % 2 else "dve"
                    P.op(eng, lambda e, k_=k_: e.tensor_copy(out=cob[k_][:], in_=cin[k_][:]), reads=[("cin", k_)], writes=[("cob", k_)])
                    P.dma("sp", dst1, cob[k_][:], reads=[("cob", k_)])
                    cast_n[0] += 1

            P.dma("sp", expd[:], tb["expand"], writes=["expd"])
            P.dma("sp", tri[:], tb["tri"], writes=["tri"])
            P.dma("sp", kbias[:], tb["keybias"], writes=["kbias"])
            P.dma("sp", cbias[:], tb["cmpbias"], writes=["cbias"])
            p_xb = p_x[:].bitcast(BF16)
            gts4 = gts[:].rearrange("p (i g h c) -> p i g h c", g=2, h=4, c=3)
            nsa4 = nsa[:].rearrange("p (g h d) -> p g h d", g=2, h=4)
            sc_i = [0]
            eb_i = [0]
            p_sa = [(p_s[0][:], ("p_s", 0)), (p_s[1][:], ("p_s", 1)), (p_x[:, 0:512], ("p_x", 0)), (p_x[:, 512:1024], ("p_x", 1))]

            def attend(i, g, chunks, first_branch, br):
                W = chunks[0]["v"].shape[-1]
                nchk = len(chunks)
                qg = qTs[64 * g:64 * g + 64, i * 512:(i + 1) * 512]
                pbs = []

                def stage_s(ci):
                    ch = chunks[ci]
                    pb = sc_i[0] % 4; sc_i[0] += 1
                    pbs.append(pb)
                    pst, pkey_ = p_sa[pb]
                    npen = len(ch["pens"])
                    P.op("pe", lambda e, ch=ch, pst=pst, npen=npen: e.matmul(pst[:], lhsT=ch["lhsT"], rhs=qg, start=True, stop=(npen == 0)),
                         reads=ch["keys_r"] + ["qTs"], writes=[pkey_])
                    for pi, (pl, pr, pk) in enumerate(ch["pens"]):
                        P.op("pe", lambda e, pl=pl, pr=pr, pst=pst, last=(pi == npen - 1): e.matmul(
                            pst[:], lhsT=pl, rhs=pr, start=False, stop=last), reads=pk, writes=[pkey_])

                def stage_ev(ci):
                    ch = chunks[ci]
                    pb = pbs[ci]
                    pst, pkey_ = p_sa[pb]
                    ei = eb_i[0] % 4; eb_i[0] += 1
                    et = eb[ei]
                    if ch["bias"] is None:
                        P.op("act", lambda e, et=et, pst=pst: e.activation(out=et[:], in_=pst[:], func=AF.Exp, scale=0.125), reads=[pkey_], writes=[("eb", ei)])
                    else:
                        P.op("act", lambda e, et=et, pst=pst, ch=ch: e.activation(out=et[:], in_=pst[:], func=AF.Exp, scale=0.125, bias=ch["bias"]),
                             reads=[pkey_, "kbias", "cbias"], writes=[("eb", ei)])
                    for h in range(4):
                        P.op("pe", lambda e, h=h, et=et, ch=ch, ci=ci: e.matmul(p_a[h][:, 0:W], lhsT=et[:, h * 128:(h + 1) * 128], rhs=ch["v"],
                                                                              start=(ci == 0), stop=(ci == nchk - 1)),
                             reads=[("eb", ei)] + ch["keys_v"], writes=[("p_a", h)])

                stage_s(0)
                if nchk > 1:
                    stage_s(1)
                for ci in range(nchk):
                    if ci + 2 < nchk:
                        stage_s(ci + 2)
                    stage_ev(ci)
                for h in range(4):
                    P.op("dve", lambda e, h=h: e.tensor_scalar(out=den[:, h:h + 1], in0=p_a[h][:, 64:65], scalar1=1e-30, scalar2=None, op0=ALU.max),
                         reads=[("p_a", h)], writes=[("den", h)])
                    P.op("dve", lambda e, h=h: e.reciprocal(out=den[:, h:h + 1], in_=den[:, h:h + 1]), reads=[("den", h)], writes=[("den", h)])
                    P.op("dve", lambda e, h=h: e.tensor_tensor(out=coef[:, h:h + 1], in0=den[:, h:h + 1], in1=gts4[:, i, g, h, br:br + 1], op=ALU.mult),
                         reads=[("den", h), "gts"], writes=[("coef", h)])
                    if first_branch:
                        P.op("dve", lambda e, h=h: e.tensor_scalar(out=nsa4[:, g, h, :], in0=p_a[h][:, 0:64], scalar1=coef[:, h:h + 1], scalar2=None, op0=ALU.mult),
                             reads=[("p_a", h), ("coef", h)], writes=[("nsa", g, h)])
                    else:
                        P.op("dve", lambda e, h=h: e.scalar_tensor_tensor(out=nsa4[:, g, h, :], in0=p_a[h][:, 0:64], scalar=coef[:, h:h + 1], in1=nsa4[:, g, h, :],
                                                                         op0=ALU.mult, op1=ALU.add),
                             reads=[("p_a", h), ("coef", h), ("nsa", g, h)], writes=[("nsa", g, h)])

            CPT = -(-len(cast_jobs) // (NO * 6))
            for i in range(NO):
                T = T0 + i
                b = i % 2
                P.dma("sp", cpen[b][:], tb["cmppen"][i * 128:(i + 1) * 128, :], writes=[("cpen", b)])
                P.dma("sp", selm[b][:], tb["selm"][i * 128:(i + 1) * 128, :], writes=[("selm", b)])
                P.dma("sp", xo[b][:], xw[T * 128:(T + 1) * 128, :], writes=[("xo", b)])
                for g in range(2):
                    gs = slice(64 * g, 64 * g + 64)
                    chunks = []
                    for c_ in range(NCH):
                        chunks.append(dict(lhsT=kcT[gs, c_ * 128:(c_ + 1) * 128], keys_r=["kcT"],
                                           pens=[(identb[:], cpen[b][:, c_ * 512:(c_ + 1) * 512], [("cpen", b), "identb"])],
                                           bias=cbias[:, c_:c_ + 1], v=vca4[:, c_, g, :], keys_v=["vca"]))
                    emit_casts(CPT)
                    attend(i, g, chunks, True, 0)
                    for h in range(4):
                        if h == 0:
                            P.op("dve", lambda e: e.tensor_scalar(out=imp[:], in0=p_a[0][:, 65:193], scalar1=den[:, 0:1], scalar2=None, op0=ALU.mult),
                                 reads=[("p_a", 0), ("den", 0)], writes=["imp"])
                        else:
                            P.op("dve", lambda e, h=h: e.scalar_tensor_tensor(out=imp[:], in0=p_a[h][:, 65:193], scalar=den[:, h:h + 1], in1=imp[:], op0=ALU.mult, op1=ALU.add),
                                 reads=[("p_a", h), ("den", h), "imp"], writes=["imp"])
                    P.op("dve", lambda e, b=b: e.tensor_tensor(out=impm[:], in0=imp[:], in1=selm[b][:, 0:128], op=ALU.mult), reads=["imp", ("selm", b)], writes=["impm"])
                    P.op("dve", lambda e, b=b: e.tensor_tensor(out=impm[:], in0=impm[:], in1=selm[b][:, 128:256], op=ALU.add), reads=["impm", ("selm", b)], writes=["impm"])
                    P.op("dve", lambda e: e.max(out=m8[:, 0:8], in_=impm[:]), reads=["impm"], writes=["m8a"])
                    P.op("dve", lambda e: e.match_replace(out=r1[:], in_to_replace=m8[:, 0:8], in_values=impm[:], imm_value=-BIG), reads=["impm", "m8a"], writes=["r1"])
                    P.op("dve", lambda e: e.max(out=m8[:, 8:16], in_=r1[:]), reads=["r1"], writes=["m8b"])
                    P.op("dve", lambda e: e.match_replace(out=r2[:], in_to_replace=m8[:, 8:16], in_values=r1[:], imm_value=-BIG), reads=["r1", "m8b"], writes=["r2"])
                    P.op("dve", lambda e: e.tensor_tensor(out=r1[:], in0=impm[:], in1=r2[:], op=ALU.subtract), reads=["impm", "r2"], writes=["r1"])
                    P.op("dve", lambda e: e.tensor_scalar(out=seln[:], in0=r1[:], scalar1=0.0, scalar2=NEG, op0=ALU.is_le, op1=ALU.mult), reads=["r1"], writes=["seln"])
                    P.op("pe", lambda e: e.transpose(p_xb[:, 0:128], seln[:], identb[:]), reads=["seln"], writes=[("p_x", 0)])
                    P.op("dve", lambda e: e.tensor_copy(out=selT[:].rearrange("p (h t) -> p h t", h=4), in_=p_xb[:, 0:128].unsqueeze(1).to_broadcast([128, 4, 128])),
                         reads=[("p_x", 0)], writes=["selT"])
                    chunks = []
                    for c_ in range(T + 1):
                        pens = [(expd[:, c_ * 128:(c_ + 1) * 128], selT[:], ["expd", "selT"])]
                        if c_ == T:
                            pens.append((identb[:], tri[:, 0:512], ["tri", "identb"]))
                        chunks.append(dict(lhsT=ksT[gs, c_ * 128:(c_ + 1) * 128], keys_r=["ksT"], pens=pens, bias=None, v=vs4[:, c_, g, :], keys_v=["vs"]))
                    emit_casts(CPT)
                    attend(i, g, chunks, False, 1)
                    chunks = []
                    for c_ in range(max(0, T - 4), T + 1):
                        wk = c_ - (NW - NWK)
                        pens = []
                        if c_ == T - 4:
                            pens.append((identb[:], tri[:, 512:1024], ["tri", "identb"]))
                        if c_ == T:
                            pens.append((identb[:], tri[:, 0:512], ["tri", "identb"]))
                        chunks.append(dict(lhsT=kwT[gs, wk * 128:(wk + 1) * 128], keys_r=["kwT"], pens=pens, bias=kbias[:, c_:c_ + 1], v=vw4[:, wk, g, :], keys_v=["vw"]))
                    emit_casts(CPT)
                    attend(i, g, chunks, False, 2)
                P.op("act", lambda e, i=i: e.copy(out=cat[:, 0:512], in_=reto[:, i * 512:(i + 1) * 512]), reads=["reto"], writes=["cat_a"])
                P.op("act", lambda e: e.copy(out=cat[:, 512:1024], in_=nsa[:]), reads=[("nsa", g_, h_) for g_ in range(2) for h_ in range(4)], writes=["cat_b"])
                for kc in range(8):
                    P.op("pe", lambda e, kc=kc: e.transpose(p_xb[:, kc * 128:(kc + 1) * 128], cat[:, kc * 128:(kc + 1) * 128], identb[:]),
                         reads=["cat_a", "cat_b"], writes=[("p_x", 0)])
                P.op("act", lambda e: e.copy(out=catT[:], in_=p_xb[:, 0:1024]), reads=[("p_x", 0)], writes=["catT"])
                for half in range(2):
                    for kc in range(8):
                        P.op("pe", lambda e, kc=kc, half=half: e.matmul(p_x[:, half * 512:(half + 1) * 512], lhsT=catT[:, kc * 128:(kc + 1) * 128],
                                                                        rhs=wob3[:, kc, half * 512:(half + 1) * 512], start=(kc == 0), stop=(kc == 7)),
                             reads=["catT", "wob"], writes=[("p_x", half)])
                P.op("dve", lambda e, b=b: e.tensor_tensor(out=x1t[b][:], in0=p_x[:], in1=GA1[:], op=ALU.mult), reads=[("p_x", 0), ("p_x", 1), "GA1"], writes=[("x1t", b)])
                P.op("pool", lambda e, b=b: e.tensor_tensor(out=x1t[b][:], in0=x1t[b][:], in1=xo[b][:], op=ALU.add), reads=[("x1t", b), ("xo", b)], writes=[("x1t", b)])
                P.dma("sp", x1d[i * 128:(i + 1) * 128, :], x1t[b][:], reads=[("x1t", b)])
            emit_casts(len(cast_jobs))
            P.emit()

        mid.close()
        for st in _blk(cfg.stop >= 4):
            sb = lambda name, shape, dt=F32: st.enter_context(nc.sbuf_tensor(name, shape, dt))
            ps = lambda name, shape, dt=F32: st.enter_context(nc.psum_tensor(name, shape, dt))
            P = Prog(nc, SEM)
            wqb = sb("wqb", [128, 8 * 2048], BF16)
            wqb3 = wqb[:].rearrange("p (k n) -> p k n", k=8)
            stg = [sb("stgd%d" % i, [128, 2048]) for i in range(2)]
            for kc in range(8):
                P.dma("sp", stg[kc % 2][:], w_pq[kc * 128:(kc + 1) * 128, :], writes=[("stg", kc % 2)])
                P.op("act", lambda e, kc=kc: e.copy(out=wqb3[:, kc, :], in_=stg[kc % 2][:]), reads=[("stg", kc % 2)], writes=["wqb"])
            A2R = sb("A2R", [128, 1024]); B2R = sb("B2R", [128, 1024]); GA2 = sb("GA2", [128, 1024]); GF = sb("GF", [128, 1024])
            P.dma("sp", GF[:], gfin.partition_broadcast(128), writes=["GF"])
            skb = sb("skb", [128, 2048], BF16)
            P.dma("sp", stg[0][:], skT, writes=[("stg", 0)])
            P.op("act", lambda e: e.copy(out=skb[:], in_=stg[0][:]), reads=[("stg", 0)], writes=["skb"])
            selrow = sb("selrow", [128, 128 * 128], BF16)
            P.dma("sp", selrow[:], tb["selrow"], writes=["selrow"])
            selrow3 = selrow[:].rearrange("p (t m) -> p t m", t=128)
            io16 = sb("io16", [128, 16])
            P.dma("sp", io16[:], tb["iota16"], writes=["io16"])
            io128 = sb("io128", [128, 128]); cm = [sb("cm%d" % i_, [128, 128], BF16) for i_ in range(3)]
            P.dma("sp", io128[:], tb["iota128"], writes=["io128"])
            x1 = sb("x1", [128, 1024]); sq = sb("sqd", [128, 1024]); ss = sb("ssd", [128, 1]); rs = sb("rsd", [128, 1])
            tmod = sb("tmodd", [128, 1024]); h2b = sb("h2b", [128, 1024], BF16); h2T = sb("h2T", [128, 1024], BF16)
            qT = sb("qT", [128, 2048], BF16)
            s_sb = sb("s_sb", [128, 2048]); s2 = sb("s2", [128, 128])
            V16 = sb("V16", [128, 256]); I16 = sb("I16", [128, 256], U32); I16f = sb("I16f", [128, 256])
            cand = sb("cand", [128, 2048]); c2 = sb("c2", [128, 256])
            TV = sb("TV", [128, 128]); TJ = sb("TJ", [128, 128], U32); TA = sb("TA", [128, 128], U32); TBb = sb("TBb", [128, 128], U32)
            TAf = sb("TAf", [128, 128]); TBf = sb("TBf", [128, 128])
            oh = sb("oh", [128, 2048]); i0 = sb("i0", [128, 128]); i1 = sb("i1", [128, 128]); ef = sb("ef", [128, 128])
            ex = sb("ex", [128, 128]); sm = sb("sm", [128, 8]); Wt = sb("Wt", [128, 128])
            idxT = sb("idxT", [128, 128], U32); WT = sb("WT", [128, 128]); aT = sb("aT", [128, 128]); cT = sb("cT", [128, 128])
            g2 = sb("g2", [128, 128]); g3 = sb("g3", [128, 128])
            NU = 8
            U = [sb("U%d" % i, [128, 1024], BF16) for i in range(NU)]
            jk = sq
            oT = tmod; x2 = sb("x2", [128, 1024]); yo = sb("yo", [128, 1024])
            p_q = ps("p_q", [128, 512]); p_s4 = ps("p_s4", [128, 2048]); p_hb = ps("p_hb", [128, 1024]); p_t = ps("p_t", [128, 512])
            bcast_rows(P, sb, [(p_q, "p_q"), (p_t, "p_t")], [(2, A2R, "A2R"), (3, B2R, "B2R"), (5, GA2, "GA2")])
            V4 = V16[:].rearrange("p (c k) -> p c k", k=16); I4 = I16[:].rearrange("p (c k) -> p c k", k=16)
            s3 = s_sb[:].rearrange("p (c n) -> p c n", n=128)
            for i in range(NO):
                P.dma("sp", x1[:], x1d[i * 128:(i + 1) * 128, :], writes=["x1"])
                P.op("act", lambda e: e.activation(out=sq[:], in_=x1[:], func=AF.Square), reads=["x1"], writes=["sq"])
                P.op("dve", lambda e: e.reduce_sum(out=ss[:], in_=sq[:], axis=AX.X), reads=["sq"], writes=["ss"])
                P.op("act", lambda e: e.activation(out=rs[:], in_=ss[:], func=AF.Sqrt, scale=1.0 / 1024, bias=epst[:]), reads=["ss"], writes=["rs"])
                P.op("dve", lambda e: e.reciprocal(out=rs[:], in_=rs[:]), reads=["rs"], writes=["rs"])
                P.op("dve", lambda e: e.scalar_tensor_tensor(out=tmod[:], in0=x1[:], scalar=rs[:], in1=A2R[:], op0=ALU.mult, op1=ALU.mult), reads=["x1", "rs", "A2R"], writes=["tmod"])
                P.op("pool", lambda e: e.tensor_tensor(out=h2b[:], in0=tmod[:], in1=B2R[:], op=ALU.add), reads=["tmod", "B2R"], writes=["h2b"])
                p_tb = p_t[:].bitcast(BF16)
                for kc in range(8):
                    P.op("pe", lambda e, kc=kc: e.transpose(p_tb[:, kc * 128:(kc + 1) * 128], h2b[:, kc * 128:(kc + 1) * 128], identb[:]), reads=["h2b"], writes=["p_t"])
                P.op("act", lambda e: e.copy(out=h2T[:], in_=p_tb[:, 0:1024]), reads=["p_t"], writes=["h2T"])
                for c4 in range(4):
                    for cq in range(4):
                        ch = c4 * 4 + cq
                        for kc in range(8):
                            P.op("pe", lambda e, ch=ch, cq=cq, kc=kc: e.matmul(p_q[:, cq * 128:(cq + 1) * 128], lhsT=wqb3[:, kc, ch * 128:(ch + 1) * 128],
                                                                              rhs=h2T[:, kc * 128:(kc + 1) * 128], start=(kc == 0), stop=(kc == 7)),
                                 reads=["wqb", "h2T"], writes=["p_q"])
                    P.op("act", lambda e, c4=c4: e.copy(out=qT[:, c4 * 512:(c4 + 1) * 512], in_=p_q[:]), reads=["p_q"], writes=[("qT", c4)])
                for ch in range(16):
                    P.op("pe", lambda e, ch=ch: e.matmul(p_s4[:, ch * 128:(ch + 1) * 128], lhsT=qT[:, ch * 128:(ch + 1) * 128], rhs=skb[:, ch * 128:(ch + 1) * 128],
                                                        start=True, stop=True), reads=[("qT", ch // 4), "skb"], writes=[("p_s4", ch // 8)])
                for c4 in range(4):
                    P.op("act", lambda e, c4=c4: e.copy(out=s_sb[:, c4 * 512:(c4 + 1) * 512], in_=p_s4[:, c4 * 512:(c4 + 1) * 512]), reads=[("p_s4", c4 // 2)], writes=[("s_sb", c4)])
                for ch in range(16):
                    sk = ("s_sb", ch // 4)
                    P.op("dve", lambda e, ch=ch: e.max(out=V4[:, ch, 0:8], in_=s3[:, ch, :]), reads=[sk], writes=[("V", ch, 0)])
                    P.op("dve", lambda e, ch=ch: e.max_index(out=I4[:, ch, 0:8], in_max=V4[:, ch, 0:8], in_values=s3[:, ch, :]), reads=[sk, ("V", ch, 0)], writes=[("I", ch, 0)])
                    P.op("dve", lambda e, ch=ch: e.match_replace(out=s2[:], in_to_replace=V4[:, ch, 0:8], in_values=s3[:, ch, :], imm_value=-BIG), reads=[sk, ("V", ch, 0)], writes=["s2"])
                    P.op("dve", lambda e, ch=ch: e.max(out=V4[:, ch, 8:16], in_=s2[:]), reads=["s2"], writes=[("V", ch, 1)])
                    P.op("dve", lambda e, ch=ch: e.max_index(out=I4[:, ch, 8:16], in_max=V4[:, ch, 8:16], in_values=s2[:]), reads=["s2", ("V", ch, 1)], writes=[("I", ch, 1)])
                allV = [("V", ch, k_) for ch in range(16) for k_ in range(2)]
                allI = [("I", ch, k_) for ch in range(16) for k_ in range(2)]
                P.op("dve", lambda e: e.tensor_copy(out=I16f[:], in_=I16[:]), reads=allI, writes=["I16f"])
                cand4 = cand[:].rearrange("p (h a b) -> p h a b", a=16, b=16)
                V5 = V16[:].rearrange("p (h t k) -> p h t k", t=2, k=16)
                I5 = I16f[:].rearrange("p (h t k) -> p h t k", t=2, k=16)
                for hd in range(8):
                    P.op("dve", lambda e, hd=hd: e.tensor_tensor(out=cand4[:, hd], in0=V5[:, hd, 0, :].unsqueeze(2).to_broadcast([128, 16, 16]),
                                                                 in1=V5[:, hd, 1, :].unsqueeze(1).to_broadcast([128, 16, 16]), op=ALU.add),
                         reads=allV, writes=[("cand", hd)])
                    cd = cand[:, hd * 256:(hd + 1) * 256]
                    P.op("dve", lambda e, hd=hd, cd=cd: e.max(out=TV[:, hd * 16:hd * 16 + 8], in_=cd), reads=[("cand", hd)], writes=[("TV", hd, 0)])
                    P.op("dve", lambda e, hd=hd, cd=cd: e.max_index(out=TJ[:, hd * 16:hd * 16 + 8], in_max=TV[:, hd * 16:hd * 16 + 8], in_values=cd),
                         reads=[("cand", hd), ("TV", hd, 0)], writes=[("TJ", hd, 0)])
                    P.op("dve", lambda e, hd=hd, cd=cd: e.match_replace(out=c2[:], in_to_replace=TV[:, hd * 16:hd * 16 + 8], in_values=cd, imm_value=-BIG),
                         reads=[("cand", hd), ("TV", hd, 0)], writes=["c2"])
                    P.op("dve", lambda e, hd=hd: e.max(out=TV[:, hd * 16 + 8:hd * 16 + 16], in_=c2[:]), reads=["c2"], writes=[("TV", hd, 1)])
                    P.op("dve", lambda e, hd=hd: e.max_index(out=TJ[:, hd * 16 + 8:hd * 16 + 16], in_max=TV[:, hd * 16 + 8:hd * 16 + 16], in_values=c2[:]),
                         reads=["c2", ("TV", hd, 1)], writes=[("TJ", hd, 1)])
                allTV = [("TV", hd, k_) for hd in range(8) for k_ in range(2)]
                allTJ = [("TJ", hd, k_) for hd in range(8) for k_ in range(2)]
                TV3 = TV[:].rearrange("p (h k) -> p h k", k=16)
                ex3 = ex[:].rearrange("p (h k) -> p h k", k=16)
                P.op("dve", lambda e: e.tensor_tensor(out=ex3, in0=TV3, in1=TV3[:, :, 0:1].to_broadcast([128, 8, 16]), op=ALU.subtract), reads=allTV, writes=["ex"])
                P.op("act", lambda e: e.activation(out=ex[:], in_=ex[:], func=AF.Exp), reads=["ex"], writes=["ex"])
                P.op("dve", lambda e: e.tensor_reduce(out=sm[:], in_=ex3, axis=AX.X, op=ALU.add), reads=["ex"], writes=["sm"])
                P.op("dve", lambda e: e.reciprocal(out=sm[:], in_=sm[:]), reads=["sm"], writes=["sm"])
                P.op("dve", lambda e: e.tensor_tensor(out=Wt[:].rearrange("p (h k) -> p h k", k=16), in0=ex3, in1=sm[:].unsqueeze(2).to_broadcast([128, 8, 16]), op=ALU.mult),
                     reads=["ex", "sm"], writes=["Wt"])
                P.op("dve", lambda e: e.tensor_single_scalar(out=TA[:], in_=TJ[:], scalar=4, op=ALU.logical_shift_right), reads=allTJ, writes=["TA"])
                P.op("dve", lambda e: e.tensor_single_scalar(out=TBb[:], in_=TJ[:], scalar=15, op=ALU.bitwise_and), reads=allTJ, writes=["TB"])
                P.op("dve", lambda e: e.tensor_copy(out=TAf[:], in_=TA[:]), reads=["TA"], writes=["TAf"])
                P.op("dve", lambda e: e.tensor_copy(out=TBf[:], in_=TBb[:]), reads=["TB"], writes=["TBf"])
                oh4 = oh[:].rearrange("p (h k a) -> p h k a", k=16, a=16)
                for which, (tf, dst) in enumerate([(TAf, i0), (TBf, i1)]):
                    tf3 = tf[:].rearrange("p (h k) -> p h k", k=16)
                    P.op("dve", lambda e, tf3=tf3: e.tensor_tensor(out=oh4, in0=tf3.unsqueeze(3).to_broadcast([128, 8, 16, 16]),
                                                                  in1=io16[:].unsqueeze(1).unsqueeze(1).to_broadcast([128, 8, 16, 16]), op=ALU.is_equal),
                         reads=["TAf", "TBf", "io16"], writes=["oh"])
                    P.op("dve", lambda e, which=which: e.tensor_tensor(out=oh4, in0=oh4, in1=I5[:, :, which, :].unsqueeze(2).to_broadcast([128, 8, 16, 16]), op=ALU.mult),
                         reads=["oh", "I16f"], writes=["oh"])
                    P.op("dve", lambda e, dst=dst: e.tensor_reduce(out=dst[:], in_=oh4, axis=AX.X, op=ALU.add), reads=["oh"], writes=["i01_%d" % which])
                P.op("dve", lambda e: e.scalar_tensor_tensor(out=ef[:], in0=i0[:], scalar=128.0, in1=i1[:], op0=ALU.mult, op1=ALU.add), reads=["i01_0", "i01_1"], writes=["ef"])
                P.op("pe", lambda e: e.transpose(p_t[:, 0:128], ef[:], identf[:]), reads=["ef"], writes=["p_t"])
                P.op("pe", lambda e: e.transpose(p_t[:, 128:256], Wt[:], identf[:]), reads=["Wt"], writes=["p_t"])
                P.op("dve", lambda e: e.tensor_copy(out=idxT[:], in_=p_t[:, 0:128]), reads=["p_t"], writes=["idxT"])
                P.op("dve", lambda e: e.tensor_copy(out=WT[:], in_=p_t[:, 128:256]), reads=["p_t"], writes=["WT"])
                for t_ in range(128):
                    ub = U[t_ % NU]
                    P.op("pool", lambda e, ub=ub, t_=t_: e.indirect_dma_start(out=ub[:], out_offset=None, in_=pdb,
                                                                             in_offset=bass.IndirectOffsetOnAxis(ap=idxT[:, t_:t_ + 1], axis=0)),
                         reads=["idxT"], writes=[("U", t_ % NU)], dma=True)
                    hbt, hbk = (p_hb[:], "p_hb") if t_ % 2 == 0 else (p_s4[:, 1024:2048], ("p_s4", 1))
                    for half in range(2):
                        P.op("pe", lambda e, t_=t_, half=half, hbt=hbt: e.matmul(hbt[:, half * 512:(half + 1) * 512], lhsT=selrow3[:, t_, :], rhs=h2b[:, half * 512:(half + 1) * 512],
                                                                                 start=True, stop=True), reads=["selrow", "h2b"], writes=[hbk])
                    P.op("dve", lambda e, ub=ub, t_=t_, hbt=hbt: e.tensor_tensor_reduce(out=jk[:], in0=ub[:], in1=hbt, scale=1.0, scalar=0.0, op0=ALU.mult, op1=ALU.add,
                                                                                       accum_out=aT[:, t_:t_ + 1]), reads=[("U", t_ % NU), hbk], writes=["sq", "aT"])
                P.op("act", lambda e: e.activation(out=g2[:], in_=aT[:], func=AF.Square), reads=["aT"], writes=["g2"])
                P.op("dve", lambda e: e.tensor_scalar(out=g2[:], in0=g2[:], scalar1=0.044715, scalar2=1.0, op0=ALU.mult, op1=ALU.add), reads=["g2"], writes=["g2"])
                P.op("dve", lambda e: e.tensor_tensor(out=g3[:], in0=g2[:], in1=aT[:], op=ALU.mult), reads=["g2", "aT"], writes=["g3"])
                P.op("act", lambda e: e.activation(out=g3[:], in_=g3[:], func=AF.Sigmoid, scale=1.5957691216), reads=["g3"], writes=["g3"])
                P.op("dve", lambda e: e.tensor_tensor(out=g3[:], in0=g3[:], in1=aT[:], op=ALU.mult), reads=["g3", "aT"], writes=["g3"])
                P.op("dve", lambda e: e.tensor_tensor(out=cT[:], in0=g3[:], in1=WT[:], op=ALU.mult), reads=["g3", "WT"], writes=["cT"])
                for t_ in range(128):
                    ub = U[t_ % NU]
                    P.op("pool", lambda e, ub=ub, t_=t_: e.indirect_dma_start(out=ub[:], out_offset=None, in_=pub,
                                                                             in_offset=bass.IndirectOffsetOnAxis(ap=idxT[:, t_:t_ + 1], axis=0)),
                         reads=["idxT"], writes=[("U", t_ % NU)], dma=True)
                    cmt = cm[t_ % 3]
                    P.op("dve", lambda e, cmt=cmt, t_=t_: e.scalar_tensor_tensor(out=cmt[:], in0=io128[:], scalar=float(t_), in1=cT[:, t_:t_ + 1].to_broadcast([128, 128]),
                                                                                op0=ALU.is_equal, op1=ALU.mult), reads=["io128", "cT"], writes=[("cm", t_ % 3)])
                    for half in range(2):
                        P.op("pe", lambda e, ub=ub, cmt=cmt, t_=t_, half=half: e.matmul(p_s4[:, half * 512:(half + 1) * 512], lhsT=cmt[:],
                                                                                      rhs=ub[:, half * 512:(half + 1) * 512], start=(t_ == 0), stop=(t_ == 127)),
                             reads=[("U", t_ % NU), ("cm", t_ % 3)], writes=[("p_s4", 0)])
                P.op("dve", lambda e: e.tensor_tensor(out=x2[:], in0=p_s4[:, 0:1024], in1=GA2[:], op=ALU.mult), reads=[("p_s4", 0), "GA2"], writes=["x2"])
                P.op("pool", lambda e: e.tensor_tensor(out=x2[:], in0=x2[:], in1=x1[:], op=ALU.add), reads=["x2", "x1"], writes=["x2"])
                P.op("act", lambda e: e.activation(out=sq[:], in_=x2[:], func=AF.Square), reads=["x2"], writes=["sq"])
                P.op("dve", lambda e: e.reduce_sum(out=ss[:], in_=sq[:], axis=AX.X), reads=["sq"], writes=["ss"])
                P.op("act", lambda e: e.activation(out=rs[:], in_=ss[:], func=AF.Sqrt, scale=1.0 / 1024, bias=epst[:]), reads=["ss"], writes=["rs"])
                P.op("dve", lambda e: e.reciprocal(out=rs[:], in_=rs[:]), reads=["rs"], writes=["rs"])
                P.op("dve", lambda e: e.scalar_tensor_tensor(out=yo[:], in0=x2[:], scalar=rs[:], in1=GF[:], op0=ALU.mult, op1=ALU.mult), reads=["x2", "rs", "GF"], writes=["yo"])
                P.dma("sp", out[i * 128:(i + 1) * 128, :], yo[:], reads=["yo"])
            P.emit()
    return nc


def make_inputs(cfg, core, inp, n_per_batch=4, S_full=None):
    NW, NO = cfg.NW, cfg.NO
    b, j = core // n_per_batch, core % n_per_batch
    off = NO * 128 * (j + 1) - NW * 128
    x = inp["x"][b]
    xw = np.zeros((NW * 128, 1024), np.float32)
    lo = max(0, -off)
    xw[lo:] = x[off + lo: off + NW * 128]
    m = {"xw": xw}
    m["ccol"] = np.ascontiguousarray(inp["c"][b].reshape(8, 128).T)
    m["w_ada"] = inp["w_ada"][0]
    m["b_adaT"] = np.ascontiguousarray(inp["b_ada"][0].reshape(48, 128).T)
    m["gmixT"] = np.ascontiguousarray(inp["g_norm_mix"][0].reshape(8, 128).T)
    m["gffnT"] = np.ascontiguousarray(inp["g_norm_ffn"][0].reshape(8, 128).T)
    m["gfin"] = inp["g_norm_final"].reshape(1, 1024)
    m["w_in"] = np.ascontiguousarray(inp["w_in"][0][:, w_in_perm()])
    m["w_out"] = inp["w_out"][0]
    for nm, key in (("w1k", "w_cmp_k1"), ("w1v", "w_cmp_v1")):
        m[nm] = np.ascontiguousarray(inp[key][0].reshape(32, 64, 256).transpose(1, 0, 2).reshape(64, 32 * 256))
    m["peTk"] = np.ascontiguousarray(inp["pe_cmp_k"][0].T)
    m["peTv"] = np.ascontiguousarray(inp["pe_cmp_v"][0].T)
    m["b1k"] = np.ascontiguousarray(inp["b_cmp_k1"][0].reshape(2, 128).T)
    m["b1v"] = np.ascontiguousarray(inp["b_cmp_v1"][0].reshape(2, 128).T)
    m["w2k"] = np.ascontiguousarray(inp["w_cmp_k2"][0].reshape(2, 128, 64).transpose(1, 0, 2).reshape(128, 128))
    m["w2v"] = np.ascontiguousarray(inp["w_cmp_v2"][0].reshape(2, 128, 64).transpose(1, 0, 2).reshape(128, 128))
    m["w_pq"] = inp["w_peer_q"][0]
    m["skT"] = np.ascontiguousarray(inp["peer_subkeys"][0].reshape(16, 128, 128).transpose(2, 0, 1).reshape(128, 2048))
    m["pdown"] = inp["peer_down"][0]
    m["pup"] = inp["peer_up"][0]
    for k_, v_ in host_tables(cfg, off).items():
        m["t_" + k_] = v_
    return m


_NC_CACHE = {}


def kernel(**inputs):
    inp = {k: np.asarray(v) for k, v in inputs.items()}
    cfg = CFG()
    if "nc" not in _NC_CACHE:
        _NC_CACHE["nc"] = build(cfg)
        mybir.codegen_inst_isa_subclasses(_NC_CACHE["nc"])
    nc = _NC_CACHE["nc"]
    in_maps = [make_inputs(cfg, c, inp) for c in range(8)]
    res = run_bass_kernel_spmd(nc, in_maps, core_ids=list(range(8)))
    out = np.zeros((2, 8192, 1024), np.float32)
    for c in range(8):
        b, j = c // 4, c % 4
        out[b, j * 2048:(j + 1) * 2048] = res.results[c]["out"]
    return out
```

```python
import numpy as np
import ml_dtypes
from contextlib import ExitStack
import concourse.bass as bass
import concourse.mybir as mybir
from concourse.bass_utils import run_bass_kernel_spmd

F32 = mybir.dt.float32
BF16 = mybir.dt.bfloat16
U32 = mybir.dt.uint32
F32R = mybir.dt.float32r
ALU = mybir.AluOpType
AF = mybir.ActivationFunctionType
AX = mybir.AxisListType

import os
ROPE_ENG = os.environ.get("ROPE_ENG", "dve")
NEG = -30000.0
BIG = 1.0e30
COMPUTE = ("pe", "act", "dve", "pool")
NSW = 8


class _Op:
    __slots__ = ("eng", "fn", "reads", "writes", "deps", "is_dma", "signal", "ev", "id")


class SemState:
    def __init__(self, nc, stack, n_dma_sems=28):
        self.n_dma_sems = n_dma_sems
        self.sems = {}
        for e in COMPUTE:
            self.sems[e] = stack.enter_context(nc.semaphore("s_" + e))
        for k in range(n_dma_sems):
            self.sems["d%d" % k] = stack.enter_context(nc.semaphore("s_d%d" % k))
        self.cnt = {e: 0 for e in COMPUTE}
        self.dma_cnt = [0] * n_dma_sems


class Prog:
    def __init__(self, nc, state):
        self.nc = nc
        self.state = state
        self.ops = []
        self.last_writer = {}
        self.readers = {}
        self.n_dma_sems = state.n_dma_sems
        self.dma_rr = 0
        self.sw_rr = 0
        self.dma_last = [None] * self.n_dma_sems
        self.dma_cnt = state.dma_cnt

    def op(self, eng, fn, reads=(), writes=(), dma=False):
        o = _Op()
        o.eng, o.fn, o.is_dma = eng, fn, dma
        o.reads, o.writes = tuple(reads), tuple(writes)
        o.deps = set()
        o.signal = False
        o.ev = None
        o.id = len(self.ops)
        for r in o.reads:
            w = self.last_writer.get(r)
            if w is not None:
                o.deps.add(w)
        for w_ in o.writes:
            w = self.last_writer.get(w_)
            if w is not None:
                o.deps.add(w)
            lastc = {}
            for rd in self.readers.get(w_, ()):
                p_ = self.ops[rd]
                if p_.is_dma:
                    o.deps.add(rd)
                else:
                    lastc[p_.eng] = rd
            o.deps.update(lastc.values())
        for r in o.reads:
            self.readers.setdefault(r, []).append(o.id)
        for w_ in o.writes:
            self.last_writer[w_] = o.id
            self.readers[w_] = []
        o.deps.discard(o.id)
        if dma:
            if eng == "pool":
                k = self.n_dma_sems - NSW + self.sw_rr
                self.sw_rr = (self.sw_rr + 1) % NSW
            else:
                k = self.dma_rr
                self.dma_rr = (self.dma_rr + 1) % (self.n_dma_sems - NSW)
            prev = self.dma_last[k]
            if prev is not None:
                o.deps.add(prev)
            self.dma_last[k] = o.id
            self.dma_cnt[k] += 16
            o.ev = ("d%d" % k, self.dma_cnt[k])
        self.ops.append(o)
        return o.id

    def dma(self, eng, out, in_, reads=(), writes=(), **kw):
        return self.op(eng, lambda e: e.dma_start(out=out, in_=in_, **kw), reads, writes, dma=True)

    def emit(self):
        nc, ops = self.nc, self.ops
        for o in ops:
            nd = set()
            for d in o.deps:
                p = ops[d]
                if p.is_dma or o.is_dma or p.eng != o.eng:
                    nd.add(d)
                elif o.eng != "pe":
                    nd.add(d)
            o.deps = nd
            for d in nd:
                if not ops[d].is_dma:
                    ops[d].signal = True
        cnt = self.state.cnt
        for o in ops:
            if not o.is_dma and o.signal:
                cnt[o.eng] += 1
                o.ev = (o.eng, cnt[o.eng])
        finals = [ops[i] for i in self.dma_last if i is not None]
        with ExitStack() as st:
            sems = self.state.sems
            block = st.enter_context(nc.Block())
            streams = {}
            for o in ops:
                streams.setdefault(o.eng, []).append(o)

            def run_stream(ename, e, final=False):
                waited = {}

                def wait(ev):
                    if waited.get(ev[0], 0) < ev[1]:
                        e.wait_ge(sems[ev[0]], ev[1])
                        waited[ev[0]] = ev[1]

                for o in streams.get(ename, []):
                    for d in sorted(o.deps):
                        wait(ops[d].ev)
                    ins = o.fn(e)
                    if o.is_dma:
                        ins.then_inc(sems[o.ev[0]], 16)
                    elif o.signal:
                        ins.then_inc(sems[o.eng], 1)
                if final:
                    for o in finals:
                        wait(o.ev)

            block.sync(lambda e: run_stream("sp", e, final=True))
            block.scalar(lambda e: run_stream("act", e))
            block.vector(lambda e: run_stream("dve", e))
            block.gpsimd(lambda e: run_stream("pool", e))
            block.tensor(lambda e: run_stream("pe", e))


def _blk(flag):
    if flag:
        with ExitStack() as st:
            yield st


class CFG:
    def __init__(self, NW=64, NO=16, NEXP=16384, stop=9):
        self.NW, self.NO, self.NEXP, self.stop = NW, NO, NEXP, stop
        self.astop = 99
        self.NB = NW * 8
        self.NCH = max(1, self.NB // 128)
        self.NWK = min(NW, NO + 4)
        self.NSB = NW * 2


LOGG = [float(np.log1p(-np.exp2(-5.0 - h))) for h in range(4)]


def host_tables(cfg, off):
    NW, NO, NB, NCH = cfg.NW, cfg.NO, cfg.NB, cfg.NCH
    S = NW * 128
    p = (off + np.arange(S)).astype(np.float32)
    t = {}
    inv128 = (10000.0 ** (-np.arange(0, 128, 2, dtype=np.float32) / 128)).astype(np.float32)
    inv64 = (10000.0 ** (-np.arange(0, 64, 2, dtype=np.float32) / 64)).astype(np.float32)
    a128 = p[:, None] * inv128[None]
    a64 = p[:, None] * inv64[None]
    t["rope"] = np.concatenate([np.cos(a128), np.sin(a128), np.cos(a64), np.sin(a64)], 1).astype(np.float32)
    valid_tile = ((off + 128 * np.arange(NW)) >= 0).astype(np.float32)
    n = np.arange(128, dtype=np.float32)
    sc = 128 ** -0.5
    zt = np.stack([np.exp((127 - n) * LOGG[h]) * sc for h in range(4)], 1)
    t["zeta"] = (zt[:, None, :] * valid_tile[None, :, None]).astype(np.float32).reshape(128, NW * 4)
    dm = np.zeros((128, 4, 128), np.float32)
    for h in range(4):
        d = n[None, :] - n[:, None]
        dm[:, h, :] = np.where(d >= 0, np.exp(np.where(d >= 0, d, 0) * LOGG[h]), 0.0) * sc
    t["dmat"] = dm.reshape(128, 512)
    xi = np.stack([np.exp((n + 1.0) * LOGG[h]) for h in range(4)], 0)
    t["xi"] = np.broadcast_to(xi[None], (128, 4, 128)).reshape(128, 512).astype(np.float32).copy()
    t["keybias"] = np.broadcast_to(np.where(valid_tile > 0, 0.0, NEG)[None], (128, NW)).astype(np.float32).copy()
    nb = np.arange(NCH * 128)
    cvalid = ((off + 16 * nb) >= 0) & (nb <= NB - 2)
    t["cmpbias"] = np.where(cvalid, 0.0, NEG).astype(np.float32).reshape(NCH, 128).T.copy()
    tq = (NW - NO) * 128 + np.arange(NO * 128)
    cp = np.where((16 * nb[:, None] + 31) <= tq[None, :], 0.0, NEG)
    cp = cp.reshape(NCH, 128, NO, 128).transpose(2, 1, 0, 3)
    cp = np.broadcast_to(cp[:, :, :, None, :], (NO, 128, NCH, 4, 128))
    t["cmppen"] = cp.astype(ml_dtypes.bfloat16).reshape(NO * 128, NCH * 512)
    k = np.arange(128)
    t["tri"] = np.concatenate([np.tile(np.where(k[:, None] <= k[None, :], 0.0, NEG), (1, 4)),
                               np.tile(np.where(k[:, None] > k[None, :], 0.0, NEG), (1, 4))], 1).astype(ml_dtypes.bfloat16)
    t["iota128"] = np.broadcast_to(np.arange(128, dtype=np.float32)[None], (128, 128)).copy()
    mb = np.arange(128)
    cur = tq // 64
    b0 = (-off) // 64
    validb = (mb[None, :] >= b0) & (mb[None, :] <= cur[:, None]) & (mb[None, :] < NW * 2)
    forced = ((mb[None, :] == b0) | (mb[None, :] == cur[:, None]) | (mb[None, :] == cur[:, None] - 1)) & validb
    m1 = (validb & ~forced).astype(np.float32)
    m2 = np.where(forced, 1e9, np.where(validb, 0.0, -BIG)).astype(np.float32)
    t["selm"] = np.concatenate([m1, m2], 1)
    key = np.arange(S)
    ex = np.zeros((128, S), np.float32)
    ex[key // 64, key] = 1.0
    t["expand"] = ex.astype(ml_dtypes.bfloat16)
    cs = 16 * nb
    ss = 64 * mb
    ov = np.clip(np.minimum(cs[:, None] + 32, ss[None, :] + 64) - np.maximum(cs[:, None], ss[None, :]), 0, None) / 16.0
    t["overlap"] = ov.reshape(NCH, 128, 128).transpose(1, 0, 2).reshape(128, NCH * 128).astype(ml_dtypes.bfloat16)
    t["ident"] = np.eye(128, dtype=np.float32)
    sel = np.zeros((128, 128, 128), np.float32)
    sel[k, k, :] = 1.0
    t["selrow"] = sel.reshape(128, 128 * 128).astype(ml_dtypes.bfloat16)
    t["iota16"] = np.broadcast_to(np.arange(16, dtype=np.float32)[None], (128, 16)).copy()
    return t


def w_in_perm():
    r = lambda a, b: list(range(a, b))
    return np.array(r(512, 1024) + r(1024, 1536) + r(2560, 2688) + r(2816, 2944) + r(3072, 3200)
                    + r(2688, 2816) + r(2944, 3072) + r(3200, 3328)
                    + r(0, 512) + r(1536, 2048)
                    + [2048 + (g * 4 + h) * 64 + d for h in range(4) for g in range(2) for d in range(64)]
                    + r(3328, 3352))


B_RK, B_RV, B_NK, B_NV, B_RQ, B_RG, B_NQ, B_GT = (0, 512), (512, 512), (1024, 384), (1408, 384), \
    (1792, 512), (2304, 512), (2816, 512), (3328, 24)


def build(cfg):
    NW, NO, NB, NCH, NWK = cfg.NW, cfg.NO, cfg.NB, cfg.NCH, cfg.NWK
    T0 = NW - NO
    S = NW * 128
    nc = bass.Bass("TRN2", target_bir_lowering=False)
    dram = lambda name, shape, dt=F32, kind="ExternalInput": nc.dram_tensor(name, shape, dt, kind=kind).ap()
    xw = dram("xw", [S, 1024])
    ccol = dram("ccol", [128, 8])
    w_ada = dram("w_ada", [1024, 6144])
    b_adaT = dram("b_adaT", [128, 48])
    gmixT = dram("gmixT", [128, 8])
    gffnT = dram("gffnT", [128, 8])
    gfin = dram("gfin", [1, 1024])
    w_in = dram("w_in", [1024, 3352])
    w_out = dram("w_out", [1024, 1024])
    w1k = dram("w1k", [64, 32 * 256])
    w1v = dram("w1v", [64, 32 * 256])
    peTk = dram("peTk", [64, 32])
    peTv = dram("peTv", [64, 32])
    b1k = dram("b1k", [128, 2])
    b1v = dram("b1v", [128, 2])
    w2k = dram("w2k", [128, 2 * 64])
    w2v = dram("w2v", [128, 2 * 64])
    w_pq = dram("w_pq", [1024, 2048])
    skT = dram("skT", [128, 2048])
    pdown = dram("pdown", [cfg.NEXP, 1024])
    pup = dram("pup", [cfg.NEXP, 1024])
    tb = {}
    for name, shape, dt in [("rope", [S, 192], F32), ("zeta", [128, NW * 4], F32), ("dmat", [128, 512], F32),
                            ("xi", [128, 512], F32), ("keybias", [128, NW], F32), ("cmpbias", [128, NCH], F32),
                            ("cmppen", [NO * 128, NCH * 512], BF16), ("tri", [128, 1024], BF16), ("iota128", [128, 128], F32),
                            ("selm", [NO * 128, 256], F32), ("expand", [128, S], BF16),
                            ("overlap", [128, NCH * 128], BF16), ("ident", [128, 128], F32),
                            ("selrow", [128, 128 * 128], BF16), ("iota16", [128, 16], F32)]:
        tb[name] = dram("t_" + name, shape, dt)
    out = dram("out", [NO * 128, 1024], kind="ExternalOutput")
    rawd = dram("rawd", [2, 128, S], BF16, kind="Internal")
    RAWLEN = max(S + 16, 16 * NCH * 128 + 32)
    x1d = dram("x1d", [NO * 128, 1024], kind="Internal")
    pdb = dram("pdb", [cfg.NEXP, 1024], BF16, kind="Internal")
    pub = dram("pub", [cfg.NEXP, 1024], BF16, kind="Internal")

    with ExitStack() as outer:
        sbo = lambda name, shape, dt=F32: outer.enter_context(nc.sbuf_tensor(name, shape, dt))
        SEM = SemState(nc, outer)
        vec = sbo("vec", [128, 48])

        _bc = [0]

        def bcast_rows(P, sb, pbrk, items):
            _bc[0] += 1
            onesf = sb("onesf%d" % _bc[0], [128, 128]); diag = [sb("diag%d_%d" % (i_, _bc[0]), [128, 128]) for i_ in range(2)]
            P.op("dve", lambda e: e.memset(onesf[:], 1.0), writes=["onesf"])
            n = 0
            for vi, dst, dkey in items:
                for half in range(2):
                    pb, pkey = pbrk[n % 2]
                    for q in range(4):
                        fc = half * 4 + q
                        dg = diag[q % 2]
                        P.op("dve", lambda e, dg=dg, vi=vi, fc=fc: e.tensor_scalar(out=dg[:], in0=identf[:], scalar1=vec[:, vi * 8 + fc:vi * 8 + fc + 1],
                                                                                  scalar2=None, op0=ALU.mult),
                             reads=["identf", "vec"], writes=[("diag", q % 2)])
                        P.op("pe", lambda e, dg=dg, pb=pb, q=q: e.matmul(pb[:, q * 128:(q + 1) * 128], lhsT=onesf[:], rhs=dg[:], start=True, stop=True),
                             reads=[("diag", q % 2), "onesf"], writes=[pkey])
                    P.op("act", lambda e, pb=pb, dst=dst, half=half: e.copy(out=dst[:, half * 512:(half + 1) * 512], in_=pb[:]),
                         reads=[pkey], writes=[dkey])
                    n += 1
        identf = sbo("identf", [128, 128]); identb = sbo("identb", [128, 128], BF16)
        epst = sbo("epst", [128, 1])
        mid = ExitStack()
        sbm = lambda name, shape, dt=F32: mid.enter_context(nc.sbuf_tensor(name, shape, dt))
        ksT = sbm("ksT", [128, S], BF16)
        vs = sbm("vs", [128, NW * 2 * 65], BF16)
        kwT = sbm("kwT", [128, NWK * 128], BF16)
        vw = sbm("vw", [128, NWK * 2 * 65], BF16)
        kcT = sbm("kcT", [128, NCH * 128], BF16)
        vca = sbm("vca", [128, NCH * 2 * 193], BF16)
        reto = sbm("reto", [128, NO * 512], BF16)
        qTs = sbm("qTs", [128, NO * 512], BF16)
        gts = sbm("gts", [128, NO * 24])
        vs4 = vs[:].rearrange("p (t g d) -> p t g d", g=2, d=65)
        vw4 = vw[:].rearrange("p (t g d) -> p t g d", g=2, d=65)
        vca4 = vca[:].rearrange("p (c g d) -> p c g d", g=2, d=193)

        for st in _blk(cfg.stop >= 0):
            sb = lambda name, shape, dt=F32: st.enter_context(nc.sbuf_tensor(name, shape, dt))
            ps = lambda name, shape, dt=F32: st.enter_context(nc.psum_tensor(name, shape, dt))
            P = Prog(nc, SEM)
            wad = [sb("wad%d" % i, [128, 8 * 1024]) for i in range(2)]
            cc = sb("cc", [128, 8]); sil = sb("sil", [128, 8])
            badT = sb("badT", [128, 48]); gm = sb("gm", [128, 8]); gf_ = sb("gf_", [128, 8])
            modT = sb("modT", [128, 48])
            pm = ps("pm", [128, 48])
            P.dma("sp", cc[:], ccol, writes=["cc"])
            P.dma("sp", badT[:], b_adaT, writes=["badT"])
            P.dma("sp", gm[:], gmixT, writes=["gm"])
            P.dma("sp", gf_[:], gffnT, writes=["gf_"])
            P.dma("sp", identf[:], tb["ident"], writes=["identf"])
            P.op("dve", lambda e: e.memset(epst[:], 1e-6), writes=["eps"])
            P.op("act", lambda e: e.activation(out=sil[:], in_=cc[:], func=AF.Silu), reads=["cc"], writes=["sil"])
            P.op("dve", lambda e: e.tensor_copy(out=identb[:], in_=identf[:]), reads=["identf"], writes=["identb"])
            for s in range(6):
                wt = wad[s % 2]
                wt3 = wt[:].rearrange("p (k n) -> p k n", k=8)
                for kc in range(8):
                    P.dma("sp" if kc % 2 == 0 else "act", wt3[:, kc, :], w_ada[kc * 128:(kc + 1) * 128, s * 1024:(s + 1) * 1024],
                          writes=[("wad", s % 2, kc)])
                for fc in range(8):
                    for kc in range(8):
                        P.op("pe", lambda e, fc=fc, kc=kc, wt3=wt3, s=s: e.matmul(
                            pm[:, s * 8 + fc:s * 8 + fc + 1], lhsT=wt3[:, kc, fc * 128:(fc + 1) * 128], rhs=sil[:, kc:kc + 1],
                            start=(kc == 0), stop=(kc == 7)), reads=[("wad", s % 2, kc), "sil"], writes=["pm"])
            P.op("dve", lambda e: e.tensor_tensor(out=modT[:], in0=pm[:], in1=badT[:], op=ALU.add), reads=["pm", "badT"], writes=["modT"])
            P.op("dve", lambda e: e.scalar_tensor_tensor(out=vec[:, 0:8], in0=modT[:, 8:16], scalar=1.0, in1=gm[:], op0=ALU.add, op1=ALU.mult),
                 reads=["modT", "gm"], writes=["vec"])
            P.op("dve", lambda e: e.scalar_tensor_tensor(out=vec[:, 16:24], in0=modT[:, 32:40], scalar=1.0, in1=gf_[:], op0=ALU.add, op1=ALU.mult),
                 reads=["modT", "gf_"], writes=["vec"])
            P.op("dve", lambda e: e.tensor_copy(out=vec[:, 8:16], in_=modT[:, 0:8]), reads=["modT"], writes=["vec"])
            P.op("dve", lambda e: e.tensor_copy(out=vec[:, 24:32], in_=modT[:, 24:32]), reads=["modT"], writes=["vec"])
            P.op("dve", lambda e: e.tensor_copy(out=vec[:, 32:40], in_=modT[:, 16:24]), reads=["modT"], writes=["vec"])
            P.op("dve", lambda e: e.tensor_copy(out=vec[:, 40:48], in_=modT[:, 40:48]), reads=["modT"], writes=["vec"])
            P.emit()

        for st in _blk(cfg.stop >= 1):
            sb = lambda name, shape, dt=F32: st.enter_context(nc.sbuf_tensor(name, shape, dt))
            ps = lambda name, shape, dt=F32: st.enter_context(nc.psum_tensor(name, shape, dt))
            P = Prog(nc, SEM)
            wib = sb("wib", [128, 8 * 3352], BF16)
            wib3 = wib[:].rearrange("p (k n) -> p k n", k=8)
            stg = [sb("stg%d" % i, [128, 419]) for i in range(2)]
            A1R = sb("A1R", [128, 1024]); B1R = sb("B1R", [128, 1024])
            n_ = 0
            for kc in range(8):
                for cq in range(8):
                    sl = slice(cq * 419, (cq + 1) * 419)
                    P.dma("sp", stg[n_ % 2][:], w_in[kc * 128:(kc + 1) * 128, sl], writes=[("stg", n_ % 2)])
                    if n_ % 2:
                        P.op("act", lambda e, kc=kc, sl=sl, n_=n_: e.copy(out=wib3[:, kc, sl], in_=stg[n_ % 2][:]), reads=[("stg", n_ % 2)], writes=["wib"])
                    else:
                        P.op("pool", lambda e, kc=kc, sl=sl, n_=n_: e.tensor_copy(out=wib3[:, kc, sl], in_=stg[n_ % 2][:]), reads=[("stg", n_ % 2)], writes=["wib"])
                    n_ += 1
            xt = [sb("xt%d" % i, [128, 1024]) for i in range(2)]
            sq = sb("sq", [128, 1024])
            ss = sb("ss", [128, 2]); rs = sb("rs", [128, 2])
            tmod = sb("tmod", [128, 1024])
            hb2 = [sb("hb%d" % i, [128, 1024], BF16) for i in range(2)]
            hT = [sb("hT%d" % i, [128, 1024], BF16) for i in range(2)]
            rp = [sb("rp%d" % i, [128, 192]) for i in range(2)]
            pj = [sb("pj%d" % i, [128, 512]) for i in range(3)]
            ra = sb("ra", [128, 256]); rb_ = sb("rb_", [128, 256]); rc_ = sb("rc_", [128, 256]); rd_ = sb("rd_", [128, 256])
            ktok = sb("ktok", [128, 512], BF16)
            qtok = sb("qtok", [128, 512], BF16)
            vtok = sb("vtok", [128, 512], BF16)
            vz = sb("vz", [128, 512], BF16)
            nk = sb("nk", [128, 384], BF16)
            nv = sb("nv", [128, 384], BF16)
            nq = sb("nq", [128, 512], BF16)
            Sst = sb("Sst", [128, 512]); Sb = sb("Sb", [128, 512], BF16)
            zt = sb("zt", [128, NW * 4]); dmat = sb("dmat", [128, 512]); xit = sb("xit", [128, 512])
            kTr = sb("kTr", [128, 512], BF16); qTr = sb("qTr", [128, 512], BF16); qxT = sb("qxT", [128, 512], BF16)
            pT = sb("pT", [128, 512], BF16)
            yv = sb("yv", [128, 512]); sg = sb("sg", [128, 512])
            st6 = sb("st6", [128, 4 * 6]); mv = sb("mv", [128, 4 * 2]); rstd4 = sb("rstd4", [128, 4])
            rawst = [sb("rawst%d" % i, [128, 256], BF16) for i in range(2)]
            p_tr = ps("p_tr", [128, 1024], BF16)
            p_mm = [ps("p_mm%d" % i, [128, 512]) for i in range(3)]
            p_kv = ps("p_kv", [128, 512])
            p_sc = ps("p_sc", [128, 512])
            p_y = ps("p_y", [128, 512])
            bcast_rows(P, sb, [(p_y, "p_y"), (p_sc, "p_sc")], [(0, A1R, "A1R"), (1, B1R, "B1R")])
            P.dma("sp", zt[:], tb["zeta"], writes=["zt"])
            P.dma("sp", dmat[:], tb["dmat"], writes=["dmat"])
            P.dma("sp", xit[:], tb["xi"], writes=["xit"])
            P.op("dve", lambda e: e.memset(Sst[:], 0.0), writes=["Sst"])
            P.op("dve", lambda e: e.memset(Sb[:], 0.0), writes=["Sb"])
            P.op("pool", lambda e: e.memset(vs[:], 1.0), writes=["vs"])
            P.op("pool", lambda e: e.memset(vw[:], 1.0), writes=["vw"])

            def rope(src, dst, H, D, cos, sin, keys_r, key_w):
                if os.environ.get("SKIP_ROPE"):
                    return
                h2 = D // 2
                s3 = src.rearrange("p (h d) -> p h d", h=H)
                d3 = dst.rearrange("p (h d) -> p h d", h=H)
                x1, x2 = s3[:, :, 0:h2], s3[:, :, h2:D]
                cb = cos.unsqueeze(1).to_broadcast([128, H, h2])
                sbb = sin.unsqueeze(1).to_broadcast([128, H, h2])
                n_ = H * h2
                v = lambda t_: t_[:, 0:n_].rearrange("p (h d) -> p h d", h=H)
                P.op("dve", lambda e: e.tensor_tensor(out=v(ra), in0=x1, in1=cb, op=ALU.mult), reads=keys_r, writes=["ra"])
                P.op(ROPE_ENG, lambda e: e.tensor_tensor(out=v(rb_), in0=x2, in1=sbb, op=ALU.mult), reads=keys_r, writes=["rb"])
                P.op("dve", lambda e: e.tensor_tensor(out=d3[:, :, 0:h2], in0=v(ra), in1=v(rb_), op=ALU.subtract), reads=["ra", "rb"], writes=[key_w + "_lo"])
                P.op(ROPE_ENG, lambda e: e.tensor_tensor(out=v(rc_), in0=x2, in1=cb, op=ALU.mult), reads=keys_r, writes=["rc"])
                P.op("dve", lambda e: e.tensor_tensor(out=v(rd_), in0=x1, in1=sbb, op=ALU.mult), reads=keys_r, writes=["rd"])
                P.op(ROPE_ENG, lambda e: e.tensor_tensor(out=d3[:, :, h2:D], in0=v(rc_), in1=v(rd_), op=ALU.add), reads=["rc", "rd"], writes=[key_w + "_hi"])

            def proj(blk, pdst, pkey, hTt, hkey):
                c0, w = blk
                for kc in range(8):
                    P.op("pe", lambda e, kc=kc: e.matmul(pdst[:, 0:w], lhsT=hTt[:, kc * 128:(kc + 1) * 128], rhs=wib3[:, kc, c0:c0 + w],
                                                        start=(kc == 0), stop=(kc == 7)), reads=[hkey, "wib"], writes=[pkey])

            for T in range(NW if cfg.astop >= 1 else 0):
                b = T % 2
                own = T >= T0
                i = T - T0
                hb = hb2[b]
                P.dma("sp", xt[b][:], xw[T * 128:(T + 1) * 128, :], writes=[("xt", b)])
                P.dma("sp", rp[b][:], tb["rope"][T * 128:(T + 1) * 128, :], writes=[("rp", b)])
                P.op("act", lambda e, b=b: e.activation(out=sq[:], in_=xt[b][:], func=AF.Square), reads=[("xt", b)], writes=["sq"])
                P.op("dve", lambda e, b=b: e.reduce_sum(out=ss[:, b:b + 1], in_=sq[:], axis=AX.X), reads=["sq"], writes=[("ss", b)])
                P.op("act", lambda e, b=b: e.activation(out=rs[:, b:b + 1], in_=ss[:, b:b + 1], func=AF.Sqrt, scale=1.0 / 1024, bias=epst[:]),
                     reads=[("ss", b), "eps"], writes=[("rs", b)])
                P.op("dve", lambda e, b=b: e.reciprocal(out=rs[:, b:b + 1], in_=rs[:, b:b + 1]), reads=[("rs", b)], writes=[("rs", b)])
                P.op("dve", lambda e, b=b: e.scalar_tensor_tensor(out=tmod[:], in0=xt[b][:], scalar=rs[:, b:b + 1], in1=A1R[:], op0=ALU.mult, op1=ALU.mult),
                     reads=[("xt", b), ("rs", b), "A1R"], writes=["tmod"])
                P.op("pool", lambda e, hb=hb: e.tensor_tensor(out=hb[:], in0=tmod[:], in1=B1R[:], op=ALU.add), reads=["tmod", "B1R"], writes=[("hb", b)])
                for kc in range(8):
                    P.op("pe", lambda e, kc=kc, hb=hb: e.transpose(p_tr[:, kc * 128:(kc + 1) * 128], hb[:, kc * 128:(kc + 1) * 128], identb[:]),
                         reads=[("hb", b)], writes=["p_tr"])
                P.op("act", lambda e, b=b: e.copy(out=hT[b][:], in_=p_tr[:]), reads=["p_tr"], writes=[("hT", b)])
                hkey = ("hT", b)
                cos128, sin128, cos64, sin64 = rp[b][:, 0:64], rp[b][:, 64:128], rp[b][:, 128:160], rp[b][:, 160:192]
                if cfg.astop < 2:
                    continue
                proj(B_RK, p_mm[0], ("p_mm", 0), hT[b], hkey)
                P.op("act", lambda e: e.copy(out=pj[0][:], in_=p_mm[0][:]), reads=[("p_mm", 0)], writes=[("pj", 0)])
                rope(pj[0][:], ktok[:], 4, 128, cos128, sin128, [("pj", 0), ("rp", b)], "ktok")
                proj(B_RV, p_mm[1], ("p_mm", 1), hT[b], hkey)
                P.op("act", lambda e: e.copy(out=vtok[:], in_=p_mm[1][:]), reads=[("p_mm", 1)], writes=["vtok"])
                for h in range(0 if os.environ.get("SKIP_VZ") else 4):
                    P.op("dve", lambda e, h=h, T=T: e.tensor_scalar(out=vz[:, h * 128:(h + 1) * 128], in0=vtok[:, h * 128:(h + 1) * 128],
                                                                    scalar1=zt[:, T * 4 + h:T * 4 + h + 1], scalar2=None, op0=ALU.mult),
                         reads=["vtok", "zt"], writes=[("vz", h)])
                if cfg.astop < 2.2:
                    continue
                proj(B_NK, p_mm[2], ("p_mm", 2), hT[b], hkey)
                P.op("act", lambda e: e.copy(out=pj[2][:, 0:384], in_=p_mm[2][:, 0:384]), reads=[("p_mm", 2)], writes=[("pj", 2)])
                if cfg.astop < 2.5:
                    continue
                rope(pj[2][:, 0:384], nk[:], 6, 64, cos64, sin64, [("pj", 2), ("rp", b)], "nk")
                if cfg.astop < 2.8:
                    continue
                proj(B_NV, p_mm[0], ("p_mm", 0), hT[b], hkey)
                P.op("act", lambda e: e.copy(out=nv[:], in_=p_mm[0][:, 0:384]), reads=[("p_mm", 0)], writes=["nv"])
                if cfg.astop < 4:
                    continue
                P.op("dve", lambda e, T=T: e.tensor_copy(out=vs4[:, T, :, 0:64], in_=nv[:, 128:256].rearrange("p (g d) -> p g d", g=2)),
                     reads=["nv"], writes=["vs"])
                wk = T - (NW - NWK)
                if wk >= 0:
                    P.op("dve", lambda e, wk=wk: e.tensor_copy(out=vw4[:, wk, :, 0:64], in_=nv[:, 256:384].rearrange("p (g d) -> p g d", g=2)),
                         reads=["nv"], writes=["vw"])
                srcs = [nk[:, 0:128], nk[:, 128:256], nk[:, 256:384], nv[:, 0:128]]
                for q, s_ in enumerate(srcs):
                    P.op("pe", lambda e, q=q, s_=s_: e.transpose(p_tr[:, q * 128:(q + 1) * 128], s_, identb[:]),
                         reads=["nk_lo", "nk_hi", "nv"], writes=["p_tr"])
                P.op("act", lambda e, T=T: e.copy(out=ksT[:, T * 128:(T + 1) * 128], in_=p_tr[:, 128:256]), reads=["p_tr"], writes=["ksT"])
                if wk >= 0:
                    P.op("act", lambda e, wk=wk: e.copy(out=kwT[:, wk * 128:(wk + 1) * 128], in_=p_tr[:, 256:384]), reads=["p_tr"], writes=["kwT"])
                rw = rawst[T % 2]
                P.op("act", lambda e, rw=rw: e.copy(out=rw[:, 0:128], in_=p_tr[:, 0:128]), reads=["p_tr"], writes=[("rawst", T % 2)])
                P.op("act", lambda e, rw=rw: e.copy(out=rw[:, 128:256], in_=p_tr[:, 384:512]), reads=["p_tr"], writes=[("rawst", T % 2)])
                for kind in range(2):
                    P.dma("sp", rawd[kind, :, T * 128:(T + 1) * 128], rw[:, kind * 128:(kind + 1) * 128], reads=[("rawst", T % 2)])
                if cfg.astop < 5:
                    continue
                if own and cfg.astop >= 6:
                    proj(B_RQ, p_mm[1], ("p_mm", 1), hT[b], hkey)
                    P.op("act", lambda e: e.copy(out=pj[1][:], in_=p_mm[1][:]), reads=[("p_mm", 1)], writes=[("pj", 1)])
                    rope(pj[1][:], qtok[:], 4, 128, cos128, sin128, [("pj", 1), ("rp", b)], "qtok")
                    for h in range(4):
                        P.op("pe", lambda e, h=h: e.transpose(p_tr[:, h * 128:(h + 1) * 128], ktok[:, h * 128:(h + 1) * 128], identb[:]),
                             reads=["ktok_lo", "ktok_hi"], writes=["p_tr"])
                    P.op("act", lambda e: e.copy(out=kTr[:], in_=p_tr[:, 0:512]), reads=["p_tr"], writes=["kTr"])
                    for h in range(4):
                        P.op("pe", lambda e, h=h: e.transpose(p_tr[:, 512 + h * 128:512 + (h + 1) * 128], qtok[:, h * 128:(h + 1) * 128], identb[:]),
                             reads=["qtok_lo", "qtok_hi"], writes=["p_tr"])
                    P.op("act", lambda e: e.copy(out=qTr[:], in_=p_tr[:, 512:1024]), reads=["p_tr"], writes=["qTr"])
                    P.op("dve", lambda e: e.tensor_tensor(out=qxT[:], in0=qTr[:], in1=xit[:], op=ALU.mult), reads=["qTr", "xit"], writes=["qxT"])
                    for h in range(4):
                        hs = slice(h * 128, (h + 1) * 128)
                        P.op("pe", lambda e, hs=hs: e.matmul(p_sc[:, hs], lhsT=kTr[:, hs], rhs=qTr[:, hs], start=True, stop=True),
                             reads=["kTr", "qTr"], writes=["p_sc"])
                    P.op("dve", lambda e: e.tensor_tensor(out=pT[:], in0=p_sc[:], in1=dmat[:], op=ALU.mult), reads=["p_sc", "dmat"], writes=["pT"])
                    for h in range(4):
                        hs = slice(h * 128, (h + 1) * 128)
                        P.op("pe", lambda e, hs=hs: e.matmul(p_y[:, hs], lhsT=pT[:, hs], rhs=vtok[:, hs], start=True, stop=False),
                             reads=["pT", "vtok"], writes=["p_y"])
                        P.op("pe", lambda e, hs=hs: e.matmul(p_y[:, hs], lhsT=qxT[:, hs], rhs=Sb[:, hs], start=False, stop=True),
                             reads=["qxT", "Sb"], writes=["p_y"])
                    P.op("act", lambda e: e.copy(out=yv[:], in_=p_y[:]), reads=["p_y"], writes=["yv"])
                    for h in range(4):
                        P.op("dve", lambda e, h=h: e.bn_stats(out=st6[:, h * 6:(h + 1) * 6], in_=yv[:, h * 128:(h + 1) * 128]), reads=["yv"], writes=[("st6", h)])
                        P.op("dve", lambda e, h=h: e.bn_aggr(out=mv[:, h * 2:(h + 1) * 2], in_=st6[:, h * 6:(h + 1) * 6]), reads=[("st6", h)], writes=[("mv", h)])
                    mv3 = mv[:].rearrange("p (h t) -> p h t", t=2)
                    P.op("act", lambda e: e.activation(out=rstd4[:], in_=mv3[:, :, 1], func=AF.Sqrt, bias=epst[:]),
                         reads=[("mv", 0), ("mv", 1), ("mv", 2), ("mv", 3), "eps"], writes=["rstd4"])
                    P.op("dve", lambda e: e.reciprocal(out=rstd4[:], in_=rstd4[:]), reads=["rstd4"], writes=["rstd4"])
                    proj(B_RG, p_mm[2], ("p_mm", 2), hT[b], hkey)
                    P.op("act", lambda e: e.activation(out=sg[:], in_=p_mm[2][:], func=AF.Silu), reads=[("p_mm", 2)], writes=["sg"])
                    for h in range(4):
                        hs = slice(h * 128, (h + 1) * 128)
                        P.op("dve", lambda e, h=h, hs=hs: e.tensor_scalar(out=yv[:, hs], in0=yv[:, hs], scalar1=mv[:, 2 * h:2 * h + 1], scalar2=rstd4[:, h:h + 1],
                                                                         op0=ALU.subtract, op1=ALU.mult),
                             reads=["yv", ("mv", h), "rstd4"], writes=[("yn", h)])
                        P.op("pool", lambda e, h=h, hs=hs, i=i: e.tensor_tensor(out=reto[:, i * 512 + h * 128:i * 512 + (h + 1) * 128], in0=yv[:, hs], in1=sg[:, hs], op=ALU.mult),
                             reads=[("yn", h), "sg"], writes=["reto"])
                    proj(B_NQ, p_mm[0], ("p_mm", 0), hT[b], hkey)
                    P.op("act", lambda e: e.copy(out=pj[0][:], in_=p_mm[0][:]), reads=[("p_mm", 0)], writes=[("pj", 0)])
                    rope(pj[0][:], nq[:], 8, 64, cos64, sin64, [("pj", 0), ("rp", b)], "nq")
                    for h in range(4):
                        P.op("pe", lambda e, h=h: e.transpose(p_tr[:, h * 128:(h + 1) * 128], nq[:, h * 128:(h + 1) * 128], identb[:]),
                             reads=["nq_lo", "nq_hi"], writes=["p_tr"])
                    P.op("act", lambda e, i=i: e.copy(out=qTs[:, i * 512:(i + 1) * 512], in_=p_tr[:, 0:512]), reads=["p_tr"], writes=["qTs"])
                    proj(B_GT, p_mm[1], ("p_mm", 1), hT[b], hkey)
                    P.op("act", lambda e, i=i: e.activation(out=gts[:, i * 24:(i + 1) * 24], in_=p_mm[1][:, 0:24], func=AF.Sigmoid),
                         reads=[("p_mm", 1)], writes=["gts"])
                for h in range(4):
                    hs = slice(h * 128, (h + 1) * 128)
                    P.op("pe", lambda e, hs=hs: e.matmul(p_kv[:, hs], lhsT=ktok[:, hs], rhs=vz[:, hs], start=True, stop=True),
                         reads=["ktok_lo", "ktok_hi", ("vz", 0), ("vz", 1), ("vz", 2), ("vz", 3)], writes=["p_kv"])
                for h in range(4):
                    hs = slice(h * 128, (h + 1) * 128)
                    P.op("dve", lambda e, h=h, hs=hs: e.scalar_tensor_tensor(out=Sst[:, hs], in0=Sst[:, hs], scalar=float(np.exp(128 * LOGG[h])), in1=p_kv[:, hs],
                                                                            op0=ALU.mult, op1=ALU.add), reads=["p_kv", "Sst"], writes=["Sst"])
                P.op("act", lambda e: e.copy(out=Sb[:], in_=Sst[:]), reads=["Sst"], writes=["Sb"])
            P.emit()

        for st in _blk(cfg.stop >= 2):
            sb = lambda name, shape, dt=F32: st.enter_context(nc.sbuf_tensor(name, shape, dt))
            ps = lambda name, shape, dt=F32: st.enter_context(nc.psum_tensor(name, shape, dt))
            P = Prog(nc, SEM)
            NBP = NCH * 128
            raw = [sb("raw%d" % k, [128, RAWLEN], BF16) for k in range(2)]
            w1s = sb("w1s", [128, 32 * 256]); w1b = [sb("w1b%d" % k, [128, 32 * 256], BF16) for k in range(2)]
            pes = sb("pes", [128, 32]); peb = [sb("peb%d" % k, [128, 32], BF16) for k in range(2)]
            b1s = [sb("b1s%d" % k, [128, 2]) for k in range(2)]
            w2s = sb("w2s", [128, 128]); w2b = [sb("w2b%d" % k, [128, 128], BF16) for k in range(2)]
            cb = sb("cb", [128, 2])
            u = sb("u", [128, 512]); u2 = sb("u2", [128, 512]); u3 = sb("u3", [128, 512])
            hid = [sb("hid%d" % i, [128, 512], BF16) for i in range(2)]
            ovl = sb("ovl", [128, NCH * 128], BF16)
            p_h = ps("p_h", [128, 512]); p_c = ps("p_c", [128, 2]); p_o = ps("p_o", [128, 512])
            P.dma("sp", ovl[:], tb["overlap"], writes=["ovl"])
            P.op("pool", lambda e: e.memset(vca[:], 1.0), writes=["vca"])
            for cc_ in range(NCH):
                for g in range(2):
                    P.op("pool", lambda e, cc_=cc_, g=g: e.tensor_copy(out=vca4[:, cc_, g, 65:193], in_=ovl[:, cc_ * 128:(cc_ + 1) * 128]), reads=["ovl"], writes=["vca"])
            for kind, (w1d, ped, b1d, w2d) in enumerate([(w1k, peTk, b1k, w2k), (w1v, peTv, b1v, w2v)]):
                P.op("pool", lambda e, kind=kind: e.memset(raw[kind][:], 0.0), writes=[("raw", kind)])
                P.dma("sp", raw[kind][:, 0:S], rawd[kind], writes=[("raw", kind)])
                for half in range(2):
                    P.dma("sp", w1s[half * 64:(half + 1) * 64, :], w1d, writes=["w1s"])
                    P.dma("sp", pes[half * 64:(half + 1) * 64, :], ped, writes=["pes"])
                P.op("act", lambda e, kind=kind: e.copy(out=w1b[kind][:], in_=w1s[:]), reads=["w1s"], writes=[("w1b", kind)])
                P.op("dve", lambda e, kind=kind: e.tensor_copy(out=peb[kind][:], in_=pes[:]), reads=["pes"], writes=[("peb", kind)])
                P.dma("sp", b1s[kind][:], b1d, writes=[("b1s", kind)])
                P.dma("sp", w2s[:], w2d, writes=["w2s"])
                P.op("dve", lambda e, kind=kind: e.tensor_copy(out=w2b[kind][:], in_=w2s[:]), reads=["w2s"], writes=[("w2b", kind)])
                w13 = w1b[kind][:].rearrange("p (l n) -> p l n", l=32)
                for hc in range(2):
                    for l in range(32):
                        P.op("pe", lambda e, hc=hc, l=l, w13=w13, kind=kind: e.matmul(p_c[:, hc:hc + 1], lhsT=w13[0:64, l, hc * 128:(hc + 1) * 128], rhs=peb[kind][0:64, l:l + 1],
                                                                                    start=(l == 0), stop=(l == 31)), reads=[("w1b", kind), ("peb", kind)], writes=["p_c"])
                P.op("dve", lambda e, kind=kind: e.tensor_tensor(out=cb[:], in0=p_c[:], in1=b1s[kind][:], op=ALU.add), reads=["p_c", ("b1s", kind)], writes=["cb"])
                for g in range(2):
                    gs = slice(64 * g, 64 * g + 64)
                    for n0 in range(0, NBP, 512):
                        nn = min(512, NBP - n0)
                        for hc in range(2):
                            for l in range(32):
                                rhs = raw[kind][gs, l + 16 * n0: l + 16 * (n0 + nn): 16]
                                P.op("pe", lambda e, hc=hc, l=l, rhs=rhs, gs=gs, w13=w13, nn=nn: e.matmul(
                                    p_h[:, 0:nn], lhsT=w13[gs, l, hc * 128:(hc + 1) * 128], rhs=rhs, start=(l == 0), stop=(l == 31)),
                                    reads=[("w1b", kind), ("raw", kind)], writes=["p_h"])
                            P.op("act", lambda e, hc=hc, nn=nn: e.activation(out=u[:, 0:nn], in_=p_h[:, 0:nn], func=AF.Identity, bias=cb[:, hc:hc + 1]),
                                 reads=["p_h", "cb"], writes=["u"])
                            P.op("act", lambda e, nn=nn: e.activation(out=u2[:, 0:nn], in_=u[:, 0:nn], func=AF.Square), reads=["u"], writes=["u2"])
                            P.op("dve", lambda e, nn=nn: e.tensor_scalar(out=u2[:, 0:nn], in0=u2[:, 0:nn], scalar1=0.044715, scalar2=1.0, op0=ALU.mult, op1=ALU.add),
                                 reads=["u2"], writes=["u2"])
                            P.op("dve", lambda e, nn=nn: e.tensor_tensor(out=u3[:, 0:nn], in0=u2[:, 0:nn], in1=u[:, 0:nn], op=ALU.mult), reads=["u2", "u"], writes=["u3"])
                            P.op("act", lambda e, nn=nn: e.activation(out=u3[:, 0:nn], in_=u3[:, 0:nn], func=AF.Sigmoid, scale=1.5957691216), reads=["u3"], writes=["u3"])
                            P.op("dve", lambda e, hc=hc, nn=nn: e.tensor_tensor(out=hid[hc][:, 0:nn], in0=u3[:, 0:nn], in1=u[:, 0:nn], op=ALU.mult),
                                 reads=["u3", "u"], writes=[("hid", hc)])
                        w23 = w2b[kind][:].rearrange("p (c d) -> p c d", c=2)
                        if kind == 0:
                            for hc in range(2):
                                P.op("pe", lambda e, hc=hc, gs=gs, nn=nn, w23=w23: e.matmul(p_o[gs, 0:nn], lhsT=w23[:, hc, :], rhs=hid[hc][:, 0:nn], start=(hc == 0), stop=(hc == 1)),
                                     reads=[("hid", 0), ("hid", 1), ("w2b", 0)], writes=["p_o"])
                            P.op("act", lambda e, gs=gs, n0=n0, nn=nn: e.copy(out=kcT[gs, n0:n0 + nn], in_=p_o[gs, 0:nn]), reads=["p_o"], writes=["kcT"])
                        else:
                            for c4 in range(nn // 128):
                                for hc in range(2):
                                    P.op("pe", lambda e, hc=hc, c4=c4, w23=w23: e.matmul(p_o[:, c4 * 64:(c4 + 1) * 64], lhsT=hid[hc][:, c4 * 128:(c4 + 1) * 128], rhs=w23[:, hc, :],
                                                                                        start=(hc == 0), stop=(hc == 1)), reads=[("hid", 0), ("hid", 1), ("w2b", 1)], writes=["p_o"])
                                P.op("act", lambda e, c4=c4, g=g, n0=n0: e.copy(out=vca4[:, n0 // 128 + c4, g, 0:64], in_=p_o[:, c4 * 64:(c4 + 1) * 64]),
                                     reads=["p_o"], writes=["vca"])
            P.emit()

        for st in _blk(cfg.stop >= 3):
            sb = lambda name, shape, dt=F32: st.enter_context(nc.sbuf_tensor(name, shape, dt))
            ps = lambda name, shape, dt=F32: st.enter_context(nc.psum_tensor(name, shape, dt))
            P = Prog(nc, SEM)
            wob = sb("wob", [128, 8 * 1024], BF16)
            wob3 = wob[:].rearrange("p (k n) -> p k n", k=8)
            stg = [sb("stgc%d" % i, [128, 1024]) for i in range(2)]
            for kc in range(8):
                P.dma("sp", stg[kc % 2][:], w_out[kc * 128:(kc + 1) * 128, :], writes=[("stg", kc % 2)])
                P.op("pool", lambda e, kc=kc: e.tensor_copy(out=wob3[:, kc, :], in_=stg[kc % 2][:]), reads=[("stg", kc % 2)], writes=["wob"])
            expd = sb("expd", [128, S], BF16); tri = sb("tri", [128, 1024], BF16)
            kbias = sb("kbias", [128, NW]); cbias = sb("cbias", [128, NCH])
            cpen = [sb("cpen%d" % i, [128, NCH * 512], BF16) for i in range(2)]
            selm = [sb("selm%d" % i, [128, 256]) for i in range(2)]
            xo = [sb("xo%d" % i, [128, 1024]) for i in range(2)]
            eb = [sb("eb%d" % i, [128, 512], BF16) for i in range(4)]
            imp = sb("imp", [128, 128]); impm = sb("impm", [128, 128]); r1 = sb("r1", [128, 128]); r2 = sb("r2", [128, 128])
            m8 = sb("m8", [128, 16]); seln = sb("seln", [128, 128], BF16); selT = sb("selT", [128, 512], BF16)
            den = sb("den", [128, 4]); coef = sb("coef", [128, 4])
            nsa = sb("nsa", [128, 512])
            cat = sb("cat", [128, 1024], BF16); catT = sb("catT", [128, 1024], BF16)
            x1t = [sb("x1t%d" % i, [128, 1024]) for i in range(2)]
            p_s = [ps("p_s%d" % i, [128, 512]) for i in range(2)]
            p_a = [ps("p_a%d" % i, [128, 512]) for i in range(4)]
            p_x = ps("p_x", [128, 1024])
            GA1 = sb("GA1", [128, 1024])
            bcast_rows(P, sb, [(p_s[0], ("p_s", 0)), (p_s[1], ("p_s", 1))], [(4, GA1, "GA1")])
            RP = 2
            NCK = cfg.NEXP // (128 * RP)
            cin = [sb("cin%d" % i_, [128, RP * 1024]) for i_ in range(2)]
            cob = [sb("cob%d" % i_, [128, RP * 1024], BF16) for i_ in range(2)]
            cast_jobs = []
            for src_, dst_ in ((pdown, pdb), (pup, pub)):
                srcv = src_.rearrange("(c p j) d -> c p (j d)", p=128, j=RP)
                dstv = dst_.rearrange("(c p j) d -> c p (j d)", p=128, j=RP)
                for c_ in range(NCK):
                    cast_jobs.append((srcv[c_], dstv[c_]))
            cast_n = [0]

            def emit_casts(n):
                for _ in range(n):
                    if cast_n[0] >= len(cast_jobs):
                        return
                    src1, dst1 = cast_jobs[cast_n[0]]
                    k_ = cast_n[0] % 2
                    P.dma("sp", cin[k_][:], src1, writes=[("cin", k_)])
                    eng = "pool" if cast_n[0] % 2 else "dve"
                    P.op(eng, lambda e, k_=k_: e.tensor_copy(out=cob[k_][:], in_=cin[k_][:]), reads=[("cin", k_)], writes=[("cob", k_)])
                    P.dma("sp", dst1, cob[k_][:], reads=[("cob", k_)])
                    cast_n[0] += 1

            P.dma("sp", expd[:], tb["expand"], writes=["expd"])
            P.dma("sp", tri[:], tb["tri"], writes=["tri"])
            P.dma("sp", kbias[:], tb["keybias"], writes=["kbias"])
            P.dma("sp", cbias[:], tb["cmpbias"], writes=["cbias"])
            p_xb = p_x[:].bitcast(BF16)
            gts4 = gts[:].rearrange("p (i g h c) -> p i g h c", g=2, h=4, c=3)
            nsa4 = nsa[:].rearrange("p (g h d) -> p g h d", g=2, h=4)
            sc_i = [0]
            eb_i = [0]
            p_sa = [(p_s[0][:], ("p_s", 0)), (p_s[1][:], ("p_s", 1)), (p_x[:, 0:512], ("p_x", 0)), (p_x[:, 512:1024], ("p_x", 1))]

            def attend(i, g, chunks, first_branch, br):
                W = chunks[0]["v"].shape[-1]
                nchk = len(chunks)
                qg = qTs[64 * g:64 * g + 64, i * 512:(i + 1) * 512]
                pbs = []

                def stage_s(ci):
                    ch = chunks[ci]
                    pb = sc_i[0] % 4; sc_i[0] += 1
                    pbs.append(pb)
                    pst, pkey_ = p_sa[pb]
                    npen = len(ch["pens"])
                    P.op("pe", lambda e, ch=ch, pst=pst, npen=npen: e.matmul(pst[:], lhsT=ch["lhsT"], rhs=qg, start=True, stop=(npen == 0)),
                         reads=ch["keys_r"] + ["qTs"], writes=[pkey_])
                    for pi, (pl, pr, pk) in enumerate(ch["pens"]):
                        P.op("pe", lambda e, pl=pl, pr=pr, pst=pst, last=(pi == npen - 1): e.matmul(
                            pst[:], lhsT=pl, rhs=pr, start=False, stop=last), reads=pk, writes=[pkey_])

                def stage_ev(ci):
                    ch = chunks[ci]
                    pb = pbs[ci]
                    pst, pkey_ = p_sa[pb]
                    ei = eb_i[0] % 4; eb_i[0] += 1
                    et = eb[ei]
                    if ch["bias"] is None:
                        P.op("act", lambda e, et=et, pst=pst: e.activation(out=et[:], in_=pst[:], func=AF.Exp, scale=0.125), reads=[pkey_], writes=[("eb", ei)])
                    else:
                        P.op("act", lambda e, et=et, pst=pst, ch=ch: e.activation(out=et[:], in_=pst[:], func=AF.Exp, scale=0.125, bias=ch["bias"]),
                             reads=[pkey_, "kbias", "cbias"], writes=[("eb", ei)])
                    for h in range(4):
                        P.op("pe", lambda e, h=h, et=et, ch=ch, ci=ci: e.matmul(p_a[h][:, 0:W], lhsT=et[:, h * 128:(h + 1) * 128], rhs=ch["v"],
                                                                              start=(ci == 0), stop=(ci == nchk - 1)),
                             reads=[("eb", ei)] + ch["keys_v"], writes=[("p_a", h)])

                stage_s(0)
                if nchk > 1:
                    stage_s(1)
                for ci in range(nchk):
                    if ci + 2 < nchk:
                        stage_s(ci + 2)
                    stage_ev(ci)
                for h in range(4):
                    P.op("dve", lambda e, h=h: e.tensor_scalar(out=den[:, h:h + 1], in0=p_a[h][:, 64:65], scalar1=1e-30, scalar2=None, op0=ALU.max),
                         reads=[("p_a", h)], writes=[("den", h)])
                    P.op("dve", lambda e, h=h: e.reciprocal(out=den[:, h:h + 1], in_=den[:, h:h + 1]), reads=[("den", h)], writes=[("den", h)])
                    P.op("dve", lambda e, h=h: e.tensor_tensor(out=coef[:, h:h + 1], in0=den[:, h:h + 1], in1=gts4[:, i, g, h, br:br + 1], op=ALU.mult),
                         reads=[("den", h), "gts"], writes=[("coef", h)])
                    if first_branch:
                        P.op("dve", lambda e, h=h: e.tensor_scalar(out=nsa4[:, g, h, :], in0=p_a[h][:, 0:64], scalar1=coef[:, h:h + 1], scalar2=None, op0=ALU.mult),
                             reads=[("p_a", h), ("coef", h)], writes=[("nsa", g, h)])
                    else:
                        P.op("dve", lambda e, h=h: e.scalar_tensor_tensor(out=nsa4[:, g, h, :], in0=p_a[h][:, 0:64], scalar=coef[:, h:h + 1], in1=nsa4[:, g, h, :],
                                                                         op0=ALU.mult, op1=ALU.add),
                             reads=[("p_a", h), ("coef", h), ("nsa", g, h)], writes=[("nsa", g, h)])

            CPT = -(-len(cast_jobs) // (NO * 6))
            for i in range(NO):
                T = T0 + i
                b = i % 2
                P.dma("sp", cpen[b][:], tb["cmppen"][i * 128:(i + 1) * 128, :], writes=[("cpen", b)])
                P.dma("sp", selm[b][:], tb["selm"][i * 128:(i + 1) * 128, :], writes=[("selm", b)])
                P.dma("sp", xo[b][:], xw[T * 128:(T + 1) * 128, :], writes=[("xo", b)])
                for g in range(2):
                    gs = slice(64 * g, 64 * g + 64)
                    chunks = []
                    for c_ in range(NCH):
                        chunks.append(dict(lhsT=kcT[gs, c_ * 128:(c_ + 1) * 128], keys_r=["kcT"],
                                           pens=[(identb[:], cpen[b][:, c_ * 512:(c_ + 1) * 512], [("cpen", b), "identb"])],
                                           bias=cbias[:, c_:c_ + 1], v=vca4[:, c_, g, :], keys_v=["vca"]))
                    emit_casts(CPT)
                    attend(i, g, chunks, True, 0)
                    for h in range(4):
                        if h == 0:
                            P.op("dve", lambda e: e.tensor_scalar(out=imp[:], in0=p_a[0][:, 65:193], scalar1=den[:, 0:1], scalar2=None, op0=ALU.mult),
                                 reads=[("p_a", 0), ("den", 0)], writes=["imp"])
                        else:
                            P.op("dve", lambda e, h=h: e.scalar_tensor_tensor(out=imp[:], in0=p_a[h][:, 65:193], scalar=den[:, h:h + 1], in1=imp[:], op0=ALU.mult, op1=ALU.add),
                                 reads=[("p_a", h), ("den", h), "imp"], writes=["imp"])
                    P.op("dve", lambda e, b=b: e.tensor_tensor(out=impm[:], in0=imp[:], in1=selm[b][:, 0:128], op=ALU.mult), reads=["imp", ("selm", b)], writes=["impm"])
                    P.op("dve", lambda e, b=b: e.tensor_tensor(out=impm[:], in0=impm[:], in1=selm[b][:, 128:256], op=ALU.add), reads=["impm", ("selm", b)], writes=["impm"])
                    P.op("dve", lambda e: e.max(out=m8[:, 0:8], in_=impm[:]), reads=["impm"], writes=["m8a"])
                    P.op("dve", lambda e: e.match_replace(out=r1[:], in_to_replace=m8[:, 0:8], in_values=impm[:], imm_value=-BIG), reads=["impm", "m8a"], writes=["r1"])
                    P.op("dve", lambda e: e.max(out=m8[:, 8:16], in_=r1[:]), reads=["r1"], writes=["m8b"])
                    P.op("dve", lambda e: e.match_replace(out=r2[:], in_to_replace=m8[:, 8:16], in_values=r1[:], imm_value=-BIG), reads=["r1", "m8b"], writes=["r2"])
                    P.op("dve", lambda e: e.tensor_tensor(out=r1[:], in0=impm[:], in1=r2[:], op=ALU.subtract), reads=["impm", "r2"], writes=["r1"])
                    P.op("dve", lambda e: e.tensor_scalar(out=seln[:], in0=r1[:], scalar1=0.0, scalar2=NEG, op0=ALU.is_le, op1=ALU.mult), reads=["r1"], writes=["seln"])
                    P.op("pe", lambda e: e.transpose(p_xb[:, 0:128], seln[:], identb[:]), reads=["seln"], writes=[("p_x", 0)])
                    P.op("dve", lambda e: e.tensor_copy(out=selT[:].rearrange("p (h t) -> p h t", h=4), in_=p_xb[:, 0:128].unsqueeze(1).to_broadcast([128, 4, 128])),
                         reads=[("p_x", 0)], writes=["selT"])
                    chunks = []
                    for c_ in range(T + 1):
                        pens = [(expd[:, c_ * 128:(c_ + 1) * 128], selT[:], ["expd", "selT"])]
                        if c_ == T:
                            pens.append((identb[:], tri[:, 0:512], ["tri", "identb"]))
                        chunks.append(dict(lhsT=ksT[gs, c_ * 128:(c_ + 1) * 128], keys_r=["ksT"], pens=pens, bias=None, v=vs4[:, c_, g, :], keys_v=["vs"]))
                    emit_casts(CPT)
                    attend(i, g, chunks, False, 1)
                    chunks = []
                    for c_ in range(max(0, T - 4), T + 1):
                        wk = c_ - (NW - NWK)
                        pens = []
                        if c_ == T - 4:
                            pens.append((identb[:], tri[:, 512:1024], ["tri", "identb"]))
                        if c_ == T:
                            pens.append((identb[:], tri[:, 0:512], ["tri", "identb"]))
                        chunks.append(dict(lhsT=kwT[gs, wk * 128:(wk + 1) * 128], keys_r=["kwT"], pens=pens, bias=kbias[:, c_:c_ + 1], v=vw4[:, wk, g, :], keys_v=["vw"]))
                    emit_casts(CPT)
                    attend(i, g, chunks, False, 2)
                P.op("act", lambda e, i=i: e.copy(out=cat[:, 0:512], in_=reto[:, i * 512:(i + 1) * 512]), reads=["reto"], writes=["cat_a"])
                P.op("act", lambda e: e.copy(out=cat[:, 512:1024], in_=nsa[:]), reads=[("nsa", g_, h_) for g_ in range(2) for h_ in range(4)], writes=["cat_b"])
                for kc in range(8):
                    P.op("pe", lambda e, kc=kc: e.transpose(p_xb[:, kc * 128:(kc + 1) * 128], cat[:, kc * 128:(kc + 1) * 128], identb[:]),
                         reads=["cat_a", "cat_b"], writes=[("p_x", 0)])
                P.op("act", lambda e: e.copy(out=catT[:], in_=p_xb[:, 0:1024]), reads=[("p_x", 0)], writes=["catT"])
                for half in range(2):
                    for kc in range(8):
                        P.op("pe", lambda e, kc=kc, half=half: e.matmul(p_x[:, half * 512:(half + 1) * 512], lhsT=catT[:, kc * 128:(kc + 1) * 128],
                                                                        rhs=wob3[:, kc, half * 512:(half + 1) * 512], start=(kc == 0), stop=(kc == 7)),
                             reads=["catT", "wob"], writes=[("p_x", half)])
                P.op("dve", lambda e, b=b: e.tensor_tensor(out=x1t[b][:], in0=p_x[:], in1=GA1[:], op=ALU.mult), reads=[("p_x", 0), ("p_x", 1), "GA1"], writes=[("x1t", b)])
                P.op("pool", lambda e, b=b: e.tensor_tensor(out=x1t[b][:], in0=x1t[b][:], in1=xo[b][:], op=ALU.add), reads=[("x1t", b), ("xo", b)], writes=[("x1t", b)])
                P.dma("sp", x1d[i * 128:(i + 1) * 128, :], x1t[b][:], reads=[("x1t", b)])
            emit_casts(len(cast_jobs))
            P.emit()

        mid.close()
        for st in _blk(cfg.stop >= 4):
            sb = lambda name, shape, dt=F32: st.enter_context(nc.sbuf_tensor(name, shape, dt))
            ps = lambda name, shape, dt=F32: st.enter_context(nc.psum_tensor(name, shape, dt))
            P = Prog(nc, SEM)
            wqb = sb("wqb", [128, 8 * 2048], BF16)
            wqb3 = wqb[:].rearrange("p (k n) -> p k n", k=8)
            stg = [sb("stgd%d" % i, [128, 2048]) for i in range(2)]
            for kc in range(8):
                P.dma("sp", stg[kc % 2][:], w_pq[kc * 128:(kc + 1) * 128, :], writes=[("stg", kc % 2)])
                P.op("act", lambda e, kc=kc: e.copy(out=wqb3[:, kc, :], in_=stg[kc % 2][:]), reads=[("stg", kc % 2)], writes=["wqb"])
            A2R = sb("A2R", [128, 1024]); B2R = sb("B2R", [128, 1024]); GA2 = sb("GA2", [128, 1024]); GF = sb("GF", [128, 1024])
            P.dma("sp", GF[:], gfin.partition_broadcast(128), writes=["GF"])
            skb = sb("skb", [128, 2048], BF16)
            P.dma("sp", stg[0][:], skT, writes=[("stg", 0)])
            P.op("act", lambda e: e.copy(out=skb[:], in_=stg[0][:]), reads=[("stg", 0)], writes=["skb"])
            selrow = sb("selrow", [128, 128 * 128], BF16)
            P.dma("sp", selrow[:], tb["selrow"], writes=["selrow"])
            selrow3 = selrow[:].rearrange("p (t m) -> p t m", t=128)
            io16 = sb("io16", [128, 16])
            P.dma("sp", io16[:], tb["iota16"], writes=["io16"])
            io128 = sb("io128", [128, 128]); cm = [sb("cm%d" % i_, [128, 128], BF16) for i_ in range(3)]
            P.dma("sp", io128[:], tb["iota128"], writes=["io128"])
            x1 = sb("x1", [128, 1024]); sq = sb("sqd", [128, 1024]); ss = sb("ssd", [128, 1]); rs = sb("rsd", [128, 1])
            tmod = sb("tmodd", [128, 1024]); h2b = sb("h2b", [128, 1024], BF16); h2T = sb("h2T", [128, 1024], BF16)
            qT = sb("qT", [128, 2048], BF16)
            s_sb = sb("s_sb", [128, 2048]); s2 = sb("s2", [128, 128])
            V16 = sb("V16", [128, 256]); I16 = sb("I16", [128, 256], U32); I16f = sb("I16f", [128, 256])
            cand = sb("cand", [128, 2048]); c2 = sb("c2", [128, 256])
            TV = sb("TV", [128, 128]); TJ = sb("TJ", [128, 128], U32); TA = sb("TA", [128, 128], U32); TBb = sb("TBb", [128, 128], U32)
            TAf = sb("TAf", [128, 128]); TBf = sb("TBf", [128, 128])
            oh = sb("oh", [128, 2048]); i0 = sb("i0", [128, 128]); i1 = sb("i1", [128, 128]); ef = sb("ef", [128, 128])
            ex = sb("ex", [128, 128]); sm = sb("sm", [128, 8]); Wt = sb("Wt", [128, 128])
            idxT = sb("idxT", [128, 128], U32); WT = sb("WT", [128, 128]); aT = sb("aT", [128, 128]); cT = sb("cT", [128, 128])
            g2 = sb("g2", [128, 128]); g3 = sb("g3", [128, 128])
            NU = 8
            U = [sb("U%d" % i, [128, 1024], BF16) for i in range(NU)]
            jk = sq
            oT = tmod; x2 = sb("x2", [128, 1024]); yo = sb("yo", [128, 1024])
            p_q = ps("p_q", [128, 512]); p_s4 = ps("p_s4", [128, 2048]); p_hb = ps("p_hb", [128, 1024]); p_t = ps("p_t", [128, 512])
            bcast_rows(P, sb, [(p_q, "p_q"), (p_t, "p_t")], [(2, A2R, "A2R"), (3, B2R, "B2R"), (5, GA2, "GA2")])
            V4 = V16[:].rearrange("p (c k) -> p c k", k=16); I4 = I16[:].rearrange("p (c k) -> p c k", k=16)
            s3 = s_sb[:].rearrange("p (c n) -> p c n", n=128)
            for i in range(NO):
                P.dma("sp", x1[:], x1d[i * 128:(i + 1) * 128, :], writes=["x1"])
                P.op("act", lambda e: e.activation(out=sq[:], in_=x1[:], func=AF.Square), reads=["x1"], writes=["sq"])
                P.op("dve", lambda e: e.reduce_sum(out=ss[:], in_=sq[:], axis=AX.X), reads=["sq"], writes=["ss"])
                P.op("act", lambda e: e.activation(out=rs[:], in_=ss[:], func=AF.Sqrt, scale=1.0 / 1024, bias=epst[:]), reads=["ss"], writes=["rs"])
                P.op("dve", lambda e: e.reciprocal(out=rs[:], in_=rs[:]), reads=["rs"], writes=["rs"])
                P.op("dve", lambda e: e.scalar_tensor_tensor(out=tmod[:], in0=x1[:], scalar=rs[:], in1=A2R[:], op0=ALU.mult, op1=ALU.mult), reads=["x1", "rs", "A2R"], writes=["tmod"])
                P.op("pool", lambda e: e.tensor_tensor(out=h2b[:], in0=tmod[:], in1=B2R[:], op=ALU.add), reads=["tmod", "B2R"], writes=["h2b"])
                p_tb = p_t[:].bitcast(BF16)
                for kc in range(8):
                    P.op("pe", lambda e, kc=kc: e.transpose(p_tb[:, kc * 128:(kc + 1) * 128], h2b[:, kc * 128:(kc + 1) * 128], identb[:]), reads=["h2b"], writes=["p_t"])
                P.op("act", lambda e: e.copy(out=h2T[:], in_=p_tb[:, 0:1024]), reads=["p_t"], writes=["h2T"])
                for c4 in range(4):
                    for cq in range(4):
                        ch = c4 * 4 + cq
                        for kc in range(8):
                            P.op("pe", lambda e, ch=ch, cq=cq, kc=kc: e.matmul(p_q[:, cq * 128:(cq + 1) * 128], lhsT=wqb3[:, kc, ch * 128:(ch + 1) * 128],
                                                                              rhs=h2T[:, kc * 128:(kc + 1) * 128], start=(kc == 0), stop=(kc == 7)),
                                 reads=["wqb", "h2T"], writes=["p_q"])
                    P.op("act", lambda e, c4=c4: e.copy(out=qT[:, c4 * 512:(c4 + 1) * 512], in_=p_q[:]), reads=["p_q"], writes=[("qT", c4)])
                for ch in range(16):
                    P.op("pe", lambda e, ch=ch: e.matmul(p_s4[:, ch * 128:(ch + 1) * 128], lhsT=qT[:, ch * 128:(ch + 1) * 128], rhs=skb[:, ch * 128:(ch + 1) * 128],
                                                        start=True, stop=True), reads=[("qT", ch // 4), "skb"], writes=[("p_s4", ch // 8)])
                for c4 in range(4):
                    P.op("act", lambda e, c4=c4: e.copy(out=s_sb[:, c4 * 512:(c4 + 1) * 512], in_=p_s4[:, c4 * 512:(c4 + 1) * 512]), reads=[("p_s4", c4 // 2)], writes=[("s_sb", c4)])
                for ch in range(16):
                    sk = ("s_sb", ch // 4)
                    P.op("dve", lambda e, ch=ch: e.max(out=V4[:, ch, 0:8], in_=s3[:, ch, :]), reads=[sk], writes=[("V", ch, 0)])
                    P.op("dve", lambda e, ch=ch: e.max_index(out=I4[:, ch, 0:8], in_max=V4[:, ch, 0:8], in_values=s3[:, ch, :]), reads=[sk, ("V", ch, 0)], writes=[("I", ch, 0)])
                    P.op("dve", lambda e, ch=ch: e.match_replace(out=s2[:], in_to_replace=V4[:, ch, 0:8], in_values=s3[:, ch, :], imm_value=-BIG), reads=[sk, ("V", ch, 0)], writes=["s2"])
                    P.op("dve", lambda e, ch=ch: e.max(out=V4[:, ch, 8:16], in_=s2[:]), reads=["s2"], writes=[("V", ch, 1)])
                    P.op("dve", lambda e, ch=ch: e.max_index(out=I4[:, ch, 8:16], in_max=V4[:, ch, 8:16], in_values=s2[:]), reads=["s2", ("V", ch, 1)], writes=[("I", ch, 1)])
                allV = [("V", ch, k_) for ch in range(16) for k_ in range(2)]
                allI = [("I", ch, k_) for ch in range(16) for k_ in range(2)]
                P.op("dve", lambda e: e.tensor_copy(out=I16f[:], in_=I16[:]), reads=allI, writes=["I16f"])
                cand4 = cand[:].rearrange("p (h a b) -> p h a b", a=16, b=16)
                V5 = V16[:].rearrange("p (h t k) -> p h t k", t=2, k=16)
                I5 = I16f[:].rearrange("p (h t k) -> p h t k", t=2, k=16)
                for hd in range(8):
                    P.op("dve", lambda e, hd=hd: e.tensor_tensor(out=cand4[:, hd], in0=V5[:, hd, 0, :].unsqueeze(2).to_broadcast([128, 16, 16]),
                                                                 in1=V5[:, hd, 1, :].unsqueeze(1).to_broadcast([128, 16, 16]), op=ALU.add),
                         reads=allV, writes=[("cand", hd)])
                    cd = cand[:, hd * 256:(hd + 1) * 256]
                    P.op("dve", lambda e, hd=hd, cd=cd: e.max(out=TV[:, hd * 16:hd * 16 + 8], in_=cd), reads=[("cand", hd)], writes=[("TV", hd, 0)])
                    P.op("dve", lambda e, hd=hd, cd=cd: e.max_index(out=TJ[:, hd * 16:hd * 16 + 8], in_max=TV[:, hd * 16:hd * 16 + 8], in_values=cd),
                         reads=[("cand", hd), ("TV", hd, 0)], writes=[("TJ", hd, 0)])
                    P.op("dve", lambda e, hd=hd, cd=cd: e.match_replace(out=c2[:], in_to_replace=TV[:, hd * 16:hd * 16 + 8], in_values=cd, imm_value=-BIG),
                         reads=[("cand", hd), ("TV", hd, 0)], writes=["c2"])
                    P.op("dve", lambda e, hd=hd: e.max(out=TV[:, hd * 16 + 8:hd * 16 + 16], in_=c2[:]), reads=["c2"], writes=[("TV", hd, 1)])
                    P.op("dve", lambda e, hd=hd: e.max_index(out=TJ[:, hd * 16 + 8:hd * 16 + 16], in_max=TV[:, hd * 16 + 8:hd * 16 + 16], in_values=c2[:]),
                         reads=["c2", ("TV", hd, 1)], writes=[("TJ", hd, 1)])
                allTV = [("TV", hd, k_) for hd in range(8) for k_ in range(2)]
                allTJ = [("TJ", hd, k_) for hd in range(8) for k_ in range(2)]
                TV3 = TV[:].rearrange("p (h k) -> p h k", k=16)
                ex3 = ex[:].rearrange("p (h k) -> p h k", k=16)
                P.op("dve", lambda e: e.tensor_tensor(out=ex3, in0=TV3, in1=TV3[:, :, 0:1].to_broadcast([128, 8, 16]), op=ALU.subtract), reads=allTV, writes=["ex"])
                P.op("act", lambda e: e.activation(out=ex[:], in_=ex[:], func=AF.Exp), reads=["ex"], writes=["ex"])
                P.op("dve", lambda e: e.tensor_reduce(out=sm[:], in_=ex3, axis=AX.X, op=ALU.add), reads=["ex"], writes=["sm"])
                P.op("dve", lambda e: e.reciprocal(out=sm[:], in_=sm[:]), reads=["sm"], writes=["sm"])
                P.op("dve", lambda e: e.tensor_tensor(out=Wt[:].rearrange("p (h k) -> p h k", k=16), in0=ex3, in1=sm[:].unsqueeze(2).to_broadcast([128, 8, 16]), op=ALU.mult),
                     reads=["ex", "sm"], writes=["Wt"])
                P.op("dve", lambda e: e.tensor_single_scalar(out=TA[:], in_=TJ[:], scalar=4, op=ALU.logical_shift_right), reads=allTJ, writes=["TA"])
                P.op("dve", lambda e: e.tensor_single_scalar(out=TBb[:], in_=TJ[:], scalar=15, op=ALU.bitwise_and), reads=allTJ, writes=["TB"])
                P.op("dve", lambda e: e.tensor_copy(out=TAf[:], in_=TA[:]), reads=["TA"], writes=["TAf"])
                P.op("dve", lambda e: e.tensor_copy(out=TBf[:], in_=TBb[:]), reads=["TB"], writes=["TBf"])
                oh4 = oh[:].rearrange("p (h k a) -> p h k a", k=16, a=16)
                for which, (tf, dst) in enumerate([(TAf, i0), (TBf, i1)]):
                    tf3 = tf[:].rearrange("p (h k) -> p h k", k=16)
                    P.op("dve", lambda e, tf3=tf3: e.tensor_tensor(out=oh4, in0=tf3.unsqueeze(3).to_broadcast([128, 8, 16, 16]),
                                                                  in1=io16[:].unsqueeze(1).unsqueeze(1).to_broadcast([128, 8, 16, 16]), op=ALU.is_equal),
                         reads=["TAf", "TBf", "io16"], writes=["oh"])
                    P.op("dve", lambda e, which=which: e.tensor_tensor(out=oh4, in0=oh4, in1=I5[:, :, which, :].unsqueeze(2).to_broadcast([128, 8, 16, 16]), op=ALU.mult),
                         reads=["oh", "I16f"], writes=["oh"])
                    P.op("dve", lambda e, dst=dst: e.tensor_reduce(out=dst[:], in_=oh4, axis=AX.X, op=ALU.add), reads=["oh"], writes=["i01_%d" % which])
                P.op("dve", lambda e: e.scalar_tensor_tensor(out=ef[:], in0=i0[:], scalar=128.0, in1=i1[:], op0=ALU.mult, op1=ALU.add), reads=["i01_0", "i01_1"], writes=["ef"])
                P.op("pe", lambda e: e.transpose(p_t[:, 0:128], ef[:], identf[:]), reads=["ef"], writes=["p_t"])
                P.op("pe", lambda e: e.transpose(p_t[:, 128:256], Wt[:], identf[:]), reads=["Wt"], writes=["p_t"])
                P.op("dve", lambda e: e.tensor_copy(out=idxT[:], in_=p_t[:, 0:128]), reads=["p_t"], writes=["idxT"])
                P.op("dve", lambda e: e.tensor_copy(out=WT[:], in_=p_t[:, 128:256]), reads=["p_t"], writes=["WT"])
                for t_ in range(128):
                    ub = U[t_ % NU]
                    P.op("pool", lambda e, ub=ub, t_=t_: e.indirect_dma_start(out=ub[:], out_offset=None, in_=pdb,
                                                                             in_offset=bass.IndirectOffsetOnAxis(ap=idxT[:, t_:t_ + 1], axis=0)),
                         reads=["idxT"], writes=[("U", t_ % NU)], dma=True)
                    hbt, hbk = (p_hb[:], "p_hb") if t_ % 2 == 0 else (p_s4[:, 1024:2048], ("p_s4", 1))
                    for half in range(2):
                        P.op("pe", lambda e, t_=t_, half=half, hbt=hbt: e.matmul(hbt[:, half * 512:(half + 1) * 512], lhsT=selrow3[:, t_, :], rhs=h2b[:, half * 512:(half + 1) * 512],
                                                                                 start=True, stop=True), reads=["selrow", "h2b"], writes=[hbk])
                    P.op("dve", lambda e, ub=ub, t_=t_, hbt=hbt: e.tensor_tensor_reduce(out=jk[:], in0=ub[:], in1=hbt, scale=1.0, scalar=0.0, op0=ALU.mult, op1=ALU.add,
                                                                                       accum_out=aT[:, t_:t_ + 1]), reads=[("U", t_ % NU), hbk], writes=["sq", "aT"])
                P.op("act", lambda e: e.activation(out=g2[:], in_=aT[:], func=AF.Square), reads=["aT"], writes=["g2"])
                P.op("dve", lambda e: e.tensor_scalar(out=g2[:], in0=g2[:], scalar1=0.044715, scalar2=1.0, op0=ALU.mult, op1=ALU.add), reads=["g2"], writes=["g2"])
                P.op("dve", lambda e: e.tensor_tensor(out=g3[:], in0=g2[:], in1=aT[:], op=ALU.mult), reads=["g2", "aT"], writes=["g3"])
                P.op("act", lambda e: e.activation(out=g3[:], in_=g3[:], func=AF.Sigmoid, scale=1.5957691216), reads=["g3"], writes=["g3"])
                P.op("dve", lambda e: e.tensor_tensor(out=g3[:], in0=g3[:], in1=aT[:], op=ALU.mult), reads=["g3", "aT"], writes=["g3"])
                P.op("dve", lambda e: e.tensor_tensor(out=cT[:], in0=g3[:], in1=WT[:], op=ALU.mult), reads=["g3", "WT"], writes=["cT"])
                for t_ in range(128):
                    ub = U[t_ % NU]
                    P.op("pool", lambda e, ub=ub, t_=t_: e.indirect_dma_start(out=ub[:], out_offset=None, in_=pub,
                                                                             in_offset=bass.IndirectOffsetOnAxis(ap=idxT[:, t_:t_ + 1], axis=0)),
                         reads=["idxT"], writes=[("U", t_ % NU)], dma=True)
                    cmt = cm[t_ % 3]
                    P.op("dve", lambda e, cmt=cmt, t_=t_: e.scalar_tensor_tensor(out=cmt[:], in0=io128[:], scalar=float(t_), in1=cT[:, t_:t_ + 1].to_broadcast([128, 128]),
                                                                                op0=ALU.is_equal, op1=ALU.mult), reads=["io128", "cT"], writes=[("cm", t_ % 3)])
                    for half in range(2):
                        P.op("pe", lambda e, ub=ub, cmt=cmt, t_=t_, half=half: e.matmul(p_s4[:, half * 512:(half + 1) * 512], lhsT=cmt[:],
                                                                                      rhs=ub[:, half * 512:(half + 1) * 512], start=(t_ == 0), stop=(t_ == 127)),
                             reads=[("U", t_ % NU), ("cm", t_ % 3)], writes=[("p_s4", 0)])
                P.op("dve", lambda e: e.tensor_tensor(out=x2[:], in0=p_s4[:, 0:1024], in1=GA2[:], op=ALU.mult), reads=[("p_s4", 0), "GA2"], writes=["x2"])
                P.op("pool", lambda e: e.tensor_tensor(out=x2[:], in0=x2[:], in1=x1[:], op=ALU.add), reads=["x2", "x1"], writes=["x2"])
                P.op("act", lambda e: e.activation(out=sq[:], in_=x2[:], func=AF.Square), reads=["x2"], writes=["sq"])
                P.op("dve", lambda e: e.reduce_sum(out=ss[:], in_=sq[:], axis=AX.X), reads=["sq"], writes=["ss"])
                P.op("act", lambda e: e.activation(out=rs[:], in_=ss[:], func=AF.Sqrt, scale=1.0 / 1024, bias=epst[:]), reads=["ss"], writes=["rs"])
                P.op("dve", lambda e: e.reciprocal(out=rs[:], in_=rs[:]), reads=["rs"], writes=["rs"])
                P.op("dve", lambda e: e.scalar_tensor_tensor(out=yo[:], in0=x2[:], scalar=rs[:], in1=GF[:], op0=ALU.mult, op1=ALU.mult), reads=["x2", "rs", "GF"], writes=["yo"])
                P.dma("sp", out[i * 128:(i + 1) * 128, :], yo[:], reads=["yo"])
            P.emit()
    return nc


def make_inputs(cfg, core, inp, n_per_batch=4, S_full=None):
    NW, NO = cfg.NW, cfg.NO
    b, j = core // n_per_batch, core % n_per_batch
    off = NO * 128 * (j + 1) - NW * 128
    x = inp["x"][b]
    xw = np.zeros((NW * 128, 1024), np.float32)
    lo = max(0, -off)
    xw[lo:] = x[off + lo: off + NW * 128]
    m = {"xw": xw}
    m["ccol"] = np.ascontiguousarray(inp["c"][b].reshape(8, 128).T)
    m["w_ada"] = inp["w_ada"][0]
    m["b_adaT"] = np.ascontiguousarray(inp["b_ada"][0].reshape(48, 128).T)
    m["gmixT"] = np.ascontiguousarray(inp["g_norm_mix"][0].reshape(8, 128).T)
    m["gffnT"] = np.ascontiguousarray(inp["g_norm_ffn"][0].reshape(8, 128).T)
    m["gfin"] = inp["g_norm_final"].reshape(1, 1024)
    m["w_in"] = np.ascontiguousarray(inp["w_in"][0][:, w_in_perm()])
    m["w_out"] = inp["w_out"][0]
    for nm, key in (("w1k", "w_cmp_k1"), ("w1v", "w_cmp_v1")):
        m[nm] = np.ascontiguousarray(inp[key][0].reshape(32, 64, 256).transpose(1, 0, 2).reshape(64, 32 * 256))
    m["peTk"] = np.ascontiguousarray(inp["pe_cmp_k"][0].T)
    m["peTv"] = np.ascontiguousarray(inp["pe_cmp_v"][0].T)
    m["b1k"] = np.ascontiguousarray(inp["b_cmp_k1"][0].reshape(2, 128).T)
    m["b1v"] = np.ascontiguousarray(inp["b_cmp_v1"][0].reshape(2, 128).T)
    m["w2k"] = np.ascontiguousarray(inp["w_cmp_k2"][0].reshape(2, 128, 64).transpose(1, 0, 2).reshape(128, 128))
    m["w2v"] = np.ascontiguousarray(inp["w_cmp_v2"][0].reshape(2, 128, 64).transpose(1, 0, 2).reshape(128, 128))
    m["w_pq"] = inp["w_peer_q"][0]
    m["skT"] = np.ascontiguousarray(inp["peer_subkeys"][0].reshape(16, 128, 128).transpose(2, 0, 1).reshape(128, 2048))
    m["pdown"] = inp["peer_down"][0]
    m["pup"] = inp["peer_up"][0]
    for k_, v_ in host_tables(cfg, off).items():
        m["t_" + k_] = v_
    return m


_NC_CACHE = {}


def kernel(**inputs):
    inp = {k: np.asarray(v) for k, v in inputs.items()}
    cfg = CFG()
    if "nc" not in _NC_CACHE:
        _NC_CACHE["nc"] = build(cfg)
        mybir.codegen_inst_isa_subclasses(_NC_CACHE["nc"])
    nc = _NC_CACHE["nc"]
    in_maps = [make_inputs(cfg, c, inp) for c in range(8)]
    res = run_bass_kernel_spmd(nc, in_maps, core_ids=list(range(8)))
    out = np.zeros((2, 8192, 1024), np.float32)
    for c in range(8):
        b, j = c // 4, c % 4
        out[b, j * 2048:(j + 1) * 2048] = res.results[c]["out"]
    return out
```
